# Optimizing a Trainium2 kernel written in Bass

```python
import jax
import jax.numpy as jnp
from jax import lax
import numpy as np

D_MODEL = 1024
BATCH = 4
SEQ = 8192
DEPTH = 2

N_GROUPS = 4
GROUP_WIDTH = D_MODEL // N_GROUPS
MIX_WIDTH = N_GROUPS * GROUP_WIDTH
ROPE_THETA = 10000.0
NORM_EPS = 1e-6
NEG_INF = -1e30
FORCE_SCORE = 1e9
Q_BLOCK = 128

LRU_WIDTH = GROUP_WIDTH
LRU_HEADS = 4
LRU_HEAD_DIM = LRU_WIDTH // LRU_HEADS
CONV_WIDTH = 4
LRU_C = 8.0

NSA_HEADS = 4
NSA_HEAD_DIM = GROUP_WIDTH // NSA_HEADS
CMP_LEN = 32
CMP_STRIDE = 16
CMP_HIDDEN = 256
SEL_LEN = 64
SEL_TOPN = 16
WINDOW = 512

GLA_HEADS = 4
GLA_DV = GROUP_WIDTH // GLA_HEADS
GLA_DK = GLA_DV // 2
GLA_GATE_RANK = 16
GLA_TAU = 16.0
GLA_CHUNK = 64

MLA_HEADS = 4
MLA_V_DIM = GROUP_WIDTH // MLA_HEADS
MLA_NOPE_DIM = 64
MLA_ROPE_DIM = 32
MLA_QK_DIM = MLA_NOPE_DIM + MLA_ROPE_DIM
MLA_Q_RANK = 192
MLA_KV_RANK = 128

_FF_RAW = -(-8 * D_MODEL // 3)
D_FF = -(-_FF_RAW // 256) * 256

IN_SPLITS = (
    LRU_WIDTH, LRU_WIDTH,
    NSA_HEADS * NSA_HEAD_DIM, 6 * NSA_HEAD_DIM, NSA_HEADS * 3,
    GLA_HEADS * GLA_DK, GLA_HEADS * GLA_DK, GLA_HEADS * GLA_DV,
    GLA_GATE_RANK, GLA_HEADS * GLA_DV,
    MLA_Q_RANK, MLA_KV_RANK, MLA_ROPE_DIM,
)
IN_COLS = sum(IN_SPLITS)

kernel_name = 'hybrid_hymba_rglru_nsa_gla_mla'


def rmsnorm(x, gain):
    xf = x.astype(jnp.float32)
    y = xf * lax.rsqrt(jnp.mean(xf * xf, axis=-1, keepdims=True) + NORM_EPS)
    return (y * gain.astype(jnp.float32)).astype(x.dtype)


def rope(x, positions):
    d = x.shape[-1]
    inv_freq = ROPE_THETA ** (-jnp.arange(0, d, 2, dtype=jnp.float32) / d)
    ang = positions.astype(jnp.float32)[..., None] * inv_freq
    cos = jnp.cos(ang)[:, :, None, :]
    sin = jnp.sin(ang)[:, :, None, :]
    xf = x.astype(jnp.float32)
    x1, x2 = xf[..., : d // 2], xf[..., d // 2:]
    return jnp.concatenate([x1 * cos - x2 * sin, x2 * cos + x1 * sin], axis=-1).astype(x.dtype)


def masked_softmax(s, mask):
    s = jnp.where(mask, s, NEG_INF)
    m = jnp.max(s, axis=-1, keepdims=True)
    p = jnp.where(mask, jnp.exp(s - m), 0.0)
    return p / jnp.maximum(jnp.sum(p, axis=-1, keepdims=True), 1e-30)


def rglru_mixer(xa, gate, conv_w, conv_b, wa, ba, wx, bx, lam):
    dtype = xa.dtype
    B, S, W = xa.shape
    xf = xa.astype(jnp.float32)
    xc = lax.conv_general_dilated(
        xf, conv_w.astype(jnp.float32)[:, None, :], window_strides=(1,),
        padding=[(CONV_WIDTH - 1, 0)], dimension_numbers=('NWC', 'WIO', 'NWC'),
        feature_group_count=W) + conv_b.astype(jnp.float32)
    xh = xc.reshape(B, S, LRU_HEADS, LRU_HEAD_DIM)
    r = jax.nn.sigmoid(jnp.einsum('bshi,hij->bshj', xh, wa.astype(jnp.float32)).reshape(B, S, W) + ba)
    i = jax.nn.sigmoid(jnp.einsum('bshi,hij->bshj', xh, wx.astype(jnp.float32)).reshape(B, S, W) + bx)
    log_a = -LRU_C * r * jax.nn.softplus(-lam.astype(jnp.float32))
    a = jnp.exp(log_a)
    u = jnp.sqrt(-jnp.expm1(2.0 * log_a)) * (i * xc)

    def combine(left, right):
        a1, b1 = left
        a2, b2 = right
        return a1 * a2, a2 * b1 + b2

    _, h = lax.associative_scan(combine, (a, u), axis=1)
    y = h * jax.nn.gelu(gate.astype(jnp.float32))
    return y.astype(dtype)


def nsa_mixer(q, kv, gates, positions, cmp_pos, cmp_w1, cmp_b1, cmp_w2):
    dtype = q.dtype
    f32 = jnp.float32
    B, S, _ = q.shape
    H, Dh = NSA_HEADS, NSA_HEAD_DIM
    q = rope(q.reshape(B, S, H, Dh), positions).astype(f32)
    k_c, v_c, k_s, v_s, k_w, v_w = jnp.split(kv.astype(f32), 6, axis=-1)
    k_c, k_s, k_w = [rope(k[:, :, None, :], positions)[:, :, 0] for k in (k_c, k_s, k_w)]

    n_cmp = (S - CMP_LEN) // CMP_STRIDE + 1
    tok_idx = jnp.arange(n_cmp)[:, None] * CMP_STRIDE + jnp.arange(CMP_LEN)[None, :]

    def compress(t, j):
        blk = t[:, tok_idx] + cmp_pos[j].astype(f32)
        hid = jax.nn.gelu(blk.reshape(B, n_cmp, CMP_LEN * Dh) @ cmp_w1[j].astype(f32) + cmp_b1[j].astype(f32))
        return hid @ cmp_w2[j].astype(f32)

    k_cmp = compress(k_c, 0)
    v_cmp = compress(v_c, 1)
    cmp_start = jnp.arange(n_cmp) * CMP_STRIDE
    cmp_end = cmp_start + CMP_LEN - 1

    n_sel = S // SEL_LEN
    topn = min(SEL_TOPN, n_sel)
    sel_j = jnp.arange(n_sel)
    sel_start = sel_j * SEL_LEN
    overlap = ((cmp_start[:, None] < sel_start[None, :] + SEL_LEN)
               & (cmp_start[:, None] + CMP_LEN > sel_start[None, :])).astype(f32)
    k_sel_blk = k_s.reshape(B, n_sel, SEL_LEN, Dh)
    v_sel_blk = v_s.reshape(B, n_sel, SEL_LEN, Dh)

    k_win_pad = jnp.pad(k_w, ((0, 0), (WINDOW, 0), (0, 0)))
    v_win_pad = jnp.pad(v_w, ((0, 0), (WINDOW, 0), (0, 0)))

    g = jax.nn.sigmoid(gates.astype(f32)).reshape(B, S, H, 3)
    scale = Dh ** -0.5
    n_qb = S // Q_BLOCK
    q_blocks = (q * scale).reshape(B, n_qb, Q_BLOCK, H, Dh).transpose(1, 0, 2, 3, 4)
    g_blocks = g.reshape(B, n_qb, Q_BLOCK, H, 3).transpose(1, 0, 2, 3, 4)
    starts = jnp.arange(n_qb, dtype=jnp.int32) * Q_BLOCK

    def block(args):
        qb, gb, s0 = args
        t = s0 + jnp.arange(Q_BLOCK, dtype=jnp.int32)
        s_c = jnp.einsum('bqhd,bnd->bhqn', qb, k_cmp)
        p_c = masked_softmax(s_c, cmp_end[None, :] <= t[:, None])
        o_c = jnp.einsum('bhqn,bnd->bqhd', p_c, v_cmp)
        imp = jnp.einsum('bhqn,ns->bqs', p_c, overlap)
        cur = t // SEL_LEN
        valid = sel_start[None, :] <= t[:, None]
        forced = (sel_j[None, :] == 0) | (sel_j[None, :] == cur[:, None]) | (sel_j[None, :] == cur[:, None] - 1)
        score = jnp.where(forced, FORCE_SCORE, jnp.where(valid, imp, NEG_INF))
        _, idx = lax.top_k(score, topn)
        ks = jax.vmap(lambda kb, ix: kb[ix])(k_sel_blk, idx)
        vs = jax.vmap(lambda vb, ix: vb[ix])(v_sel_blk, idx)
        key_pos = idx[..., None] * SEL_LEN + jnp.arange(SEL_LEN)
        m_s = (key_pos <= t[None, :, None, None]).reshape(B, 1, Q_BLOCK, topn * SEL_LEN)
        s_s = jnp.einsum('bqhd,bqnld->bhqnl', qb, ks).reshape(B, H, Q_BLOCK, topn * SEL_LEN)
        p_s = masked_softmax(s_s, m_s)
        o_s = jnp.einsum('bhqm,bqmd->bqhd', p_s, vs.reshape(B, Q_BLOCK, topn * SEL_LEN, Dh))
        kw = lax.dynamic_slice_in_dim(k_win_pad, s0, WINDOW + Q_BLOCK, axis=1)
        vw = lax.dynamic_slice_in_dim(v_win_pad, s0, WINDOW + Q_BLOCK, axis=1)
        kpos = s0 - WINDOW + jnp.arange(WINDOW + Q_BLOCK, dtype=jnp.int32)
        m_w = (kpos[None, :] <= t[:, None]) & (kpos[None, :] > t[:, None] - WINDOW) & (kpos[None, :] >= 0)
        s_w = jnp.einsum('bqhd,bkd->bhqk', qb, kw)
        p_w = masked_softmax(s_w, m_w)
        o_w = jnp.einsum('bhqk,bkd->bqhd', p_w, vw)
        return gb[..., 0:1] * o_c + gb[..., 1:2] * o_s + gb[..., 2:3] * o_w

    out = lax.map(block, (q_blocks, g_blocks, starts))
    return out.transpose(1, 0, 2, 3, 4).reshape(B, S, H * Dh).astype(dtype)


def gla_mixer(q, k, v, g_lr, og, w_g2, b_g2, norm_g):
    dtype = q.dtype
    f32 = jnp.float32
    B, S, _ = q.shape
    H, C = GLA_HEADS, GLA_CHUNK
    n_c = S // C
    log_a = jax.nn.log_sigmoid(g_lr.astype(f32) @ w_g2.astype(f32) + b_g2.astype(f32)) / GLA_TAU

    def to_chunks(t, d):
        return t.astype(f32).reshape(B, n_c, C, H, d).transpose(1, 0, 3, 2, 4)

    qc = to_chunks(q, GLA_DK) * (GLA_DK ** -0.5)
    kc = to_chunks(k, GLA_DK)
    vc = to_chunks(v, GLA_DV)
    gc = to_chunks(log_a, GLA_DK)
    causal = jnp.tril(jnp.ones((C, C), dtype=bool))

    def step(state, xs):
        qi, ki, vi, gi = xs
        b = jnp.cumsum(gi, axis=2)
        o_inter = jnp.einsum('bhck,bhkv->bhcv', qi * jnp.exp(b), state)
        diff = b[:, :, :, None, :] - b[:, :, None, :, :]
        decay = jnp.exp(jnp.where(causal[:, :, None], diff, NEG_INF))
        attn = jnp.einsum('bhik,bhjk,bhijk->bhij', qi, ki, decay)
        o = o_inter + jnp.einsum('bhij,bhjv->bhiv', attn, vi)
        b_last = b[:, :, -1:, :]
        state = (jnp.exp(b_last[:, :, 0, :])[..., None] * state
                 + jnp.einsum('bhjk,bhjv->bhkv', ki * jnp.exp(b_last - b), vi))
        return state, o

    s_init = jnp.zeros((B, H, GLA_DK, GLA_DV), f32)
    _, o = lax.scan(step, s_init, (qc, kc, vc, gc))
    o = o.transpose(1, 0, 3, 2, 4).reshape(B, S, H, GLA_DV)
    o = rmsnorm(o, norm_g) * jax.nn.silu(og.astype(f32).reshape(B, S, H, GLA_DV))
    return o.reshape(B, S, H * GLA_DV).astype(dtype)


def mla_mixer(c_q, c_kv, k_rope, positions, q_norm, kv_norm, w_uq, w_ukv):
    dtype = c_q.dtype
    f32 = jnp.float32
    B, S, _ = c_q.shape
    H = MLA_HEADS
    q = (rmsnorm(c_q, q_norm) @ w_uq).reshape(B, S, H, MLA_QK_DIM)
    q = jnp.concatenate([q[..., :MLA_NOPE_DIM], rope(q[..., MLA_NOPE_DIM:], positions)], axis=-1)
    kv = (rmsnorm(c_kv, kv_norm) @ w_ukv).reshape(B, S, H, MLA_NOPE_DIM + MLA_V_DIM)
    kr = rope(k_rope[:, :, None, :], positions)
    k = jnp.concatenate([kv[..., :MLA_NOPE_DIM], jnp.broadcast_to(kr, (B, S, H, MLA_ROPE_DIM))], axis=-1).astype(f32)
    v = kv[..., MLA_NOPE_DIM:].astype(f32)
    n_qb = S // Q_BLOCK
    q_blocks = (q.astype(f32) * (MLA_QK_DIM ** -0.5)).reshape(B, n_qb, Q_BLOCK, H, MLA_QK_DIM).transpose(1, 0, 2, 3, 4)
    starts = jnp.arange(n_qb, dtype=jnp.int32) * Q_BLOCK
    key_idx = jnp.arange(S, dtype=jnp.int32)

    def block(args):
        qb, s0 = args
        t = s0 + jnp.arange(Q_BLOCK, dtype=jnp.int32)
        s = jnp.einsum('bqhd,bkhd->bhqk', qb, k)
        p = masked_softmax(s, key_idx[None, :] <= t[:, None])
        return jnp.einsum('bhqk,bkhd->bqhd', p, v)

    o = lax.map(block, (q_blocks, starts))
    return o.transpose(1, 0, 2, 3, 4).reshape(B, S, H * MLA_V_DIM).astype(dtype)


def setup_inputs(seed: int = 0) -> dict:
    key = jax.random.key(seed)
    ks = jax.random.split(key, 32)
    f32 = jnp.float32

    def nrm(k, shape, scale):
        return jax.random.normal(k, shape, f32) * scale

    def gain(k, shape):
        return 1.0 + 0.02 * jax.random.normal(k, shape, f32)

    u = jax.random.uniform(ks[10], (DEPTH, LRU_WIDTH), f32, minval=0.9, maxval=0.999)
    s = u ** (1.0 / LRU_C)
    lru_lambda = jnp.log(s) - jnp.log1p(-s)
    positions = (jnp.arange(SEQ, dtype=jnp.int32)[None, :]
                 + jax.random.randint(ks[1], (BATCH, 1), 0, SEQ, dtype=jnp.int32))
    return {
        'x': nrm(ks[0], (BATCH, SEQ, D_MODEL), 1.0),
        'positions': positions,
        'norm_mix': gain(ks[2], (DEPTH, D_MODEL)),
        'w_in': nrm(ks[3], (DEPTH, D_MODEL, IN_COLS), D_MODEL ** -0.5),
        'conv_w': nrm(ks[4], (DEPTH, CONV_WIDTH, LRU_WIDTH), CONV_WIDTH ** -0.5),
        'conv_b': nrm(ks[5], (DEPTH, LRU_WIDTH), 0.02),
        'lru_wa': nrm(ks[6], (DEPTH, LRU_HEADS, LRU_HEAD_DIM, LRU_HEAD_DIM), LRU_HEAD_DIM ** -0.5),
        'lru_ba': nrm(ks[7], (DEPTH, LRU_WIDTH), 0.02),
        'lru_wx': nrm(ks[8], (DEPTH, LRU_HEADS, LRU_HEAD_DIM, LRU_HEAD_DIM), LRU_HEAD_DIM ** -0.5),
        'lru_bx': nrm(ks[9], (DEPTH, LRU_WIDTH), 0.02),
        'lru_lambda': lru_lambda,
        'cmp_pos': nrm(ks[11], (DEPTH, 2, CMP_LEN, NSA_HEAD_DIM), 0.02),
        'cmp_w1': nrm(ks[12], (DEPTH, 2, CMP_LEN * NSA_HEAD_DIM, CMP_HIDDEN), (CMP_LEN * NSA_HEAD_DIM) ** -0.5),
        'cmp_b1': nrm(ks[13], (DEPTH, 2, CMP_HIDDEN), 0.02),
        'cmp_w2': nrm(ks[14], (DEPTH, 2, CMP_HIDDEN, NSA_HEAD_DIM), CMP_HIDDEN ** -0.5),
        'gla_wg2': nrm(ks[15], (DEPTH, GLA_GATE_RANK, GLA_HEADS * GLA_DK), GLA_GATE_RANK ** -0.5),
        'gla_bg2': nrm(ks[16], (DEPTH, GLA_HEADS * GLA_DK), 0.02),
        'gla_norm': gain(ks[17], (DEPTH, GLA_DV)),
        'mla_q_norm': gain(ks[18], (DEPTH, MLA_Q_RANK)),
        'mla_kv_norm': gain(ks[19], (DEPTH, MLA_KV_RANK)),
        'mla_w_uq': nrm(ks[20], (DEPTH, MLA_Q_RANK, MLA_HEADS * MLA_QK_DIM), MLA_Q_RANK ** -0.5),
        'mla_w_ukv': nrm(ks[21], (DEPTH, MLA_KV_RANK, MLA_HEADS * (MLA_NOPE_DIM + MLA_V_DIM)), MLA_KV_RANK ** -0.5),
        'group_norm': gain(ks[22], (DEPTH, MIX_WIDTH)),
        'w_out': nrm(ks[23], (DEPTH, MIX_WIDTH, D_MODEL), MIX_WIDTH ** -0.5),
        'norm_ffn': gain(ks[24], (DEPTH, D_MODEL)),
        'w_gate_up': nrm(ks[25], (DEPTH, D_MODEL, 2 * D_FF), D_MODEL ** -0.5),
        'w_down': nrm(ks[26], (DEPTH, D_FF, D_MODEL), D_FF ** -0.5),
        'final_norm': gain(ks[27], (D_MODEL,)),
    }


def reference(x, positions, norm_mix, w_in, conv_w, conv_b, lru_wa, lru_ba, lru_wx, lru_bx,
              lru_lambda, cmp_pos, cmp_w1, cmp_b1, cmp_w2, gla_wg2, gla_bg2, gla_norm,
              mla_q_norm, mla_kv_norm, mla_w_uq, mla_w_ukv, group_norm, w_out, norm_ffn,
              w_gate_up, w_down, final_norm):
    B, S, _ = x.shape
    offsets = [int(o) for o in np.cumsum(IN_SPLITS)[:-1]]
    for l in range(DEPTH):
        h = rmsnorm(x, norm_mix[l])
        (a_x, a_gate, b_q, b_kv, b_gate, c_q, c_k, c_v, c_glr, c_og,
         d_cq, d_ckv, d_kr) = jnp.split(h @ w_in[l], offsets, axis=-1)
        y_a = rglru_mixer(a_x, a_gate, conv_w[l], conv_b[l], lru_wa[l], lru_ba[l],
                          lru_wx[l], lru_bx[l], lru_lambda[l])
        y_b = nsa_mixer(b_q, b_kv, b_gate, positions, cmp_pos[l], cmp_w1[l], cmp_b1[l], cmp_w2[l])
        y_c = gla_mixer(c_q, c_k, c_v, c_glr, c_og, gla_wg2[l], gla_bg2[l], gla_norm[l])
        y_d = mla_mixer(d_cq, d_ckv, d_kr, positions, mla_q_norm[l], mla_kv_norm[l],
                        mla_w_uq[l], mla_w_ukv[l])
        y = jnp.stack([y_a, y_b, y_c, y_d], axis=2)
        y = rmsnorm(y, group_norm[l].reshape(N_GROUPS, GROUP_WIDTH)).reshape(B, S, MIX_WIDTH)
        x = x + y @ w_out[l]
        h = rmsnorm(x, norm_ffn[l])
        gate, up = jnp.split(h @ w_gate_up[l], 2, axis=-1)
        x = x + (jax.nn.silu(gate) * up) @ w_down[l]
    return rmsnorm(x, final_norm)
```

```python
import numpy as np
from contextlib import ExitStack
import concourse.bass as bass
import concourse.mybir as mybir
from concourse.bass_utils import run_bass_kernel_spmd

F32 = mybir.dt.float32
BF16 = mybir.dt.bfloat16
I32 = mybir.dt.int32
AF = mybir.ActivationFunctionType
ALU = mybir.AluOpType
AX = mybir.AxisListType

ENGS = ("pe", "act", "dve", "pool", "sp")
NDMA_SEMS = 8
EPS = 1e-6


class Sched:
    def __init__(self, nc, es):
        self.nc = nc
        self.sem = {e: es.enter_context(nc.semaphore("s_" + e)) for e in ENGS}
        self.cnt = {e: 0 for e in ENGS}
        self.dsem = {e: [es.enter_context(nc.semaphore(f"d_{e}{i}")) for i in range(NDMA_SEMS)]
                     for e in ("sp", "pool", "act")}
        self.dval = {e: [0] * NDMA_SEMS for e in self.dsem}
        self.drr = {e: 0 for e in self.dsem}
        self.ops = {e: [] for e in ENGS}
        self.known = {e: {} for e in ENGS}
        self.semobj = {}
        self.last_w = {}
        self.readers = {}
        self.ccsem = es.enter_context(nc.semaphore("s_cc"))
        self.semobj["cc"] = self.ccsem
        self.ccn = 0
        self.rt = {}
        for e in ENGS:
            self.semobj["c_" + e] = self.sem[e]
        for e in self.dsem:
            for i in range(NDMA_SEMS):
                self.semobj[f"d_{e}{i}"] = self.dsem[e][i]

    def _need(self, eng, tok, waits):
        if tok is None:
            return
        sk, val, teng = tok
        if teng == "pe" and eng == "pe" and sk == "c_pe":
            return
        if self.known[eng].get(sk, 0) >= val:
            return
        self.known[eng][sk] = val
        waits[sk] = max(waits.get(sk, 0), val)

    def _deps(self, eng, reads, writes):
        waits = {}
        for k in reads:
            self._need(eng, self.last_w.get(k), waits)
        for k in writes:
            self._need(eng, self.last_w.get(k), waits)
            for t in self.readers.get(k, ()):
                self._need(eng, t, waits)
        return waits

    def _commit(self, tok, reads, writes):
        for k in reads:
            self.readers.setdefault(k, []).append(tok)
        for k in writes:
            self.last_w[k] = tok
            self.readers[k] = []

    def op(self, eng, emit, reads=(), writes=()):
        waits = self._deps(eng, reads, writes)
        self.cnt[eng] += 1
        tok = ("c_" + eng, self.cnt[eng], eng)
        self.ops[eng].append((waits, emit, (self.sem[eng], 1)))
        self._commit(tok, reads, writes)
        return tok

    def dma(self, eng, emit, reads=(), writes=()):
        waits = self._deps(eng, reads, writes)
        i = self.drr[eng]
        self.drr[eng] = (i + 1) % NDMA_SEMS
        sk = f"d_{eng}{i}"
        prev = self.dval[eng][i]
        if prev and self.known[eng].get(sk, 0) < prev:
            self.known[eng][sk] = prev
            waits[sk] = prev
        self.dval[eng][i] = prev + 16
        tok = (sk, prev + 16, eng)
        self.ops[eng].append((waits, emit, (self.dsem[eng][i], 16)))
        self._commit(tok, reads, writes)
        return tok

    def barrier(self):
        for eng in ENGS:
            waits = {}
            for e in ENGS:
                if e != eng and self.cnt[e]:
                    self._need(eng, ("c_" + e, self.cnt[e], e), waits)
            for e in self.dsem:
                for i in range(NDMA_SEMS):
                    if self.dval[e][i]:
                        self._need(eng, (f"d_{e}{i}", self.dval[e][i], "dma"), waits)
            if eng != "pe" and self.cnt[eng]:
                self._need(eng, ("c_" + eng, self.cnt[eng], eng), waits)
            if waits:
                self.ops[eng].append((waits, None, None))
        self.last_w = {}
        self.readers = {}

    def collective(self, emits):
        self.barrier()
        for emit in emits:
            w = {"cc": self.ccn} if self.ccn else {}
            self.ccn += 1
            self.ops["pool"].append((w, emit, (self.ccsem, 1)))
        for eng in ENGS:
            self.known[eng]["cc"] = self.ccn
            self.ops[eng].append(({"cc": self.ccn}, None, None))

    def collective_async(self, emit, reads=()):
        waits = {}
        for k in reads:
            self._need("pool", self.last_w.get(k), waits)
        if self.ccn:
            waits["cc"] = self.ccn
        self.ccn += 1
        self.ops["pool"].append((waits, emit, (self.ccsem, 1)))

    def cc_wait_all(self):
        self.barrier()
        for eng in ENGS:
            if self.known[eng].get("cc", 0) < self.ccn:
                self.known[eng]["cc"] = self.ccn
                self.ops[eng].append(({"cc": self.ccn}, None, None))

    def finish(self):
        self.barrier()
        semobj = self.semobj

        def run(engobj, lst):
            for waits, emit, inc in lst:
                for sk, v in waits.items():
                    engobj.wait_ge(semobj[sk], v)
                if emit is not None:
                    emit(engobj).then_inc(inc[0], inc[1])

        with self.nc.Block() as block:
            @block.tensor
            def _(e):
                run(e, self.ops["pe"])

            @block.scalar
            def _(e):
                self.rt["hp_act"] = e.partition_id() % 2
                run(e, self.ops["act"])

            @block.vector
            def _(e):
                run(e, self.ops["dve"])

            @block.gpsimd
            def _(e):
                run(e, self.ops["pool"])

            @block.sync
            def _(e):
                self.rt["hp_sp"] = e.partition_id() % 2
                run(e, self.ops["sp"])


class KB:
    def __init__(self, arena_cols=50000):
        self.nc = bass.Bass("TRN2", target_bir_lowering=False)
        self.es = ExitStack()
        self.S = Sched(self.nc, self.es)
        self.arena = self.es.enter_context(self.nc.sbuf_tensor("arena", [128, arena_cols], F32))
        self.acols = arena_cols
        self.top = 0
        self.psb = [self.es.enter_context(self.nc.psum_tensor(f"psb{i}", [128, 512], F32)) for i in range(8)]
        self.uid = 0

    def dint(self, name, shape, dt=F32):
        return self.nc.dram_tensor(name, list(shape), dt).ap()

    def din(self, name, shape, dt=F32):
        return self.nc.dram_tensor(name, list(shape), dt, kind="ExternalInput").ap()

    def dout(self, name, shape, dt=F32):
        return self.nc.dram_tensor(name, list(shape), dt, kind="ExternalOutput").ap()

    def alloc(self, cols, dt=F32):
        n32 = cols if dt != BF16 else (cols + 1) // 2
        a = self.top
        self.top += n32
        assert self.top <= self.acols, f"arena overflow {self.top}"
        v = self.arena[:, a:a + n32]
        if dt == BF16:
            v = v.bitcast(BF16)
        elif dt == I32:
            v = v.bitcast(I32)
        return v

    def mark(self):
        return self.top

    def release(self, m):
        self.top = m

    def key(self, base="k"):
        self.uid += 1
        return f"{base}{self.uid}"

    def close(self):
        self.S.finish()
        self.es.close()
        return self.nc


NCOL = 2300
NCOLP = 1920
NCOLW = NCOLP + 384
VCOLS = [(NCOLP, NCOLW)]
NV = 384
OFF = dict(a_x=0, a_gate=256, b_q=512, b_kv=768, c_q=1024, c_k=1152, c_og=1280, d_cq=1536, d_kr=1728, c_glr=1760,
           b_gate=1776, d_ckv=1792)


def perm_cols():
    o = dict(a_x=0, a_gate=256, b_q=512, b_kv=768, b_gate=1152, c_q=1164, c_k=1292, c_v=1420, c_glr=1676, c_og=1692,
             d_cq=1948, d_ckv=2140, d_kr=2268)
    r = lambda a, n: list(range(a, a + n))
    p = (r(o["a_x"], 256) + r(o["a_gate"], 256) + r(o["b_q"], 256)
         + r(o["b_kv"], 64) + r(o["b_kv"] + 64, 64) + r(o["b_kv"] + 128, 64) + r(o["b_kv"] + 256, 64)
         + r(o["c_q"], 128) + r(o["c_k"], 128) + r(o["c_og"], 256) + r(o["d_cq"], 192) + r(o["d_kr"], 32)
         + r(o["c_glr"], 16) + r(o["b_gate"], 12) + r(o["b_gate"], 4) + r(o["d_ckv"], 128))
    assert len(p) == NCOLP
    p += r(o["b_kv"] + 192, 64) + r(o["b_kv"] + 320, 64) + r(o["c_v"], 256)
    assert len(p) == NCOLW
    return np.array(p)
DFF = 2816


def load_cast_weight(kb, w_dram, wsb, kchunks, ncols, stage, key, piece=1024):
    S = kb.S
    i = 0
    for c in range(kchunks):
        for c0 in range(0, ncols, piece):
            c1 = min(ncols, c0 + piece)
            st = stage[i % 2]
            sk = f"wstage{i % 2}"
            S.dma("sp", lambda e, st=st, c=c, c0=c0, c1=c1: e.dma_start(out=st[:, 0:c1 - c0], in_=w_dram[c * 128:(c + 1) * 128, c0:c1]),
                  writes=[sk])
            eng = "act" if i % 2 == 0 else "pool"
            if eng == "act":
                S.op("act", lambda e, st=st, c=c, c0=c0, c1=c1: e.copy(out=wsb[:, c, c0:c1], in_=st[:, 0:c1 - c0]), reads=[sk], writes=[key])
            else:
                S.op("pool", lambda e, st=st, c=c, c0=c0, c1=c1: e.tensor_copy(out=wsb[:, c, c0:c1], in_=st[:, 0:c1 - c0]), reads=[sk], writes=[key])
            i += 1


def rms_stats(kb, src3, nch, T, sq, ones_bf, ps_ap, rstd, denom, keys_in, key_sq, key_ps, key_rstd):
    S = kb.S
    S.op("act", lambda e: e.activation(out=sq, in_=src3, func=AF.Square), reads=keys_in, writes=[key_sq])
    for c in range(nch):
        S.op("pe", lambda e, c=c: e.matmul(ps_ap, lhsT=ones_bf, rhs=sq[:, c, :], start=(c == 0), stop=(c == nch - 1)),
             reads=[key_sq, "ones_bf"], writes=[key_ps])
    S.op("act", lambda e: e.activation(out=rstd, in_=ps_ap, func=AF.Sqrt, bias=EPS, scale=1.0 / denom), writes=[key_ps, key_rstd])
    S.op("dve", lambda e: e.reciprocal(out=rstd, in_=rstd), reads=[key_rstd], writes=[key_rstd])


def phase_A(kb, xT, gain, w, pT, pV, GP, GV, ag):
    S = kb.S
    m0 = kb.mark()
    T = 512
    NT = 8
    xv = xT.rearrange("(c p) t -> p c t", p=128)

    wsb = kb.alloc(8 * NCOLW, BF16).rearrange("p (c n) -> p c n", c=8)
    gsb = kb.alloc(8)
    ones_bf = kb.alloc(128, BF16)
    stage = [kb.alloc(1024), kb.alloc(1024)]
    xt = [kb.alloc(8 * T).rearrange("p (c t) -> p c t", c=8) for _ in range(2)]
    sq = kb.alloc(8 * T, BF16).rearrange("p (c t) -> p c t", c=8)
    hb = kb.alloc(8 * 4096, BF16).rearrange("p (c t) -> p c t", c=8)
    rstd = kb.alloc(T)
    ost = [kb.alloc(T) for _ in range(4)]
    P = [p[:] for p in kb.psb]

    S.dma("sp", lambda e: e.dma_start(out=gsb, in_=gain[:, :]), writes=["gsb"])
    S.op("pool", lambda e: e.memset(ones_bf, 1.0), writes=["ones_bf"])
    S.dma("sp", lambda e: e.dma_start(out=xt[0], in_=xv[:, :, 0:T]), writes=["xt0"])
    load_cast_weight(kb, w, wsb, 8, NCOLW, stage, "wsb")
    for t in range(NT):
        b = t % 2
        if t + 1 < NT:
            S.dma("sp", lambda e, t=t: e.dma_start(out=xt[(t + 1) % 2], in_=xv[:, :, (t + 1) * T:(t + 2) * T]),
                  writes=[f"xt{(t + 1) % 2}"])
        rms_stats(kb, xt[b], 8, T, sq, ones_bf, P[0], rstd, 1024.0, [f"xt{b}"], "sq", "psb0", "rstd")
        for c in range(8):
            S.op("dve", lambda e, c=c, b=b, t=t: e.scalar_tensor_tensor(out=hb[:, c, t * T:(t + 1) * T], in0=xt[b][:, c, :], scalar=gsb[:, c:c + 1], in1=rstd,
                                                                   op0=ALU.mult, op1=ALU.mult),
                 reads=[f"xt{b}", "rstd", "gsb"], writes=[f"hb{t}"])
    oi = 0
    for t in range(NT):
        for tb in range(4):
            pb = 5 + (tb % 2)
            for c in range(8):
                S.op("pe", lambda e, c=c, t=t, tb=tb, pb=pb: e.matmul(
                    P[pb][:, 0:NV], lhsT=hb[:, c, t * T + tb * 128:t * T + (tb + 1) * 128], rhs=wsb[:, c, NCOLP:NCOLW], start=(c == 0), stop=(c == 7)),
                    reads=[f"hb{t}", "wsb"], writes=[f"psb{pb}"])
            o = oi % 4
            oi += 1
            S.op("act", lambda e, o=o, pb=pb: e.copy(out=ost[o][:, 0:NV], in_=P[pb][:, 0:NV]), writes=[f"psb{pb}", f"ost{o}"])
            S.dma("sp", lambda e, o=o, t=t, tb=tb: e.dma_start(out=pV[t * T + tb * 128: t * T + (tb + 1) * 128, :], in_=ost[o][:, 0:NV]),
                  reads=[f"ost{o}"], writes=[f"XV{t}_{tb}"])
        if t % 2 == 1:
            j = t // 2
            S.collective_async(ag(pV[j * 1024:(j + 1) * 1024, :], GV[j * 2048:(j + 1) * 2048, :]),
                               reads=[f"XV{tt}_{tb}" for tt in (t - 1, t) for tb in range(4)])
    for k in range(NCOLP // 128):
        c0, c1 = k * 128, (k + 1) * 128
        for t in range(NT):
            pb = 1 + (oi % 4)
            for c in range(8):
                S.op("pe", lambda e, c=c, t=t, c0=c0, c1=c1, pb=pb: e.matmul(P[pb][:, :], lhsT=wsb[:, c, c0:c1], rhs=hb[:, c, t * T:(t + 1) * T],
                                                                             start=(c == 0), stop=(c == 7)),
                     reads=[f"hb{t}", "wsb"], writes=[f"psb{pb}"])
            o = oi % 4
            oi += 1
            if t % 2 == 0:
                S.op("act", lambda e, o=o, pb=pb: e.copy(out=ost[o], in_=P[pb][:, :]), writes=[f"psb{pb}", f"ost{o}"])
            else:
                S.op("dve", lambda e, o=o, pb=pb: e.tensor_copy(out=ost[o], in_=P[pb][:, :]), writes=[f"psb{pb}", f"ost{o}"])
            S.dma("sp", lambda e, o=o, c0=c0, c1=c1, t=t: e.dma_start(out=pT[c0:c1, t * T:(t + 1) * T], in_=ost[o]),
                  reads=[f"ost{o}"], writes=[f"XA{k}_{t}"])
        S.collective_async(ag(pT[c0:c1, :], GP[k * 256:(k + 1) * 256, :]), reads=[f"XA{k}_{t}" for t in range(NT)])
    S.cc_wait_all()
    kb.release(m0)


def phase_C(kb, final, xT, GY, gng, nfg, fing, w_out, w_gu, w_dn, xo, NT=16):
    S = kb.S
    m0 = kb.mark()
    T = 256
    xv = xT.rearrange("(c p) t -> p c t", p=128)
    ov = xo.rearrange("(c p) t -> p c t", p=128)

    wo = kb.alloc(8 * 1024, BF16).rearrange("p (c n) -> p c n", c=8)
    wgu = kb.alloc(8 * 2 * DFF, BF16).rearrange("p (c n) -> p c n", c=8)
    wdn = kb.alloc(22 * 1024, BF16).rearrange("p (c n) -> p c n", c=22)
    g3 = kb.alloc(24)
    ones_bf = kb.alloc(128, BF16)
    xt = kb.alloc(8 * T).rearrange("p (c t) -> p c t", c=8)
    yt = kb.alloc(8 * T).rearrange("p (c t) -> p c t", c=8)
    ytf = yt.rearrange("p c t -> p (c t)")
    stage = [ytf[:, 0:1024], ytf[:, 1024:2048]]
    sq = kb.alloc(8 * T, BF16).rearrange("p (c t) -> p c t", c=8)
    hb = kb.alloc(8 * T, BF16).rearrange("p (c t) -> p c t", c=8)
    aT = kb.alloc(22 * T, BF16).rearrange("p (c t) -> p c t", c=22)
    rstd4 = kb.alloc(4 * T).rearrange("p (c t) -> p c t", c=4)
    rstd = kb.alloc(T)
    sg = [kb.alloc(T), kb.alloc(T)]
    P = [p[:] for p in kb.psb]

    S.dma("sp", lambda e: e.dma_start(out=g3[:, 0:8], in_=gng[:, :]), writes=["g3"])
    S.dma("sp", lambda e: e.dma_start(out=g3[:, 8:16], in_=nfg[:, :]), writes=["g3"])
    S.dma("sp", lambda e: e.dma_start(out=g3[:, 16:24], in_=fing[:, :]), writes=["g3"])
    S.op("pool", lambda e: e.memset(ones_bf, 1.0), writes=["ones_bf"])
    load_cast_weight(kb, w_out, wo, 8, 1024, stage, "wo")
    load_cast_weight(kb, w_gu, wgu, 8, 2 * DFF, stage, "wgu")
    load_cast_weight(kb, w_dn, wdn, 22, 1024, stage, "wdn")
    S.barrier()

    for t in range(NT):
        ts = slice(t * T, (t + 1) * T)
        S.dma("sp", lambda e, ts=ts: e.dma_start(out=xt, in_=xv[:, :, ts]), writes=["xt"])
        for cc_ in range(8):
            r0 = cc_ * 128
            S.dma("sp", lambda e, cc_=cc_, r0=r0, t=t: e.dma_start(out=yt[:, cc_, :], in_=GY[r0:r0 + 128, t * T:(t + 1) * T]),
                  writes=["yt"])
        S.op("act", lambda e: e.activation(out=sq, in_=yt, func=AF.Square), reads=["yt"], writes=["sq"])
        for g in range(4):
            pa = P[g][:, 0:T]
            for j in range(2):
                S.op("pe", lambda e, g=g, j=j, pa=pa: e.matmul(pa, lhsT=ones_bf, rhs=sq[:, 2 * g + j, :], start=(j == 0), stop=(j == 1)),
                     reads=["sq", "ones_bf"], writes=[f"psb{g}"])
            S.op("act", lambda e, g=g, pa=pa: e.activation(out=rstd4[:, g, :], in_=pa, func=AF.Sqrt, bias=EPS, scale=1.0 / 256.0),
                 writes=[f"psb{g}", f"rstd4_{g}"])
            S.op("dve", lambda e, g=g: e.reciprocal(out=rstd4[:, g, :], in_=rstd4[:, g, :]), reads=[f"rstd4_{g}"], writes=[f"rstd4_{g}"])
        for c in range(8):
            eng = "dve"
            S.op(eng, lambda e, c=c: e.scalar_tensor_tensor(out=hb[:, c, :], in0=yt[:, c, :], scalar=g3[:, c:c + 1], in1=rstd4[:, c // 2, :],
                                                         op0=ALU.mult, op1=ALU.mult),
                 reads=["yt", f"rstd4_{c // 2}", "g3"], writes=[f"hb{c}"])
        for m in range(8):
            pa = P[4 + m % 4][:, 0:T]
            for c in range(8):
                S.op("pe", lambda e, m=m, c=c, pa=pa: e.matmul(pa, lhsT=wo[:, c, m * 128:(m + 1) * 128], rhs=hb[:, c, :], start=(c == 0), stop=(c == 7)),
                     reads=[f"hb{c}", "wo"], writes=[f"psb{4 + m % 4}"])
            S.op("dve", lambda e, m=m, pa=pa: e.tensor_tensor(out=xt[:, m, :], in0=xt[:, m, :], in1=pa, op=ALU.add),
                 reads=["xt"], writes=["xt", f"psb{4 + m % 4}"])
        rms_stats(kb, xt, 8, T, sq, ones_bf, P[0][:, 0:T], rstd, 1024.0, ["xt"], "sq", "psb0", "rstd")
        for c in range(8):
            eng = "dve"
            S.op(eng, lambda e, c=c: e.scalar_tensor_tensor(out=hb[:, c, :], in0=xt[:, c, :], scalar=g3[:, 8 + c:9 + c], in1=rstd,
                                                         op0=ALU.mult, op1=ALU.mult),
                 reads=["xt", "rstd", "g3"], writes=[f"hb{c}"])
        for j in range(22):
            pg = P[j % 2][:, 0:T]
            pu = P[2 + j % 2][:, 0:T]
            for c in range(8):
                S.op("pe", lambda e, j=j, c=c, pg=pg: e.matmul(pg, lhsT=wgu[:, c, j * 128:(j + 1) * 128], rhs=hb[:, c, :], start=(c == 0), stop=(c == 7)),
                     reads=[f"hb{c}", "wgu"], writes=[f"psb{j % 2}"])
            for c in range(8):
                S.op("pe", lambda e, j=j, c=c, pu=pu: e.matmul(pu, lhsT=wgu[:, c, DFF + j * 128:DFF + (j + 1) * 128], rhs=hb[:, c, :], start=(c == 0), stop=(c == 7)),
                     reads=[f"hb{c}", "wgu"], writes=[f"psb{2 + j % 2}"])
            S.op("act", lambda e, j=j, pg=pg: e.activation(out=sg[j % 2], in_=pg, func=AF.Silu), writes=[f"psb{j % 2}", f"sg{j % 2}"])
            S.op("dve", lambda e, j=j, pu=pu: e.tensor_tensor(out=aT[:, j, :], in0=sg[j % 2], in1=pu, op=ALU.mult),
                 reads=[f"sg{j % 2}"], writes=[f"aT{j}", f"psb{2 + j % 2}"])
        for m in range(8):
            pa = P[4 + m % 4][:, 0:T]
            for k in range(22):
                S.op("pe", lambda e, m=m, k=k, pa=pa: e.matmul(pa, lhsT=wdn[:, k, m * 128:(m + 1) * 128], rhs=aT[:, k, :], start=(k == 0), stop=(k == 21)),
                     reads=[f"aT{k}", "wdn"], writes=[f"psb{4 + m % 4}"])
            S.op("dve", lambda e, m=m, pa=pa: e.tensor_tensor(out=xt[:, m, :], in0=xt[:, m, :], in1=pa, op=ALU.add),
                 reads=["xt"], writes=["xt", f"psb{4 + m % 4}"])
        if final:
            rms_stats(kb, xt, 8, T, sq, ones_bf, P[0][:, 0:T], rstd, 1024.0, ["xt"], "sq", "psb0", "rstd")
            for c in range(8):
                eng = "dve"
                S.op(eng, lambda e, c=c: e.scalar_tensor_tensor(out=yt[:, c, :], in0=xt[:, c, :], scalar=g3[:, 16 + c:17 + c], in1=rstd,
                                                             op0=ALU.mult, op1=ALU.mult),
                     reads=["xt", "rstd", "g3"], writes=["yt"])
            S.dma("sp", lambda e, ts=ts: e.dma_start(out=ov[:, :, ts], in_=yt), reads=["yt"])
        else:
            S.dma("sp", lambda e, ts=ts: e.dma_start(out=ov[:, :, ts], in_=xt), reads=["xt"])
    S.barrier()
    kb.release(m0)


N = 8192
TQ = 512
NQT = N // TQ
NEG = -30000.0
PI = float(np.pi)
TWO_PI = float(2 * np.pi)
THETA = 10000.0


def pk(b):
    return f"psb{b}"


def host_consts():
    c = {}
    c["ident"] = np.eye(128, dtype=np.float32)
    R = np.zeros((128, 128), np.float32)
    for blk in range(2):
        for m in range(64):
            if m < 32:
                R[blk * 64 + m + 32, blk * 64 + m] = -1.0
            else:
                R[blk * 64 + m - 32, blk * 64 + m] = 1.0
    c["rbd"] = R
    R32 = np.zeros((128, 32), np.float32)
    for m in range(32):
        if m < 16:
            R32[64 + m + 16, m] = -1.0
        else:
            R32[64 + m - 16, m] = 1.0
    c["r32"] = R32
    invf = np.zeros((128, 2), np.float32)
    for p in range(128):
        invf[p, 0] = np.float32(THETA) ** np.float32(-(2.0 * ((p % 64) % 32)) / 64.0)
    for p in range(64, 96):
        invf[p, 1] = np.float32(THETA) ** np.float32(-(2.0 * ((p - 64) % 16)) / 32.0)
    c["invf"] = invf
    k = np.arange(128)[:, None]
    q = np.arange(512)[None, :]
    c["tri"] = np.stack([np.where(k + i * 128 <= q, 0.0, NEG) for i in range(4)]).astype(np.float32)
    c["win"] = np.stack([np.where((k + (i - 4) * 128 <= q) & (k + (i - 4) * 128 > q - 512), 0.0, NEG) for i in range(8)]).astype(np.float32)
    c["cmpm"] = np.stack([np.where(16 * k + 31 <= i * 512 + q, 0.0, NEG) for i in range(5)]).astype(np.float32)
    n = np.arange(512)
    s = np.arange(128)
    ov = ((n[:, None] * 16 < s[None, :] * 64 + 64) & (n[:, None] * 16 + 32 > s[None, :] * 64)).astype(np.float32)
    ov[511] = 0.0
    ovl = np.zeros((4, 128, 129), np.float32)
    ovl[:, :, :128] = ov.reshape(4, 128, 128)
    ovl[:, :, 128] = 1.0
    c["ovl"] = ovl
    c["eall"] = (np.arange(N)[None, :] // 64 == np.arange(128)[:, None]).astype(np.float32)
    ql = np.arange(128)[:, None] // 64
    j = np.arange(254)[None, :] - 126
    c["bv"] = (j <= ql - 2).astype(np.float32)
    c["bf"] = (np.where((j == ql) | (j == ql - 1), 1e9, 0.0) + np.where(j > ql, -1.0, 0.0)).astype(np.float32)
    selg = np.zeros((8, 6 * 64), np.float32)
    for r in range(6):
        selg[r, r * 64:(r + 1) * 64] = 1.0
    c["selg"] = selg
    c["tri64"] = (np.arange(64)[:, None] <= np.arange(64)[None, :]).astype(np.float32)
    return c


CONST_SHAPES = {"ident": [128, 128], "rbd": [128, 128], "r32": [128, 32], "invf": [128, 2], "tri": [4, 128, 512],
                "win": [8, 128, 512], "cmpm": [5, 128, 512], "ovl": [4, 128, 129], "eall": [128, N], "bv": [128, 254],
                "bf": [128, 254], "selg": [8, 384], "tri64": [64, 64]}

IN_SHAPES = {
    "pos": ([1, N], I32),
    "lru_x": ([2, 128, N], F32), "lru_w": ([2, 2, 64, 64], F32), "lru_v": ([128, 8], F32),
    "mla_cq": ([192, N], F32), "mla_ckv": ([128, N], F32), "mla_kr": ([32, N], F32),
    "mla_wuq": ([192, 192], F32), "mla_wk": ([128, 128], F32), "mla_wv": ([128, 128], F32), "mla_v": ([128, 3], F32),
    "nsa_q": ([2, 128, N], F32), "nsa_k": ([3, 64, N], F32), "nsa_vc": ([64, N], F32), "nsa_vs": ([N, 64], F32),
    "nsa_vw": ([N, 64], F32), "nsa_g": ([6, N], F32), "nsa_pos": ([128, 32], F32), "nsa_w1": ([2, 2048, 256], F32),
    "nsa_b1": ([128, 4], F32), "nsa_w2": ([2, 256, 64], F32),
    "gla_q": ([64, N], F32), "gla_k": ([64, N], F32), "gla_v": ([N, 128], F32), "gla_glr": ([16, N], F32),
    "gla_og": ([128, N], F32), "gla_wg2": ([16, 64], F32), "gla_v2": ([128, 2], F32),
}


class Ctx:
    pass


class YView:
    def __init__(self, ap):
        self.ap = ap

    def __getitem__(self, key):
        rs, cs = key
        half = cs.start // 4096
        assert (cs.stop - 1) // 4096 == half
        return self.ap[half * 512 + rs.start:half * 512 + rs.stop, cs.start - half * 4096:cs.stop - half * 4096]


SEL = [("a_x", 128, False, 128), ("a_gate", 128, False, 128), ("b_q", 128, False, 128), ("b_q", 128, True, 128),
       ("b_gate", 6, False, 6), ("c_q", 64, False, 64), ("c_k", 64, False, 64), ("c_og", 128, False, 128)]
SELROWS = sum(x[3] for x in SEL)


class Loader:
    def __init__(self, S, GP, GV, MYP, MYV):
        self.S = S
        self.GP, self.GV, self.MYP, self.MYV = GP, GV, MYP, MYV
        self.row0 = {}
        r = 0
        for nm, hpm, inv, n in SEL:
            self.row0[(nm, inv)] = r
            r += n

    @staticmethod
    def gprow(r, half):
        return (r // 128) * 256 + half * 128 + r % 128

    @staticmethod
    def gvrow(t):
        half, tl = t // 4096, t % 4096
        return (tl // 1024) * 2048 + half * 1024 + tl % 1024

    def select(self):
        S = self.S
        i = 0
        for nm, hpm, inv, n in SEL:
            r0 = self.row0[(nm, inv)]
            mult = 256 if hpm == 128 else hpm
            for hf in range(2):
                q = "sp" if i % 2 == 0 else "act"
                i += 1

                def emit(e, nm=nm, mult=mult, inv=inv, n=n, r0=r0, hf=hf, q=q):
                    hp = S.rt["hp_" + q]
                    start = ((1 - hp) if inv else hp) * mult + self.gprow(OFF[nm], hf)
                    return e.dma_start(out=self.MYP[r0:r0 + n, hf * 4096:(hf + 1) * 4096], in_=self.GP[bass.ds(start, n), :])
                S.dma(q, emit, writes=["MYP"])
        for j in range(4):
            S.dma("sp", lambda e, j=j: e.dma_start(out=self.MYV[j * 2048:(j + 1) * 2048, :],
                                                   in_=self.GV[:, bass.ds(S.rt["hp_sp"] * 128 + 128, 128)][j * 2048:(j + 1) * 2048, :]), writes=["MYV"])
        S.barrier()

    def pt(self, off, hpm, n, c0, c1, inv=False):
        if hpm == 0:
            half = c0 // 4096
            assert off // 128 == (off + n - 1) // 128
            base = self.gprow(off, half)
            return self.GP[base:base + n, c0 - half * 4096:c1 - half * 4096]
        for nm, hm, iv, nn in SEL:
            if hm == hpm and iv == inv and OFF[nm] <= off and (off - OFF[nm]) + n <= nn:
                r0 = self.row0[(nm, inv)] + (off - OFF[nm])
                return self.MYP[r0:r0 + n, c0:c1]
        raise KeyError((off, hpm, n, inv))

    def pv(self, coff, hpm, n, t0, t1):
        assert t0 // 1024 == (t1 - 1) // 1024
        g0 = self.gvrow(t0)
        if hpm == 0:
            return self.GV[g0:g0 + (t1 - t0), coff:coff + n]
        assert coff == 128 and n == 128
        return self.MYV[g0:g0 + (t1 - t0), :]


def common_setup(kb, D):
    S = kb.S
    c = Ctx()
    c.ident = kb.alloc(128)
    c.ones_f = kb.alloc(128)
    c.ones_bf = kb.alloc(128, BF16)
    c.ident_bf = kb.alloc(128, BF16)
    S.dma("sp", lambda e: e.dma_start(out=c.ident, in_=D["ident"][:, :]), writes=["ident"])
    S.op("pool", lambda e: e.memset(c.ones_f, 1.0), writes=["ones_f"])
    S.op("pool", lambda e: e.memset(c.ones_bf, 1.0), writes=["ones_bf"])
    S.op("act", lambda e: e.copy(out=c.ident_bf, in_=c.ident), reads=["ident"], writes=["ident_bf"])
    c.invf = kb.alloc(2)
    S.dma("sp", lambda e: e.dma_start(out=c.invf, in_=D["invf"][:, :]), writes=["invf"])
    return c


def rope_tables(kb, c, posf, posk, r0, r1, col, T, bank, tag):
    S = kb.S
    P = kb.psb[bank]
    n = r1 - r0
    rs = slice(r0, r1)
    a, kf, ki, sn, cs = T["ang"], T["kf"], T["ki"], T["sin"], T["cos"]
    S.op("pe", lambda e: e.matmul(P[rs, :], lhsT=c.ones_f[0:1, 0:n], rhs=posf[0:1, :], start=True, stop=True),
         reads=["ones_f", posk], writes=[pk(bank)])
    S.op("dve", lambda e: e.tensor_scalar(out=a[rs, :], in0=P[rs, :], scalar1=c.invf[rs, col:col + 1], scalar2=None, op0=ALU.mult),
         reads=["invf"], writes=[pk(bank), tag + "ang"])
    S.op("dve", lambda e: e.tensor_scalar(out=ki[rs, :], in0=a[rs, :], scalar1=1.0 / TWO_PI, scalar2=None, op0=ALU.mult),
         reads=[tag + "ang"], writes=[tag + "ki"])
    S.op("dve", lambda e: e.tensor_copy(out=kf[rs, :], in_=ki[rs, :]), reads=[tag + "ki"], writes=[tag + "kf"])
    S.op("dve", lambda e: e.scalar_tensor_tensor(out=a[rs, :], in0=kf[rs, :], scalar=-TWO_PI, in1=a[rs, :], op0=ALU.mult, op1=ALU.add),
         reads=[tag + "kf"], writes=[tag + "ang"])
    S.op("dve", lambda e: e.tensor_scalar(out=kf[rs, :], in0=a[rs, :], scalar1=PI, scalar2=-TWO_PI, op0=ALU.is_gt, op1=ALU.mult),
         reads=[tag + "ang"], writes=[tag + "kf"])
    S.op("dve", lambda e: e.tensor_tensor(out=sn[rs, :], in0=a[rs, :], in1=kf[rs, :], op=ALU.add),
         reads=[tag + "ang", tag + "kf"], writes=[tag + "sin"])
    S.op("dve", lambda e: e.tensor_scalar(out=a[rs, :], in0=a[rs, :], scalar1=PI / 2, scalar2=None, op0=ALU.add),
         reads=[], writes=[tag + "ang"])
    S.op("dve", lambda e: e.tensor_scalar(out=kf[rs, :], in0=a[rs, :], scalar1=PI, scalar2=-TWO_PI, op0=ALU.is_gt, op1=ALU.mult),
         reads=[tag + "ang"], writes=[tag + "kf"])
    S.op("dve", lambda e: e.tensor_tensor(out=cs[rs, :], in0=a[rs, :], in1=kf[rs, :], op=ALU.add),
         reads=[tag + "ang", tag + "kf"], writes=[tag + "cos"])
    S.op("act", lambda e: e.activation(out=sn[rs, :], in_=sn[rs, :], func=AF.Sin), writes=[tag + "sin"])
    S.op("act", lambda e: e.activation(out=cs[rs, :], in_=cs[rs, :], func=AF.Sin), writes=[tag + "cos"])


def alloc_tables(kb):
    return {"ang": kb.alloc(512), "kf": kb.alloc(512), "ki": kb.alloc(512, I32), "sin": kb.alloc(512), "cos": kb.alloc(512)}


def load_pos(kb, D, posi, posf, t):
    S = kb.S
    S.dma("sp", lambda e: e.dma_start(out=posi[0:1, :], in_=D["pos"][0:1, t * TQ:(t + 1) * TQ]), writes=["posi"])
    S.op("dve", lambda e: e.tensor_copy(out=posf[0:1, :], in_=posi[0:1, :]), reads=["posi"], writes=["posf"])


class Attn:
    def __init__(self, kb, sbanks=(0, 1), npt=3):
        self.kb = kb
        self.sbanks = sbanks
        self.PT = [kb.alloc(512, BF16) for _ in range(npt)]
        self.pti = 0
        self.si = 0

    def run(self, blocks, obank, defer=None):
        kb = self.kb
        S = kb.S
        n = len(blocks)
        O = kb.psb[obank]
        banks = []

        def scores(i):
            bank = self.sbanks[self.si % len(self.sbanks)]
            self.si += 1
            banks.append(bank)
            mms = blocks[i][0]
            for j, (l, r, ks) in enumerate(mms):
                S.op("pe", lambda e, l=l, r=r, j=j, bank=bank, nm=len(mms): e.matmul(kb.psb[bank][:, :], lhsT=l, rhs=r, start=(j == 0), stop=(j == nm - 1)),
                     reads=ks, writes=[pk(bank)])

        scores(0)
        for i in range(n):
            if i + 1 < n:
                scores(i + 1)
            bank = banks[i]
            pi_ = self.pti % len(self.PT)
            self.pti += 1
            pt = self.PT[pi_]
            S.op("act", lambda e, pt=pt, bank=bank: e.activation(out=pt, in_=kb.psb[bank][:, :], func=AF.Exp), writes=[pk(bank), f"PT{pi_}"])
            v, vk = blocks[i][1], blocks[i][2]
            S.op("pe", lambda e, v=v, pt=pt, i=i: e.matmul(O[0:65, :], lhsT=v, rhs=pt, start=(i == 0), stop=(i == n - 1)),
                 reads=[f"PT{pi_}"] + vk, writes=[pk(obank)])
            if defer and i == min(2, n - 1):
                for f in defer:
                    f()
                del defer[:]


def norm_coef(kb, c, obank, rowbuf, bcbank, bcs):
    S = kb.S
    O = kb.psb[obank]
    B = kb.psb[bcbank]
    S.op("dve", lambda e: e.tensor_scalar_max(out=rowbuf[64:65, :], in0=O[64:65, :], scalar1=1e-30), writes=[pk(obank), "rowbuf"])
    S.op("dve", lambda e: e.reciprocal(out=rowbuf[64:65, :], in_=rowbuf[64:65, :]), writes=["rowbuf"])
    S.op("pe", lambda e: e.matmul(B[0:64, :], lhsT=c.ones_f[64:65, 0:64], rhs=rowbuf[64:65, :], start=True, stop=True),
         reads=["rowbuf", "ones_f"], writes=[pk(bcbank)])
    S.op("act", lambda e: e.copy(out=bcs[0:64, :], in_=B[0:64, :]), writes=[pk(bcbank), "bcs"])


def part_lru(kb, c, D, yT, L):
    S = kb.S
    m0 = kb.mark()
    xa = kb.alloc(N + 4)
    xc = kb.alloc(N)
    A = kb.alloc(N)
    U = kb.alloc(N)
    G = kb.alloc(N)
    xcb = kb.alloc(N, BF16)
    vec = kb.alloc(16)
    wtmp = kb.alloc(256)
    wbd = kb.alloc(256, BF16)
    T1 = xa[:, 0:N]
    P = kb.psb
    S.op("pool", lambda e: e.memset(xa[:, 0:3], 0.0), writes=["xa_pad"])
    for hf in range(2):
        S.dma("sp", lambda e, hf=hf: e.dma_start(out=xa[:, 3 + hf * 4096:3 + (hf + 1) * 4096], in_=L.pt(OFF["a_x"], 128, 128, hf * 4096, (hf + 1) * 4096)), writes=["xa"])
        S.dma("sp", lambda e, hf=hf: e.dma_start(out=G[:, hf * 4096:(hf + 1) * 4096], in_=L.pt(OFF["a_gate"], 128, 128, hf * 4096, (hf + 1) * 4096)), writes=["G"])
    S.dma("sp", lambda e: e.dma_start(out=vec[:, 0:8], in_=D["lru_v"][:, :]), writes=["vec"])
    S.op("pool", lambda e: e.memset(wtmp, 0.0), writes=["wtmp"])
    for a in range(2):
        for b in range(2):
            S.dma("sp", lambda e, a=a, b=b: e.dma_start(out=wtmp[b * 64:(b + 1) * 64, a * 128 + b * 64:a * 128 + (b + 1) * 64], in_=D["lru_w"][a, b]),
                  writes=["wtmp"])
    S.op("act", lambda e: e.copy(out=wbd, in_=wtmp), reads=["wtmp"], writes=["wbd"])
    S.op("act", lambda e: e.activation(out=vec[:, 8:9], in_=vec[:, 7:8], func=AF.Exp, scale=-1.0), reads=["vec"], writes=["vec8"])
    S.op("act", lambda e: e.activation(out=vec[:, 8:9], in_=vec[:, 8:9], func=AF.Ln, bias=1.0), writes=["vec8"])
    S.op("dve", lambda e: e.tensor_scalar(out=vec[:, 9:10], in0=vec[:, 8:9], scalar1=-8.0, scalar2=None, op0=ALU.mult), reads=["vec8"], writes=["vec9"])
    S.op("dve", lambda e: e.tensor_scalar(out=xc, in0=xa[:, 0:N], scalar1=vec[:, 0:1], scalar2=vec[:, 4:5], op0=ALU.mult, op1=ALU.add),
         reads=["xa", "xa_pad", "vec"], writes=["xc"])
    for j in range(1, 4):
        S.op("dve", lambda e, j=j: e.scalar_tensor_tensor(out=xc, in0=xa[:, j:j + N], scalar=vec[:, j:j + 1], in1=xc, op0=ALU.mult, op1=ALU.add),
             reads=["xa", "xa_pad", "vec"], writes=["xc"])
    S.op("act", lambda e: e.copy(out=xcb, in_=xc), reads=["xc"], writes=["xcb"])
    allA = [f"A{t}" for t in range(16)]
    allU = [f"U{t}" for t in range(16)]
    for t in range(16):
        ts = slice(t * 512, (t + 1) * 512)
        b0, b1 = 2 * (t % 2), 2 * (t % 2) + 1
        S.op("pe", lambda e, ts=ts, b0=b0: e.matmul(P[b0][:, :], lhsT=wbd[:, 0:128], rhs=xcb[:, ts], start=True, stop=True),
             reads=["xcb", "wbd"], writes=[pk(b0)])
        S.op("pe", lambda e, ts=ts, b1=b1: e.matmul(P[b1][:, :], lhsT=wbd[:, 128:256], rhs=xcb[:, ts], start=True, stop=True),
             reads=["xcb", "wbd"], writes=[pk(b1)])
        S.op("act", lambda e, ts=ts, b0=b0: e.activation(out=A[:, ts], in_=P[b0][:, :], func=AF.Sigmoid, bias=vec[:, 5:6]),
             reads=["vec"], writes=[pk(b0), f"A{t}"])
        S.op("act", lambda e, ts=ts, b1=b1: e.activation(out=U[:, ts], in_=P[b1][:, :], func=AF.Sigmoid, bias=vec[:, 6:7]),
             reads=["vec"], writes=[pk(b1), f"U{t}"])
    S.op("act", lambda e: e.activation(out=A, in_=A, func=AF.Exp, scale=vec[:, 9:10]), reads=["vec9"], writes=["A"] + allA)
    S.op("pool", lambda e: e.tensor_tensor(out=T1, in0=A, in1=A, op=ALU.mult), reads=["A"], writes=["xa", "xa_pad"])
    S.op("act", lambda e: e.activation(out=T1, in_=T1, func=AF.Sqrt, bias=1.0, scale=-1.0), writes=["xa"])
    S.op("dve", lambda e: e.tensor_tensor(out=U, in0=U, in1=xc, op=ALU.mult), reads=["xc"], writes=["U"] + allU)
    S.op("dve", lambda e: e.tensor_tensor(out=U, in0=U, in1=T1, op=ALU.mult), reads=["xa"], writes=["U"])
    S.op("dve", lambda e: e.tensor_tensor_scan(out=xc, data0=A, data1=U, initial=0.0, op0=ALU.mult, op1=ALU.add), reads=["A", "U"], writes=["xc"])
    S.op("pool", lambda e: e.tensor_tensor(out=T1, in0=G, in1=G, op=ALU.mult), reads=["G", "U"], writes=["xa"])
    S.op("pool", lambda e: e.tensor_scalar(out=T1, in0=T1, scalar1=0.044715, scalar2=1.0, op0=ALU.mult, op1=ALU.add), writes=["xa"])
    S.op("pool", lambda e: e.tensor_tensor(out=T1, in0=T1, in1=G, op=ALU.mult), reads=["G"], writes=["xa"])
    S.op("act", lambda e: e.activation(out=T1, in_=T1, func=AF.Sigmoid, scale=1.5957691216057308), writes=["xa"])
    S.op("dve", lambda e: e.tensor_tensor(out=G, in0=G, in1=T1, op=ALU.mult), reads=["xa"], writes=["G"])
    S.op("dve", lambda e: e.tensor_tensor(out=A, in0=xc, in1=G, op=ALU.mult), reads=["xc", "G"], writes=["A"])
    for hf in range(2):
        S.dma("sp", lambda e, hf=hf: e.dma_start(out=yT[0:128, hf * 4096:(hf + 1) * 4096], in_=A[:, hf * 4096:(hf + 1) * 4096]), reads=["A"])
    S.barrier()
    kb.release(m0)


def part_mla(kb, c, D, yT, L):
    S = kb.S
    m0 = kb.mark()
    P = kb.psb
    SC = float(96 ** -0.5)
    QD = [kb.alloc(N, BF16) for _ in range(2)]
    KD = [kb.alloc(N, BF16) for _ in range(2)]
    VD = kb.alloc(64 * 2 * 66, BF16).rearrange("p (b h d) -> p b h d", b=64, h=2)
    tri = kb.alloc(4 * 512, BF16).rearrange("p (i q) -> p i q", i=4)
    wuq = kb.alloc(2 * 192, BF16).rearrange("p (c n) -> p c n", c=2)
    wk = kb.alloc(128, BF16)
    wv = kb.alloc(128, BF16)
    vec = kb.alloc(4)
    r32 = kb.alloc(32)
    st = kb.alloc(512)
    S.op("pool", lambda e: e.memset(VD, 1.0), writes=["VD"])
    S.dma("sp", lambda e: e.dma_start(out=vec[:, 0:3], in_=D["mla_v"][:, :]), writes=["mvec"])
    S.dma("sp", lambda e: e.dma_start(out=r32, in_=D["r32"][:, :]), writes=["r32"])
    for i in range(4):
        S.dma("sp", lambda e, i=i: e.dma_start(out=st, in_=D["tri"][i]), writes=["st"])
        S.op("act", lambda e, i=i: e.copy(out=tri[:, i, :], in_=st), reads=["st"], writes=["tri"])
    S.dma("sp", lambda e: e.dma_start(out=st[:, 0:192], in_=D["mla_wuq"][0:128, :]), writes=["st"])
    S.op("act", lambda e: e.copy(out=wuq[:, 0, :], in_=st[:, 0:192]), reads=["st"], writes=["wuq"])
    S.dma("sp", lambda e: e.dma_start(out=st[0:64, 0:192], in_=D["mla_wuq"][128:192, :]), writes=["st"])
    S.op("act", lambda e: e.copy(out=wuq[0:64, 1, :], in_=st[0:64, 0:192]), reads=["st"], writes=["wuq"])
    S.dma("sp", lambda e: e.dma_start(out=st[:, 0:128], in_=D["mla_wk"][:, :]), writes=["st"])
    S.op("act", lambda e: e.copy(out=wk, in_=st[:, 0:128]), reads=["st"], writes=["wk"])
    S.dma("sp", lambda e: e.dma_start(out=st[:, 0:128], in_=D["mla_wv"][:, :]), writes=["st"])
    S.op("act", lambda e: e.copy(out=wv, in_=st[:, 0:128]), reads=["st"], writes=["wv"])

    m1 = kb.mark()
    cq0 = kb.alloc(512)
    cq1 = kb.alloc(512)
    ckv = kb.alloc(512)
    krt = kb.alloc(512)
    sq0 = kb.alloc(512, BF16)
    sq1 = kb.alloc(512, BF16)
    cn0 = kb.alloc(512, BF16)
    cn1 = kb.alloc(512, BF16)
    ckn = kb.alloc(512, BF16)
    rstd = kb.alloc(512)
    qr = kb.alloc(512)
    t1 = kb.alloc(512)
    t2 = kb.alloc(512)
    posi = kb.alloc(512, I32)
    posf = kb.alloc(512)
    T = alloc_tables(kb)
    R = slice(64, 96)
    for t in range(NQT):
        ts = slice(t * TQ, (t + 1) * TQ)
        load_pos(kb, D, posi, posf, t)
        S.dma("sp", lambda e, ts=ts: e.dma_start(out=cq0, in_=L.pt(OFF["d_cq"], 0, 128, ts.start, ts.stop)), writes=["cq0"])
        S.dma("sp", lambda e, ts=ts: e.dma_start(out=cq1[0:64, :], in_=L.pt(OFF["d_cq"] + 128, 0, 64, ts.start, ts.stop)), writes=["cq1"])
        S.dma("sp", lambda e, ts=ts: e.dma_start(out=ckv, in_=L.pt(OFF["d_ckv"], 0, 128, ts.start, ts.stop)), writes=["ckv"])
        S.dma("sp", lambda e, ts=ts: e.dma_start(out=krt[R, :], in_=L.pt(OFF["d_kr"], 0, 32, ts.start, ts.stop)), writes=["krt"])
        rope_tables(kb, c, posf, "posf", 64, 96, 1, T, 6, "m")
        S.op("act", lambda e: e.activation(out=sq0, in_=cq0, func=AF.Square), reads=["cq0"], writes=["sq0"])
        S.op("act", lambda e: e.activation(out=sq1[0:64, :], in_=cq1[0:64, :], func=AF.Square), reads=["cq1"], writes=["sq1"])
        S.op("pe", lambda e: e.matmul(P[0][:, :], lhsT=c.ones_bf, rhs=sq0, start=True, stop=False), reads=["sq0", "ones_bf"], writes=[pk(0)])
        S.op("pe", lambda e: e.matmul(P[0][:, :], lhsT=c.ones_bf[0:64, :], rhs=sq1[0:64, :], start=False, stop=True), reads=["sq1", "ones_bf"], writes=[pk(0)])
        S.op("act", lambda e: e.activation(out=rstd, in_=P[0][:, :], func=AF.Sqrt, bias=EPS, scale=1.0 / 192.0), writes=[pk(0), "rstd"])
        S.op("dve", lambda e: e.reciprocal(out=rstd, in_=rstd), writes=["rstd"])
        S.op("dve", lambda e: e.scalar_tensor_tensor(out=cn0, in0=cq0, scalar=vec[:, 0:1], in1=rstd, op0=ALU.mult, op1=ALU.mult),
             reads=["cq0", "rstd", "mvec"], writes=["cn0"])
        S.op("dve", lambda e: e.scalar_tensor_tensor(out=cn1[0:64, :], in0=cq1[0:64, :], scalar=vec[0:64, 1:2], in1=rstd[0:64, :], op0=ALU.mult, op1=ALU.mult),
             reads=["cq1", "rstd", "mvec"], writes=["cn1"])
        for h in range(2):
            hs = slice(h * 96, (h + 1) * 96)
            S.op("pe", lambda e, hs=hs: e.matmul(P[1][0:96, :], lhsT=wuq[:, 0, hs], rhs=cn0, start=True, stop=False), reads=["cn0", "wuq"], writes=[pk(1)])
            S.op("pe", lambda e, hs=hs: e.matmul(P[1][0:96, :], lhsT=wuq[0:64, 1, hs], rhs=cn1[0:64, :], start=False, stop=True), reads=["cn1", "wuq"], writes=[pk(1)])
            S.op("act", lambda e, h=h, ts=ts: e.mul(out=QD[h][0:64, ts], in_=P[1][0:64, :], mul=SC), writes=[pk(1), f"QD{h}"])
            S.op("dve", lambda e: e.tensor_copy(out=qr[R, :], in_=P[1][R, :]), writes=[pk(1), "qr"])
            S.op("pe", lambda e: e.matmul(P[2][R, :], lhsT=r32[R, 0:32], rhs=qr[R, :], start=True, stop=True), reads=["qr", "r32"], writes=[pk(2)])
            S.op("dve", lambda e: e.scalar_tensor_tensor(out=t1[R, :], in0=qr[R, :], scalar=SC, in1=T["cos"][R, :], op0=ALU.mult, op1=ALU.mult),
                 reads=["qr", "mcos"], writes=["t1"])
            S.op("dve", lambda e: e.scalar_tensor_tensor(out=t2[R, :], in0=P[2][R, :], scalar=SC, in1=T["sin"][R, :], op0=ALU.mult, op1=ALU.mult),
                 reads=["msin"], writes=[pk(2), "t2"])
            S.op("dve", lambda e, h=h, ts=ts: e.tensor_tensor(out=QD[h][R, ts], in0=t1[R, :], in1=t2[R, :], op=ALU.add), reads=["t1", "t2"], writes=[f"QD{h}"])
        S.op("act", lambda e: e.activation(out=sq0, in_=ckv, func=AF.Square), reads=["ckv"], writes=["sq0"])
        S.op("pe", lambda e: e.matmul(P[7][:, :], lhsT=c.ones_bf, rhs=sq0, start=True, stop=True), reads=["sq0", "ones_bf"], writes=[pk(7)])
        S.op("act", lambda e: e.activation(out=rstd, in_=P[7][:, :], func=AF.Sqrt, bias=EPS, scale=1.0 / 128.0), writes=[pk(7), "rstd"])
        S.op("dve", lambda e: e.reciprocal(out=rstd, in_=rstd), writes=["rstd"])
        S.op("dve", lambda e: e.scalar_tensor_tensor(out=ckn, in0=ckv, scalar=vec[:, 2:3], in1=rstd, op0=ALU.mult, op1=ALU.mult),
             reads=["ckv", "rstd", "mvec"], writes=["ckn"])
        for h in range(2):
            S.op("pe", lambda e, h=h: e.matmul(P[3][0:64, :], lhsT=wk[:, h * 64:(h + 1) * 64], rhs=ckn, start=True, stop=True), reads=["ckn", "wk"], writes=[pk(3)])
            S.op("act", lambda e, h=h, ts=ts: e.copy(out=KD[h][0:64, ts], in_=P[3][0:64, :]), writes=[pk(3), f"KD{h}"])
        for tb in range(4):
            S.op("pe", lambda e, tb=tb: e.matmul(P[4][:, 0:128], lhsT=ckn[:, tb * 128:(tb + 1) * 128], rhs=wv, start=True, stop=True), reads=["ckn", "wv"], writes=[pk(4)])
            S.op("act", lambda e, tb=tb, t=t: e.copy(out=VD[:, 4 * t + tb, :, 0:64], in_=P[4][:, 0:128].rearrange("p (h d) -> p h d", h=2)),
                 writes=[pk(4), "VD"])
        S.op("pe", lambda e: e.matmul(P[5][R, :], lhsT=r32[R, 0:32], rhs=krt[R, :], start=True, stop=True), reads=["krt", "r32"], writes=[pk(5)])
        S.op("dve", lambda e: e.tensor_tensor(out=t1[R, :], in0=krt[R, :], in1=T["cos"][R, :], op=ALU.mult), reads=["krt", "mcos"], writes=["t1"])
        S.op("dve", lambda e: e.tensor_tensor(out=t2[R, :], in0=P[5][R, :], in1=T["sin"][R, :], op=ALU.mult), reads=["msin"], writes=[pk(5), "t2"])
        for h in range(2):
            S.op("dve", lambda e, h=h, ts=ts: e.tensor_tensor(out=KD[h][R, ts], in0=t1[R, :], in1=t2[R, :], op=ALU.add), reads=["t1", "t2"], writes=[f"KD{h}"])
    S.barrier()
    kb.release(m1)
    att = Attn(kb, sbanks=(0, 1))
    rowbuf = kb.alloc(512)
    bcs = kb.alloc(512)
    yst = [kb.alloc(512), kb.alloc(512)]
    it = 0
    pending = []
    for qt in range(NQT):
        qs = slice(qt * TQ, (qt + 1) * TQ)
        for h in range(2):
            blocks = []
            for kbk in range(4 * qt + 4):
                mms = [(KD[h][0:96, kbk * 128:(kbk + 1) * 128], QD[h][0:96, qs], [f"KD{h}", f"QD{h}"])]
                if kbk >= 4 * qt:
                    mms.append((c.ident_bf, tri[:, kbk - 4 * qt, :], ["ident_bf", "tri"]))
                blocks.append((mms, VD[:, kbk, h, 0:65], ["VD"]))
            ob = 2 + it % 2
            att.run(blocks, ob, defer=pending)

            def fin(ob=ob, ys=yst[it % 2], yk=f"yst{it % 2}", h=h, qs=qs):
                norm_coef(kb, c, ob, rowbuf, 4, bcs)
                S.op("dve", lambda e: e.tensor_tensor(out=ys[0:64, :], in0=P[ob][0:64, :], in1=bcs[0:64, :], op=ALU.mult),
                     reads=["bcs"], writes=[pk(ob), yk])
                S.dma("sp", lambda e: e.dma_start(out=yT[384 + h * 64:384 + (h + 1) * 64, qs], in_=ys[0:64, :]), reads=[yk])
            pending.append(fin)
            it += 1
    for f in pending:
        f()
    S.barrier()
    kb.release(m0)


def load_cast(kb, src_ap, dst_ap, stage, skey, dkey, eng="act", rows=slice(0, 128)):
    S = kb.S
    S.dma("sp", lambda e: e.dma_start(out=stage, in_=(src_ap() if callable(src_ap) else src_ap)), writes=[skey])
    if eng == "act":
        S.op("act", lambda e: e.copy(out=dst_ap, in_=stage), reads=[skey], writes=[dkey])
    else:
        S.op("pool", lambda e: e.tensor_copy(out=dst_ap, in_=stage), reads=[skey], writes=[dkey])


def gelu_tanh(kb, z, u, out, zk, uk, outk):
    S = kb.S
    S.op("pool", lambda e: e.tensor_tensor(out=u, in0=z, in1=z, op=ALU.mult), reads=[zk], writes=[uk])
    S.op("pool", lambda e: e.tensor_scalar(out=u, in0=u, scalar1=0.044715, scalar2=1.0, op0=ALU.mult, op1=ALU.add), writes=[uk])
    S.op("pool", lambda e: e.tensor_tensor(out=u, in0=u, in1=z, op=ALU.mult), reads=[zk], writes=[uk])
    S.op("act", lambda e: e.activation(out=u, in_=u, func=AF.Sigmoid, scale=1.5957691216057308), writes=[uk])
    S.op("dve", lambda e: e.tensor_tensor(out=out, in0=z, in1=u, op=ALU.mult), reads=[zk, uk], writes=[outk])


def part_nsa(kb, c, D, yT, L):
    S = kb.S
    P = kb.psb
    m0 = kb.mark()
    Qm = kb.alloc(N, BF16)
    Qo = kb.alloc(N, BF16)
    KS2 = kb.alloc(N, BF16)
    KW2 = kb.alloc(N, BF16)
    VS = kb.alloc(64 * 66, BF16).rearrange("p (b d) -> p b d", b=64)
    VW = kb.alloc(64 * 66, BF16).rearrange("p (b d) -> p b d", b=64)
    tri = kb.alloc(4 * 512, BF16).rearrange("p (i q) -> p i q", i=4)
    win = kb.alloc(8 * 512, BF16).rearrange("p (i q) -> p i q", i=8)
    cmpm = kb.alloc(5 * 512, BF16).rearrange("p (i q) -> p i q", i=5)
    rbd = kb.alloc(128)
    ovl = kb.alloc(4 * 130, BF16).rearrange("p (i q) -> p i q", i=4)
    bv = kb.alloc(254)
    bf = kb.alloc(254)
    selg = kb.alloc(384)
    KCMP2 = kb.alloc(512, BF16)
    VCMP = kb.alloc(4 * 66, BF16).rearrange("p (b d) -> p b d", b=4)
    st = kb.alloc(1024)
    st3 = st.rearrange("p (b d) -> p b d", d=64)
    S.op("pool", lambda e: e.memset(VS, 1.0), writes=["VS"])
    S.op("pool", lambda e: e.memset(VW, 1.0), writes=["VW"])
    S.op("pool", lambda e: e.memset(VCMP, 1.0), writes=["VCMP"])
    S.dma("sp", lambda e: e.dma_start(out=rbd, in_=D["rbd"][:, :]), writes=["rbd"])
    S.dma("sp", lambda e: e.dma_start(out=bv, in_=D["bv"][:, :]), writes=["bv"])
    S.dma("sp", lambda e: e.dma_start(out=bf, in_=D["bf"][:, :]), writes=["bf"])
    S.dma("sp", lambda e: e.dma_start(out=selg[0:8, :], in_=D["selg"][:, :]), writes=["selg"])
    i = 0
    for nm, dst, cnt in (("tri", tri, 4), ("win", win, 8), ("cmpm", cmpm, 5)):
        for j in range(cnt):
            load_cast(kb, D[nm][j], dst[:, j, :], st[:, 0:512], "st", nm, "act" if i % 2 == 0 else "pool")
            i += 1
    for j in range(4):
        load_cast(kb, D["ovl"][j], ovl[:, j, 0:129], st[:, 0:129], "st", "ovl", "act")
    for nm, dst, coff in (("nsa_vs", VS, 0), ("nsa_vw", VW, 64)):
        for b0 in range(0, 64, 8):
            load_cast(kb, (lambda b0=b0, coff=coff: L.pv(coff, 0, 64, b0 * 128, (b0 + 8) * 128).rearrange("(b p) d -> p b d", p=128)),
                      dst[:, b0:b0 + 8, 0:64], st3[:, 0:8, :], "st", nm[-2:].upper(), "act" if (b0 // 8) % 2 == 0 else "pool")

    m1 = kb.mark()
    KCV = kb.alloc(N)
    xs = [kb.alloc(512), kb.alloc(512)]
    t1s = [kb.alloc(512), kb.alloc(512)]
    t2s = [kb.alloc(512), kb.alloc(512)]
    posi = kb.alloc(512, I32)
    posf = kb.alloc(512)
    T = alloc_tables(kb)
    for hf in range(2):
        S.dma("sp", lambda e, hf=hf: e.dma_start(out=KCV[64:128, hf * 4096:(hf + 1) * 4096], in_=L.pt(OFF["b_kv"] + 64, 0, 64, hf * 4096, (hf + 1) * 4096)), writes=["KCVv"])
    it = 0
    for t in range(NQT):
        ts = slice(t * TQ, (t + 1) * TQ)
        load_pos(kb, D, posi, posf, t)
        rope_tables(kb, c, posf, "posf", 0, 128, 0, T, 6, "n")
        a0, a1 = ts.start, ts.stop
        items = [("q0", [lambda a0=a0, a1=a1: L.pt(OFF["b_q"], 128, 128, a0, a1)], Qm, 0.125, 128),
                 ("q1", [lambda a0=a0, a1=a1: L.pt(OFF["b_q"], 128, 128, a0, a1, inv=True)], Qo, 0.125, 128),
                 ("ks", [lambda a0=a0, a1=a1: L.pt(OFF["b_kv"] + 128, 0, 64, a0, a1)] * 2, KS2, 1.0, 128),
                 ("kw", [lambda a0=a0, a1=a1: L.pt(OFF["b_kv"] + 192, 0, 64, a0, a1)] * 2, KW2, 1.0, 128),
                 ("kc", [lambda a0=a0, a1=a1: L.pt(OFF["b_kv"], 0, 64, a0, a1)], KCV, 1.0, 64)]
        for nm, srcs, dst, sc, rows in items:
            x = xs[it % 2]
            xk = f"xs{it % 2}"
            t1 = t1s[it % 2]
            t2 = t2s[it % 2]
            bank = 4 + it % 2
            if len(srcs) == 2:
                S.dma("sp", lambda e, x=x, srcs=srcs: e.dma_start(out=x[0:64, :], in_=srcs[0]()), writes=[xk])
                S.dma("sp", lambda e, x=x, srcs=srcs: e.dma_start(out=x[64:128, :], in_=srcs[1]()), writes=[xk])
            else:
                S.dma("sp", lambda e, x=x, srcs=srcs, rows=rows: e.dma_start(out=x[0:rows, :], in_=srcs[0]()), writes=[xk])
            R = slice(0, rows)
            S.op("pe", lambda e, x=x, R=R, bank=bank, rows=rows: e.matmul(P[bank][R, :], lhsT=rbd[R, 0:rows], rhs=x[R, :], start=True, stop=True),
                 reads=[xk, "rbd"], writes=[pk(bank)])
            S.op("dve", lambda e, x=x, R=R, t1=t1, sc=sc: e.scalar_tensor_tensor(out=t1[R, :], in0=x[R, :], scalar=sc, in1=T["cos"][R, :], op0=ALU.mult, op1=ALU.mult),
                 reads=[xk, "ncos"], writes=[f"t1{it % 2}"])
            S.op("dve", lambda e, R=R, t2=t2, sc=sc, bank=bank: e.scalar_tensor_tensor(out=t2[R, :], in0=P[bank][R, :], scalar=sc, in1=T["sin"][R, :], op0=ALU.mult, op1=ALU.mult),
                 reads=["nsin"], writes=[pk(bank), f"t2{it % 2}"])
            dk = "KCVk" if nm == "kc" else nm
            S.op("pool", lambda e, R=R, t1=t1, t2=t2, dst=dst, ts=ts: e.tensor_tensor(out=dst[R, ts], in0=t1[R, :], in1=t2[R, :], op=ALU.add),
                 reads=[f"t1{it % 2}", f"t2{it % 2}"], writes=[dk])
            it += 1
    S.barrier()
    kb.release(m1)
    KCV = kb.alloc(N)
    BLK = kb.alloc(32 * 512, BF16).rearrange("p (l n) -> p l n", l=32)
    W1 = kb.alloc(32 * 256, BF16).rearrange("p (l h) -> p l h", l=32)
    pos2 = kb.alloc(32)
    b1 = kb.alloc(4)
    w2 = kb.alloc(2 * 2 * 64, BF16).rearrange("p (k m d) -> p k m d", k=2, m=2)
    HID = kb.alloc(2 * 2 * 512, BF16).rearrange("p (k m n) -> p k m n", k=2, m=2)
    zt = kb.alloc(512)
    ut = kb.alloc(512)
    stw = kb.alloc(1024).rearrange("p (l h) -> p l h", l=4)
    S.dma("sp", lambda e: e.dma_start(out=pos2, in_=D["nsa_pos"][:, :]), writes=["pos2"])
    S.dma("sp", lambda e: e.dma_start(out=b1, in_=D["nsa_b1"][:, :]), writes=["b1"])
    for kv in range(2):
        load_cast(kb, D["nsa_w2"][kv].rearrange("(m p) d -> p m d", p=128), w2[:, kv, :, :], st[:, 0:128].rearrange("p (m d) -> p m d", m=2), "st", "w2", "act")
    for l0 in range(0, 32, 4):
        for kv in range(2):
            src = D["nsa_w1"][kv].rearrange("(l d) h -> d l h", d=64)[:, l0:l0 + 4, :]
            S.dma("sp", lambda e, src=src, kv=kv: e.dma_start(out=stw[kv * 64:(kv + 1) * 64, :, :], in_=src), writes=["stw"])
        if (l0 // 4) % 2 == 0:
            S.op("act", lambda e, l0=l0: e.copy(out=W1[:, l0:l0 + 4, :], in_=stw), reads=["stw"], writes=["W1"])
        else:
            S.op("pool", lambda e, l0=l0: e.tensor_copy(out=W1[:, l0:l0 + 4, :], in_=stw), reads=["stw"], writes=["W1"])
    S.op("pool", lambda e: e.memset(BLK[:, :, 511:512], 0.0), writes=["BLKpad"])
    K3 = KCV.rearrange("p (g r) -> p g r", r=16)
    for l in range(32):
        src = K3[:, 0:511, l] if l < 16 else K3[:, 1:512, l - 16]
        eng = "dve" if l % 2 == 0 else "pool"
        S.op(eng, lambda e, l=l, src=src: e.tensor_scalar(out=BLK[:, l, 0:511], in0=src, scalar1=pos2[:, l:l + 1], scalar2=None, op0=ALU.add),
             reads=["pos2"], writes=[f"BLK{l}"])
    for kv in range(2):
        R = slice(kv * 64, kv * 64 + 64)
        for m in range(2):
            bank = 2 * kv + m
            for l in range(32):
                S.op("pe", lambda e, l=l, R=R, m=m, bank=bank: e.matmul(P[bank][:, :], lhsT=W1[R, l, m * 128:(m + 1) * 128], rhs=BLK[R, l, :], start=(l == 0), stop=(l == 31)),
                     reads=["W1", f"BLK{l}", "BLKpad"], writes=[pk(bank)])
            S.op("act", lambda e, kv=kv, m=m, bank=bank: e.activation(out=zt, in_=P[bank][:, :], func=AF.Identity, bias=b1[:, kv * 2 + m:kv * 2 + m + 1]),
                 reads=["b1"], writes=[pk(bank), "zt"])
            gelu_tanh(kb, zt, ut, HID[:, kv, m, :], "zt", "ut", f"HID{kv}")
    for m in range(2):
        S.op("pe", lambda e, m=m: e.matmul(P[4][0:64, :], lhsT=w2[:, 0, m, :], rhs=HID[:, 0, m, :], start=(m == 0), stop=(m == 1)), reads=["w2", "HID0"], writes=[pk(4)])
    S.op("act", lambda e: e.copy(out=KCMP2[0:64, :], in_=P[4][0:64, :]), writes=[pk(4), "KCMP2"])
    S.op("dve", lambda e: e.tensor_copy(out=KCMP2[64:128, :], in_=P[4][0:64, :]), writes=[pk(4), "KCMP2"])
    for nb in range(4):
        for m in range(2):
            S.op("pe", lambda e, m=m, nb=nb: e.matmul(P[5][:, 0:64], lhsT=HID[:, 1, m, nb * 128:(nb + 1) * 128], rhs=w2[:, 1, m, :], start=(m == 0), stop=(m == 1)),
                 reads=["w2", "HID1"], writes=[pk(5)])
        S.op("act", lambda e, nb=nb: e.copy(out=VCMP[:, nb, 0:64], in_=P[5][:, 0:64]), writes=[pk(5), "VCMP"])
    S.barrier()
    kb.release(m1)
    NST = kb.alloc(N, BF16)
    EALL = kb.alloc(N, BF16)
    for j in range(16):
        load_cast(kb, D["eall"][:, j * 512:(j + 1) * 512], EALL[:, j * 512:(j + 1) * 512], st[:, 0:512], "st", "EALL", "act" if j % 2 == 0 else "pool")
    ET = [kb.alloc(512, BF16) for _ in range(4)]
    IMP = kb.alloc(512).rearrange("p (a s) -> p a s", a=4)
    scr = kb.alloc(128)
    mr = kb.alloc(128)
    nsb = kb.alloc(128)
    mx = kb.alloc(16)
    thr = kb.alloc(2)
    rsum = kb.alloc(2)
    g6 = kb.alloc(512)
    gs = [[kb.alloc(512) for _ in range(3)] for _ in range(2)]
    acc = [kb.alloc(512), kb.alloc(512)]
    ctmp = kb.alloc(512)
    otmp = kb.alloc(512)
    rowbuf = kb.alloc(512)
    bcs = kb.alloc(512)
    att = Attn(kb, sbanks=(0, 1))
    oi = 0

    def combine(ob, hA, br, first):
        norm_coef(kb, c, ob, rowbuf, 4, bcs)
        S.op("dve", lambda e: e.tensor_tensor(out=ctmp[0:64, :], in0=bcs[0:64, :], in1=gs[hA][br][0:64, :], op=ALU.mult),
             reads=["bcs", f"gs{hA}{br}"], writes=["ctmp"])
        if first:
            S.op("dve", lambda e: e.tensor_tensor(out=acc[hA][0:64, :], in0=P[ob][0:64, :], in1=ctmp[0:64, :], op=ALU.mult),
                 reads=["ctmp"], writes=[pk(ob), f"acc{hA}"])
        else:
            S.op("dve", lambda e: e.tensor_tensor(out=otmp[0:64, :], in0=P[ob][0:64, :], in1=ctmp[0:64, :], op=ALU.mult),
                 reads=["ctmp"], writes=[pk(ob), "otmp"])
            S.op("pool", lambda e: e.tensor_tensor(out=acc[hA][0:64, :], in0=acc[hA][0:64, :], in1=otmp[0:64, :], op=ALU.add),
                 reads=["otmp"], writes=[f"acc{hA}"])

    pending = []
    for qt in range(NQT):
        qs = slice(qt * TQ, (qt + 1) * TQ)
        for f in pending:
            f()
        del pending[:]
        S.dma("sp", lambda e, qs=qs: e.dma_start(out=g6[0:6, :], in_=L.pt(OFF["b_gate"], 6, 6, qs.start, qs.stop)), writes=["g6"])
        for hA in range(2):
            for br in range(3):
                r = hA * 3 + br
                S.op("pe", lambda e, r=r: e.matmul(P[5][0:64, :], lhsT=selg[0:6, r * 64:(r + 1) * 64], rhs=g6[0:6, :], start=True, stop=True),
                     reads=["g6", "selg"], writes=[pk(5)])
                S.op("act", lambda e, hA=hA, br=br: e.activation(out=gs[hA][br][0:64, :], in_=P[5][0:64, :], func=AF.Sigmoid), writes=[pk(5), f"gs{hA}{br}"])
        nbm = (512 * qt + 480) // 2048
        for hh in range(4):
            Qt = (Qm if hh < 2 else Qo)
            qk = "q0" if hh < 2 else "q1"
            R = slice(64 * (hh % 2), 64 * (hh % 2) + 64)
            ob = 2 + oi % 2
            for nb in range(nbm + 1):
                bank = nb % 2
                dl = 512 * qt - 2048 * nb
                mms = [(KCMP2[R, nb * 128:(nb + 1) * 128], Qt[R, qs], ["KCMP2", qk])]
                if dl < 2560:
                    mms.append((c.ident_bf, cmpm[:, dl // 512, :], ["ident_bf", "cmpm"]))
                for j, (l_, r_, ks) in enumerate(mms):
                    S.op("pe", lambda e, l_=l_, r_=r_, j=j, bank=bank, nm=len(mms): e.matmul(P[bank][:, :], lhsT=l_, rhs=r_, start=(j == 0), stop=(j == nm - 1)),
                         reads=ks, writes=[pk(bank)])
                S.op("act", lambda e, nb=nb, bank=bank: e.activation(out=ET[nb], in_=P[bank][:, :], func=AF.Exp), writes=[pk(bank), f"ET{nb}"])
                if hh < 2:
                    S.op("pe", lambda e, nb=nb, ob=ob, nbm=nbm: e.matmul(P[ob][0:65, :], lhsT=VCMP[:, nb, 0:65], rhs=ET[nb], start=(nb == 0), stop=(nb == nbm)),
                         reads=[f"ET{nb}", "VCMP"], writes=[pk(ob)])
            for s4 in range(4):
                for nb in range(nbm + 1):
                    S.op("pe", lambda e, nb=nb, s4=s4, nbm=nbm: e.matmul(P[6][:, 0:129], lhsT=ET[nb][:, s4 * 128:(s4 + 1) * 128], rhs=ovl[:, nb, 0:129], start=(nb == 0), stop=(nb == nbm)),
                         reads=[f"ET{nb}", "ovl"], writes=[pk(6)])
                S.op("dve", lambda e: e.tensor_scalar_max(out=rsum[:, 0:1], in0=P[6][:, 128:129], scalar1=1e-30), writes=[pk(6), "rsum"])
                S.op("dve", lambda e: e.reciprocal(out=rsum[:, 0:1], in_=rsum[:, 0:1]), writes=["rsum"])
                if hh == 0:
                    S.op("dve", lambda e, s4=s4: e.tensor_scalar(out=IMP[:, s4, :], in0=P[6][:, 0:128], scalar1=rsum[:, 0:1], scalar2=None, op0=ALU.mult),
                         reads=["rsum"], writes=[pk(6), f"IMP{s4}"])
                else:
                    S.op("dve", lambda e, s4=s4: e.scalar_tensor_tensor(out=IMP[:, s4, :], in0=P[6][:, 0:128], scalar=rsum[:, 0:1], in1=IMP[:, s4, :], op0=ALU.mult, op1=ALU.add),
                         reads=["rsum"], writes=[pk(6), f"IMP{s4}"])
            if hh < 2:
                combine(ob, hh, 0, True)
                oi += 1
        for s4 in range(4):
            qb = 4 * qt + s4
            o0 = 126 - 2 * qb
            S.op("dve", lambda e, s4=s4, o0=o0: e.tensor_tensor(out=scr, in0=IMP[:, s4, :], in1=bv[:, o0:o0 + 128], op=ALU.mult), reads=[f"IMP{s4}", "bv"], writes=["scr"])
            S.op("dve", lambda e, o0=o0: e.tensor_tensor(out=scr, in0=scr, in1=bf[:, o0:o0 + 128], op=ALU.add), reads=["bf"], writes=["scr"])
            S.op("dve", lambda e: e.memset(scr[:, 0:1], 1e9), writes=["scr"])
            S.op("dve", lambda e: e.max(out=mx[:, 0:8], in_=scr), reads=["scr"], writes=["mx"])
            S.op("dve", lambda e: e.match_replace(out=mr, in_to_replace=mx[:, 0:8], in_values=scr, imm_value=-2.0), reads=["scr", "mx"], writes=["mr"])
            S.op("dve", lambda e: e.max(out=mx[:, 8:16], in_=mr), reads=["mr"], writes=["mx"])
            S.op("dve", lambda e: e.tensor_reduce(out=thr[:, 0:1], in_=mx[:, 8:16], axis=AX.X, op=ALU.min), reads=["mx"], writes=["thr"])
            S.op("dve", lambda e: e.tensor_scalar(out=nsb, in0=scr, scalar1=thr[:, 0:1], scalar2=-NEG, op0=ALU.is_ge, op1=ALU.mult), reads=["scr", "thr"], writes=["nsb"])
            S.op("pool", lambda e: e.tensor_scalar(out=nsb, in0=nsb, scalar1=NEG, scalar2=None, op0=ALU.add), writes=["nsb"])
            S.op("pe", lambda e: e.transpose(P[7][:, 0:128], nsb, c.ident), reads=["nsb", "ident"], writes=[pk(7)])
            S.op("act", lambda e, qb=qb: e.copy(out=NST[:, qb * 128:(qb + 1) * 128], in_=P[7][:, 0:128]), writes=[pk(7), "NST"])
        for hA in range(2):
            R = slice(64 * hA, 64 * hA + 64)
            blocks = []
            for kbk in range(4 * qt + 4):
                mms = [(KS2[R, kbk * 128:(kbk + 1) * 128], Qm[R, qs], ["ks", "q0"]),
                       (EALL[:, kbk * 128:(kbk + 1) * 128], NST[:, qs], ["EALL", "NST"])]
                if kbk >= 4 * qt:
                    mms.append((c.ident_bf, tri[:, kbk - 4 * qt, :], ["ident_bf", "tri"]))
                blocks.append((mms, VS[:, kbk, 0:65], ["VS"]))
            ob = 2 + oi % 2
            oi += 1
            att.run(blocks, ob, defer=pending)
            pending.append(lambda ob=ob, hA=hA: combine(ob, hA, 1, False))
            blocks = []
            for kbk in range(max(0, 4 * qt - 4), 4 * qt + 4):
                mms = [(KW2[R, kbk * 128:(kbk + 1) * 128], Qm[R, qs], ["kw", "q0"]),
                       (c.ident_bf, win[:, kbk - 4 * qt + 4, :], ["ident_bf", "win"])]
                blocks.append((mms, VW[:, kbk, 0:65], ["VW"]))
            ob = 2 + oi % 2
            oi += 1
            att.run(blocks, ob, defer=pending)

            def fin(ob=ob, hA=hA, qs=qs):
                combine(ob, hA, 2, False)
                S.dma("sp", lambda e: e.dma_start(out=yT[128 + hA * 64:128 + (hA + 1) * 64, qs], in_=acc[hA][0:64, :]), reads=[f"acc{hA}"])
            pending.append(fin)
    for f in pending:
        f()
    S.barrier()
    kb.release(m0)


def part_gla(kb, c, D, yT, L):
    STAGE = 9
    S = kb.S
    P = kb.psb
    m0 = kb.mark()
    QS = float(32 ** -0.5)
    A = kb.alloc(N)
    B = kb.alloc(N)
    C = kb.alloc(2048)
    QG = kb.alloc(N, BF16)
    KG = kb.alloc(N, BF16)
    KHT = kb.alloc(128 * 64, BF16).rearrange("p (b d) -> p b d", b=128)
    V = kb.alloc(128 * 128, BF16).rearrange("p (b d) -> p b d", b=128)
    SB = kb.alloc(128 * 64, BF16).rearrange("p (c v) -> p c v", c=128)
    DC = kb.alloc(128)
    wg2 = kb.alloc(64)
    glr = kb.alloc(512)
    v2 = kb.alloc(4)
    tri64 = kb.alloc(64)
    st = kb.alloc(1024)
    S.dma("sp", lambda e: e.dma_start(out=wg2[0:16, :], in_=D["gla_wg2"][:, :]), writes=["wg2"])
    S.dma("sp", lambda e: e.dma_start(out=v2[:, 0:2], in_=D["gla_v2"][:, :]), writes=["v2"])
    S.dma("sp", lambda e: e.dma_start(out=tri64[0:64, :], in_=D["tri64"][:, :]), writes=["tri64"])
    S.dma("sp", lambda e: e.dma_start(out=tri64[64:128, :], in_=D["tri64"][:, :]), writes=["tri64"])
    S.op("dve", lambda e: e.tensor_scalar(out=v2[:, 2:3], in0=v2[:, 0:1], scalar1=-1.0, scalar2=None, op0=ALU.mult), reads=["v2"], writes=["v2n"])
    R = slice(0, 64)
    st3 = st.rearrange("p (b d) -> p b d", d=128)
    for b0 in range(0, 128, 8):
        load_cast(kb, (lambda b0=b0: L.pv(128, 128, 128, b0 * 64, (b0 + 8) * 64).rearrange("(b p) d -> p b d", p=64)),
                  V[R, b0:b0 + 8, :], st3[R, :, :], "st", "V", "act" if (b0 // 8) % 2 == 0 else "pool")
    for t in range(NQT):
        ts = slice(t * TQ, (t + 1) * TQ)
        bank = t % 2
        S.dma("sp", lambda e, ts=ts: e.dma_start(out=glr[0:16, :], in_=L.pt(OFF["c_glr"], 0, 16, ts.start, ts.stop)), writes=["glr"])
        S.op("pe", lambda e, bank=bank: e.matmul(P[bank][R, :], lhsT=wg2[0:16, 0:64], rhs=glr[0:16, :], start=True, stop=True), reads=["glr", "wg2"], writes=[pk(bank)])
        S.op("act", lambda e, bank=bank, ts=ts: e.activation(out=A[R, ts], in_=P[bank][R, :], func=AF.Exp, bias=v2[R, 2:3], scale=-1.0),
             reads=["v2n"], writes=[pk(bank), "A"])
    S.op("act", lambda e: e.activation(out=A[R, :], in_=A[R, :], func=AF.Ln, bias=1.0), writes=["A"])
    for ch in range(128):
        cs = slice(ch * 64, (ch + 1) * 64)
        S.op("dve", lambda e, cs=cs: e.tensor_tensor_scan(out=B[R, cs], data0=c.ones_f[R, 0:64], data1=A[R, cs], initial=0.0, op0=ALU.mult, op1=ALU.add),
             reads=["A", "ones_f"], writes=["B"])
    if STAGE <= 1:
        S.barrier(); kb.release(m0); return
    B3 = B.rearrange("p (c j) -> p c j", j=64)
    A3 = A.rearrange("p (c j) -> p c j", j=64)
    S.op("act", lambda e: e.activation(out=DC[R, :], in_=B3[R, :, 63], func=AF.Exp, scale=-1.0 / 16.0), reads=["B"], writes=["DC"])
    S.op("act", lambda e: e.activation(out=A[R, :], in_=B[R, :], func=AF.Exp, scale=-1.0 / 16.0), reads=["B"], writes=["A"])
    for pc in range(4):
        ps_ = slice(pc * 2048, (pc + 1) * 2048)
        S.dma("sp", lambda e, ps_=ps_: e.dma_start(out=C[R, :], in_=L.pt(OFF["c_q"], 64, 64, ps_.start, ps_.stop)), writes=["C"])
        S.op("dve", lambda e, ps_=ps_: e.scalar_tensor_tensor(out=QG[R, ps_], in0=C[R, :], scalar=QS, in1=A[R, ps_], op0=ALU.mult, op1=ALU.mult),
             reads=["C", "A"], writes=["QG"])
    S.op("act", lambda e: e.activation(out=A[R, :], in_=B[R, :], func=AF.Exp, scale=1.0 / 16.0), reads=["B", "QG"], writes=["A"])
    for pc in range(4):
        ps_ = slice(pc * 2048, (pc + 1) * 2048)
        S.dma("sp", lambda e, ps_=ps_: e.dma_start(out=C[R, :], in_=L.pt(OFF["c_k"], 64, 64, ps_.start, ps_.stop)), writes=["C"])
        S.op("dve", lambda e, ps_=ps_: e.tensor_tensor(out=KG[R, ps_], in0=C[R, :], in1=A[R, ps_], op=ALU.mult), reads=["C", "A"], writes=["KG"])
    S.op("dve", lambda e: e.tensor_tensor(out=A3[R, :, :], in0=B3[R, :, :], in1=B3[R, :, 63:64].to_broadcast([64, 128, 64]), op=ALU.subtract),
         reads=["B", "KG"], writes=["A"])
    S.op("act", lambda e: e.activation(out=A[R, :], in_=A[R, :], func=AF.Exp, scale=1.0 / 16.0), writes=["A"])
    for pc in range(4):
        ps_ = slice(pc * 2048, (pc + 1) * 2048)
        S.dma("sp", lambda e, ps_=ps_: e.dma_start(out=C[R, :], in_=L.pt(OFF["c_k"], 64, 64, ps_.start, ps_.stop)), writes=["C"])
        S.op("dve", lambda e, ps_=ps_: e.tensor_tensor(out=A[R, ps_], in0=C[R, :], in1=A[R, ps_], op=ALU.mult), reads=["C"], writes=["A"])
    if STAGE <= 2:
        S.barrier(); kb.release(m0); return
    for blk in range(64):
        bank = blk % 2
        S.op("pe", lambda e, blk=blk, bank=bank: e.transpose(P[bank][:, 0:64], A[R, blk * 128:(blk + 1) * 128], c.ident[0:64, 0:64]),
             reads=["A", "ident"], writes=[pk(bank)])
        S.op("act", lambda e, blk=blk, bank=bank: e.copy(out=KHT[R, 2 * blk, :], in_=P[bank][0:64, 0:64]), writes=[pk(bank), "KHT"])
        S.op("dve", lambda e, blk=blk, bank=bank: e.tensor_copy(out=KHT[R, 2 * blk + 1, :], in_=P[bank][64:128, 0:64]), writes=[pk(bank), "KHT"])
    if STAGE <= 3:
        S.barrier(); kb.release(m0); return
    U3 = B.rearrange("p (v c) -> p v c", c=128)
    uev = [kb.alloc(512), kb.alloc(512)]
    S3 = A.rearrange("p (v c) -> p v c", c=128)
    for g in range(32):
        bank = 2 + g % 2
        for j in range(4):
            ch = 4 * g + j
            S.op("pe", lambda e, ch=ch, j=j, bank=bank: e.matmul(P[bank][R, j * 128:(j + 1) * 128], lhsT=KHT[R, ch, :], rhs=V[R, ch, :], start=True, stop=True),
                 reads=["KHT", "V"], writes=[pk(bank)])
        ue = uev[g % 2]
        S.op("act", lambda e, bank=bank, ue=ue: e.copy(out=ue[R, :], in_=P[bank][R, :]), writes=[pk(bank), f"uev{g % 2}"])
        for h in range(2):
            hr = slice(32 * h, 32 * h + 32)
            for j in range(4):
                eng = "dve" if j % 2 == 0 else "pool"
                S.op(eng, lambda e, hr=hr, ue=ue, g=g, j=j, h=h: e.tensor_copy(out=U3[hr, :, 4 * g + j], in_=ue[hr, j * 128 + h * 64:j * 128 + (h + 1) * 64]),
                     reads=[f"uev{g % 2}"], writes=["U3", "B"])
    if STAGE <= 4:
        S.barrier(); kb.release(m0); return
    for v in range(64):
        S.op("dve", lambda e, v=v: e.tensor_tensor_scan(out=S3[R, v, :], data0=DC[R, :], data1=U3[R, v, :], initial=0.0, op0=ALU.mult, op1=ALU.add),
             reads=["U3", "DC", "KHT"], writes=["S3", "A"])
    S.op("pool", lambda e: e.memset(SB[R, 0, :], 0.0), writes=["SB0"])
    S.op("act", lambda e: e.copy(out=SB[R, 1:128, :], in_=S3[R, :, 0:127].rearrange("p v c -> p c v")), reads=["S3"], writes=["SB"])
    if STAGE <= 5:
        S.barrier(); kb.release(m0); return
    osb = kb.alloc(512)
    sqb = kb.alloc(512, BF16)
    rstd = kb.alloc(512)
    og = kb.alloc(512)
    at = [kb.alloc(64, BF16), kb.alloc(64, BF16)]
    ai = 0
    for t in range(NQT):
        ts = slice(t * TQ, (t + 1) * TQ)
        for h in range(2):
            hr = slice(32 * h, 32 * h + 32)
            ob = 4 + (2 * t + h) % 2
            for j in range(8):
                ch = 8 * t + j
                cs = slice(ch * 64, (ch + 1) * 64)
                rr = R
                sbk = ai % 2
                a_t = at[ai % 2]
                ak = f"at{ai % 2}"
                ai += 1
                S.op("pe", lambda e, hr=hr, cs=cs, rr=rr, sbk=sbk: e.matmul(P[sbk][rr, 0:64], lhsT=KG[hr, cs], rhs=QG[hr, cs], start=True, stop=True),
                     reads=["KG", "QG"], writes=[pk(sbk)])
                S.op("dve", lambda e, rr=rr, sbk=sbk, a_t=a_t: e.tensor_tensor(out=a_t[rr, :], in0=P[sbk][rr, 0:64], in1=tri64[rr, :], op=ALU.mult),
                     reads=["tri64"], writes=[pk(sbk), ak])
                S.op("pe", lambda e, rr=rr, ch=ch, h=h, a_t=a_t, ob=ob, j=j: e.matmul(P[ob][R, j * 64:(j + 1) * 64], lhsT=V[rr, ch, h * 64:(h + 1) * 64], rhs=a_t[rr, :], start=True, stop=False),
                     reads=[ak, "V"], writes=[pk(ob)])
                S.op("pe", lambda e, hr=hr, ch=ch, cs=cs, ob=ob, j=j: e.matmul(P[ob][R, j * 64:(j + 1) * 64], lhsT=SB[hr, ch, :], rhs=QG[hr, cs], start=False, stop=True),
                     reads=["SB", "SB0", "QG"], writes=[pk(ob)])
            S.op("act", lambda e, ob=ob: e.copy(out=osb[R, :], in_=P[ob][R, :]), writes=[pk(ob), "osb"])
            S.op("act", lambda e: e.activation(out=sqb[R, :], in_=osb[R, :], func=AF.Square), reads=["osb"], writes=["sqb"])
            S.op("pe", lambda e: e.matmul(P[6][R, :], lhsT=c.ones_bf[R, 0:64], rhs=sqb[R, :], start=True, stop=True), reads=["sqb", "ones_bf"], writes=[pk(6)])
            S.op("act", lambda e: e.activation(out=rstd[R, :], in_=P[6][R, :], func=AF.Sqrt, bias=EPS, scale=1.0 / 64.0), writes=[pk(6), "rstd"])
            S.op("dve", lambda e: e.reciprocal(out=rstd[R, :], in_=rstd[R, :]), writes=["rstd"])
            S.op("dve", lambda e: e.scalar_tensor_tensor(out=osb[R, :], in0=osb[R, :], scalar=v2[R, 1:2], in1=rstd[R, :], op0=ALU.mult, op1=ALU.mult),
                 reads=["rstd", "v2"], writes=["osb"])
            S.dma("sp", lambda e, h=h, ts=ts: e.dma_start(out=og[R, :], in_=L.pt(OFF["c_og"] + h * 64, 128, 64, ts.start, ts.stop)), writes=["og"])
            S.op("act", lambda e: e.activation(out=og[R, :], in_=og[R, :], func=AF.Silu), writes=["og"])
            S.op("dve", lambda e: e.tensor_tensor(out=osb[R, :], in0=osb[R, :], in1=og[R, :], op=ALU.mult), reads=["og"], writes=["osb"])
            S.dma("sp", lambda e, h=h, ts=ts: e.dma_start(out=yT[256 + h * 64:256 + (h + 1) * 64, ts], in_=osb[R, :]), reads=["osb"])
    S.barrier()
    kb.release(m0)


WB = ["lru_w", "lru_v", "mla_wuq", "mla_wk", "mla_wv", "mla_v", "nsa_pos", "nsa_w1", "nsa_b1", "nsa_w2", "gla_wg2", "gla_v2"]
WAC = (("gain", [128, 8]), ("w_in", [1024, NCOLW]), ("gng", [128, 8]), ("nfg", [128, 8]), ("w_out", [1024, 1024]),
       ("w_gu", [1024, 2 * DFF]), ("w_dn", [DFF, 1024]))
RG = [[0, 1], [2, 3], [4, 5], [6, 7]]


def build_fused():
    kb = KB(arena_cols=52500)
    S = kb.S
    D = {k: kb.din(k, shp) for k, shp in CONST_SHAPES.items()}
    D["pos"] = kb.din("pos", [1, N], I32)
    xT = kb.din("xT", [1024, 4096])
    out = kb.dout("out", [1024, 4096])
    LW = []
    for l in range(2):
        d = {k: kb.din(f"{k}_{l}", IN_SHAPES[k][0]) for k in WB}
        for k, shp in WAC:
            d[k] = kb.din(f"{k}_{l}", shp)
        LW.append(d)
    fing = kb.din("fing", [128, 8])
    XA = kb.dint("XA", [NCOLP, 4096])
    XV = kb.dint("XV", [4096, NV])
    GP = kb.dint("GP", [2 * NCOLP, 4096])
    GV = kb.dint("GV", [N, NV])
    YB = kb.dint("YB", [1024, 4096])
    YV = YView(YB)
    GY = kb.dint("GY", [2048, 4096])
    XO = kb.dint("XO", [1024, 4096])
    MYP = kb.dint("MYP", [SELROWS, N])
    MYV = kb.dint("MYV", [N, 128])
    MYY = kb.dint("MYY", [1024, 4096])
    c = common_setup(kb, D)
    L = Loader(S, GP, GV, MYP, MYV)
    xs = xT
    for l in range(2):
        W = LW[l]
        ag = lambda i_, o_: (lambda e: e.collective_compute("AllGather", ALU.bypass, replica_groups=RG, ins=[i_], outs=[o_]))
        phase_A(kb, xs, W["gain"], W["w_in"], XA, XV, GP, GV, ag)
        L.select()
        Dl = dict(D)
        Dl.update({k: W[k] for k in WB})
        for part, g in ((part_lru, 0), (part_gla, 2), (part_mla, 3), (part_nsa, 1)):
            part(kb, c, Dl, YV, L)
            for ch in range(2):
                S.collective_async(ag(YB[ch * 512 + g * 128:ch * 512 + (g + 1) * 128, :], GY[(g * 2 + ch) * 256:(g * 2 + ch + 1) * 256, :]))
        S.cc_wait_all()
        GY4 = GY.rearrange("(g ch q) t -> g ch q t", g=4, ch=2)
        S.dma("act", lambda e: e.dma_start(out=MYY.rearrange("(g q) t -> g q t", g=4), in_=GY4[:, bass.ds(S.rt["hp_act"], 1), :, :].rearrange("g o q t -> g (o q) t")),
              writes=["MYY"])
        S.barrier()
        phase_C(kb, l == 1, xs, MYY, W["gng"], W["nfg"], fing, W["w_out"], W["w_gu"], W["w_dn"], out if l == 1 else XO)
        xs = XO
    return kb.close()


def prep_W(W, l, hp):
    A = np.ascontiguousarray
    o = {}
    ch = slice(hp * 128, hp * 128 + 128)
    o["lru_w"] = A(np.stack([W["lru_wa"][l][2 * hp:2 * hp + 2], W["lru_wx"][l][2 * hp:2 * hp + 2]]))
    o["lru_v"] = A(np.stack([W["conv_w"][l][0, ch], W["conv_w"][l][1, ch], W["conv_w"][l][2, ch], W["conv_w"][l][3, ch],
                             W["conv_b"][l][ch], W["lru_ba"][l][ch], W["lru_bx"][l][ch], W["lru_lambda"][l][ch]], axis=1))
    o["mla_wuq"] = A(W["mla_w_uq"][l][:, hp * 192:(hp + 1) * 192])
    wkv = W["mla_w_ukv"][l].reshape(128, 4, 128)
    o["mla_wk"] = A(wkv[:, 2 * hp:2 * hp + 2, 0:64].reshape(128, 128))
    o["mla_wv"] = A(wkv[:, 2 * hp:2 * hp + 2, 64:128].reshape(128, 128))
    mv = np.zeros((128, 3), np.float32)
    mv[:, 0] = W["mla_q_norm"][l][0:128]
    mv[0:64, 1] = W["mla_q_norm"][l][128:192]
    mv[:, 2] = W["mla_kv_norm"][l]
    o["mla_v"] = mv
    o["nsa_pos"] = A(np.concatenate([W["cmp_pos"][l][0].T, W["cmp_pos"][l][1].T], axis=0))
    o["nsa_w1"] = A(W["cmp_w1"][l])
    o["nsa_b1"] = A(W["cmp_b1"][l].reshape(2, 2, 128).transpose(2, 0, 1).reshape(128, 4))
    o["nsa_w2"] = A(W["cmp_w2"][l])
    o["gla_wg2"] = A(W["gla_wg2"][l][:, hp * 64:(hp + 1) * 64])
    g2 = np.zeros((128, 2), np.float32)
    g2[0:64, 0] = W["gla_bg2"][l][hp * 64:(hp + 1) * 64]
    g2[:, 1] = np.tile(W["gla_norm"][l], 2)
    o["gla_v2"] = g2
    return o


_PROG = {}


def _arr8(g):
    return np.ascontiguousarray(np.asarray(g, np.float32).reshape(8, 128).T)


def kernel(**inp):
    W = {k: np.asarray(v) for k, v in inp.items()}
    x = W["x"]
    Bn, Sn, Dm = x.shape
    HT = Sn // 2
    cores = [(b, r) for b in range(Bn) for r in range(2)]
    if "F" not in _PROG:
        _PROG["F"] = build_fused()
    nc = _PROG["F"]
    consts = host_consts()
    pc = perm_cols()
    ins = []
    for (b, r) in cores:
        d = dict(consts)
        d["pos"] = np.ascontiguousarray(W["positions"][b][None, :].astype(np.int32))
        d["xT"] = np.ascontiguousarray(x[b, r * HT:(r + 1) * HT].T)
        d["fing"] = _arr8(W["final_norm"])
        for l in range(2):
            for k, v in prep_W(W, l, r).items():
                d[f"{k}_{l}"] = v
            d[f"gain_{l}"] = _arr8(W["norm_mix"][l])
            d[f"w_in_{l}"] = np.ascontiguousarray(W["w_in"][l][:, pc])
            d[f"gng_{l}"] = _arr8(W["group_norm"][l])
            d[f"nfg_{l}"] = _arr8(W["norm_ffn"][l])
            d[f"w_out_{l}"] = np.ascontiguousarray(W["w_out"][l])
            d[f"w_gu_{l}"] = np.ascontiguousarray(W["w_gate_up"][l])
            d[f"w_dn_{l}"] = np.ascontiguousarray(W["w_down"][l])
        ins.append(d)
    res = run_bass_kernel_spmd(nc, ins, core_ids=list(range(8))).results
    out = np.empty((Bn, Sn, Dm), np.float32)
    for ci, (b, r) in enumerate(cores):
        out[b, r * HT:(r + 1) * HT] = res[ci]["out"].T
    return out
```

```python
import numpy as np
from contextlib import ExitStack
import concourse.bass as bass
import concourse.mybir as mybir
from concourse.bass_utils import run_bass_kernel_spmd

F32 = mybir.dt.float32
BF16 = mybir.dt.bfloat16
I32 = mybir.dt.int32
AF = mybir.ActivationFunctionType
ALU = mybir.AluOpType
AX = mybir.AxisListType

ENGS = ("pe", "act", "dve", "pool", "sp")
NDMA_SEMS = 8
EPS = 1e-6


class Sched:
    def __init__(self, nc, es):
        self.nc = nc
        self.sem = {e: es.enter_context(nc.semaphore("s_" + e)) for e in ENGS}
        self.cnt = {e: 0 for e in ENGS}
        self.dsem = {e: [es.enter_context(nc.semaphore(f"d_{e}{i}")) for i in range(NDMA_SEMS)]
                     for e in ("sp", "pool", "act")}
        self.dval = {e: [0] * NDMA_SEMS for e in self.dsem}
        self.drr = {e: 0 for e in self.dsem}
        self.ops = {e: [] for e in ENGS}
        self.known = {e: {} for e in ENGS}
        self.semobj = {}
        self.last_w = {}
        self.readers = {}
        self.ccsem = es.enter_context(nc.semaphore("s_cc"))
        self.semobj["cc"] = self.ccsem
        self.ccn = 0
        self.rt = {}
        for e in ENGS:
            self.semobj["c_" + e] = self.sem[e]
        for e in self.dsem:
            for i in range(NDMA_SEMS):
                self.semobj[f"d_{e}{i}"] = self.dsem[e][i]

    def _need(self, eng, tok, waits):
        if tok is None:
            return
        sk, val, teng = tok
        if teng == "pe" and eng == "pe" and sk == "c_pe":
            return
        if self.known[eng].get(sk, 0) >= val:
            return
        self.known[eng][sk] = val
        waits[sk] = max(waits.get(sk, 0), val)

    def _deps(self, eng, reads, writes):
        waits = {}
        for k in reads:
            self._need(eng, self.last_w.get(k), waits)
        for k in writes:
            self._need(eng, self.last_w.get(k), waits)
            for t in self.readers.get(k, ()):
                self._need(eng, t, waits)
        return waits

    def _commit(self, tok, reads, writes):
        for k in reads:
            self.readers.setdefault(k, []).append(tok)
        for k in writes:
            self.last_w[k] = tok
            self.readers[k] = []

    def op(self, eng, emit, reads=(), writes=()):
        waits = self._deps(eng, reads, writes)
        self.cnt[eng] += 1
        tok = ("c_" + eng, self.cnt[eng], eng)
        self.ops[eng].append((waits, emit, (self.sem[eng], 1)))
        self._commit(tok, reads, writes)
        return tok

    def dma(self, eng, emit, reads=(), writes=()):
        waits = self._deps(eng, reads, writes)
        i = self.drr[eng]
        self.drr[eng] = (i + 1) % NDMA_SEMS
        sk = f"d_{eng}{i}"
        prev = self.dval[eng][i]
        if prev and self.known[eng].get(sk, 0) < prev:
            self.known[eng][sk] = prev
            waits[sk] = prev
        self.dval[eng][i] = prev + 16
        tok = (sk, prev + 16, eng)
        self.ops[eng].append((waits, emit, (self.dsem[eng][i], 16)))
        self._commit(tok, reads, writes)
        return tok

    def barrier(self):
        for eng in ENGS:
            waits = {}
            for e in ENGS:
                if e != eng and self.cnt[e]:
                    self._need(eng, ("c_" + e, self.cnt[e], e), waits)
            for e in self.dsem:
                for i in range(NDMA_SEMS):
                    if self.dval[e][i]:
                        self._need(eng, (f"d_{e}{i}", self.dval[e][i], "dma"), waits)
            if eng != "pe" and self.cnt[eng]:
                self._need(eng, ("c_" + eng, self.cnt[eng], eng), waits)
            if waits:
                self.ops[eng].append((waits, None, None))
        self.last_w = {}
        self.readers = {}

    def collective(self, emits):
        self.barrier()
        for emit in emits:
            w = {"cc": self.ccn} if self.ccn else {}
            self.ccn += 1
            self.ops["pool"].append((w, emit, (self.ccsem, 1)))
        for eng in ENGS:
            self.known[eng]["cc"] = self.ccn
            self.ops[eng].append(({"cc": self.ccn}, None, None))

    def collective_async(self, emit, reads=()):
        waits = {}
        for k in reads:
            self._need("pool", self.last_w.get(k), waits)
        if self.ccn:
            waits["cc"] = self.ccn
        self.ccn += 1
        self.ops["pool"].append((waits, emit, (self.ccsem, 1)))

    def cc_wait_all(self):
        self.barrier()
        for eng in ENGS:
            if self.known[eng].get("cc", 0) < self.ccn:
                self.known[eng]["cc"] = self.ccn
                self.ops[eng].append(({"cc": self.ccn}, None, None))

    def finish(self):
        self.barrier()
        semobj = self.semobj

        def run(engobj, lst):
            for waits, emit, inc in lst:
                for sk, v in waits.items():
                    engobj.wait_ge(semobj[sk], v)
                if emit is not None:
                    emit(engobj).then_inc(inc[0], inc[1])

        with self.nc.Block() as block:
            @block.tensor
            def _(e):
                run(e, self.ops["pe"])

            @block.scalar
            def _(e):
                self.rt["hp_act"] = e.partition_id() % 2
                run(e, self.ops["act"])

            @block.vector
            def _(e):
                run(e, self.ops["dve"])

            @block.gpsimd
            def _(e):
                run(e, self.ops["pool"])

            @block.sync
            def _(e):
                self.rt["hp_sp"] = e.partition_id() % 2
                run(e, self.ops["sp"])


class KB:
    def __init__(self, arena_cols=50000):
        self.nc = bass.Bass("TRN2", target_bir_lowering=False)
        self.es = ExitStack()
        self.S = Sched(self.nc, self.es)
        self.arena = self.es.enter_context(self.nc.sbuf_tensor("arena", [128, arena_cols], F32))
        self.acols = arena_cols
        self.top = 0
        self.psb = [self.es.enter_context(self.nc.psum_tensor(f"psb{i}", [128, 512], F32)) for i in range(8)]
        self.uid = 0

    def dint(self, name, shape, dt=F32):
        return self.nc.dram_tensor(name, list(shape), dt).ap()

    def din(self, name, shape, dt=F32):
        return self.nc.dram_tensor(name, list(shape), dt, kind="ExternalInput").ap()

    def dout(self, name, shape, dt=F32):
        return self.nc.dram_tensor(name, list(shape), dt, kind="ExternalOutput").ap()

    def alloc(self, cols, dt=F32):
        n32 = cols if dt != BF16 else (cols + 1) // 2
        a = self.top
        self.top += n32
        assert self.top <= self.acols, f"arena overflow {self.top}"
        v = self.arena[:, a:a + n32]
        if dt == BF16:
            v = v.bitcast(BF16)
        elif dt == I32:
            v = v.bitcast(I32)
        return v

    def mark(self):
        return self.top

    def release(self, m):
        self.top = m

    def key(self, base="k"):
        self.uid += 1
        return f"{base}{self.uid}"

    def close(self):
        self.S.finish()
        self.es.close()
        return self.nc


NCOL = 2300
NCOLP = 1920
NCOLW = NCOLP + 384
VCOLS = [(NCOLP, NCOLW)]
NV = 384
OFF = dict(a_x=0, a_gate=256, b_q=512, b_kv=768, c_q=1024, c_k=1152, c_og=1280, d_cq=1536, d_kr=1728, c_glr=1760,
           b_gate=1776, d_ckv=1792)


def perm_cols():
    o = dict(a_x=0, a_gate=256, b_q=512, b_kv=768, b_gate=1152, c_q=1164, c_k=1292, c_v=1420, c_glr=1676, c_og=1692,
             d_cq=1948, d_ckv=2140, d_kr=2268)
    r = lambda a, n: list(range(a, a + n))
    p = (r(o["a_x"], 256) + r(o["a_gate"], 256) + r(o["b_q"], 256)
         + r(o["b_kv"], 64) + r(o["b_kv"] + 64, 64) + r(o["b_kv"] + 128, 64) + r(o["b_kv"] + 256, 64)
         + r(o["c_q"], 128) + r(o["c_k"], 128) + r(o["c_og"], 256) + r(o["d_cq"], 192) + r(o["d_kr"], 32)
         + r(o["c_glr"], 16) + r(o["b_gate"], 12) + r(o["b_gate"], 4) + r(o["d_ckv"], 128))
    assert len(p) == NCOLP
    p += r(o["b_kv"] + 192, 64) + r(o["b_kv"] + 320, 64) + r(o["c_v"], 256)
    assert len(p) == NCOLW
    return np.array(p)
DFF = 2816


def load_cast_weight(kb, w_dram, wsb, kchunks, ncols, stage, key, piece=1024):
    S = kb.S
    i = 0
    for c in range(kchunks):
        for c0 in range(0, ncols, piece):
            c1 = min(ncols, c0 + piece)
            st = stage[i % 2]
            sk = f"wstage{i % 2}"
            S.dma("sp", lambda e, st=st, c=c, c0=c0, c1=c1: e.dma_start(out=st[:, 0:c1 - c0], in_=w_dram[c * 128:(c + 1) * 128, c0:c1]),
                  writes=[sk])
            eng = "act" if i % 2 == 0 else "pool"
            if eng == "act":
                S.op("act", lambda e, st=st, c=c, c0=c0, c1=c1: e.copy(out=wsb[:, c, c0:c1], in_=st[:, 0:c1 - c0]), reads=[sk], writes=[key])
            else:
                S.op("pool", lambda e, st=st, c=c, c0=c0, c1=c1: e.tensor_copy(out=wsb[:, c, c0:c1], in_=st[:, 0:c1 - c0]), reads=[sk], writes=[key])
            i += 1


def rms_stats(kb, src3, nch, T, sq, ones_bf, ps_ap, rstd, denom, keys_in, key_sq, key_ps, key_rstd):
    S = kb.S
    S.op("act", lambda e: e.activation(out=sq, in_=src3, func=AF.Square), reads=keys_in, writes=[key_sq])
    for c in range(nch):
        S.op("pe", lambda e, c=c: e.matmul(ps_ap, lhsT=ones_bf, rhs=sq[:, c, :], start=(c == 0), stop=(c == nch - 1)),
             reads=[key_sq, "ones_bf"], writes=[key_ps])
    S.op("act", lambda e: e.activation(out=rstd, in_=ps_ap, func=AF.Sqrt, bias=EPS, scale=1.0 / denom), writes=[key_ps, key_rstd])
    S.op("dve", lambda e: e.reciprocal(out=rstd, in_=rstd), reads=[key_rstd], writes=[key_rstd])


def phase_A(kb, xT, gain, w, pT, pV, GP, GV, ag):
    S = kb.S
    m0 = kb.mark()
    T = 512
    NT = 8
    xv = xT.rearrange("(c p) t -> p c t", p=128)

    wsb = kb.alloc(8 * NCOLW, BF16).rearrange("p (c n) -> p c n", c=8)
    gsb = kb.alloc(8)
    ones_bf = kb.alloc(128, BF16)
    stage = [kb.alloc(1024), kb.alloc(1024)]
    xt = [kb.alloc(8 * T).rearrange("p (c t) -> p c t", c=8) for _ in range(2)]
    sq = kb.alloc(8 * T, BF16).rearrange("p (c t) -> p c t", c=8)
    hb = kb.alloc(8 * 4096, BF16).rearrange("p (c t) -> p c t", c=8)
    rstd = kb.alloc(T)
    ost = [kb.alloc(T) for _ in range(4)]
    P = [p[:] for p in kb.psb]

    S.dma("sp", lambda e: e.dma_start(out=gsb, in_=gain[:, :]), writes=["gsb"])
    S.op("pool", lambda e: e.memset(ones_bf, 1.0), writes=["ones_bf"])
    S.dma("sp", lambda e: e.dma_start(out=xt[0], in_=xv[:, :, 0:T]), writes=["xt0"])
    load_cast_weight(kb, w, wsb, 8, NCOLW, stage, "wsb")
    for t in range(NT):
        b = t % 2
        if t + 1 < NT:
            S.dma("sp", lambda e, t=t: e.dma_start(out=xt[(t + 1) % 2], in_=xv[:, :, (t + 1) * T:(t + 2) * T]),
                  writes=[f"xt{(t + 1) % 2}"])
        rms_stats(kb, xt[b], 8, T, sq, ones_bf, P[0], rstd, 1024.0, [f"xt{b}"], "sq", "psb0", "rstd")
        for c in range(8):
            S.op("dve", lambda e, c=c, b=b, t=t: e.scalar_tensor_tensor(out=hb[:, c, t * T:(t + 1) * T], in0=xt[b][:, c, :], scalar=gsb[:, c:c + 1], in1=rstd,
                                                                   op0=ALU.mult, op1=ALU.mult),
                 reads=[f"xt{b}", "rstd", "gsb"], writes=[f"hb{t}"])
    oi = 0
    for t in range(NT):
        for tb in range(4):
            pb = 5 + (tb % 2)
            for c in range(8):
                S.op("pe", lambda e, c=c, t=t, tb=tb, pb=pb: e.matmul(
                    P[pb][:, 0:NV], lhsT=hb[:, c, t * T + tb * 128:t * T + (tb + 1) * 128], rhs=wsb[:, c, NCOLP:NCOLW], start=(c == 0), stop=(c == 7)),
                    reads=[f"hb{t}", "wsb"], writes=[f"psb{pb}"])
            o = oi % 4
            oi += 1
            S.op("act", lambda e, o=o, pb=pb: e.copy(out=ost[o][:, 0:NV], in_=P[pb][:, 0:NV]), writes=[f"psb{pb}", f"ost{o}"])
            S.dma("sp", lambda e, o=o, t=t, tb=tb: e.dma_start(out=pV[t * T + tb * 128: t * T + (tb + 1) * 128, :], in_=ost[o][:, 0:NV]),
                  reads=[f"ost{o}"], writes=[f"XV{t}_{tb}"])
        if t % 2 == 1:
            j = t // 2
            S.collective_async(ag(pV[j * 1024:(j + 1) * 1024, :], GV[j * 2048:(j + 1) * 2048, :]),
                               reads=[f"XV{tt}_{tb}" for tt in (t - 1, t) for tb in range(4)])
    for k in range(NCOLP // 128):
        c0, c1 = k * 128, (k + 1) * 128
        for t in range(NT):
            pb = 1 + (oi % 4)
            for c in range(8):
                S.op("pe", lambda e, c=c, t=t, c0=c0, c1=c1, pb=pb: e.matmul(P[pb][:, :], lhsT=wsb[:, c, c0:c1], rhs=hb[:, c, t * T:(t + 1) * T],
                                                                             start=(c == 0), stop=(c == 7)),
                     reads=[f"hb{t}", "wsb"], writes=[f"psb{pb}"])
            o = oi % 4
            oi += 1
            if t % 2 == 0:
                S.op("act", lambda e, o=o, pb=pb: e.copy(out=ost[o], in_=P[pb][:, :]), writes=[f"psb{pb}", f"ost{o}"])
            else:
                S.op("dve", lambda e, o=o, pb=pb: e.tensor_copy(out=ost[o], in_=P[pb][:, :]), writes=[f"psb{pb}", f"ost{o}"])
            S.dma("sp", lambda e, o=o, c0=c0, c1=c1, t=t: e.dma_start(out=pT[c0:c1, t * T:(t + 1) * T], in_=ost[o]),
                  reads=[f"ost{o}"], writes=[f"XA{k}_{t}"])
        S.collective_async(ag(pT[c0:c1, :], GP[k * 256:(k + 1) * 256, :]), reads=[f"XA{k}_{t}" for t in range(NT)])
    S.cc_wait_all()
    kb.release(m0)


def phase_C(kb, final, xT, GY, gng, nfg, fing, w_out, w_gu, w_dn, xo, NT=16):
    S = kb.S
    m0 = kb.mark()
    T = 256
    xv = xT.rearrange("(c p) t -> p c t", p=128)
    ov = xo.rearrange("(c p) t -> p c t", p=128)

    wo = kb.alloc(8 * 1024, BF16).rearrange("p (c n) -> p c n", c=8)
    wgu = kb.alloc(8 * 2 * DFF, BF16).rearrange("p (c n) -> p c n", c=8)
    wdn = kb.alloc(22 * 1024, BF16).rearrange("p (c n) -> p c n", c=22)
    g3 = kb.alloc(24)
    ones_bf = kb.alloc(128, BF16)
    xt = kb.alloc(8 * T).rearrange("p (c t) -> p c t", c=8)
    yt = kb.alloc(8 * T).rearrange("p (c t) -> p c t", c=8)
    ytf = yt.rearrange("p c t -> p (c t)")
    stage = [ytf[:, 0:1024], ytf[:, 1024:2048]]
    sq = kb.alloc(8 * T, BF16).rearrange("p (c t) -> p c t", c=8)
    hb = kb.alloc(8 * T, BF16).rearrange("p (c t) -> p c t", c=8)
    aT = kb.alloc(22 * T, BF16).rearrange("p (c t) -> p c t", c=22)
    rstd4 = kb.alloc(4 * T).rearrange("p (c t) -> p c t", c=4)
    rstd = kb.alloc(T)
    sg = [kb.alloc(T), kb.alloc(T)]
    P = [p[:] for p in kb.psb]

    S.dma("sp", lambda e: e.dma_start(out=g3[:, 0:8], in_=gng[:, :]), writes=["g3"])
    S.dma("sp", lambda e: e.dma_start(out=g3[:, 8:16], in_=nfg[:, :]), writes=["g3"])
    S.dma("sp", lambda e: e.dma_start(out=g3[:, 16:24], in_=fing[:, :]), writes=["g3"])
    S.op("pool", lambda e: e.memset(ones_bf, 1.0), writes=["ones_bf"])
    load_cast_weight(kb, w_out, wo, 8, 1024, stage, "wo")
    load_cast_weight(kb, w_gu, wgu, 8, 2 * DFF, stage, "wgu")
    load_cast_weight(kb, w_dn, wdn, 22, 1024, stage, "wdn")
    S.barrier()

    for t in range(NT):
        ts = slice(t * T, (t + 1) * T)
        S.dma("sp", lambda e, ts=ts: e.dma_start(out=xt, in_=xv[:, :, ts]), writes=["xt"])
        for cc_ in range(8):
            r0 = cc_ * 128
            S.dma("sp", lambda e, cc_=cc_, r0=r0, t=t: e.dma_start(out=yt[:, cc_, :], in_=GY[r0:r0 + 128, t * T:(t + 1) * T]),
                  writes=["yt"])
        S.op("act", lambda e: e.activation(out=sq, in_=yt, func=AF.Square), reads=["yt"], writes=["sq"])
        for g in range(4):
            pa = P[g][:, 0:T]
            for j in range(2):
                S.op("pe", lambda e, g=g, j=j, pa=pa: e.matmul(pa, lhsT=ones_bf, rhs=sq[:, 2 * g + j, :], start=(j == 0), stop=(j == 1)),
                     reads=["sq", "ones_bf"], writes=[f"psb{g}"])
            S.op("act", lambda e, g=g, pa=pa: e.activation(out=rstd4[:, g, :], in_=pa, func=AF.Sqrt, bias=EPS, scale=1.0 / 256.0),
                 writes=[f"psb{g}", f"rstd4_{g}"])
            S.op("dve", lambda e, g=g: e.reciprocal(out=rstd4[:, g, :], in_=rstd4[:, g, :]), reads=[f"rstd4_{g}"], writes=[f"rstd4_{g}"])
        for c in range(8):
            eng = "dve"
            S.op(eng, lambda e, c=c: e.scalar_tensor_tensor(out=hb[:, c, :], in0=yt[:, c, :], scalar=g3[:, c:c + 1], in1=rstd4[:, c // 2, :],
                                                         op0=ALU.mult, op1=ALU.mult),
                 reads=["yt", f"rstd4_{c // 2}", "g3"], writes=[f"hb{c}"])
        for m in range(8):
            pa = P[4 + m % 4][:, 0:T]
            for c in range(8):
                S.op("pe", lambda e, m=m, c=c, pa=pa: e.matmul(pa, lhsT=wo[:, c, m * 128:(m + 1) * 128], rhs=hb[:, c, :], start=(c == 0), stop=(c == 7)),
                     reads=[f"hb{c}", "wo"], writes=[f"psb{4 + m % 4}"])
            S.op("dve", lambda e, m=m, pa=pa: e.tensor_tensor(out=xt[:, m, :], in0=xt[:, m, :], in1=pa, op=ALU.add),
                 reads=["xt"], writes=["xt", f"psb{4 + m % 4}"])
        rms_stats(kb, xt, 8, T, sq, ones_bf, P[0][:, 0:T], rstd, 1024.0, ["xt"], "sq", "psb0", "rstd")
        for c in range(8):
            eng = "dve"
            S.op(eng, lambda e, c=c: e.scalar_tensor_tensor(out=hb[:, c, :], in0=xt[:, c, :], scalar=g3[:, 8 + c:9 + c], in1=rstd,
                                                         op0=ALU.mult, op1=ALU.mult),
                 reads=["xt", "rstd", "g3"], writes=[f"hb{c}"])
        for j in range(22):
            pg = P[j % 2][:, 0:T]
            pu = P[2 + j % 2][:, 0:T]
            for c in range(8):
                S.op("pe", lambda e, j=j, c=c, pg=pg: e.matmul(pg, lhsT=wgu[:, c, j * 128:(j + 1) * 128], rhs=hb[:, c, :], start=(c == 0), stop=(c == 7)),
                     reads=[f"hb{c}", "wgu"], writes=[f"psb{j % 2}"])
            for c in range(8):
                S.op("pe", lambda e, j=j, c=c, pu=pu: e.matmul(pu, lhsT=wgu[:, c, DFF + j * 128:DFF + (j + 1) * 128], rhs=hb[:, c, :], start=(c == 0), stop=(c == 7)),
                     reads=[f"hb{c}", "wgu"], writes=[f"psb{2 + j % 2}"])
            S.op("act", lambda e, j=j, pg=pg: e.activation(out=sg[j % 2], in_=pg, func=AF.Silu), writes=[f"psb{j % 2}", f"sg{j % 2}"])
            S.op("dve", lambda e, j=j, pu=pu: e.tensor_tensor(out=aT[:, j, :], in0=sg[j % 2], in1=pu, op=ALU.mult),
                 reads=[f"sg{j % 2}"], writes=[f"aT{j}", f"psb{2 + j % 2}"])
        for m in range(8):
            pa = P[4 + m % 4][:, 0:T]
            for k in range(22):
                S.op("pe", lambda e, m=m, k=k, pa=pa: e.matmul(pa, lhsT=wdn[:, k, m * 128:(m + 1) * 128], rhs=aT[:, k, :], start=(k == 0), stop=(k == 21)),
                     reads=[f"aT{k}", "wdn"], writes=[f"psb{4 + m % 4}"])
            S.op("dve", lambda e, m=m, pa=pa: e.tensor_tensor(out=xt[:, m, :], in0=xt[:, m, :], in1=pa, op=ALU.add),
                 reads=["xt"], writes=["xt", f"psb{4 + m % 4}"])
        if final:
            rms_stats(kb, xt, 8, T, sq, ones_bf, P[0][:, 0:T], rstd, 1024.0, ["xt"], "sq", "psb0", "rstd")
            for c in range(8):
                eng = "dve"
                S.op(eng, lambda e, c=c: e.scalar_tensor_tensor(out=yt[:, c, :], in0=xt[:, c, :], scalar=g3[:, 16 + c:17 + c], in1=rstd,
                                                             op0=ALU.mult, op1=ALU.mult),
                     reads=["xt", "rstd", "g3"], writes=["yt"])
            S.dma("sp", lambda e, ts=ts: e.dma_start(out=ov[:, :, ts], in_=yt), reads=["yt"])
        else:
            S.dma("sp", lambda e, ts=ts: e.dma_start(out=ov[:, :, ts], in_=xt), reads=["xt"])
    S.barrier()
    kb.release(m0)


N = 8192
TQ = 512
NQT = N // TQ
NEG = -30000.0
PI = float(np.pi)
TWO_PI = float(2 * np.pi)
THETA = 10000.0


def pk(b):
    return f"psb{b}"


def host_consts():
    c = {}
    c["ident"] = np.eye(128, dtype=np.float32)
    R = np.zeros((128, 128), np.float32)
    for blk in range(2):
        for m in range(64):
            if m < 32:
                R[blk * 64 + m + 32, blk * 64 + m] = -1.0
            else:
                R[blk * 64 + m - 32, blk * 64 + m] = 1.0
    c["rbd"] = R
    R32 = np.zeros((128, 32), np.float32)
    for m in range(32):
        if m < 16:
            R32[64 + m + 16, m] = -1.0
        else:
            R32[64 + m - 16, m] = 1.0
    c["r32"] = R32
    invf = np.zeros((128, 2), np.float32)
    for p in range(128):
        invf[p, 0] = np.float32(THETA) ** np.float32(-(2.0 * ((p % 64) % 32)) / 64.0)
    for p in range(64, 96):
        invf[p, 1] = np.float32(THETA) ** np.float32(-(2.0 * ((p - 64) % 16)) / 32.0)
    c["invf"] = invf
    k = np.arange(128)[:, None]
    q = np.arange(512)[None, :]
    c["tri"] = np.stack([np.where(k + i * 128 <= q, 0.0, NEG) for i in range(4)]).astype(np.float32)
    c["win"] = np.stack([np.where((k + (i - 4) * 128 <= q) & (k + (i - 4) * 128 > q - 512), 0.0, NEG) for i in range(8)]).astype(np.float32)
    c["cmpm"] = np.stack([np.where(16 * k + 31 <= i * 512 + q, 0.0, NEG) for i in range(5)]).astype(np.float32)
    n = np.arange(512)
    s = np.arange(128)
    ov = ((n[:, None] * 16 < s[None, :] * 64 + 64) & (n[:, None] * 16 + 32 > s[None, :] * 64)).astype(np.float32)
    ov[511] = 0.0
    ovl = np.zeros((4, 128, 129), np.float32)
    ovl[:, :, :128] = ov.reshape(4, 128, 128)
    ovl[:, :, 128] = 1.0
    c["ovl"] = ovl
    c["eall"] = (np.arange(N)[None, :] // 64 == np.arange(128)[:, None]).astype(np.float32)
    ql = np.arange(128)[:, None] // 64
    j = np.arange(254)[None, :] - 126
    c["bv"] = (j <= ql - 2).astype(np.float32)
    c["bf"] = (np.where((j == ql) | (j == ql - 1), 1e9, 0.0) + np.where(j > ql, -1.0, 0.0)).astype(np.float32)
    selg = np.zeros((8, 6 * 64), np.float32)
    for r in range(6):
        selg[r, r * 64:(r + 1) * 64] = 1.0
    c["selg"] = selg
    c["tri64"] = (np.arange(64)[:, None] <= np.arange(64)[None, :]).astype(np.float32)
    return c


CONST_SHAPES = {"ident": [128, 128], "rbd": [128, 128], "r32": [128, 32], "invf": [128, 2], "tri": [4, 128, 512],
                "win": [8, 128, 512], "cmpm": [5, 128, 512], "ovl": [4, 128, 129], "eall": [128, N], "bv": [128, 254],
                "bf": [128, 254], "selg": [8, 384], "tri64": [64, 64]}

IN_SHAPES = {
    "pos": ([1, N], I32),
    "lru_x": ([2, 128, N], F32), "lru_w": ([2, 2, 64, 64], F32), "lru_v": ([128, 8], F32),
    "mla_cq": ([192, N], F32), "mla_ckv": ([128, N], F32), "mla_kr": ([32, N], F32),
    "mla_wuq": ([192, 192], F32), "mla_wk": ([128, 128], F32), "mla_wv": ([128, 128], F32), "mla_v": ([128, 3], F32),
    "nsa_q": ([2, 128, N], F32), "nsa_k": ([3, 64, N], F32), "nsa_vc": ([64, N], F32), "nsa_vs": ([N, 64], F32),
    "nsa_vw": ([N, 64], F32), "nsa_g": ([6, N], F32), "nsa_pos": ([128, 32], F32), "nsa_w1": ([2, 2048, 256], F32),
    "nsa_b1": ([128, 4], F32), "nsa_w2": ([2, 256, 64], F32),
    "gla_q": ([64, N], F32), "gla_k": ([64, N], F32), "gla_v": ([N, 128], F32), "gla_glr": ([16, N], F32),
    "gla_og": ([128, N], F32), "gla_wg2": ([16, 64], F32), "gla_v2": ([128, 2], F32),
}


class Ctx:
    pass


class YView:
    def __init__(self, ap):
        self.ap = ap

    def __getitem__(self, key):
        rs, cs = key
        half = cs.start // 4096
        assert (cs.stop - 1) // 4096 == half
        return self.ap[half * 512 + rs.start:half * 512 + rs.stop, cs.start - half * 4096:cs.stop - half * 4096]


SEL = [("a_x", 128, False, 128), ("a_gate", 128, False, 128), ("b_q", 128, False, 128), ("b_q", 128, True, 128),
       ("b_gate", 6, False, 6), ("c_q", 64, False, 64), ("c_k", 64, False, 64), ("c_og", 128, False, 128)]
SELROWS = sum(x[3] for x in SEL)


class Loader:
    def __init__(self, S, GP, GV, MYP, MYV):
        self.S = S
        self.GP, self.GV, self.MYP, self.MYV = GP, GV, MYP, MYV
        self.row0 = {}
        r = 0
        for nm, hpm, inv, n in SEL:
            self.row0[(nm, inv)] = r
            r += n

    @staticmethod
    def gprow(r, half):
        return (r // 128) * 256 + half * 128 + r % 128

    @staticmethod
    def gvrow(t):
        half, tl = t // 4096, t % 4096
        return (tl // 1024) * 2048 + half * 1024 + tl % 1024

    def select(self):
        S = self.S
        i = 0
        for nm, hpm, inv, n in SEL:
            r0 = self.row0[(nm, inv)]
            mult = 256 if hpm == 128 else hpm
            for hf in range(2):
                q = "sp" if i % 2 == 0 else "act"
                i += 1

                def emit(e, nm=nm, mult=mult, inv=inv, n=n, r0=r0, hf=hf, q=q):
                    hp = S.rt["hp_" + q]
                    start = ((1 - hp) if inv else hp) * mult + self.gprow(OFF[nm], hf)
                    return e.dma_start(out=self.MYP[r0:r0 + n, hf * 4096:(hf + 1) * 4096], in_=self.GP[bass.ds(start, n), :])
                S.dma(q, emit, writes=["MYP"])
        for j in range(4):
            S.dma("sp", lambda e, j=j: e.dma_start(out=self.MYV[j * 2048:(j + 1) * 2048, :],
                                                   in_=self.GV[:, bass.ds(S.rt["hp_sp"] * 128 + 128, 128)][j * 2048:(j + 1) * 2048, :]), writes=["MYV"])
        S.barrier()

    def pt(self, off, hpm, n, c0, c1, inv=False):
        if hpm == 0:
            half = c0 // 4096
            assert off // 128 == (off + n - 1) // 128
            base = self.gprow(off, half)
            return self.GP[base:base + n, c0 - half * 4096:c1 - half * 4096]
        for nm, hm, iv, nn in SEL:
            if hm == hpm and iv == inv and OFF[nm] <= off and (off - OFF[nm]) + n <= nn:
                r0 = self.row0[(nm, inv)] + (off - OFF[nm])
                return self.MYP[r0:r0 + n, c0:c1]
        raise KeyError((off, hpm, n, inv))

    def pv(self, coff, hpm, n, t0, t1):
        assert t0 // 1024 == (t1 - 1) // 1024
        g0 = self.gvrow(t0)
        if hpm == 0:
            return self.GV[g0:g0 + (t1 - t0), coff:coff + n]
        assert coff == 128 and n == 128
        return self.MYV[g0:g0 + (t1 - t0), :]


def common_setup(kb, D):
    S = kb.S
    c = Ctx()
    c.ident = kb.alloc(128)
    c.ones_f = kb.alloc(128)
    c.ones_bf = kb.alloc(128, BF16)
    c.ident_bf = kb.alloc(128, BF16)
    S.dma("sp", lambda e: e.dma_start(out=c.ident, in_=D["ident"][:, :]), writes=["ident"])
    S.op("pool", lambda e: e.memset(c.ones_f, 1.0), writes=["ones_f"])
    S.op("pool", lambda e: e.memset(c.ones_bf, 1.0), writes=["ones_bf"])
    S.op("act", lambda e: e.copy(out=c.ident_bf, in_=c.ident), reads=["ident"], writes=["ident_bf"])
    c.invf = kb.alloc(2)
    S.dma("sp", lambda e: e.dma_start(out=c.invf, in_=D["invf"][:, :]), writes=["invf"])
    return c


def rope_tables(kb, c, posf, posk, r0, r1, col, T, bank, tag):
    S = kb.S
    P = kb.psb[bank]
    n = r1 - r0
    rs = slice(r0, r1)
    a, kf, ki, sn, cs = T["ang"], T["kf"], T["ki"], T["sin"], T["cos"]
    S.op("pe", lambda e: e.matmul(P[rs, :], lhsT=c.ones_f[0:1, 0:n], rhs=posf[0:1, :], start=True, stop=True),
         reads=["ones_f", posk], writes=[pk(bank)])
    S.op("dve", lambda e: e.tensor_scalar(out=a[rs, :], in0=P[rs, :], scalar1=c.invf[rs, col:col + 1], scalar2=None, op0=ALU.mult),
         reads=["invf"], writes=[pk(bank), tag + "ang"])
    S.op("dve", lambda e: e.tensor_scalar(out=ki[rs, :], in0=a[rs, :], scalar1=1.0 / TWO_PI, scalar2=None, op0=ALU.mult),
         reads=[tag + "ang"], writes=[tag + "ki"])
    S.op("dve", lambda e: e.tensor_copy(out=kf[rs, :], in_=ki[rs, :]), reads=[tag + "ki"], writes=[tag + "kf"])
    S.op("dve", lambda e: e.scalar_tensor_tensor(out=a[rs, :], in0=kf[rs, :], scalar=-TWO_PI, in1=a[rs, :], op0=ALU.mult, op1=ALU.add),
         reads=[tag + "kf"], writes=[tag + "ang"])
    S.op("dve", lambda e: e.tensor_scalar(out=kf[rs, :], in0=a[rs, :], scalar1=PI, scalar2=-TWO_PI, op0=ALU.is_gt, op1=ALU.mult),
         reads=[tag + "ang"], writes=[tag + "kf"])
    S.op("dve", lambda e: e.tensor_tensor(out=sn[rs, :], in0=a[rs, :], in1=kf[rs, :], op=ALU.add),
         reads=[tag + "ang", tag + "kf"], writes=[tag + "sin"])
    S.op("dve", lambda e: e.tensor_scalar(out=a[rs, :], in0=a[rs, :], scalar1=PI / 2, scalar2=None, op0=ALU.add),
         reads=[], writes=[tag + "ang"])
    S.op("dve", lambda e: e.tensor_scalar(out=kf[rs, :], in0=a[rs, :], scalar1=PI, scalar2=-TWO_PI, op0=ALU.is_gt, op1=ALU.mult),
         reads=[tag + "ang"], writes=[tag + "kf"])
    S.op("dve", lambda e: e.tensor_tensor(out=cs[rs, :], in0=a[rs, :], in1=kf[rs, :], op=ALU.add),
         reads=[tag + "ang", tag + "kf"], writes=[tag + "cos"])
    S.op("act", lambda e: e.activation(out=sn[rs, :], in_=sn[rs, :], func=AF.Sin), writes=[tag + "sin"])
    S.op("act", lambda e: e.activation(out=cs[rs, :], in_=cs[rs, :], func=AF.Sin), writes=[tag + "cos"])


def alloc_tables(kb):
    return {"ang": kb.alloc(512), "kf": kb.alloc(512), "ki": kb.alloc(512, I32), "sin": kb.alloc(512), "cos": kb.alloc(512)}


def load_pos(kb, D, posi, posf, t):
    S = kb.S
    S.dma("sp", lambda e: e.dma_start(out=posi[0:1, :], in_=D["pos"][0:1, t * TQ:(t + 1) * TQ]), writes=["posi"])
    S.op("dve", lambda e: e.tensor_copy(out=posf[0:1, :], in_=posi[0:1, :]), reads=["posi"], writes=["posf"])


class Attn:
    def __init__(self, kb, sbanks=(0, 1), npt=3):
        self.kb = kb
        self.sbanks = sbanks
        self.PT = [kb.alloc(512, BF16) for _ in range(npt)]
        self.pti = 0
        self.si = 0

    def run(self, blocks, obank, defer=None):
        kb = self.kb
        S = kb.S
        n = len(blocks)
        O = kb.psb[obank]
        banks = []

        def scores(i):
            bank = self.sbanks[self.si % len(self.sbanks)]
            self.si += 1
            banks.append(bank)
            mms = blocks[i][0]
            for j, (l, r, ks) in enumerate(mms):
                S.op("pe", lambda e, l=l, r=r, j=j, bank=bank, nm=len(mms): e.matmul(kb.psb[bank][:, :], lhsT=l, rhs=r, start=(j == 0), stop=(j == nm - 1)),
                     reads=ks, writes=[pk(bank)])

        scores(0)
        for i in range(n):
            if i + 1 < n:
                scores(i + 1)
            bank = banks[i]
            pi_ = self.pti % len(self.PT)
            self.pti += 1
            pt = self.PT[pi_]
            S.op("act", lambda e, pt=pt, bank=bank: e.activation(out=pt, in_=kb.psb[bank][:, :], func=AF.Exp), writes=[pk(bank), f"PT{pi_}"])
            v, vk = blocks[i][1], blocks[i][2]
            S.op("pe", lambda e, v=v, pt=pt, i=i: e.matmul(O[0:65, :], lhsT=v, rhs=pt, start=(i == 0), stop=(i == n - 1)),
                 reads=[f"PT{pi_}"] + vk, writes=[pk(obank)])
            if defer and i == min(2, n - 1):
                for f in defer:
                    f()
                del defer[:]


def norm_coef(kb, c, obank, rowbuf, bcbank, bcs):
    S = kb.S
    O = kb.psb[obank]
    B = kb.psb[bcbank]
    S.op("dve", lambda e: e.tensor_scalar_max(out=rowbuf[64:65, :], in0=O[64:65, :], scalar1=1e-30), writes=[pk(obank), "rowbuf"])
    S.op("dve", lambda e: e.reciprocal(out=rowbuf[64:65, :], in_=rowbuf[64:65, :]), writes=["rowbuf"])
    S.op("pe", lambda e: e.matmul(B[0:64, :], lhsT=c.ones_f[64:65, 0:64], rhs=rowbuf[64:65, :], start=True, stop=True),
         reads=["rowbuf", "ones_f"], writes=[pk(bcbank)])
    S.op("act", lambda e: e.copy(out=bcs[0:64, :], in_=B[0:64, :]), writes=[pk(bcbank), "bcs"])


def part_lru(kb, c, D, yT, L):
    S = kb.S
    m0 = kb.mark()
    xa = kb.alloc(N + 4)
    xc = kb.alloc(N)
    A = kb.alloc(N)
    U = kb.alloc(N)
    G = kb.alloc(N)
    xcb = kb.alloc(N, BF16)
    vec = kb.alloc(16)
    wtmp = kb.alloc(256)
    wbd = kb.alloc(256, BF16)
    T1 = xa[:, 0:N]
    P = kb.psb
    S.op("pool", lambda e: e.memset(xa[:, 0:3], 0.0), writes=["xa_pad"])
    for hf in range(2):
        S.dma("sp", lambda e, hf=hf: e.dma_start(out=xa[:, 3 + hf * 4096:3 + (hf + 1) * 4096], in_=L.pt(OFF["a_x"], 128, 128, hf * 4096, (hf + 1) * 4096)), writes=["xa"])
        S.dma("sp", lambda e, hf=hf: e.dma_start(out=G[:, hf * 4096:(hf + 1) * 4096], in_=L.pt(OFF["a_gate"], 128, 128, hf * 4096, (hf + 1) * 4096)), writes=["G"])
    S.dma("sp", lambda e: e.dma_start(out=vec[:, 0:8], in_=D["lru_v"][:, :]), writes=["vec"])
    S.op("pool", lambda e: e.memset(wtmp, 0.0), writes=["wtmp"])
    for a in range(2):
        for b in range(2):
            S.dma("sp", lambda e, a=a, b=b: e.dma_start(out=wtmp[b * 64:(b + 1) * 64, a * 128 + b * 64:a * 128 + (b + 1) * 64], in_=D["lru_w"][a, b]),
                  writes=["wtmp"])
    S.op("act", lambda e: e.copy(out=wbd, in_=wtmp), reads=["wtmp"], writes=["wbd"])
    S.op("act", lambda e: e.activation(out=vec[:, 8:9], in_=vec[:, 7:8], func=AF.Exp, scale=-1.0), reads=["vec"], writes=["vec8"])
    S.op("act", lambda e: e.activation(out=vec[:, 8:9], in_=vec[:, 8:9], func=AF.Ln, bias=1.0), writes=["vec8"])
    S.op("dve", lambda e: e.tensor_scalar(out=vec[:, 9:10], in0=vec[:, 8:9], scalar1=-8.0, scalar2=None, op0=ALU.mult), reads=["vec8"], writes=["vec9"])
    S.op("dve", lambda e: e.tensor_scalar(out=xc, in0=xa[:, 0:N], scalar1=vec[:, 0:1], scalar2=vec[:, 4:5], op0=ALU.mult, op1=ALU.add),
         reads=["xa", "xa_pad", "vec"], writes=["xc"])
    for j in range(1, 4):
        S.op("dve", lambda e, j=j: e.scalar_tensor_tensor(out=xc, in0=xa[:, j:j + N], scalar=vec[:, j:j + 1], in1=xc, op0=ALU.mult, op1=ALU.add),
             reads=["xa", "xa_pad", "vec"], writes=["xc"])
    S.op("act", lambda e: e.copy(out=xcb, in_=xc), reads=["xc"], writes=["xcb"])
    allA = [f"A{t}" for t in range(16)]
    allU = [f"U{t}" for t in range(16)]
    for t in range(16):
        ts = slice(t * 512, (t + 1) * 512)
        b0, b1 = 2 * (t % 2), 2 * (t % 2) + 1
        S.op("pe", lambda e, ts=ts, b0=b0: e.matmul(P[b0][:, :], lhsT=wbd[:, 0:128], rhs=xcb[:, ts], start=True, stop=True),
             reads=["xcb", "wbd"], writes=[pk(b0)])
        S.op("pe", lambda e, ts=ts, b1=b1: e.matmul(P[b1][:, :], lhsT=wbd[:, 128:256], rhs=xcb[:, ts], start=True, stop=True),
             reads=["xcb", "wbd"], writes=[pk(b1)])
        S.op("act", lambda e, ts=ts, b0=b0: e.activation(out=A[:, ts], in_=P[b0][:, :], func=AF.Sigmoid, bias=vec[:, 5:6]),
             reads=["vec"], writes=[pk(b0), f"A{t}"])
        S.op("act", lambda e, ts=ts, b1=b1: e.activation(out=U[:, ts], in_=P[b1][:, :], func=AF.Sigmoid, bias=vec[:, 6:7]),
             reads=["vec"], writes=[pk(b1), f"U{t}"])
    NP = 4
    W_ = N // NP
    for p_ in range(NP):
        cs = slice(p_ * W_, (p_ + 1) * W_)
        tA = [f"A{t}" for t in range(16) if p_ * W_ <= t * 512 < (p_ + 1) * W_]
        tU = [f"U{t}" for t in range(16) if p_ * W_ <= t * 512 < (p_ + 1) * W_]
        kA, kU, kT, kG, kH = f"Ap{p_}", f"Up{p_}", f"Tp{p_}", f"Gp{p_}", f"Hp{p_}"
        S.op("act", lambda e, cs=cs: e.activation(out=A[:, cs], in_=A[:, cs], func=AF.Exp, scale=vec[:, 9:10]), reads=["vec9"], writes=[kA] + tA)
        S.op("pool", lambda e, cs=cs: e.tensor_tensor(out=T1[:, cs], in0=A[:, cs], in1=A[:, cs], op=ALU.mult), reads=[kA, "xc"], writes=[kT, "xa", "xa_pad"])
        S.op("act", lambda e, cs=cs: e.activation(out=T1[:, cs], in_=T1[:, cs], func=AF.Sqrt, bias=1.0, scale=-1.0), writes=[kT])
        S.op("dve", lambda e, cs=cs: e.tensor_tensor(out=U[:, cs], in0=U[:, cs], in1=xc[:, cs], op=ALU.mult), reads=["xc"], writes=[kU] + tU)
        S.op("dve", lambda e, cs=cs: e.tensor_tensor(out=U[:, cs], in0=U[:, cs], in1=T1[:, cs], op=ALU.mult), reads=[kT], writes=[kU])
    for p_ in range(NP):
        cs = slice(p_ * W_, (p_ + 1) * W_)
        init = 0.0 if p_ == 0 else xc[:, p_ * W_ - 1:p_ * W_]
        S.op("dve", lambda e, cs=cs, init=init: e.tensor_tensor_scan(out=xc[:, cs], data0=A[:, cs], data1=U[:, cs], initial=init, op0=ALU.mult, op1=ALU.add),
             reads=[f"Ap{p_}", f"Up{p_}"] + [f"Up{q_}" for q_ in range(NP)], writes=["xc", f"Hp{p_}"])
    for p_ in range(NP):
        cs = slice(p_ * W_, (p_ + 1) * W_)
        kT, kG = f"Tp{p_}", f"Gp{p_}"
        S.op("pool", lambda e, cs=cs: e.tensor_tensor(out=T1[:, cs], in0=G[:, cs], in1=G[:, cs], op=ALU.mult), reads=["G", f"Up{p_}"], writes=[kT])
        S.op("pool", lambda e, cs=cs: e.tensor_scalar(out=T1[:, cs], in0=T1[:, cs], scalar1=0.044715, scalar2=1.0, op0=ALU.mult, op1=ALU.add), writes=[kT])
        S.op("pool", lambda e, cs=cs: e.tensor_tensor(out=T1[:, cs], in0=T1[:, cs], in1=G[:, cs], op=ALU.mult), reads=["G"], writes=[kT])
        S.op("act", lambda e, cs=cs: e.activation(out=T1[:, cs], in_=T1[:, cs], func=AF.Sigmoid, scale=1.5957691216057308), writes=[kT])
        S.op("dve", lambda e, cs=cs: e.tensor_tensor(out=G[:, cs], in0=G[:, cs], in1=T1[:, cs], op=ALU.mult), reads=[kT], writes=[kG])
        S.op("dve", lambda e, cs=cs: e.tensor_tensor(out=A[:, cs], in0=xc[:, cs], in1=G[:, cs], op=ALU.mult), reads=[f"Hp{p_}", kG], writes=[f"Ap{p_}"])
        for hf in range(2):
            if hf * 4096 >= p_ * W_ and (hf + 1) * 4096 <= (p_ + 1) * W_ or (p_ * W_ >= hf * 4096 and (p_ + 1) * W_ <= (hf + 1) * 4096):
                c0, c1 = max(hf * 4096, p_ * W_), min((hf + 1) * 4096, (p_ + 1) * W_)
                S.dma("sp", lambda e, c0=c0, c1=c1: e.dma_start(out=yT[0:128, c0:c1], in_=A[:, c0:c1]), reads=[f"Ap{p_}"])
    S.barrier()
    kb.release(m0)


def part_mla(kb, c, D, yT, L):
    S = kb.S
    m0 = kb.mark()
    P = kb.psb
    SC = float(96 ** -0.5)
    QD = [kb.alloc(N, BF16) for _ in range(2)]
    KD = [kb.alloc(N, BF16) for _ in range(2)]
    VD = kb.alloc(64 * 2 * 66, BF16).rearrange("p (b h d) -> p b h d", b=64, h=2)
    tri = kb.alloc(4 * 512, BF16).rearrange("p (i q) -> p i q", i=4)
    wuq = kb.alloc(2 * 192, BF16).rearrange("p (c n) -> p c n", c=2)
    wk = kb.alloc(128, BF16)
    wv = kb.alloc(128, BF16)
    vec = kb.alloc(4)
    r32 = kb.alloc(32)
    st = kb.alloc(512)
    S.op("pool", lambda e: e.memset(VD, 1.0), writes=["VD"])
    S.dma("sp", lambda e: e.dma_start(out=vec[:, 0:3], in_=D["mla_v"][:, :]), writes=["mvec"])
    S.dma("sp", lambda e: e.dma_start(out=r32, in_=D["r32"][:, :]), writes=["r32"])
    for i in range(4):
        S.dma("sp", lambda e, i=i: e.dma_start(out=st, in_=D["tri"][i]), writes=["st"])
        S.op("act", lambda e, i=i: e.copy(out=tri[:, i, :], in_=st), reads=["st"], writes=["tri"])
    S.dma("sp", lambda e: e.dma_start(out=st[:, 0:192], in_=D["mla_wuq"][0:128, :]), writes=["st"])
    S.op("act", lambda e: e.copy(out=wuq[:, 0, :], in_=st[:, 0:192]), reads=["st"], writes=["wuq"])
    S.dma("sp", lambda e: e.dma_start(out=st[0:64, 0:192], in_=D["mla_wuq"][128:192, :]), writes=["st"])
    S.op("act", lambda e: e.copy(out=wuq[0:64, 1, :], in_=st[0:64, 0:192]), reads=["st"], writes=["wuq"])
    S.dma("sp", lambda e: e.dma_start(out=st[:, 0:128], in_=D["mla_wk"][:, :]), writes=["st"])
    S.op("act", lambda e: e.copy(out=wk, in_=st[:, 0:128]), reads=["st"], writes=["wk"])
    S.dma("sp", lambda e: e.dma_start(out=st[:, 0:128], in_=D["mla_wv"][:, :]), writes=["st"])
    S.op("act", lambda e: e.copy(out=wv, in_=st[:, 0:128]), reads=["st"], writes=["wv"])

    m1 = kb.mark()
    cq0 = kb.alloc(512)
    cq1 = kb.alloc(512)
    ckv = kb.alloc(512)
    krt = kb.alloc(512)
    sq0 = kb.alloc(512, BF16)
    sq1 = kb.alloc(512, BF16)
    cn0 = kb.alloc(512, BF16)
    cn1 = kb.alloc(512, BF16)
    ckn = kb.alloc(512, BF16)
    rstd = kb.alloc(512)
    qr = kb.alloc(512)
    t1 = kb.alloc(512)
    t2 = kb.alloc(512)
    posi = kb.alloc(512, I32)
    posf = kb.alloc(512)
    T = alloc_tables(kb)
    R = slice(64, 96)
    for t in range(NQT):
        ts = slice(t * TQ, (t + 1) * TQ)
        load_pos(kb, D, posi, posf, t)
        S.dma("sp", lambda e, ts=ts: e.dma_start(out=cq0, in_=L.pt(OFF["d_cq"], 0, 128, ts.start, ts.stop)), writes=["cq0"])
        S.dma("sp", lambda e, ts=ts: e.dma_start(out=cq1[0:64, :], in_=L.pt(OFF["d_cq"] + 128, 0, 64, ts.start, ts.stop)), writes=["cq1"])
        S.dma("sp", lambda e, ts=ts: e.dma_start(out=ckv, in_=L.pt(OFF["d_ckv"], 0, 128, ts.start, ts.stop)), writes=["ckv"])
        S.dma("sp", lambda e, ts=ts: e.dma_start(out=krt[R, :], in_=L.pt(OFF["d_kr"], 0, 32, ts.start, ts.stop)), writes=["krt"])
        rope_tables(kb, c, posf, "posf", 64, 96, 1, T, 6, "m")
        S.op("act", lambda e: e.activation(out=sq0, in_=cq0, func=AF.Square), reads=["cq0"], writes=["sq0"])
        S.op("act", lambda e: e.activation(out=sq1[0:64, :], in_=cq1[0:64, :], func=AF.Square), reads=["cq1"], writes=["sq1"])
        S.op("pe", lambda e: e.matmul(P[0][:, :], lhsT=c.ones_bf, rhs=sq0, start=True, stop=False), reads=["sq0", "ones_bf"], writes=[pk(0)])
        S.op("pe", lambda e: e.matmul(P[0][:, :], lhsT=c.ones_bf[0:64, :], rhs=sq1[0:64, :], start=False, stop=True), reads=["sq1", "ones_bf"], writes=[pk(0)])
        S.op("act", lambda e: e.activation(out=rstd, in_=P[0][:, :], func=AF.Sqrt, bias=EPS, scale=1.0 / 192.0), writes=[pk(0), "rstd"])
        S.op("dve", lambda e: e.reciprocal(out=rstd, in_=rstd), writes=["rstd"])
        S.op("dve", lambda e: e.scalar_tensor_tensor(out=cn0, in0=cq0, scalar=vec[:, 0:1], in1=rstd, op0=ALU.mult, op1=ALU.mult),
             reads=["cq0", "rstd", "mvec"], writes=["cn0"])
        S.op("dve", lambda e: e.scalar_tensor_tensor(out=cn1[0:64, :], in0=cq1[0:64, :], scalar=vec[0:64, 1:2], in1=rstd[0:64, :], op0=ALU.mult, op1=ALU.mult),
             reads=["cq1", "rstd", "mvec"], writes=["cn1"])
        for h in range(2):
            hs = slice(h * 96, (h + 1) * 96)
            S.op("pe", lambda e, hs=hs: e.matmul(P[1][0:96, :], lhsT=wuq[:, 0, hs], rhs=cn0, start=True, stop=False), reads=["cn0", "wuq"], writes=[pk(1)])
            S.op("pe", lambda e, hs=hs: e.matmul(P[1][0:96, :], lhsT=wuq[0:64, 1, hs], rhs=cn1[0:64, :], start=False, stop=True), reads=["cn1", "wuq"], writes=[pk(1)])
            S.op("act", lambda e, h=h, ts=ts: e.mul(out=QD[h][0:64, ts], in_=P[1][0:64, :], mul=SC), writes=[pk(1), f"QD{h}"])
            S.op("dve", lambda e: e.tensor_copy(out=qr[R, :], in_=P[1][R, :]), writes=[pk(1), "qr"])
            S.op("pe", lambda e: e.matmul(P[2][R, :], lhsT=r32[R, 0:32], rhs=qr[R, :], start=True, stop=True), reads=["qr", "r32"], writes=[pk(2)])
            S.op("dve", lambda e: e.scalar_tensor_tensor(out=t1[R, :], in0=qr[R, :], scalar=SC, in1=T["cos"][R, :], op0=ALU.mult, op1=ALU.mult),
                 reads=["qr", "mcos"], writes=["t1"])
            S.op("dve", lambda e: e.scalar_tensor_tensor(out=t2[R, :], in0=P[2][R, :], scalar=SC, in1=T["sin"][R, :], op0=ALU.mult, op1=ALU.mult),
                 reads=["msin"], writes=[pk(2), "t2"])
            S.op("dve", lambda e, h=h, ts=ts: e.tensor_tensor(out=QD[h][R, ts], in0=t1[R, :], in1=t2[R, :], op=ALU.add), reads=["t1", "t2"], writes=[f"QD{h}"])
        S.op("act", lambda e: e.activation(out=sq0, in_=ckv, func=AF.Square), reads=["ckv"], writes=["sq0"])
        S.op("pe", lambda e: e.matmul(P[7][:, :], lhsT=c.ones_bf, rhs=sq0, start=True, stop=True), reads=["sq0", "ones_bf"], writes=[pk(7)])
        S.op("act", lambda e: e.activation(out=rstd, in_=P[7][:, :], func=AF.Sqrt, bias=EPS, scale=1.0 / 128.0), writes=[pk(7), "rstd"])
        S.op("dve", lambda e: e.reciprocal(out=rstd, in_=rstd), writes=["rstd"])
        S.op("dve", lambda e: e.scalar_tensor_tensor(out=ckn, in0=ckv, scalar=vec[:, 2:3], in1=rstd, op0=ALU.mult, op1=ALU.mult),
             reads=["ckv", "rstd", "mvec"], writes=["ckn"])
        for h in range(2):
            S.op("pe", lambda e, h=h: e.matmul(P[3][0:64, :], lhsT=wk[:, h * 64:(h + 1) * 64], rhs=ckn, start=True, stop=True), reads=["ckn", "wk"], writes=[pk(3)])
            S.op("act", lambda e, h=h, ts=ts: e.copy(out=KD[h][0:64, ts], in_=P[3][0:64, :]), writes=[pk(3), f"KD{h}"])
        for tb in range(4):
            S.op("pe", lambda e, tb=tb: e.matmul(P[4][:, 0:128], lhsT=ckn[:, tb * 128:(tb + 1) * 128], rhs=wv, start=True, stop=True), reads=["ckn", "wv"], writes=[pk(4)])
            S.op("act", lambda e, tb=tb, t=t: e.copy(out=VD[:, 4 * t + tb, :, 0:64], in_=P[4][:, 0:128].rearrange("p (h d) -> p h d", h=2)),
                 writes=[pk(4), "VD"])
        S.op("pe", lambda e: e.matmul(P[5][R, :], lhsT=r32[R, 0:32], rhs=krt[R, :], start=True, stop=True), reads=["krt", "r32"], writes=[pk(5)])
        S.op("dve", lambda e: e.tensor_tensor(out=t1[R, :], in0=krt[R, :], in1=T["cos"][R, :], op=ALU.mult), reads=["krt", "mcos"], writes=["t1"])
        S.op("dve", lambda e: e.tensor_tensor(out=t2[R, :], in0=P[5][R, :], in1=T["sin"][R, :], op=ALU.mult), reads=["msin"], writes=[pk(5), "t2"])
        for h in range(2):
            S.op("dve", lambda e, h=h, ts=ts: e.tensor_tensor(out=KD[h][R, ts], in0=t1[R, :], in1=t2[R, :], op=ALU.add), reads=["t1", "t2"], writes=[f"KD{h}"])
    S.barrier()
    kb.release(m1)
    att = Attn(kb, sbanks=(0, 1))
    rowbuf = kb.alloc(512)
    bcs = kb.alloc(512)
    yst = [kb.alloc(512), kb.alloc(512)]
    it = 0
    pending = []
    for qt in range(NQT):
        qs = slice(qt * TQ, (qt + 1) * TQ)
        for h in range(2):
            blocks = []
            for kbk in range(4 * qt + 4):
                mms = [(KD[h][0:96, kbk * 128:(kbk + 1) * 128], QD[h][0:96, qs], [f"KD{h}", f"QD{h}"])]
                if kbk >= 4 * qt:
                    mms.append((c.ident_bf, tri[:, kbk - 4 * qt, :], ["ident_bf", "tri"]))
                blocks.append((mms, VD[:, kbk, h, 0:65], ["VD"]))
            ob = 2 + it % 2
            att.run(blocks, ob, defer=pending)

            def fin(ob=ob, ys=yst[it % 2], yk=f"yst{it % 2}", h=h, qs=qs):
                norm_coef(kb, c, ob, rowbuf, 4, bcs)
                S.op("dve", lambda e: e.tensor_tensor(out=ys[0:64, :], in0=P[ob][0:64, :], in1=bcs[0:64, :], op=ALU.mult),
                     reads=["bcs"], writes=[pk(ob), yk])
                S.dma("sp", lambda e: e.dma_start(out=yT[384 + h * 64:384 + (h + 1) * 64, qs], in_=ys[0:64, :]), reads=[yk])
            pending.append(fin)
            it += 1
    for f in pending:
        f()
    S.barrier()
    kb.release(m0)


def load_cast(kb, src_ap, dst_ap, stage, skey, dkey, eng="act", rows=slice(0, 128)):
    S = kb.S
    S.dma("sp", lambda e: e.dma_start(out=stage, in_=(src_ap() if callable(src_ap) else src_ap)), writes=[skey])
    if eng == "act":
        S.op("act", lambda e: e.copy(out=dst_ap, in_=stage), reads=[skey], writes=[dkey])
    else:
        S.op("pool", lambda e: e.tensor_copy(out=dst_ap, in_=stage), reads=[skey], writes=[dkey])


def gelu_tanh(kb, z, u, out, zk, uk, outk):
    S = kb.S
    S.op("pool", lambda e: e.tensor_tensor(out=u, in0=z, in1=z, op=ALU.mult), reads=[zk], writes=[uk])
    S.op("pool", lambda e: e.tensor_scalar(out=u, in0=u, scalar1=0.044715, scalar2=1.0, op0=ALU.mult, op1=ALU.add), writes=[uk])
    S.op("pool", lambda e: e.tensor_tensor(out=u, in0=u, in1=z, op=ALU.mult), reads=[zk], writes=[uk])
    S.op("act", lambda e: e.activation(out=u, in_=u, func=AF.Sigmoid, scale=1.5957691216057308), writes=[uk])
    S.op("dve", lambda e: e.tensor_tensor(out=out, in0=z, in1=u, op=ALU.mult), reads=[zk, uk], writes=[outk])


def part_nsa(kb, c, D, yT, L):
    S = kb.S
    P = kb.psb
    m0 = kb.mark()
    Qm = kb.alloc(N, BF16)
    Qo = kb.alloc(N, BF16)
    KSA = kb.alloc(N, BF16)
    KSB = kb.alloc(N, BF16)
    KW2 = kb.alloc(N, BF16)
    tri = kb.alloc(4 * 512, BF16).rearrange("p (i q) -> p i q", i=4)
    win = kb.alloc(8 * 512, BF16).rearrange("p (i q) -> p i q", i=8)
    cmpm = kb.alloc(5 * 512, BF16).rearrange("p (i q) -> p i q", i=5)
    rbd = kb.alloc(128)
    ovl = kb.alloc(4 * 130, BF16).rearrange("p (i q) -> p i q", i=4)
    bv = kb.alloc(254)
    bf = kb.alloc(254)
    selg = kb.alloc(384)
    KCMP2 = kb.alloc(512, BF16)
    VCMP = kb.alloc(4 * 66, BF16).rearrange("p (b d) -> p b d", b=4)
    st = kb.alloc(1024)
    st3 = st.rearrange("p (b d) -> p b d", d=64)
    S.op("pool", lambda e: e.memset(KSA[64:128, :], 0.0), writes=["ksz"])
    S.op("pool", lambda e: e.memset(KSB[0:64, :], 0.0), writes=["ksz"])
    S.op("pool", lambda e: e.memset(VCMP, 1.0), writes=["VCMP"])
    S.dma("sp", lambda e: e.dma_start(out=rbd, in_=D["rbd"][:, :]), writes=["rbd"])
    S.dma("sp", lambda e: e.dma_start(out=bv, in_=D["bv"][:, :]), writes=["bv"])
    S.dma("sp", lambda e: e.dma_start(out=bf, in_=D["bf"][:, :]), writes=["bf"])
    S.dma("sp", lambda e: e.dma_start(out=selg[0:8, :], in_=D["selg"][:, :]), writes=["selg"])
    i = 0
    for nm, dst, cnt in (("tri", tri, 4), ("win", win, 8), ("cmpm", cmpm, 5)):
        for j in range(cnt):
            load_cast(kb, D[nm][j], dst[:, j, :], st[:, 0:512], "st", nm, "act" if i % 2 == 0 else "pool")
            i += 1
    for j in range(4):
        load_cast(kb, D["ovl"][j], ovl[:, j, 0:129], st[:, 0:129], "st", "ovl", "act")

    m1 = kb.mark()
    KCV = kb.alloc(N)
    xs = [kb.alloc(512), kb.alloc(512)]
    t1s = [kb.alloc(512), kb.alloc(512)]
    t2s = [kb.alloc(512), kb.alloc(512)]
    posi = kb.alloc(512, I32)
    posf = kb.alloc(512)
    T = alloc_tables(kb)
    for hf in range(2):
        S.dma("sp", lambda e, hf=hf: e.dma_start(out=KCV[64:128, hf * 4096:(hf + 1) * 4096], in_=L.pt(OFF["b_kv"] + 64, 0, 64, hf * 4096, (hf + 1) * 4096)), writes=["KCVv"])
    it = 0
    for t in range(NQT):
        ts = slice(t * TQ, (t + 1) * TQ)
        load_pos(kb, D, posi, posf, t)
        rope_tables(kb, c, posf, "posf", 0, 128, 0, T, 6, "n")
        a0, a1 = ts.start, ts.stop
        items = [("q0", [lambda a0=a0, a1=a1: L.pt(OFF["b_q"], 128, 128, a0, a1)], Qm, 0.125, 128),
                 ("q1", [lambda a0=a0, a1=a1: L.pt(OFF["b_q"], 128, 128, a0, a1, inv=True)], Qo, 0.125, 128),
                 ("ks", [lambda a0=a0, a1=a1: L.pt(OFF["b_kv"] + 128, 0, 64, a0, a1)] * 2, None, 1.0, 128),
                 ("kw", [lambda a0=a0, a1=a1: L.pt(OFF["b_kv"] + 192, 0, 64, a0, a1)] * 2, KW2, 1.0, 128),
                 ("kc", [lambda a0=a0, a1=a1: L.pt(OFF["b_kv"], 0, 64, a0, a1)], KCV, 1.0, 64)]
        for nm, srcs, dst, sc, rows in items:
            x = xs[it % 2]
            xk = f"xs{it % 2}"
            t1 = t1s[it % 2]
            t2 = t2s[it % 2]
            bank = 4 + it % 2
            if len(srcs) == 2:
                S.dma("sp", lambda e, x=x, srcs=srcs: e.dma_start(out=x[0:64, :], in_=srcs[0]()), writes=[xk])
                S.dma("sp", lambda e, x=x, srcs=srcs: e.dma_start(out=x[64:128, :], in_=srcs[1]()), writes=[xk])
            else:
                S.dma("sp", lambda e, x=x, srcs=srcs, rows=rows: e.dma_start(out=x[0:rows, :], in_=srcs[0]()), writes=[xk])
            R = slice(0, rows)
            S.op("pe", lambda e, x=x, R=R, bank=bank, rows=rows: e.matmul(P[bank][R, :], lhsT=rbd[R, 0:rows], rhs=x[R, :], start=True, stop=True),
                 reads=[xk, "rbd"], writes=[pk(bank)])
            S.op("dve", lambda e, x=x, R=R, t1=t1, sc=sc: e.scalar_tensor_tensor(out=t1[R, :], in0=x[R, :], scalar=sc, in1=T["cos"][R, :], op0=ALU.mult, op1=ALU.mult),
                 reads=[xk, "ncos"], writes=[f"t1{it % 2}"])
            S.op("dve", lambda e, R=R, t2=t2, sc=sc, bank=bank: e.scalar_tensor_tensor(out=t2[R, :], in0=P[bank][R, :], scalar=sc, in1=T["sin"][R, :], op0=ALU.mult, op1=ALU.mult),
                 reads=["nsin"], writes=[pk(bank), f"t2{it % 2}"])
            dk = "KCVk" if nm == "kc" else nm
            if nm == "ks":
                for dst_, RR in ((KSA, slice(0, 64)), (KSB, slice(64, 128))):
                    S.op("pool", lambda e, RR=RR, t1=t1, t2=t2, dst_=dst_, ts=ts: e.tensor_tensor(out=dst_[RR, ts], in0=t1[RR, :], in1=t2[RR, :], op=ALU.add),
                         reads=[f"t1{it % 2}", f"t2{it % 2}"], writes=[dk])
            else:
                S.op("pool", lambda e, R=R, t1=t1, t2=t2, dst=dst, ts=ts: e.tensor_tensor(out=dst[R, ts], in0=t1[R, :], in1=t2[R, :], op=ALU.add),
                     reads=[f"t1{it % 2}", f"t2{it % 2}"], writes=[dk])
            it += 1
    S.barrier()
    kb.release(m1)
    KCV = kb.alloc(N)
    BLK = kb.alloc(32 * 512, BF16).rearrange("p (l n) -> p l n", l=32)
    W1 = kb.alloc(32 * 256, BF16).rearrange("p (l h) -> p l h", l=32)
    pos2 = kb.alloc(32)
    b1 = kb.alloc(4)
    w2 = kb.alloc(2 * 2 * 64, BF16).rearrange("p (k m d) -> p k m d", k=2, m=2)
    HID = kb.alloc(2 * 2 * 512, BF16).rearrange("p (k m n) -> p k m n", k=2, m=2)
    zt = kb.alloc(512)
    ut = kb.alloc(512)
    stw = kb.alloc(1024).rearrange("p (l h) -> p l h", l=4)
    S.dma("sp", lambda e: e.dma_start(out=pos2, in_=D["nsa_pos"][:, :]), writes=["pos2"])
    S.dma("sp", lambda e: e.dma_start(out=b1, in_=D["nsa_b1"][:, :]), writes=["b1"])
    for kv in range(2):
        load_cast(kb, D["nsa_w2"][kv].rearrange("(m p) d -> p m d", p=128), w2[:, kv, :, :], st[:, 0:128].rearrange("p (m d) -> p m d", m=2), "st", "w2", "act")
    for l0 in range(0, 32, 4):
        for kv in range(2):
            src = D["nsa_w1"][kv].rearrange("(l d) h -> d l h", d=64)[:, l0:l0 + 4, :]
            S.dma("sp", lambda e, src=src, kv=kv: e.dma_start(out=stw[kv * 64:(kv + 1) * 64, :, :], in_=src), writes=["stw"])
        if (l0 // 4) % 2 == 0:
            S.op("act", lambda e, l0=l0: e.copy(out=W1[:, l0:l0 + 4, :], in_=stw), reads=["stw"], writes=["W1"])
        else:
            S.op("pool", lambda e, l0=l0: e.tensor_copy(out=W1[:, l0:l0 + 4, :], in_=stw), reads=["stw"], writes=["W1"])
    S.op("pool", lambda e: e.memset(BLK[:, :, 511:512], 0.0), writes=["BLKpad"])
    K3 = KCV.rearrange("p (g r) -> p g r", r=16)
    for l in range(32):
        src = K3[:, 0:511, l] if l < 16 else K3[:, 1:512, l - 16]
        eng = "dve" if l % 2 == 0 else "pool"
        S.op(eng, lambda e, l=l, src=src: e.tensor_scalar(out=BLK[:, l, 0:511], in0=src, scalar1=pos2[:, l:l + 1], scalar2=None, op0=ALU.add),
             reads=["pos2"], writes=[f"BLK{l}"])
    for kv in range(2):
        R = slice(kv * 64, kv * 64 + 64)
        for m in range(2):
            bank = 2 * kv + m
            for l in range(32):
                S.op("pe", lambda e, l=l, R=R, m=m, bank=bank: e.matmul(P[bank][:, :], lhsT=W1[R, l, m * 128:(m + 1) * 128], rhs=BLK[R, l, :], start=(l == 0), stop=(l == 31)),
                     reads=["W1", f"BLK{l}", "BLKpad"], writes=[pk(bank)])
            S.op("act", lambda e, kv=kv, m=m, bank=bank: e.activation(out=zt, in_=P[bank][:, :], func=AF.Identity, bias=b1[:, kv * 2 + m:kv * 2 + m + 1]),
                 reads=["b1"], writes=[pk(bank), "zt"])
            gelu_tanh(kb, zt, ut, HID[:, kv, m, :], "zt", "ut", f"HID{kv}")
    for m in range(2):
        S.op("pe", lambda e, m=m: e.matmul(P[4][0:64, :], lhsT=w2[:, 0, m, :], rhs=HID[:, 0, m, :], start=(m == 0), stop=(m == 1)), reads=["w2", "HID0"], writes=[pk(4)])
    S.op("act", lambda e: e.copy(out=KCMP2[0:64, :], in_=P[4][0:64, :]), writes=[pk(4), "KCMP2"])
    S.op("dve", lambda e: e.tensor_copy(out=KCMP2[64:128, :], in_=P[4][0:64, :]), writes=[pk(4), "KCMP2"])
    for nb in range(4):
        for m in range(2):
            S.op("pe", lambda e, m=m, nb=nb: e.matmul(P[5][:, 0:64], lhsT=HID[:, 1, m, nb * 128:(nb + 1) * 128], rhs=w2[:, 1, m, :], start=(m == 0), stop=(m == 1)),
                 reads=["w2", "HID1"], writes=[pk(5)])
        S.op("act", lambda e, nb=nb: e.copy(out=VCMP[:, nb, 0:64], in_=P[5][:, 0:64]), writes=[pk(5), "VCMP"])
    S.barrier()
    kb.release(m1)
    VS = kb.alloc(64 * 66, BF16).rearrange("p (b d) -> p b d", b=64)
    VW = kb.alloc(64 * 66, BF16).rearrange("p (b d) -> p b d", b=64)
    S.op("pool", lambda e: e.memset(VS, 1.0), writes=["VS"])
    S.op("pool", lambda e: e.memset(VW, 1.0), writes=["VW"])
    for nm, dst, coff in (("nsa_vs", VS, 0), ("nsa_vw", VW, 64)):
        for b0 in range(0, 64, 8):
            load_cast(kb, (lambda b0=b0, coff=coff: L.pv(coff, 0, 64, b0 * 128, (b0 + 8) * 128).rearrange("(b p) d -> p b d", p=128)),
                      dst[:, b0:b0 + 8, 0:64], st3[:, 0:8, :], "st", nm[-2:].upper(), "act" if (b0 // 8) % 2 == 0 else "pool")

    NST = kb.alloc(N, BF16)
    EALL = kb.alloc(N, BF16)
    for j in range(16):
        load_cast(kb, D["eall"][:, j * 512:(j + 1) * 512], EALL[:, j * 512:(j + 1) * 512], st[:, 0:512], "st", "EALL", "act" if j % 2 == 0 else "pool")
    ET = [kb.alloc(512, BF16) for _ in range(4)]
    IMP = kb.alloc(512).rearrange("p (a s) -> p a s", a=4)
    scr = kb.alloc(128)
    mr = kb.alloc(128)
    nsb = kb.alloc(128)
    mx = kb.alloc(16)
    thr = kb.alloc(2)
    rsum = kb.alloc(2)
    g6 = kb.alloc(512)
    gs = [[kb.alloc(512) for _ in range(3)] for _ in range(2)]
    acc = [kb.alloc(512), kb.alloc(512)]
    ctmp = kb.alloc(512)
    otmp = kb.alloc(512)
    rowbuf = kb.alloc(512)
    bcs = kb.alloc(512)
    att = Attn(kb, sbanks=(0, 1))
    oi = 0

    def combine(ob, hA, br, first):
        norm_coef(kb, c, ob, rowbuf, 4, bcs)
        S.op("dve", lambda e: e.tensor_tensor(out=ctmp[0:64, :], in0=bcs[0:64, :], in1=gs[hA][br][0:64, :], op=ALU.mult),
             reads=["bcs", f"gs{hA}{br}"], writes=["ctmp"])
        if first:
            S.op("dve", lambda e: e.tensor_tensor(out=acc[hA][0:64, :], in0=P[ob][0:64, :], in1=ctmp[0:64, :], op=ALU.mult),
                 reads=["ctmp"], writes=[pk(ob), f"acc{hA}"])
        else:
            S.op("dve", lambda e: e.tensor_tensor(out=otmp[0:64, :], in0=P[ob][0:64, :], in1=ctmp[0:64, :], op=ALU.mult),
                 reads=["ctmp"], writes=[pk(ob), "otmp"])
            S.op("pool", lambda e: e.tensor_tensor(out=acc[hA][0:64, :], in0=acc[hA][0:64, :], in1=otmp[0:64, :], op=ALU.add),
                 reads=["otmp"], writes=[f"acc{hA}"])

    pending = []
    for qt in range(NQT):
        qs = slice(qt * TQ, (qt + 1) * TQ)
        for f in pending:
            f()
        del pending[:]
        S.dma("sp", lambda e, qs=qs: e.dma_start(out=g6[0:6, :], in_=L.pt(OFF["b_gate"], 6, 6, qs.start, qs.stop)), writes=["g6"])
        for hA in range(2):
            for br in range(3):
                r = hA * 3 + br
                S.op("pe", lambda e, r=r: e.matmul(P[5][0:64, :], lhsT=selg[0:6, r * 64:(r + 1) * 64], rhs=g6[0:6, :], start=True, stop=True),
                     reads=["g6", "selg"], writes=[pk(5)])
                S.op("act", lambda e, hA=hA, br=br: e.activation(out=gs[hA][br][0:64, :], in_=P[5][0:64, :], func=AF.Sigmoid), writes=[pk(5), f"gs{hA}{br}"])
        nbm = (512 * qt + 480) // 2048
        for hh in range(4):
            Qt = (Qm if hh < 2 else Qo)
            qk = "q0" if hh < 2 else "q1"
            R = slice(64 * (hh % 2), 64 * (hh % 2) + 64)
            ob = 2 + oi % 2
            for nb in range(nbm + 1):
                bank = nb % 2
                dl = 512 * qt - 2048 * nb
                mms = [(KCMP2[R, nb * 128:(nb + 1) * 128], Qt[R, qs], ["KCMP2", qk])]
                if dl < 2560:
                    mms.append((c.ident_bf, cmpm[:, dl // 512, :], ["ident_bf", "cmpm"]))
                for j, (l_, r_, ks) in enumerate(mms):
                    S.op("pe", lambda e, l_=l_, r_=r_, j=j, bank=bank, nm=len(mms): e.matmul(P[bank][:, :], lhsT=l_, rhs=r_, start=(j == 0), stop=(j == nm - 1)),
                         reads=ks, writes=[pk(bank)])
                S.op("act", lambda e, nb=nb, bank=bank: e.activation(out=ET[nb], in_=P[bank][:, :], func=AF.Exp), writes=[pk(bank), f"ET{nb}"])
                if hh < 2:
                    S.op("pe", lambda e, nb=nb, ob=ob, nbm=nbm: e.matmul(P[ob][0:65, :], lhsT=VCMP[:, nb, 0:65], rhs=ET[nb], start=(nb == 0), stop=(nb == nbm)),
                         reads=[f"ET{nb}", "VCMP"], writes=[pk(ob)])
            for s4 in range(4):
                for nb in range(nbm + 1):
                    S.op("pe", lambda e, nb=nb, s4=s4, nbm=nbm: e.matmul(P[6][:, 0:129], lhsT=ET[nb][:, s4 * 128:(s4 + 1) * 128], rhs=ovl[:, nb, 0:129], start=(nb == 0), stop=(nb == nbm)),
                         reads=[f"ET{nb}", "ovl"], writes=[pk(6)])
                S.op("dve", lambda e: e.tensor_scalar_max(out=rsum[:, 0:1], in0=P[6][:, 128:129], scalar1=1e-30), writes=[pk(6), "rsum"])
                S.op("dve", lambda e: e.reciprocal(out=rsum[:, 0:1], in_=rsum[:, 0:1]), writes=["rsum"])
                if hh == 0:
                    S.op("dve", lambda e, s4=s4: e.tensor_scalar(out=IMP[:, s4, :], in0=P[6][:, 0:128], scalar1=rsum[:, 0:1], scalar2=None, op0=ALU.mult),
                         reads=["rsum"], writes=[pk(6), f"IMP{s4}"])
                else:
                    S.op("dve", lambda e, s4=s4: e.scalar_tensor_tensor(out=IMP[:, s4, :], in0=P[6][:, 0:128], scalar=rsum[:, 0:1], in1=IMP[:, s4, :], op0=ALU.mult, op1=ALU.add),
                         reads=["rsum"], writes=[pk(6), f"IMP{s4}"])
            if hh < 2:
                combine(ob, hh, 0, True)
                oi += 1
        for s4 in range(4):
            qb = 4 * qt + s4
            o0 = 126 - 2 * qb
            S.op("dve", lambda e, s4=s4, o0=o0: e.tensor_tensor(out=scr, in0=IMP[:, s4, :], in1=bv[:, o0:o0 + 128], op=ALU.mult), reads=[f"IMP{s4}", "bv"], writes=["scr"])
            S.op("dve", lambda e, o0=o0: e.tensor_tensor(out=scr, in0=scr, in1=bf[:, o0:o0 + 128], op=ALU.add), reads=["bf"], writes=["scr"])
            S.op("dve", lambda e: e.memset(scr[:, 0:1], 1e9), writes=["scr"])
            S.op("dve", lambda e: e.max(out=mx[:, 0:8], in_=scr), reads=["scr"], writes=["mx"])
            S.op("dve", lambda e: e.match_replace(out=mr, in_to_replace=mx[:, 0:8], in_values=scr, imm_value=-2.0), reads=["scr", "mx"], writes=["mr"])
            S.op("dve", lambda e: e.max(out=mx[:, 8:16], in_=mr), reads=["mr"], writes=["mx"])
            S.op("dve", lambda e: e.tensor_reduce(out=thr[:, 0:1], in_=mx[:, 8:16], axis=AX.X, op=ALU.min), reads=["mx"], writes=["thr"])
            S.op("dve", lambda e: e.tensor_scalar(out=nsb, in0=scr, scalar1=thr[:, 0:1], scalar2=-NEG, op0=ALU.is_ge, op1=ALU.mult), reads=["scr", "thr"], writes=["nsb"])
            S.op("pool", lambda e: e.tensor_scalar(out=nsb, in0=nsb, scalar1=NEG, scalar2=None, op0=ALU.add), writes=["nsb"])
            S.op("pe", lambda e: e.transpose(P[7][:, 0:128], nsb, c.ident), reads=["nsb", "ident"], writes=[pk(7)])
            S.op("act", lambda e, qb=qb: e.copy(out=NST[:, qb * 128:(qb + 1) * 128], in_=P[7][:, 0:128]), writes=[pk(7), "NST"])
        for hA in range(2):
            R = slice(64 * hA, 64 * hA + 64)
            blocks = []
            for kbk in range(4 * qt + 4):
                mms = [((KSA if hA == 0 else KSB)[:, kbk * 128:(kbk + 1) * 128], Qm[:, qs], ["ks", "q0", "ksz"]),
                       (EALL[:, kbk * 128:(kbk + 1) * 128], NST[:, qs], ["EALL", "NST"])]
                if kbk >= 4 * qt:
                    mms.append((c.ident_bf, tri[:, kbk - 4 * qt, :], ["ident_bf", "tri"]))
                blocks.append((mms, VS[:, kbk, 0:65], ["VS"]))
            ob = 2 + oi % 2
            oi += 1
            att.run(blocks, ob, defer=pending)
            pending.append(lambda ob=ob, hA=hA: combine(ob, hA, 1, False))
            blocks = []
            for kbk in range(max(0, 4 * qt - 4), 4 * qt + 4):
                mms = [(KW2[R, kbk * 128:(kbk + 1) * 128], Qm[R, qs], ["kw", "q0"]),
                       (c.ident_bf, win[:, kbk - 4 * qt + 4, :], ["ident_bf", "win"])]
                blocks.append((mms, VW[:, kbk, 0:65], ["VW"]))
            ob = 2 + oi % 2
            oi += 1
            att.run(blocks, ob, defer=pending)

            def fin(ob=ob, hA=hA, qs=qs):
                combine(ob, hA, 2, False)
                S.dma("sp", lambda e: e.dma_start(out=yT[128 + hA * 64:128 + (hA + 1) * 64, qs], in_=acc[hA][0:64, :]), reads=[f"acc{hA}"])
            pending.append(fin)
    for f in pending:
        f()
    S.barrier()
    kb.release(m0)


def part_gla(kb, c, D, yT, L):
    STAGE = 9
    S = kb.S
    P = kb.psb
    m0 = kb.mark()
    QS = float(32 ** -0.5)
    A = kb.alloc(N)
    B = kb.alloc(N)
    C = kb.alloc(2048)
    QG = kb.alloc(N, BF16)
    KG = kb.alloc(N, BF16)
    KHT = kb.alloc(128 * 64, BF16).rearrange("p (b d) -> p b d", b=128)
    V = kb.alloc(128 * 128, BF16).rearrange("p (b d) -> p b d", b=128)
    SB = kb.alloc(128 * 64, BF16).rearrange("p (c v) -> p c v", c=128)
    DC = kb.alloc(128)
    wg2 = kb.alloc(64)
    glr = kb.alloc(512)
    v2 = kb.alloc(4)
    tri64 = kb.alloc(64)
    st = kb.alloc(1024)
    S.dma("sp", lambda e: e.dma_start(out=wg2[0:16, :], in_=D["gla_wg2"][:, :]), writes=["wg2"])
    S.dma("sp", lambda e: e.dma_start(out=v2[:, 0:2], in_=D["gla_v2"][:, :]), writes=["v2"])
    S.dma("sp", lambda e: e.dma_start(out=tri64[0:64, :], in_=D["tri64"][:, :]), writes=["tri64"])
    S.dma("sp", lambda e: e.dma_start(out=tri64[64:128, :], in_=D["tri64"][:, :]), writes=["tri64"])
    S.op("dve", lambda e: e.tensor_scalar(out=v2[:, 2:3], in0=v2[:, 0:1], scalar1=-1.0, scalar2=None, op0=ALU.mult), reads=["v2"], writes=["v2n"])
    R = slice(0, 64)
    st3 = st.rearrange("p (b d) -> p b d", d=128)
    for b0 in range(0, 128, 8):
        load_cast(kb, (lambda b0=b0: L.pv(128, 128, 128, b0 * 64, (b0 + 8) * 64).rearrange("(b p) d -> p b d", p=64)),
                  V[R, b0:b0 + 8, :], st3[R, :, :], "st", "V", "act" if (b0 // 8) % 2 == 0 else "pool")
    for t in range(NQT):
        ts = slice(t * TQ, (t + 1) * TQ)
        bank = t % 2
        S.dma("sp", lambda e, ts=ts: e.dma_start(out=glr[0:16, :], in_=L.pt(OFF["c_glr"], 0, 16, ts.start, ts.stop)), writes=["glr"])
        S.op("pe", lambda e, bank=bank: e.matmul(P[bank][R, :], lhsT=wg2[0:16, 0:64], rhs=glr[0:16, :], start=True, stop=True), reads=["glr", "wg2"], writes=[pk(bank)])
        S.op("act", lambda e, bank=bank, ts=ts: e.activation(out=A[R, ts], in_=P[bank][R, :], func=AF.Exp, bias=v2[R, 2:3], scale=-1.0),
             reads=["v2n"], writes=[pk(bank), "A"])
    S.op("act", lambda e: e.activation(out=A[R, :], in_=A[R, :], func=AF.Ln, bias=1.0), writes=["A"])
    for ch in range(128):
        cs = slice(ch * 64, (ch + 1) * 64)
        S.op("dve", lambda e, cs=cs: e.tensor_tensor_scan(out=B[R, cs], data0=c.ones_f[R, 0:64], data1=A[R, cs], initial=0.0, op0=ALU.mult, op1=ALU.add),
             reads=["A", "ones_f"], writes=["B"])
    if STAGE <= 1:
        S.barrier(); kb.release(m0); return
    B3 = B.rearrange("p (c j) -> p c j", j=64)
    A3 = A.rearrange("p (c j) -> p c j", j=64)
    S.op("act", lambda e: e.activation(out=DC[R, :], in_=B3[R, :, 63], func=AF.Exp, scale=-1.0 / 16.0), reads=["B"], writes=["DC"])
    S.op("act", lambda e: e.activation(out=A[R, :], in_=B[R, :], func=AF.Exp, scale=-1.0 / 16.0), reads=["B"], writes=["A"])
    for pc in range(4):
        ps_ = slice(pc * 2048, (pc + 1) * 2048)
        S.dma("sp", lambda e, ps_=ps_: e.dma_start(out=C[R, :], in_=L.pt(OFF["c_q"], 64, 64, ps_.start, ps_.stop)), writes=["C"])
        S.op("dve", lambda e, ps_=ps_: e.scalar_tensor_tensor(out=QG[R, ps_], in0=C[R, :], scalar=QS, in1=A[R, ps_], op0=ALU.mult, op1=ALU.mult),
             reads=["C", "A"], writes=["QG"])
    S.op("act", lambda e: e.activation(out=A[R, :], in_=B[R, :], func=AF.Exp, scale=1.0 / 16.0), reads=["B", "QG"], writes=["A"])
    for pc in range(4):
        ps_ = slice(pc * 2048, (pc + 1) * 2048)
        S.dma("sp", lambda e, ps_=ps_: e.dma_start(out=C[R, :], in_=L.pt(OFF["c_k"], 64, 64, ps_.start, ps_.stop)), writes=["C"])
        S.op("dve", lambda e, ps_=ps_: e.tensor_tensor(out=KG[R, ps_], in0=C[R, :], in1=A[R, ps_], op=ALU.mult), reads=["C", "A"], writes=["KG"])
    S.op("dve", lambda e: e.tensor_tensor(out=A3[R, :, :], in0=B3[R, :, :], in1=B3[R, :, 63:64].to_broadcast([64, 128, 64]), op=ALU.subtract),
         reads=["B", "KG"], writes=["A"])
    S.op("act", lambda e: e.activation(out=A[R, :], in_=A[R, :], func=AF.Exp, scale=1.0 / 16.0), writes=["A"])
    for pc in range(4):
        ps_ = slice(pc * 2048, (pc + 1) * 2048)
        S.dma("sp", lambda e, ps_=ps_: e.dma_start(out=C[R, :], in_=L.pt(OFF["c_k"], 64, 64, ps_.start, ps_.stop)), writes=["C"])
        S.op("dve", lambda e, ps_=ps_: e.tensor_tensor(out=A[R, ps_], in0=C[R, :], in1=A[R, ps_], op=ALU.mult), reads=["C"], writes=["A"])
    if STAGE <= 2:
        S.barrier(); kb.release(m0); return
    for blk in range(64):
        bank = blk % 2
        S.op("pe", lambda e, blk=blk, bank=bank: e.transpose(P[bank][:, 0:64], A[R, blk * 128:(blk + 1) * 128], c.ident[0:64, 0:64]),
             reads=["A", "ident"], writes=[pk(bank)])
        S.op("act", lambda e, blk=blk, bank=bank: e.copy(out=KHT[R, 2 * blk, :], in_=P[bank][0:64, 0:64]), writes=[pk(bank), "KHT"])
        S.op("dve", lambda e, blk=blk, bank=bank: e.tensor_copy(out=KHT[R, 2 * blk + 1, :], in_=P[bank][64:128, 0:64]), writes=[pk(bank), "KHT"])
    if STAGE <= 3:
        S.barrier(); kb.release(m0); return
    U3 = B.rearrange("p (v c) -> p v c", c=128)
    uev = [kb.alloc(512), kb.alloc(512)]
    S3 = A.rearrange("p (v c) -> p v c", c=128)
    for g in range(32):
        bank = 2 + g % 2
        for j in range(4):
            ch = 4 * g + j
            S.op("pe", lambda e, ch=ch, j=j, bank=bank: e.matmul(P[bank][R, j * 128:(j + 1) * 128], lhsT=KHT[R, ch, :], rhs=V[R, ch, :], start=True, stop=True),
                 reads=["KHT", "V"], writes=[pk(bank)])
        ue = uev[g % 2]
        S.op("act", lambda e, bank=bank, ue=ue: e.copy(out=ue[R, :], in_=P[bank][R, :]), writes=[pk(bank), f"uev{g % 2}"])
        for h in range(2):
            hr = slice(32 * h, 32 * h + 32)
            for j in range(4):
                eng = "dve" if j % 2 == 0 else "pool"
                S.op(eng, lambda e, hr=hr, ue=ue, g=g, j=j, h=h: e.tensor_copy(out=U3[hr, :, 4 * g + j], in_=ue[hr, j * 128 + h * 64:j * 128 + (h + 1) * 64]),
                     reads=[f"uev{g % 2}"], writes=["U3", "B"])
    if STAGE <= 4:
        S.barrier(); kb.release(m0); return
    for v in range(64):
        S.op("dve", lambda e, v=v: e.tensor_tensor_scan(out=S3[R, v, :], data0=DC[R, :], data1=U3[R, v, :], initial=0.0, op0=ALU.mult, op1=ALU.add),
             reads=["U3", "DC", "KHT"], writes=["S3", "A"])
    S.op("pool", lambda e: e.memset(SB[R, 0, :], 0.0), writes=["SB0"])
    S.op("act", lambda e: e.copy(out=SB[R, 1:128, :], in_=S3[R, :, 0:127].rearrange("p v c -> p c v")), reads=["S3"], writes=["SB"])
    if STAGE <= 5:
        S.barrier(); kb.release(m0); return
    osb = kb.alloc(512)
    sqb = kb.alloc(512, BF16)
    rstd = kb.alloc(512)
    og = kb.alloc(512)
    at = [kb.alloc(64, BF16), kb.alloc(64, BF16)]
    ai = 0
    for t in range(NQT):
        ts = slice(t * TQ, (t + 1) * TQ)
        for h in range(2):
            hr = slice(32 * h, 32 * h + 32)
            ob = 4 + (2 * t + h) % 2
            for j in range(8):
                ch = 8 * t + j
                cs = slice(ch * 64, (ch + 1) * 64)
                rr = R
                sbk = ai % 2
                a_t = at[ai % 2]
                ak = f"at{ai % 2}"
                ai += 1
                S.op("pe", lambda e, hr=hr, cs=cs, rr=rr, sbk=sbk: e.matmul(P[sbk][rr, 0:64], lhsT=KG[hr, cs], rhs=QG[hr, cs], start=True, stop=True),
                     reads=["KG", "QG"], writes=[pk(sbk)])
                S.op("dve", lambda e, rr=rr, sbk=sbk, a_t=a_t: e.tensor_tensor(out=a_t[rr, :], in0=P[sbk][rr, 0:64], in1=tri64[rr, :], op=ALU.mult),
                     reads=["tri64"], writes=[pk(sbk), ak])
                S.op("pe", lambda e, rr=rr, ch=ch, h=h, a_t=a_t, ob=ob, j=j: e.matmul(P[ob][R, j * 64:(j + 1) * 64], lhsT=V[rr, ch, h * 64:(h + 1) * 64], rhs=a_t[rr, :], start=True, stop=False),
                     reads=[ak, "V"], writes=[pk(ob)])
                S.op("pe", lambda e, hr=hr, ch=ch, cs=cs, ob=ob, j=j: e.matmul(P[ob][R, j * 64:(j + 1) * 64], lhsT=SB[hr, ch, :], rhs=QG[hr, cs], start=False, stop=True),
                     reads=["SB", "SB0", "QG"], writes=[pk(ob)])
            S.op("act", lambda e, ob=ob: e.copy(out=osb[R, :], in_=P[ob][R, :]), writes=[pk(ob), "osb"])
            S.op("act", lambda e: e.activation(out=sqb[R, :], in_=osb[R, :], func=AF.Square), reads=["osb"], writes=["sqb"])
            S.op("pe", lambda e: e.matmul(P[6][R, :], lhsT=c.ones_bf[R, 0:64], rhs=sqb[R, :], start=True, stop=True), reads=["sqb", "ones_bf"], writes=[pk(6)])
            S.op("act", lambda e: e.activation(out=rstd[R, :], in_=P[6][R, :], func=AF.Sqrt, bias=EPS, scale=1.0 / 64.0), writes=[pk(6), "rstd"])
            S.op("dve", lambda e: e.reciprocal(out=rstd[R, :], in_=rstd[R, :]), writes=["rstd"])
            S.op("dve", lambda e: e.scalar_tensor_tensor(out=osb[R, :], in0=osb[R, :], scalar=v2[R, 1:2], in1=rstd[R, :], op0=ALU.mult, op1=ALU.mult),
                 reads=["rstd", "v2"], writes=["osb"])
            S.dma("sp", lambda e, h=h, ts=ts: e.dma_start(out=og[R, :], in_=L.pt(OFF["c_og"] + h * 64, 128, 64, ts.start, ts.stop)), writes=["og"])
            S.op("act", lambda e: e.activation(out=og[R, :], in_=og[R, :], func=AF.Silu), writes=["og"])
            S.op("dve", lambda e: e.tensor_tensor(out=osb[R, :], in0=osb[R, :], in1=og[R, :], op=ALU.mult), reads=["og"], writes=["osb"])
            S.dma("sp", lambda e, h=h, ts=ts: e.dma_start(out=yT[256 + h * 64:256 + (h + 1) * 64, ts], in_=osb[R, :]), reads=["osb"])
    S.barrier()
    kb.release(m0)


WB = ["lru_w", "lru_v", "mla_wuq", "mla_wk", "mla_wv", "mla_v", "nsa_pos", "nsa_w1", "nsa_b1", "nsa_w2", "gla_wg2", "gla_v2"]
WAC = (("gain", [128, 8]), ("w_in", [1024, NCOLW]), ("gng", [128, 8]), ("nfg", [128, 8]), ("w_out", [1024, 1024]),
       ("w_gu", [1024, 2 * DFF]), ("w_dn", [DFF, 1024]))
RG = [[0, 1], [2, 3], [4, 5], [6, 7]]


def build_fused():
    kb = KB(arena_cols=52500)
    S = kb.S
    D = {k: kb.din(k, shp) for k, shp in CONST_SHAPES.items()}
    D["pos"] = kb.din("pos", [1, N], I32)
    xT = kb.din("xT", [1024, 4096])
    out = kb.dout("out", [1024, 4096])
    LW = []
    for l in range(2):
        d = {k: kb.din(f"{k}_{l}", IN_SHAPES[k][0]) for k in WB}
        for k, shp in WAC:
            d[k] = kb.din(f"{k}_{l}", shp)
        LW.append(d)
    fing = kb.din("fing", [128, 8])
    XA = kb.dint("XA", [NCOLP, 4096])
    XV = kb.dint("XV", [4096, NV])
    GP = kb.dint("GP", [2 * NCOLP, 4096])
    GV = kb.dint("GV", [N, NV])
    YB = kb.dint("YB", [1024, 4096])
    YV = YView(YB)
    GY = kb.dint("GY", [2048, 4096])
    XO = kb.dint("XO", [1024, 4096])
    MYP = kb.dint("MYP", [SELROWS, N])
    MYV = kb.dint("MYV", [N, 128])
    MYY = kb.dint("MYY", [1024, 4096])
    c = common_setup(kb, D)
    L = Loader(S, GP, GV, MYP, MYV)
    xs = xT
    for l in range(2):
        W = LW[l]
        ag = lambda i_, o_: (lambda e: e.collective_compute("AllGather", ALU.bypass, replica_groups=RG, ins=[i_], outs=[o_]))
        phase_A(kb, xs, W["gain"], W["w_in"], XA, XV, GP, GV, ag)
        L.select()
        Dl = dict(D)
        Dl.update({k: W[k] for k in WB})
        for part, g in ((part_lru, 0), (part_gla, 2), (part_mla, 3), (part_nsa, 1)):
            part(kb, c, Dl, YV, L)
            for ch in range(2):
                S.collective_async(ag(YB[ch * 512 + g * 128:ch * 512 + (g + 1) * 128, :], GY[(g * 2 + ch) * 256:(g * 2 + ch + 1) * 256, :]))
        S.cc_wait_all()
        GY4 = GY.rearrange("(g ch q) t -> g ch q t", g=4, ch=2)
        S.dma("act", lambda e: e.dma_start(out=MYY.rearrange("(g q) t -> g q t", g=4), in_=GY4[:, bass.ds(S.rt["hp_act"], 1), :, :].rearrange("g o q t -> g (o q) t")),
              writes=["MYY"])
        S.barrier()
        phase_C(kb, l == 1, xs, MYY, W["gng"], W["nfg"], fing, W["w_out"], W["w_gu"], W["w_dn"], out if l == 1 else XO)
        xs = XO
    return kb.close()


def prep_W(W, l, hp):
    A = np.ascontiguousarray
    o = {}
    ch = slice(hp * 128, hp * 128 + 128)
    o["lru_w"] = A(np.stack([W["lru_wa"][l][2 * hp:2 * hp + 2], W["lru_wx"][l][2 * hp:2 * hp + 2]]))
    o["lru_v"] = A(np.stack([W["conv_w"][l][0, ch], W["conv_w"][l][1, ch], W["conv_w"][l][2, ch], W["conv_w"][l][3, ch],
                             W["conv_b"][l][ch], W["lru_ba"][l][ch], W["lru_bx"][l][ch], W["lru_lambda"][l][ch]], axis=1))
    o["mla_wuq"] = A(W["mla_w_uq"][l][:, hp * 192:(hp + 1) * 192])
    wkv = W["mla_w_ukv"][l].reshape(128, 4, 128)
    o["mla_wk"] = A(wkv[:, 2 * hp:2 * hp + 2, 0:64].reshape(128, 128))
    o["mla_wv"] = A(wkv[:, 2 * hp:2 * hp + 2, 64:128].reshape(128, 128))
    mv = np.zeros((128, 3), np.float32)
    mv[:, 0] = W["mla_q_norm"][l][0:128]
    mv[0:64, 1] = W["mla_q_norm"][l][128:192]
    mv[:, 2] = W["mla_kv_norm"][l]
    o["mla_v"] = mv
    o["nsa_pos"] = A(np.concatenate([W["cmp_pos"][l][0].T, W["cmp_pos"][l][1].T], axis=0))
    o["nsa_w1"] = A(W["cmp_w1"][l])
    o["nsa_b1"] = A(W["cmp_b1"][l].reshape(2, 2, 128).transpose(2, 0, 1).reshape(128, 4))
    o["nsa_w2"] = A(W["cmp_w2"][l])
    o["gla_wg2"] = A(W["gla_wg2"][l][:, hp * 64:(hp + 1) * 64])
    g2 = np.zeros((128, 2), np.float32)
    g2[0:64, 0] = W["gla_bg2"][l][hp * 64:(hp + 1) * 64]
    g2[:, 1] = np.tile(W["gla_norm"][l], 2)
    o["gla_v2"] = g2
    return o


_PROG = {}


def _arr8(g):
    return np.ascontiguousarray(np.asarray(g, np.float32).reshape(8, 128).T)


def kernel(**inp):
    W = {k: np.asarray(v) for k, v in inp.items()}
    x = W["x"]
    Bn, Sn, Dm = x.shape
    HT = Sn // 2
    cores = [(b, r) for b in range(Bn) for r in range(2)]
    if "F" not in _PROG:
        _PROG["F"] = build_fused()
    nc = _PROG["F"]
    consts = host_consts()
    pc = perm_cols()
    ins = []
    for (b, r) in cores:
        d = dict(consts)
        d["pos"] = np.ascontiguousarray(W["positions"][b][None, :].astype(np.int32))
        d["xT"] = np.ascontiguousarray(x[b, r * HT:(r + 1) * HT].T)
        d["fing"] = _arr8(W["final_norm"])
        for l in range(2):
            for k, v in prep_W(W, l, r).items():
                d[f"{k}_{l}"] = v
            d[f"gain_{l}"] = _arr8(W["norm_mix"][l])
            d[f"w_in_{l}"] = np.ascontiguousarray(W["w_in"][l][:, pc])
            d[f"gng_{l}"] = _arr8(W["group_norm"][l])
            d[f"nfg_{l}"] = _arr8(W["norm_ffn"][l])
            d[f"w_out_{l}"] = np.ascontiguousarray(W["w_out"][l])
            d[f"w_gu_{l}"] = np.ascontiguousarray(W["w_gate_up"][l])
            d[f"w_dn_{l}"] = np.ascontiguousarray(W["w_down"][l])
        ins.append(d)
    res = run_bass_kernel_spmd(nc, ins, core_ids=list(range(8))).results
    out = np.empty((Bn, Sn, Dm), np.float32)
    for ci, (b, r) in enumerate(cores):
        out[b, r * HT:(r + 1) * HT] = res[ci]["out"].T
    return out
```

```python
import numpy as np
from contextlib import ExitStack
import concourse.bass as bass
import concourse.mybir as mybir
from concourse.bass_utils import run_bass_kernel_spmd

F32 = mybir.dt.float32
BF16 = mybir.dt.bfloat16
I32 = mybir.dt.int32
AF = mybir.ActivationFunctionType
ALU = mybir.AluOpType
AX = mybir.AxisListType

ENGS = ("pe", "act", "dve", "pool", "sp")
NDMA_SEMS = 8
EPS = 1e-6


class Sched:
    def __init__(self, nc, es):
        self.nc = nc
        self.sem = {e: es.enter_context(nc.semaphore("s_" + e)) for e in ENGS}
        self.cnt = {e: 0 for e in ENGS}
        self.dsem = {e: [es.enter_context(nc.semaphore(f"d_{e}{i}")) for i in range(NDMA_SEMS)]
                     for e in ("sp", "pool", "act")}
        self.dval = {e: [0] * NDMA_SEMS for e in self.dsem}
        self.drr = {e: 0 for e in self.dsem}
        self.ops = {e: [] for e in ENGS}
        self.known = {e: {} for e in ENGS}
        self.semobj = {}
        self.last_w = {}
        self.readers = {}
        self.ccsem = es.enter_context(nc.semaphore("s_cc"))
        self.semobj["cc"] = self.ccsem
        self.ccn = 0
        self.rt = {}
        for e in ENGS:
            self.semobj["c_" + e] = self.sem[e]
        for e in self.dsem:
            for i in range(NDMA_SEMS):
                self.semobj[f"d_{e}{i}"] = self.dsem[e][i]

    def _need(self, eng, tok, waits):
        if tok is None:
            return
        sk, val, teng = tok
        if teng == "pe" and eng == "pe" and sk == "c_pe":
            return
        if self.known[eng].get(sk, 0) >= val:
            return
        self.known[eng][sk] = val
        waits[sk] = max(waits.get(sk, 0), val)

    def _deps(self, eng, reads, writes):
        waits = {}
        for k in reads:
            self._need(eng, self.last_w.get(k), waits)
        for k in writes:
            self._need(eng, self.last_w.get(k), waits)
            for t in self.readers.get(k, ()):
                self._need(eng, t, waits)
        return waits

    def _commit(self, tok, reads, writes):
        for k in reads:
            self.readers.setdefault(k, []).append(tok)
        for k in writes:
            self.last_w[k] = tok
            self.readers[k] = []

    def op(self, eng, emit, reads=(), writes=()):
        waits = self._deps(eng, reads, writes)
        self.cnt[eng] += 1
        tok = ("c_" + eng, self.cnt[eng], eng)
        self.ops[eng].append((waits, emit, (self.sem[eng], 1)))
        self._commit(tok, reads, writes)
        return tok

    def dma(self, eng, emit, reads=(), writes=()):
        waits = self._deps(eng, reads, writes)
        i = self.drr[eng]
        self.drr[eng] = (i + 1) % NDMA_SEMS
        sk = f"d_{eng}{i}"
        prev = self.dval[eng][i]
        if prev and self.known[eng].get(sk, 0) < prev:
            self.known[eng][sk] = prev
            waits[sk] = prev
        self.dval[eng][i] = prev + 16
        tok = (sk, prev + 16, eng)
        self.ops[eng].append((waits, emit, (self.dsem[eng][i], 16)))
        self._commit(tok, reads, writes)
        return tok

    def barrier(self):
        for eng in ENGS:
            waits = {}
            for e in ENGS:
                if e != eng and self.cnt[e]:
                    self._need(eng, ("c_" + e, self.cnt[e], e), waits)
            for e in self.dsem:
                for i in range(NDMA_SEMS):
                    if self.dval[e][i]:
                        self._need(eng, (f"d_{e}{i}", self.dval[e][i], "dma"), waits)
            if eng != "pe" and self.cnt[eng]:
                self._need(eng, ("c_" + eng, self.cnt[eng], eng), waits)
            if waits:
                self.ops[eng].append((waits, None, None))
        self.last_w = {}
        self.readers = {}

    def collective(self, emits):
        self.barrier()
        for emit in emits:
            w = {"cc": self.ccn} if self.ccn else {}
            self.ccn += 1
            self.ops["pool"].append((w, emit, (self.ccsem, 1)))
        for eng in ENGS:
            self.known[eng]["cc"] = self.ccn
            self.ops[eng].append(({"cc": self.ccn}, None, None))

    def collective_async(self, emit, reads=()):
        waits = {}
        for k in reads:
            self._need("pool", self.last_w.get(k), waits)
        if self.ccn:
            waits["cc"] = self.ccn
        self.ccn += 1
        self.ops["pool"].append((waits, emit, (self.ccsem, 1)))

    def cc_wait_all(self):
        self.barrier()
        for eng in ENGS:
            if self.known[eng].get("cc", 0) < self.ccn:
                self.known[eng]["cc"] = self.ccn
                self.ops[eng].append(({"cc": self.ccn}, None, None))

    def finish(self):
        self.barrier()
        semobj = self.semobj

        def run(engobj, lst):
            for waits, emit, inc in lst:
                for sk, v in waits.items():
                    engobj.wait_ge(semobj[sk], v)
                if emit is not None:
                    emit(engobj).then_inc(inc[0], inc[1])

        with self.nc.Block() as block:
            @block.tensor
            def _(e):
                run(e, self.ops["pe"])

            @block.scalar
            def _(e):
                self.rt["hp_act"] = e.partition_id() % 2
                run(e, self.ops["act"])

            @block.vector
            def _(e):
                run(e, self.ops["dve"])

            @block.gpsimd
            def _(e):
                run(e, self.ops["pool"])

            @block.sync
            def _(e):
                self.rt["hp_sp"] = e.partition_id() % 2
                run(e, self.ops["sp"])


class KB:
    def __init__(self, arena_cols=50000):
        self.nc = bass.Bass("TRN2", target_bir_lowering=False)
        self.es = ExitStack()
        self.S = Sched(self.nc, self.es)
        self.arena = self.es.enter_context(self.nc.sbuf_tensor("arena", [128, arena_cols], F32))
        self.acols = arena_cols
        self.top = 0
        self.psb = [self.es.enter_context(self.nc.psum_tensor(f"psb{i}", [128, 512], F32)) for i in range(8)]
        self.uid = 0

    def dint(self, name, shape, dt=F32):
        return self.nc.dram_tensor(name, list(shape), dt).ap()

    def din(self, name, shape, dt=F32):
        return self.nc.dram_tensor(name, list(shape), dt, kind="ExternalInput").ap()

    def dout(self, name, shape, dt=F32):
        return self.nc.dram_tensor(name, list(shape), dt, kind="ExternalOutput").ap()

    def alloc(self, cols, dt=F32):
        n32 = cols if dt != BF16 else (cols + 1) // 2
        a = self.top
        self.top += n32
        assert self.top <= self.acols, f"arena overflow {self.top}"
        v = self.arena[:, a:a + n32]
        if dt == BF16:
            v = v.bitcast(BF16)
        elif dt == I32:
            v = v.bitcast(I32)
        return v

    def mark(self):
        return self.top

    def release(self, m):
        self.top = m

    def key(self, base="k"):
        self.uid += 1
        return f"{base}{self.uid}"

    def close(self):
        self.S.finish()
        self.es.close()
        return self.nc


NCOL = 2300
NCOLP = 1920
NCOLW = NCOLP + 384
VCOLS = [(NCOLP, NCOLW)]
NV = 384
OFF = dict(a_x=0, a_gate=256, b_q=512, b_kv=768, c_q=1024, c_k=1152, c_og=1280, d_cq=1536, d_kr=1728, c_glr=1760,
           b_gate=1776, d_ckv=1792)


def perm_cols():
    o = dict(a_x=0, a_gate=256, b_q=512, b_kv=768, b_gate=1152, c_q=1164, c_k=1292, c_v=1420, c_glr=1676, c_og=1692,
             d_cq=1948, d_ckv=2140, d_kr=2268)
    r = lambda a, n: list(range(a, a + n))
    p = (r(o["a_x"], 256) + r(o["a_gate"], 256) + r(o["b_q"], 256)
         + r(o["b_kv"], 64) + r(o["b_kv"] + 64, 64) + r(o["b_kv"] + 128, 64) + r(o["b_kv"] + 256, 64)
         + r(o["c_q"], 128) + r(o["c_k"], 128) + r(o["c_og"], 256) + r(o["d_cq"], 192) + r(o["d_kr"], 32)
         + r(o["c_glr"], 16) + r(o["b_gate"], 12) + r(o["b_gate"], 4) + r(o["d_ckv"], 128))
    assert len(p) == NCOLP
    p += r(o["b_kv"] + 192, 64) + r(o["b_kv"] + 320, 64) + r(o["c_v"], 256)
    assert len(p) == NCOLW
    return np.array(p)
DFF = 2816


def load_cast_weight(kb, w_dram, wsb, kchunks, ncols, stage, key, piece=1024):
    S = kb.S
    i = 0
    for c in range(kchunks):
        for c0 in range(0, ncols, piece):
            c1 = min(ncols, c0 + piece)
            st = stage[i % 2]
            sk = f"wstage{i % 2}"
            S.dma("sp", lambda e, st=st, c=c, c0=c0, c1=c1: e.dma_start(out=st[:, 0:c1 - c0], in_=w_dram[c * 128:(c + 1) * 128, c0:c1]),
                  writes=[sk])
            eng = "act" if i % 2 == 0 else "pool"
            if eng == "act":
                S.op("act", lambda e, st=st, c=c, c0=c0, c1=c1: e.copy(out=wsb[:, c, c0:c1], in_=st[:, 0:c1 - c0]), reads=[sk], writes=[key])
            else:
                S.op("pool", lambda e, st=st, c=c, c0=c0, c1=c1: e.tensor_copy(out=wsb[:, c, c0:c1], in_=st[:, 0:c1 - c0]), reads=[sk], writes=[key])
            i += 1


def rms_stats(kb, src3, nch, T, sq, ones_bf, ps_ap, rstd, denom, keys_in, key_sq, key_ps, key_rstd):
    S = kb.S
    S.op("act", lambda e: e.activation(out=sq, in_=src3, func=AF.Square), reads=keys_in, writes=[key_sq])
    for c in range(nch):
        S.op("pe", lambda e, c=c: e.matmul(ps_ap, lhsT=ones_bf, rhs=sq[:, c, :], start=(c == 0), stop=(c == nch - 1)),
             reads=[key_sq, "ones_bf"], writes=[key_ps])
    S.op("act", lambda e: e.activation(out=rstd, in_=ps_ap, func=AF.Sqrt, bias=EPS, scale=1.0 / denom), writes=[key_ps, key_rstd])
    S.op("dve", lambda e: e.reciprocal(out=rstd, in_=rstd), reads=[key_rstd], writes=[key_rstd])


def phase_A(kb, xT, gain, w, pT, pV, GP, GV, ag):
    S = kb.S
    m0 = kb.mark()
    T = 512
    NT = 8
    xv = xT.rearrange("(c p) t -> p c t", p=128)

    wsb = kb.alloc(8 * NCOLW, BF16).rearrange("p (c n) -> p c n", c=8)
    gsb = kb.alloc(8)
    ones_bf = kb.alloc(128, BF16)
    stage = [kb.alloc(1024), kb.alloc(1024)]
    xt = [kb.alloc(8 * T).rearrange("p (c t) -> p c t", c=8) for _ in range(2)]
    sq = kb.alloc(8 * T, BF16).rearrange("p (c t) -> p c t", c=8)
    hb = kb.alloc(8 * 4096, BF16).rearrange("p (c t) -> p c t", c=8)
    rstd = kb.alloc(T)
    ost = [kb.alloc(T) for _ in range(4)]
    P = [p[:] for p in kb.psb]

    S.dma("sp", lambda e: e.dma_start(out=gsb, in_=gain[:, :]), writes=["gsb"])
    S.op("pool", lambda e: e.memset(ones_bf, 1.0), writes=["ones_bf"])
    S.dma("sp", lambda e: e.dma_start(out=xt[0], in_=xv[:, :, 0:T]), writes=["xt0"])
    load_cast_weight(kb, w, wsb, 8, NCOLW, stage, "wsb")
    for t in range(NT):
        b = t % 2
        if t + 1 < NT:
            S.dma("sp", lambda e, t=t: e.dma_start(out=xt[(t + 1) % 2], in_=xv[:, :, (t + 1) * T:(t + 2) * T]),
                  writes=[f"xt{(t + 1) % 2}"])
        rms_stats(kb, xt[b], 8, T, sq, ones_bf, P[0], rstd, 1024.0, [f"xt{b}"], "sq", "psb0", "rstd")
        for c in range(8):
            S.op("dve", lambda e, c=c, b=b, t=t: e.scalar_tensor_tensor(out=hb[:, c, t * T:(t + 1) * T], in0=xt[b][:, c, :], scalar=gsb[:, c:c + 1], in1=rstd,
                                                                   op0=ALU.mult, op1=ALU.mult),
                 reads=[f"xt{b}", "rstd", "gsb"], writes=[f"hb{t}"])
    oi = 0
    for t in range(NT):
        for tb in range(4):
            pb = 5 + (tb % 2)
            for c in range(8):
                S.op("pe", lambda e, c=c, t=t, tb=tb, pb=pb: e.matmul(
                    P[pb][:, 0:NV], lhsT=hb[:, c, t * T + tb * 128:t * T + (tb + 1) * 128], rhs=wsb[:, c, NCOLP:NCOLW], start=(c == 0), stop=(c == 7)),
                    reads=[f"hb{t}", "wsb"], writes=[f"psb{pb}"])
            o = oi % 4
            oi += 1
            S.op("act", lambda e, o=o, pb=pb: e.copy(out=ost[o][:, 0:NV], in_=P[pb][:, 0:NV]), writes=[f"psb{pb}", f"ost{o}"])
            S.dma("sp", lambda e, o=o, t=t, tb=tb: e.dma_start(out=pV[t * T + tb * 128: t * T + (tb + 1) * 128, :], in_=ost[o][:, 0:NV]),
                  reads=[f"ost{o}"], writes=[f"XV{t}_{tb}"])
        if t % 2 == 1:
            j = t // 2
            S.collective_async(ag(pV[j * 1024:(j + 1) * 1024, :], GV[j * 2048:(j + 1) * 2048, :]),
                               reads=[f"XV{tt}_{tb}" for tt in (t - 1, t) for tb in range(4)])
    for k in range(NCOLP // 128):
        c0, c1 = k * 128, (k + 1) * 128
        for t in range(NT):
            pb = 1 + (oi % 4)
            for c in range(8):
                S.op("pe", lambda e, c=c, t=t, c0=c0, c1=c1, pb=pb: e.matmul(P[pb][:, :], lhsT=wsb[:, c, c0:c1], rhs=hb[:, c, t * T:(t + 1) * T],
                                                                             start=(c == 0), stop=(c == 7)),
                     reads=[f"hb{t}", "wsb"], writes=[f"psb{pb}"])
            o = oi % 4
            oi += 1
            if t % 2 == 0:
                S.op("act", lambda e, o=o, pb=pb: e.copy(out=ost[o], in_=P[pb][:, :]), writes=[f"psb{pb}", f"ost{o}"])
            else:
                S.op("dve", lambda e, o=o, pb=pb: e.tensor_copy(out=ost[o], in_=P[pb][:, :]), writes=[f"psb{pb}", f"ost{o}"])
            S.dma("sp", lambda e, o=o, c0=c0, c1=c1, t=t: e.dma_start(out=pT[c0:c1, t * T:(t + 1) * T], in_=ost[o]),
                  reads=[f"ost{o}"], writes=[f"XA{k}_{t}"])
        S.collective_async(ag(pT[c0:c1, :], GP[k * 256:(k + 1) * 256, :]), reads=[f"XA{k}_{t}" for t in range(NT)])
    S.cc_wait_all()
    kb.release(m0)


def phase_C(kb, final, xT, GY, gng, nfg, fing, w_out, w_gu, w_dn, xo, NT=16):
    S = kb.S
    m0 = kb.mark()
    T = 256
    xv = xT.rearrange("(c p) t -> p c t", p=128)
    ov = xo.rearrange("(c p) t -> p c t", p=128)

    wo = kb.alloc(8 * 1024, BF16).rearrange("p (c n) -> p c n", c=8)
    wgu = kb.alloc(8 * 2 * DFF, BF16).rearrange("p (c n) -> p c n", c=8)
    wdn = kb.alloc(22 * 1024, BF16).rearrange("p (c n) -> p c n", c=22)
    g3 = kb.alloc(24)
    ones_bf = kb.alloc(128, BF16)
    xt = kb.alloc(8 * T).rearrange("p (c t) -> p c t", c=8)
    yt = kb.alloc(8 * T).rearrange("p (c t) -> p c t", c=8)
    ytf = yt.rearrange("p c t -> p (c t)")
    stage = [ytf[:, 0:1024], ytf[:, 1024:2048]]
    sq = kb.alloc(8 * T, BF16).rearrange("p (c t) -> p c t", c=8)
    hb = kb.alloc(8 * T, BF16).rearrange("p (c t) -> p c t", c=8)
    aT = kb.alloc(22 * T, BF16).rearrange("p (c t) -> p c t", c=22)
    rstd4 = kb.alloc(4 * T).rearrange("p (c t) -> p c t", c=4)
    rstd = kb.alloc(T)
    sg = [kb.alloc(T), kb.alloc(T)]
    P = [p[:] for p in kb.psb]

    S.dma("sp", lambda e: e.dma_start(out=g3[:, 0:8], in_=gng[:, :]), writes=["g3"])
    S.dma("sp", lambda e: e.dma_start(out=g3[:, 8:16], in_=nfg[:, :]), writes=["g3"])
    S.dma("sp", lambda e: e.dma_start(out=g3[:, 16:24], in_=fing[:, :]), writes=["g3"])
    S.op("pool", lambda e: e.memset(ones_bf, 1.0), writes=["ones_bf"])
    load_cast_weight(kb, w_out, wo, 8, 1024, stage, "wo")
    load_cast_weight(kb, w_gu, wgu, 8, 2 * DFF, stage, "wgu")
    load_cast_weight(kb, w_dn, wdn, 22, 1024, stage, "wdn")
    S.barrier()

    for t in range(NT):
        ts = slice(t * T, (t + 1) * T)
        S.dma("sp", lambda e, ts=ts: e.dma_start(out=xt, in_=xv[:, :, ts]), writes=["xt"])
        for cc_ in range(8):
            r0 = cc_ * 128
            S.dma("sp", lambda e, cc_=cc_, r0=r0, t=t: e.dma_start(out=yt[:, cc_, :], in_=GY[r0:r0 + 128, t * T:(t + 1) * T]),
                  writes=["yt"])
        S.op("act", lambda e: e.activation(out=sq, in_=yt, func=AF.Square), reads=["yt"], writes=["sq"])
        for g in range(4):
            pa = P[g][:, 0:T]
            for j in range(2):
                S.op("pe", lambda e, g=g, j=j, pa=pa: e.matmul(pa, lhsT=ones_bf, rhs=sq[:, 2 * g + j, :], start=(j == 0), stop=(j == 1)),
                     reads=["sq", "ones_bf"], writes=[f"psb{g}"])
            S.op("act", lambda e, g=g, pa=pa: e.activation(out=rstd4[:, g, :], in_=pa, func=AF.Sqrt, bias=EPS, scale=1.0 / 256.0),
                 writes=[f"psb{g}", f"rstd4_{g}"])
            S.op("dve", lambda e, g=g: e.reciprocal(out=rstd4[:, g, :], in_=rstd4[:, g, :]), reads=[f"rstd4_{g}"], writes=[f"rstd4_{g}"])
        for c in range(8):
            eng = "dve"
            S.op(eng, lambda e, c=c: e.scalar_tensor_tensor(out=hb[:, c, :], in0=yt[:, c, :], scalar=g3[:, c:c + 1], in1=rstd4[:, c // 2, :],
                                                         op0=ALU.mult, op1=ALU.mult),
                 reads=["yt", f"rstd4_{c // 2}", "g3"], writes=[f"hb{c}"])
        for m in range(8):
            pa = P[4 + m % 4][:, 0:T]
            for c in range(8):
                S.op("pe", lambda e, m=m, c=c, pa=pa: e.matmul(pa, lhsT=wo[:, c, m * 128:(m + 1) * 128], rhs=hb[:, c, :], start=(c == 0), stop=(c == 7)),
                     reads=[f"hb{c}", "wo"], writes=[f"psb{4 + m % 4}"])
            S.op("dve", lambda e, m=m, pa=pa: e.tensor_tensor(out=xt[:, m, :], in0=xt[:, m, :], in1=pa, op=ALU.add),
                 reads=["xt"], writes=["xt", f"psb{4 + m % 4}"])
        rms_stats(kb, xt, 8, T, sq, ones_bf, P[0][:, 0:T], rstd, 1024.0, ["xt"], "sq", "psb0", "rstd")
        for c in range(8):
            eng = "dve"
            S.op(eng, lambda e, c=c: e.scalar_tensor_tensor(out=hb[:, c, :], in0=xt[:, c, :], scalar=g3[:, 8 + c:9 + c], in1=rstd,
                                                         op0=ALU.mult, op1=ALU.mult),
                 reads=["xt", "rstd", "g3"], writes=[f"hb{c}"])
        for j in range(22):
            pg = P[j % 2][:, 0:T]
            pu = P[2 + j % 2][:, 0:T]
            for c in range(8):
                S.op("pe", lambda e, j=j, c=c, pg=pg: e.matmul(pg, lhsT=wgu[:, c, j * 128:(j + 1) * 128], rhs=hb[:, c, :], start=(c == 0), stop=(c == 7)),
                     reads=[f"hb{c}", "wgu"], writes=[f"psb{j % 2}"])
            for c in range(8):
                S.op("pe", lambda e, j=j, c=c, pu=pu: e.matmul(pu, lhsT=wgu[:, c, DFF + j * 128:DFF + (j + 1) * 128], rhs=hb[:, c, :], start=(c == 0), stop=(c == 7)),
                     reads=[f"hb{c}", "wgu"], writes=[f"psb{2 + j % 2}"])
            S.op("act", lambda e, j=j, pg=pg: e.activation(out=sg[j % 2], in_=pg, func=AF.Silu), writes=[f"psb{j % 2}", f"sg{j % 2}"])
            S.op("dve", lambda e, j=j, pu=pu: e.tensor_tensor(out=aT[:, j, :], in0=sg[j % 2], in1=pu, op=ALU.mult),
                 reads=[f"sg{j % 2}"], writes=[f"aT{j}", f"psb{2 + j % 2}"])
        for m in range(8):
            pa = P[4 + m % 4][:, 0:T]
            for k in range(22):
                S.op("pe", lambda e, m=m, k=k, pa=pa: e.matmul(pa, lhsT=wdn[:, k, m * 128:(m + 1) * 128], rhs=aT[:, k, :], start=(k == 0), stop=(k == 21)),
                     reads=[f"aT{k}", "wdn"], writes=[f"psb{4 + m % 4}"])
            S.op("dve", lambda e, m=m, pa=pa: e.tensor_tensor(out=xt[:, m, :], in0=xt[:, m, :], in1=pa, op=ALU.add),
                 reads=["xt"], writes=["xt", f"psb{4 + m % 4}"])
        if final:
            rms_stats(kb, xt, 8, T, sq, ones_bf, P[0][:, 0:T], rstd, 1024.0, ["xt"], "sq", "psb0", "rstd")
            for c in range(8):
                eng = "dve"
                S.op(eng, lambda e, c=c: e.scalar_tensor_tensor(out=yt[:, c, :], in0=xt[:, c, :], scalar=g3[:, 16 + c:17 + c], in1=rstd,
                                                             op0=ALU.mult, op1=ALU.mult),
                     reads=["xt", "rstd", "g3"], writes=["yt"])
            S.dma("sp", lambda e, ts=ts: e.dma_start(out=ov[:, :, ts], in_=yt), reads=["yt"])
        else:
            S.dma("sp", lambda e, ts=ts: e.dma_start(out=ov[:, :, ts], in_=xt), reads=["xt"])
    S.barrier()
    kb.release(m0)


N = 8192
TQ = 512
NQT = N // TQ
NEG = -30000.0
PI = float(np.pi)
TWO_PI = float(2 * np.pi)
THETA = 10000.0


def pk(b):
    return f"psb{b}"


def host_consts():
    c = {}
    c["ident"] = np.eye(128, dtype=np.float32)
    R = np.zeros((128, 128), np.float32)
    for blk in range(2):
        for m in range(64):
            if m < 32:
                R[blk * 64 + m + 32, blk * 64 + m] = -1.0
            else:
                R[blk * 64 + m - 32, blk * 64 + m] = 1.0
    c["rbd"] = R
    R32 = np.zeros((128, 32), np.float32)
    for m in range(32):
        if m < 16:
            R32[64 + m + 16, m] = -1.0
        else:
            R32[64 + m - 16, m] = 1.0
    c["r32"] = R32
    invf = np.zeros((128, 2), np.float32)
    for p in range(128):
        invf[p, 0] = np.float32(THETA) ** np.float32(-(2.0 * ((p % 64) % 32)) / 64.0)
    for p in range(64, 96):
        invf[p, 1] = np.float32(THETA) ** np.float32(-(2.0 * ((p - 64) % 16)) / 32.0)
    c["invf"] = invf
    k = np.arange(128)[:, None]
    q = np.arange(512)[None, :]
    c["tri"] = np.stack([np.where(k + i * 128 <= q, 0.0, NEG) for i in range(4)]).astype(np.float32)
    c["win"] = np.stack([np.where((k + (i - 4) * 128 <= q) & (k + (i - 4) * 128 > q - 512), 0.0, NEG) for i in range(8)]).astype(np.float32)
    c["cmpm"] = np.stack([np.where(16 * k + 31 <= i * 512 + q, 0.0, NEG) for i in range(5)]).astype(np.float32)
    n = np.arange(512)
    s = np.arange(128)
    ov = ((n[:, None] * 16 < s[None, :] * 64 + 64) & (n[:, None] * 16 + 32 > s[None, :] * 64)).astype(np.float32)
    ov[511] = 0.0
    ovl = np.zeros((4, 128, 129), np.float32)
    ovl[:, :, :128] = ov.reshape(4, 128, 128)
    ovl[:, :, 128] = 1.0
    c["ovl"] = ovl
    c["eall"] = (np.arange(N)[None, :] // 64 == np.arange(128)[:, None]).astype(np.float32)
    ql = np.arange(128)[:, None] // 64
    j = np.arange(254)[None, :] - 126
    c["bv"] = (j <= ql - 2).astype(np.float32)
    c["bf"] = (np.where((j == ql) | (j == ql - 1), 1e9, 0.0) + np.where(j > ql, -1.0, 0.0)).astype(np.float32)
    selg = np.zeros((8, 6 * 64), np.float32)
    for r in range(6):
        selg[r, r * 64:(r + 1) * 64] = 1.0
    c["selg"] = selg
    c["tri64"] = (np.arange(64)[:, None] <= np.arange(64)[None, :]).astype(np.float32)
    return c


CONST_SHAPES = {"ident": [128, 128], "rbd": [128, 128], "r32": [128, 32], "invf": [128, 2], "tri": [4, 128, 512],
                "win": [8, 128, 512], "cmpm": [5, 128, 512], "ovl": [4, 128, 129], "eall": [128, N], "bv": [128, 254],
                "bf": [128, 254], "selg": [8, 384], "tri64": [64, 64]}

IN_SHAPES = {
    "pos": ([1, N], I32),
    "lru_x": ([2, 128, N], F32), "lru_w": ([2, 2, 64, 64], F32), "lru_v": ([128, 8], F32),
    "mla_cq": ([192, N], F32), "mla_ckv": ([128, N], F32), "mla_kr": ([32, N], F32),
    "mla_wuq": ([192, 192], F32), "mla_wk": ([128, 128], F32), "mla_wv": ([128, 128], F32), "mla_v": ([128, 3], F32),
    "nsa_q": ([2, 128, N], F32), "nsa_k": ([3, 64, N], F32), "nsa_vc": ([64, N], F32), "nsa_vs": ([N, 64], F32),
    "nsa_vw": ([N, 64], F32), "nsa_g": ([6, N], F32), "nsa_pos": ([128, 32], F32), "nsa_w1": ([2, 2048, 256], F32),
    "nsa_b1": ([128, 4], F32), "nsa_w2": ([2, 256, 64], F32),
    "gla_q": ([64, N], F32), "gla_k": ([64, N], F32), "gla_v": ([N, 128], F32), "gla_glr": ([16, N], F32),
    "gla_og": ([128, N], F32), "gla_wg2": ([16, 64], F32), "gla_v2": ([128, 2], F32),
}


class Ctx:
    pass


class YView:
    def __init__(self, ap):
        self.ap = ap

    def __getitem__(self, key):
        rs, cs = key
        half = cs.start // 4096
        assert (cs.stop - 1) // 4096 == half
        return self.ap[half * 512 + rs.start:half * 512 + rs.stop, cs.start - half * 4096:cs.stop - half * 4096]


SEL = [("a_x", 128, False, 128), ("a_gate", 128, False, 128), ("b_q", 128, False, 128), ("b_q", 128, True, 128),
       ("b_gate", 6, False, 6), ("c_q", 64, False, 64), ("c_k", 64, False, 64), ("c_og", 128, False, 128)]
SELROWS = sum(x[3] for x in SEL)


class Loader:
    def __init__(self, S, GP, GV, MYP, MYV):
        self.S = S
        self.GP, self.GV, self.MYP, self.MYV = GP, GV, MYP, MYV
        self.row0 = {}
        r = 0
        for nm, hpm, inv, n in SEL:
            self.row0[(nm, inv)] = r
            r += n

    @staticmethod
    def gprow(r, half):
        return (r // 128) * 256 + half * 128 + r % 128

    @staticmethod
    def gvrow(t):
        half, tl = t // 4096, t % 4096
        return (tl // 1024) * 2048 + half * 1024 + tl % 1024

    def select(self):
        S = self.S
        i = 0
        for nm, hpm, inv, n in SEL:
            r0 = self.row0[(nm, inv)]
            mult = 256 if hpm == 128 else hpm
            for hf in range(2):
                q = "sp" if i % 2 == 0 else "act"
                i += 1

                def emit(e, nm=nm, mult=mult, inv=inv, n=n, r0=r0, hf=hf, q=q):
                    hp = S.rt["hp_" + q]
                    start = ((1 - hp) if inv else hp) * mult + self.gprow(OFF[nm], hf)
                    return e.dma_start(out=self.MYP[r0:r0 + n, hf * 4096:(hf + 1) * 4096], in_=self.GP[bass.ds(start, n), :])
                S.dma(q, emit, writes=["MYP"])
        for j in range(4):
            S.dma("sp", lambda e, j=j: e.dma_start(out=self.MYV[j * 2048:(j + 1) * 2048, :],
                                                   in_=self.GV[:, bass.ds(S.rt["hp_sp"] * 128 + 128, 128)][j * 2048:(j + 1) * 2048, :]), writes=["MYV"])
        S.barrier()

    def pt(self, off, hpm, n, c0, c1, inv=False):
        if hpm == 0:
            half = c0 // 4096
            assert off // 128 == (off + n - 1) // 128
            base = self.gprow(off, half)
            return self.GP[base:base + n, c0 - half * 4096:c1 - half * 4096]
        for nm, hm, iv, nn in SEL:
            if hm == hpm and iv == inv and OFF[nm] <= off and (off - OFF[nm]) + n <= nn:
                r0 = self.row0[(nm, inv)] + (off - OFF[nm])
                return self.MYP[r0:r0 + n, c0:c1]
        raise KeyError((off, hpm, n, inv))

    def pv(self, coff, hpm, n, t0, t1):
        assert t0 // 1024 == (t1 - 1) // 1024
        g0 = self.gvrow(t0)
        if hpm == 0:
            return self.GV[g0:g0 + (t1 - t0), coff:coff + n]
        assert coff == 128 and n == 128
        return self.MYV[g0:g0 + (t1 - t0), :]


def common_setup(kb, D):
    S = kb.S
    c = Ctx()
    c.ident = kb.alloc(128)
    c.ones_f = kb.alloc(128)
    c.ones_bf = kb.alloc(128, BF16)
    c.ident_bf = kb.alloc(128, BF16)
    S.dma("sp", lambda e: e.dma_start(out=c.ident, in_=D["ident"][:, :]), writes=["ident"])
    S.op("pool", lambda e: e.memset(c.ones_f, 1.0), writes=["ones_f"])
    S.op("pool", lambda e: e.memset(c.ones_bf, 1.0), writes=["ones_bf"])
    S.op("act", lambda e: e.copy(out=c.ident_bf, in_=c.ident), reads=["ident"], writes=["ident_bf"])
    c.invf = kb.alloc(2)
    S.dma("sp", lambda e: e.dma_start(out=c.invf, in_=D["invf"][:, :]), writes=["invf"])
    return c


def rope_tables(kb, c, posf, posk, r0, r1, col, T, bank, tag):
    S = kb.S
    P = kb.psb[bank]
    n = r1 - r0
    rs = slice(r0, r1)
    a, kf, ki, sn, cs = T["ang"], T["kf"], T["ki"], T["sin"], T["cos"]
    S.op("pe", lambda e: e.matmul(P[rs, :], lhsT=c.ones_f[0:1, 0:n], rhs=posf[0:1, :], start=True, stop=True),
         reads=["ones_f", posk], writes=[pk(bank)])
    S.op("dve", lambda e: e.tensor_scalar(out=a[rs, :], in0=P[rs, :], scalar1=c.invf[rs, col:col + 1], scalar2=None, op0=ALU.mult),
         reads=["invf"], writes=[pk(bank), tag + "ang"])
    S.op("dve", lambda e: e.tensor_scalar(out=ki[rs, :], in0=a[rs, :], scalar1=1.0 / TWO_PI, scalar2=None, op0=ALU.mult),
         reads=[tag + "ang"], writes=[tag + "ki"])
    S.op("dve", lambda e: e.tensor_copy(out=kf[rs, :], in_=ki[rs, :]), reads=[tag + "ki"], writes=[tag + "kf"])
    S.op("dve", lambda e: e.scalar_tensor_tensor(out=a[rs, :], in0=kf[rs, :], scalar=-TWO_PI, in1=a[rs, :], op0=ALU.mult, op1=ALU.add),
         reads=[tag + "kf"], writes=[tag + "ang"])
    S.op("dve", lambda e: e.tensor_scalar(out=kf[rs, :], in0=a[rs, :], scalar1=PI, scalar2=-TWO_PI, op0=ALU.is_gt, op1=ALU.mult),
         reads=[tag + "ang"], writes=[tag + "kf"])
    S.op("dve", lambda e: e.tensor_tensor(out=sn[rs, :], in0=a[rs, :], in1=kf[rs, :], op=ALU.add),
         reads=[tag + "ang", tag + "kf"], writes=[tag + "sin"])
    S.op("dve", lambda e: e.tensor_scalar(out=a[rs, :], in0=a[rs, :], scalar1=PI / 2, scalar2=None, op0=ALU.add),
         reads=[], writes=[tag + "ang"])
    S.op("dve", lambda e: e.tensor_scalar(out=kf[rs, :], in0=a[rs, :], scalar1=PI, scalar2=-TWO_PI, op0=ALU.is_gt, op1=ALU.mult),
         reads=[tag + "ang"], writes=[tag + "kf"])
    S.op("dve", lambda e: e.tensor_tensor(out=cs[rs, :], in0=a[rs, :], in1=kf[rs, :], op=ALU.add),
         reads=[tag + "ang", tag + "kf"], writes=[tag + "cos"])
    S.op("act", lambda e: e.activation(out=sn[rs, :], in_=sn[rs, :], func=AF.Sin), writes=[tag + "sin"])
    S.op("act", lambda e: e.activation(out=cs[rs, :], in_=cs[rs, :], func=AF.Sin), writes=[tag + "cos"])


def alloc_tables(kb):
    return {"ang": kb.alloc(512), "kf": kb.alloc(512), "ki": kb.alloc(512, I32), "sin": kb.alloc(512), "cos": kb.alloc(512)}


def load_pos(kb, D, posi, posf, t):
    S = kb.S
    S.dma("sp", lambda e: e.dma_start(out=posi[0:1, :], in_=D["pos"][0:1, t * TQ:(t + 1) * TQ]), writes=["posi"])
    S.op("dve", lambda e: e.tensor_copy(out=posf[0:1, :], in_=posi[0:1, :]), reads=["posi"], writes=["posf"])


class Attn:
    def __init__(self, kb, sbanks=(0, 1), npt=3):
        self.kb = kb
        self.sbanks = sbanks
        self.PT = [kb.alloc(512, BF16) for _ in range(npt)]
        self.pti = 0
        self.si = 0

    def run(self, blocks, obank, defer=None):
        kb = self.kb
        S = kb.S
        n = len(blocks)
        O = kb.psb[obank]
        banks = []

        def scores(i):
            bank = self.sbanks[self.si % len(self.sbanks)]
            self.si += 1
            banks.append(bank)
            mms = blocks[i][0]
            for j, (l, r, ks) in enumerate(mms):
                S.op("pe", lambda e, l=l, r=r, j=j, bank=bank, nm=len(mms): e.matmul(kb.psb[bank][:, :], lhsT=l, rhs=r, start=(j == 0), stop=(j == nm - 1)),
                     reads=ks, writes=[pk(bank)])

        scores(0)
        for i in range(n):
            if i + 1 < n:
                scores(i + 1)
            bank = banks[i]
            pi_ = self.pti % len(self.PT)
            self.pti += 1
            pt = self.PT[pi_]
            S.op("act", lambda e, pt=pt, bank=bank: e.activation(out=pt, in_=kb.psb[bank][:, :], func=AF.Exp), writes=[pk(bank), f"PT{pi_}"])
            v, vk = blocks[i][1], blocks[i][2]
            S.op("pe", lambda e, v=v, pt=pt, i=i: e.matmul(O[0:128, :], lhsT=v, rhs=pt, start=(i == 0), stop=(i == n - 1)),
                 reads=[f"PT{pi_}"] + vk, writes=[pk(obank)])
            if defer and i == min(2, n - 1):
                for f in defer:
                    f()
                del defer[:]


def norm_coef(kb, c, obank, rowbuf, bcbank, bcs):
    S = kb.S
    O = kb.psb[obank]
    B = kb.psb[bcbank]
    S.op("dve", lambda e: e.tensor_scalar_max(out=rowbuf[64:65, :], in0=O[64:65, :], scalar1=1e-30), writes=[pk(obank), "rowbuf"])
    S.op("dve", lambda e: e.reciprocal(out=rowbuf[64:65, :], in_=rowbuf[64:65, :]), writes=["rowbuf"])
    S.op("pe", lambda e: e.matmul(B[0:64, :], lhsT=c.ones_f[64:65, 0:64], rhs=rowbuf[64:65, :], start=True, stop=True),
         reads=["rowbuf", "ones_f"], writes=[pk(bcbank)])
    S.op("act", lambda e: e.copy(out=bcs[0:64, :], in_=B[0:64, :]), writes=[pk(bcbank), "bcs"])


def part_lru(kb, c, D, yT, L):
    S = kb.S
    m0 = kb.mark()
    xa = kb.alloc(N + 4)
    xc = kb.alloc(N)
    A = kb.alloc(N)
    U = kb.alloc(N)
    G = kb.alloc(N)
    xcb = kb.alloc(N, BF16)
    vec = kb.alloc(16)
    wtmp = kb.alloc(256)
    wbd = kb.alloc(256, BF16)
    T1 = xa[:, 0:N]
    P = kb.psb
    S.op("pool", lambda e: e.memset(xa[:, 0:3], 0.0), writes=["xa_pad"])
    for hf in range(2):
        S.dma("sp", lambda e, hf=hf: e.dma_start(out=xa[:, 3 + hf * 4096:3 + (hf + 1) * 4096], in_=L.pt(OFF["a_x"], 128, 128, hf * 4096, (hf + 1) * 4096)), writes=["xa"])
        S.dma("sp", lambda e, hf=hf: e.dma_start(out=G[:, hf * 4096:(hf + 1) * 4096], in_=L.pt(OFF["a_gate"], 128, 128, hf * 4096, (hf + 1) * 4096)), writes=["G"])
    S.dma("sp", lambda e: e.dma_start(out=vec[:, 0:8], in_=D["lru_v"][:, :]), writes=["vec"])
    S.op("pool", lambda e: e.memset(wtmp, 0.0), writes=["wtmp"])
    for a in range(2):
        for b in range(2):
            S.dma("sp", lambda e, a=a, b=b: e.dma_start(out=wtmp[b * 64:(b + 1) * 64, a * 128 + b * 64:a * 128 + (b + 1) * 64], in_=D["lru_w"][a, b]),
                  writes=["wtmp"])
    S.op("act", lambda e: e.copy(out=wbd, in_=wtmp), reads=["wtmp"], writes=["wbd"])
    S.op("act", lambda e: e.activation(out=vec[:, 8:9], in_=vec[:, 7:8], func=AF.Exp, scale=-1.0), reads=["vec"], writes=["vec8"])
    S.op("act", lambda e: e.activation(out=vec[:, 8:9], in_=vec[:, 8:9], func=AF.Ln, bias=1.0), writes=["vec8"])
    S.op("dve", lambda e: e.tensor_scalar(out=vec[:, 9:10], in0=vec[:, 8:9], scalar1=-8.0, scalar2=None, op0=ALU.mult), reads=["vec8"], writes=["vec9"])
    S.op("dve", lambda e: e.tensor_scalar(out=xc, in0=xa[:, 0:N], scalar1=vec[:, 0:1], scalar2=vec[:, 4:5], op0=ALU.mult, op1=ALU.add),
         reads=["xa", "xa_pad", "vec"], writes=["xc"])
    for j in range(1, 4):
        S.op("dve", lambda e, j=j: e.scalar_tensor_tensor(out=xc, in0=xa[:, j:j + N], scalar=vec[:, j:j + 1], in1=xc, op0=ALU.mult, op1=ALU.add),
             reads=["xa", "xa_pad", "vec"], writes=["xc"])
    S.op("act", lambda e: e.copy(out=xcb, in_=xc), reads=["xc"], writes=["xcb"])
    allA = [f"A{t}" for t in range(16)]
    allU = [f"U{t}" for t in range(16)]
    for t in range(16):
        ts = slice(t * 512, (t + 1) * 512)
        b0, b1 = 2 * (t % 2), 2 * (t % 2) + 1
        S.op("pe", lambda e, ts=ts, b0=b0: e.matmul(P[b0][:, :], lhsT=wbd[:, 0:128], rhs=xcb[:, ts], start=True, stop=True),
             reads=["xcb", "wbd"], writes=[pk(b0)])
        S.op("pe", lambda e, ts=ts, b1=b1: e.matmul(P[b1][:, :], lhsT=wbd[:, 128:256], rhs=xcb[:, ts], start=True, stop=True),
             reads=["xcb", "wbd"], writes=[pk(b1)])
        S.op("act", lambda e, ts=ts, b0=b0: e.activation(out=A[:, ts], in_=P[b0][:, :], func=AF.Sigmoid, bias=vec[:, 5:6]),
             reads=["vec"], writes=[pk(b0), f"A{t}"])
        S.op("act", lambda e, ts=ts, b1=b1: e.activation(out=U[:, ts], in_=P[b1][:, :], func=AF.Sigmoid, bias=vec[:, 6:7]),
             reads=["vec"], writes=[pk(b1), f"U{t}"])
    NP = 4
    W_ = N // NP
    for p_ in range(NP):
        cs = slice(p_ * W_, (p_ + 1) * W_)
        tA = [f"A{t}" for t in range(16) if p_ * W_ <= t * 512 < (p_ + 1) * W_]
        tU = [f"U{t}" for t in range(16) if p_ * W_ <= t * 512 < (p_ + 1) * W_]
        kA, kU, kT, kG, kH = f"Ap{p_}", f"Up{p_}", f"Tp{p_}", f"Gp{p_}", f"Hp{p_}"
        S.op("act", lambda e, cs=cs: e.activation(out=A[:, cs], in_=A[:, cs], func=AF.Exp, scale=vec[:, 9:10]), reads=["vec9"], writes=[kA] + tA)
        S.op("pool", lambda e, cs=cs: e.tensor_tensor(out=T1[:, cs], in0=A[:, cs], in1=A[:, cs], op=ALU.mult), reads=[kA, "xc"], writes=[kT, "xa", "xa_pad"])
        S.op("act", lambda e, cs=cs: e.activation(out=T1[:, cs], in_=T1[:, cs], func=AF.Sqrt, bias=1.0, scale=-1.0), writes=[kT])
        S.op("dve", lambda e, cs=cs: e.tensor_tensor(out=U[:, cs], in0=U[:, cs], in1=xc[:, cs], op=ALU.mult), reads=["xc"], writes=[kU] + tU)
        S.op("dve", lambda e, cs=cs: e.tensor_tensor(out=U[:, cs], in0=U[:, cs], in1=T1[:, cs], op=ALU.mult), reads=[kT], writes=[kU])
    for p_ in range(NP):
        cs = slice(p_ * W_, (p_ + 1) * W_)
        init = 0.0 if p_ == 0 else xc[:, p_ * W_ - 1:p_ * W_]
        S.op("dve", lambda e, cs=cs, init=init: e.tensor_tensor_scan(out=xc[:, cs], data0=A[:, cs], data1=U[:, cs], initial=init, op0=ALU.mult, op1=ALU.add),
             reads=[f"Ap{p_}", f"Up{p_}"] + [f"Up{q_}" for q_ in range(NP)], writes=["xc", f"Hp{p_}"])
    for p_ in range(NP):
        cs = slice(p_ * W_, (p_ + 1) * W_)
        kT, kG = f"Tp{p_}", f"Gp{p_}"
        S.op("pool", lambda e, cs=cs: e.tensor_tensor(out=T1[:, cs], in0=G[:, cs], in1=G[:, cs], op=ALU.mult), reads=["G", f"Up{p_}"], writes=[kT])
        S.op("pool", lambda e, cs=cs: e.tensor_scalar(out=T1[:, cs], in0=T1[:, cs], scalar1=0.044715, scalar2=1.0, op0=ALU.mult, op1=ALU.add), writes=[kT])
        S.op("pool", lambda e, cs=cs: e.tensor_tensor(out=T1[:, cs], in0=T1[:, cs], in1=G[:, cs], op=ALU.mult), reads=["G"], writes=[kT])
        S.op("act", lambda e, cs=cs: e.activation(out=T1[:, cs], in_=T1[:, cs], func=AF.Sigmoid, scale=1.5957691216057308), writes=[kT])
        S.op("dve", lambda e, cs=cs: e.tensor_tensor(out=G[:, cs], in0=G[:, cs], in1=T1[:, cs], op=ALU.mult), reads=[kT], writes=[kG])
        S.op("dve", lambda e, cs=cs: e.tensor_tensor(out=A[:, cs], in0=xc[:, cs], in1=G[:, cs], op=ALU.mult), reads=[f"Hp{p_}", kG], writes=[f"Ap{p_}"])
        for hf in range(2):
            if hf * 4096 >= p_ * W_ and (hf + 1) * 4096 <= (p_ + 1) * W_ or (p_ * W_ >= hf * 4096 and (p_ + 1) * W_ <= (hf + 1) * 4096):
                c0, c1 = max(hf * 4096, p_ * W_), min((hf + 1) * 4096, (p_ + 1) * W_)
                S.dma("sp", lambda e, c0=c0, c1=c1: e.dma_start(out=yT[0:128, c0:c1], in_=A[:, c0:c1]), reads=[f"Ap{p_}"])
    S.barrier()
    kb.release(m0)


def part_mla(kb, c, D, yT, L):
    S = kb.S
    m0 = kb.mark()
    P = kb.psb
    SC = float(96 ** -0.5)
    QD = [kb.alloc(N, BF16) for _ in range(2)]
    KD = [kb.alloc(N, BF16) for _ in range(2)]
    VD = kb.alloc(64 * 2 * 128, BF16).rearrange("p (b h d) -> p b h d", b=64, h=2)
    tri = kb.alloc(4 * 512, BF16).rearrange("p (i q) -> p i q", i=4)
    wuq = kb.alloc(2 * 192, BF16).rearrange("p (c n) -> p c n", c=2)
    wk = kb.alloc(128, BF16)
    wv = kb.alloc(128, BF16)
    vec = kb.alloc(4)
    r32 = kb.alloc(32)
    st = kb.alloc(512)
    S.op("pool", lambda e: e.memset(VD, 0.0), writes=["VD"])
    S.op("pool", lambda e: e.memset(VD[:, :, :, 64:65], 1.0), writes=["VD"])
    S.dma("sp", lambda e: e.dma_start(out=vec[:, 0:3], in_=D["mla_v"][:, :]), writes=["mvec"])
    S.dma("sp", lambda e: e.dma_start(out=r32, in_=D["r32"][:, :]), writes=["r32"])
    for i in range(4):
        S.dma("sp", lambda e, i=i: e.dma_start(out=st, in_=D["tri"][i]), writes=["st"])
        S.op("act", lambda e, i=i: e.copy(out=tri[:, i, :], in_=st), reads=["st"], writes=["tri"])
    S.dma("sp", lambda e: e.dma_start(out=st[:, 0:192], in_=D["mla_wuq"][0:128, :]), writes=["st"])
    S.op("act", lambda e: e.copy(out=wuq[:, 0, :], in_=st[:, 0:192]), reads=["st"], writes=["wuq"])
    S.dma("sp", lambda e: e.dma_start(out=st[0:64, 0:192], in_=D["mla_wuq"][128:192, :]), writes=["st"])
    S.op("act", lambda e: e.copy(out=wuq[0:64, 1, :], in_=st[0:64, 0:192]), reads=["st"], writes=["wuq"])
    S.dma("sp", lambda e: e.dma_start(out=st[:, 0:128], in_=D["mla_wk"][:, :]), writes=["st"])
    S.op("act", lambda e: e.copy(out=wk, in_=st[:, 0:128]), reads=["st"], writes=["wk"])
    S.dma("sp", lambda e: e.dma_start(out=st[:, 0:128], in_=D["mla_wv"][:, :]), writes=["st"])
    S.op("act", lambda e: e.copy(out=wv, in_=st[:, 0:128]), reads=["st"], writes=["wv"])

    m1 = kb.mark()
    cq0 = kb.alloc(512)
    cq1 = kb.alloc(512)
    ckv = kb.alloc(512)
    krt = kb.alloc(512)
    sq0 = kb.alloc(512, BF16)
    sq1 = kb.alloc(512, BF16)
    cn0 = kb.alloc(512, BF16)
    cn1 = kb.alloc(512, BF16)
    ckn = kb.alloc(512, BF16)
    rstd = kb.alloc(512)
    qr = kb.alloc(512)
    t1 = kb.alloc(512)
    t2 = kb.alloc(512)
    posi = kb.alloc(512, I32)
    posf = kb.alloc(512)
    T = alloc_tables(kb)
    R = slice(64, 96)
    for t in range(NQT):
        ts = slice(t * TQ, (t + 1) * TQ)
        load_pos(kb, D, posi, posf, t)
        S.dma("sp", lambda e, ts=ts: e.dma_start(out=cq0, in_=L.pt(OFF["d_cq"], 0, 128, ts.start, ts.stop)), writes=["cq0"])
        S.dma("sp", lambda e, ts=ts: e.dma_start(out=cq1[0:64, :], in_=L.pt(OFF["d_cq"] + 128, 0, 64, ts.start, ts.stop)), writes=["cq1"])
        S.dma("sp", lambda e, ts=ts: e.dma_start(out=ckv, in_=L.pt(OFF["d_ckv"], 0, 128, ts.start, ts.stop)), writes=["ckv"])
        S.dma("sp", lambda e, ts=ts: e.dma_start(out=krt[R, :], in_=L.pt(OFF["d_kr"], 0, 32, ts.start, ts.stop)), writes=["krt"])
        rope_tables(kb, c, posf, "posf", 64, 96, 1, T, 6, "m")
        S.op("act", lambda e: e.activation(out=sq0, in_=cq0, func=AF.Square), reads=["cq0"], writes=["sq0"])
        S.op("act", lambda e: e.activation(out=sq1[0:64, :], in_=cq1[0:64, :], func=AF.Square), reads=["cq1"], writes=["sq1"])
        S.op("pe", lambda e: e.matmul(P[0][:, :], lhsT=c.ones_bf, rhs=sq0, start=True, stop=False), reads=["sq0", "ones_bf"], writes=[pk(0)])
        S.op("pe", lambda e: e.matmul(P[0][:, :], lhsT=c.ones_bf[0:64, :], rhs=sq1[0:64, :], start=False, stop=True), reads=["sq1", "ones_bf"], writes=[pk(0)])
        S.op("act", lambda e: e.activation(out=rstd, in_=P[0][:, :], func=AF.Sqrt, bias=EPS, scale=1.0 / 192.0), writes=[pk(0), "rstd"])
        S.op("dve", lambda e: e.reciprocal(out=rstd, in_=rstd), writes=["rstd"])
        S.op("dve", lambda e: e.scalar_tensor_tensor(out=cn0, in0=cq0, scalar=vec[:, 0:1], in1=rstd, op0=ALU.mult, op1=ALU.mult),
             reads=["cq0", "rstd", "mvec"], writes=["cn0"])
        S.op("dve", lambda e: e.scalar_tensor_tensor(out=cn1[0:64, :], in0=cq1[0:64, :], scalar=vec[0:64, 1:2], in1=rstd[0:64, :], op0=ALU.mult, op1=ALU.mult),
             reads=["cq1", "rstd", "mvec"], writes=["cn1"])
        for h in range(2):
            hs = slice(h * 96, (h + 1) * 96)
            S.op("pe", lambda e, hs=hs: e.matmul(P[1][0:96, :], lhsT=wuq[:, 0, hs], rhs=cn0, start=True, stop=False), reads=["cn0", "wuq"], writes=[pk(1)])
            S.op("pe", lambda e, hs=hs: e.matmul(P[1][0:96, :], lhsT=wuq[0:64, 1, hs], rhs=cn1[0:64, :], start=False, stop=True), reads=["cn1", "wuq"], writes=[pk(1)])
            S.op("act", lambda e, h=h, ts=ts: e.mul(out=QD[h][0:64, ts], in_=P[1][0:64, :], mul=SC), writes=[pk(1), f"QD{h}"])
            S.op("dve", lambda e: e.tensor_copy(out=qr[R, :], in_=P[1][R, :]), writes=[pk(1), "qr"])
            S.op("pe", lambda e: e.matmul(P[2][R, :], lhsT=r32[R, 0:32], rhs=qr[R, :], start=True, stop=True), reads=["qr", "r32"], writes=[pk(2)])
            S.op("dve", lambda e: e.scalar_tensor_tensor(out=t1[R, :], in0=qr[R, :], scalar=SC, in1=T["cos"][R, :], op0=ALU.mult, op1=ALU.mult),
                 reads=["qr", "mcos"], writes=["t1"])
            S.op("dve", lambda e: e.scalar_tensor_tensor(out=t2[R, :], in0=P[2][R, :], scalar=SC, in1=T["sin"][R, :], op0=ALU.mult, op1=ALU.mult),
                 reads=["msin"], writes=[pk(2), "t2"])
            S.op("dve", lambda e, h=h, ts=ts: e.tensor_tensor(out=QD[h][R, ts], in0=t1[R, :], in1=t2[R, :], op=ALU.add), reads=["t1", "t2"], writes=[f"QD{h}"])
        S.op("act", lambda e: e.activation(out=sq0, in_=ckv, func=AF.Square), reads=["ckv"], writes=["sq0"])
        S.op("pe", lambda e: e.matmul(P[7][:, :], lhsT=c.ones_bf, rhs=sq0, start=True, stop=True), reads=["sq0", "ones_bf"], writes=[pk(7)])
        S.op("act", lambda e: e.activation(out=rstd, in_=P[7][:, :], func=AF.Sqrt, bias=EPS, scale=1.0 / 128.0), writes=[pk(7), "rstd"])
        S.op("dve", lambda e: e.reciprocal(out=rstd, in_=rstd), writes=["rstd"])
        S.op("dve", lambda e: e.scalar_tensor_tensor(out=ckn, in0=ckv, scalar=vec[:, 2:3], in1=rstd, op0=ALU.mult, op1=ALU.mult),
             reads=["ckv", "rstd", "mvec"], writes=["ckn"])
        for h in range(2):
            S.op("pe", lambda e, h=h: e.matmul(P[3][0:64, :], lhsT=wk[:, h * 64:(h + 1) * 64], rhs=ckn, start=True, stop=True), reads=["ckn", "wk"], writes=[pk(3)])
            S.op("act", lambda e, h=h, ts=ts: e.copy(out=KD[h][0:64, ts], in_=P[3][0:64, :]), writes=[pk(3), f"KD{h}"])
        for tb in range(4):
            S.op("pe", lambda e, tb=tb: e.matmul(P[4][:, 0:128], lhsT=ckn[:, tb * 128:(tb + 1) * 128], rhs=wv, start=True, stop=True), reads=["ckn", "wv"], writes=[pk(4)])
            S.op("act", lambda e, tb=tb, t=t: e.copy(out=VD[:, 4 * t + tb, :, 0:64], in_=P[4][:, 0:128].rearrange("p (h d) -> p h d", h=2)),
                 writes=[pk(4), "VD"])
        S.op("pe", lambda e: e.matmul(P[5][R, :], lhsT=r32[R, 0:32], rhs=krt[R, :], start=True, stop=True), reads=["krt", "r32"], writes=[pk(5)])
        S.op("dve", lambda e: e.tensor_tensor(out=t1[R, :], in0=krt[R, :], in1=T["cos"][R, :], op=ALU.mult), reads=["krt", "mcos"], writes=["t1"])
        S.op("dve", lambda e: e.tensor_tensor(out=t2[R, :], in0=P[5][R, :], in1=T["sin"][R, :], op=ALU.mult), reads=["msin"], writes=[pk(5), "t2"])
        for h in range(2):
            S.op("dve", lambda e, h=h, ts=ts: e.tensor_tensor(out=KD[h][R, ts], in0=t1[R, :], in1=t2[R, :], op=ALU.add), reads=["t1", "t2"], writes=[f"KD{h}"])
    S.barrier()
    kb.release(m1)
    att = Attn(kb, sbanks=(0, 1))
    rowbuf = kb.alloc(512)
    bcs = kb.alloc(512)
    yst = [kb.alloc(512), kb.alloc(512)]
    it = 0
    pending = []
    for qt in range(NQT):
        qs = slice(qt * TQ, (qt + 1) * TQ)
        for h in range(2):
            blocks = []
            for kbk in range(4 * qt + 4):
                mms = [(KD[h][0:96, kbk * 128:(kbk + 1) * 128], QD[h][0:96, qs], [f"KD{h}", f"QD{h}"])]
                if kbk >= 4 * qt:
                    mms.append((c.ident_bf, tri[:, kbk - 4 * qt, :], ["ident_bf", "tri"]))
                blocks.append((mms, VD[:, kbk, h, :], ["VD"]))
            ob = 2 + it % 2
            att.run(blocks, ob, defer=pending)

            def fin(ob=ob, ys=yst[it % 2], yk=f"yst{it % 2}", h=h, qs=qs):
                norm_coef(kb, c, ob, rowbuf, 4, bcs)
                S.op("dve", lambda e: e.tensor_tensor(out=ys[0:64, :], in0=P[ob][0:64, :], in1=bcs[0:64, :], op=ALU.mult),
                     reads=["bcs"], writes=[pk(ob), yk])
                S.dma("sp", lambda e: e.dma_start(out=yT[384 + h * 64:384 + (h + 1) * 64, qs], in_=ys[0:64, :]), reads=[yk])
            pending.append(fin)
            it += 1
    for f in pending:
        f()
    S.barrier()
    kb.release(m0)


def load_cast(kb, src_ap, dst_ap, stage, skey, dkey, eng="act", rows=slice(0, 128)):
    S = kb.S
    S.dma("sp", lambda e: e.dma_start(out=stage, in_=(src_ap() if callable(src_ap) else src_ap)), writes=[skey])
    if eng == "act":
        S.op("act", lambda e: e.copy(out=dst_ap, in_=stage), reads=[skey], writes=[dkey])
    else:
        S.op("pool", lambda e: e.tensor_copy(out=dst_ap, in_=stage), reads=[skey], writes=[dkey])


def gelu_tanh(kb, z, u, out, zk, uk, outk):
    S = kb.S
    S.op("pool", lambda e: e.tensor_tensor(out=u, in0=z, in1=z, op=ALU.mult), reads=[zk], writes=[uk])
    S.op("pool", lambda e: e.tensor_scalar(out=u, in0=u, scalar1=0.044715, scalar2=1.0, op0=ALU.mult, op1=ALU.add), writes=[uk])
    S.op("pool", lambda e: e.tensor_tensor(out=u, in0=u, in1=z, op=ALU.mult), reads=[zk], writes=[uk])
    S.op("act", lambda e: e.activation(out=u, in_=u, func=AF.Sigmoid, scale=1.5957691216057308), writes=[uk])
    S.op("dve", lambda e: e.tensor_tensor(out=out, in0=z, in1=u, op=ALU.mult), reads=[zk, uk], writes=[outk])


def part_nsa(kb, c, D, yT, L):
    S = kb.S
    P = kb.psb
    m0 = kb.mark()
    Qm = kb.alloc(N, BF16)
    Qo = kb.alloc(N, BF16)
    KSA = kb.alloc(N, BF16)
    KSB = kb.alloc(N, BF16)
    KW2 = kb.alloc(N, BF16)
    tri = kb.alloc(4 * 512, BF16).rearrange("p (i q) -> p i q", i=4)
    win = kb.alloc(8 * 512, BF16).rearrange("p (i q) -> p i q", i=8)
    cmpm = kb.alloc(5 * 512, BF16).rearrange("p (i q) -> p i q", i=5)
    rbd = kb.alloc(128)
    ovl = kb.alloc(4 * 130, BF16).rearrange("p (i q) -> p i q", i=4)
    bv = kb.alloc(254)
    bf = kb.alloc(254)
    selg = kb.alloc(384)
    KCMP2 = kb.alloc(512, BF16)
    VCMP = kb.alloc(4 * 128, BF16).rearrange("p (b d) -> p b d", b=4)
    st = kb.alloc(512)
    st3 = st.rearrange("p (b d) -> p b d", d=64)
    S.op("pool", lambda e: e.memset(KSA[64:128, :], 0.0), writes=["ksz"])
    S.op("pool", lambda e: e.memset(KSB[0:64, :], 0.0), writes=["ksz"])
    S.op("pool", lambda e: e.memset(VCMP, 0.0), writes=["VCMP"])
    S.op("pool", lambda e: e.memset(VCMP[:, :, 64:65], 1.0), writes=["VCMP"])
    S.dma("sp", lambda e: e.dma_start(out=rbd, in_=D["rbd"][:, :]), writes=["rbd"])
    S.dma("sp", lambda e: e.dma_start(out=bv, in_=D["bv"][:, :]), writes=["bv"])
    S.dma("sp", lambda e: e.dma_start(out=bf, in_=D["bf"][:, :]), writes=["bf"])
    S.dma("sp", lambda e: e.dma_start(out=selg[0:8, :], in_=D["selg"][:, :]), writes=["selg"])
    i = 0
    for nm, dst, cnt in (("tri", tri, 4), ("win", win, 8), ("cmpm", cmpm, 5)):
        for j in range(cnt):
            load_cast(kb, D[nm][j], dst[:, j, :], st[:, 0:512], "st", nm, "act" if i % 2 == 0 else "pool")
            i += 1
    for j in range(4):
        load_cast(kb, D["ovl"][j], ovl[:, j, 0:129], st[:, 0:129], "st", "ovl", "act")

    m1 = kb.mark()
    KCV = kb.alloc(N)
    xs = [kb.alloc(512), kb.alloc(512)]
    t1s = [kb.alloc(512), kb.alloc(512)]
    t2s = [kb.alloc(512), kb.alloc(512)]
    posi = kb.alloc(512, I32)
    posf = kb.alloc(512)
    T = alloc_tables(kb)
    for hf in range(2):
        S.dma("sp", lambda e, hf=hf: e.dma_start(out=KCV[64:128, hf * 4096:(hf + 1) * 4096], in_=L.pt(OFF["b_kv"] + 64, 0, 64, hf * 4096, (hf + 1) * 4096)), writes=["KCVv"])
    it = 0
    for t in range(NQT):
        ts = slice(t * TQ, (t + 1) * TQ)
        load_pos(kb, D, posi, posf, t)
        rope_tables(kb, c, posf, "posf", 0, 128, 0, T, 6, "n")
        a0, a1 = ts.start, ts.stop
        items = [("q0", [lambda a0=a0, a1=a1: L.pt(OFF["b_q"], 128, 128, a0, a1)], Qm, 0.125, 128),
                 ("q1", [lambda a0=a0, a1=a1: L.pt(OFF["b_q"], 128, 128, a0, a1, inv=True)], Qo, 0.125, 128),
                 ("ks", [lambda a0=a0, a1=a1: L.pt(OFF["b_kv"] + 128, 0, 64, a0, a1)] * 2, None, 1.0, 128),
                 ("kw", [lambda a0=a0, a1=a1: L.pt(OFF["b_kv"] + 192, 0, 64, a0, a1)] * 2, KW2, 1.0, 128),
                 ("kc", [lambda a0=a0, a1=a1: L.pt(OFF["b_kv"], 0, 64, a0, a1)], KCV, 1.0, 64)]
        for nm, srcs, dst, sc, rows in items:
            x = xs[it % 2]
            xk = f"xs{it % 2}"
            t1 = t1s[it % 2]
            t2 = t2s[it % 2]
            bank = 4 + it % 2
            if len(srcs) == 2:
                S.dma("sp", lambda e, x=x, srcs=srcs: e.dma_start(out=x[0:64, :], in_=srcs[0]()), writes=[xk])
                S.dma("sp", lambda e, x=x, srcs=srcs: e.dma_start(out=x[64:128, :], in_=srcs[1]()), writes=[xk])
            else:
                S.dma("sp", lambda e, x=x, srcs=srcs, rows=rows: e.dma_start(out=x[0:rows, :], in_=srcs[0]()), writes=[xk])
            R = slice(0, rows)
            S.op("pe", lambda e, x=x, R=R, bank=bank, rows=rows: e.matmul(P[bank][R, :], lhsT=rbd[R, 0:rows], rhs=x[R, :], start=True, stop=True),
                 reads=[xk, "rbd"], writes=[pk(bank)])
            S.op("dve", lambda e, x=x, R=R, t1=t1, sc=sc: e.scalar_tensor_tensor(out=t1[R, :], in0=x[R, :], scalar=sc, in1=T["cos"][R, :], op0=ALU.mult, op1=ALU.mult),
                 reads=[xk, "ncos"], writes=[f"t1{it % 2}"])
            S.op("dve", lambda e, R=R, t2=t2, sc=sc, bank=bank: e.scalar_tensor_tensor(out=t2[R, :], in0=P[bank][R, :], scalar=sc, in1=T["sin"][R, :], op0=ALU.mult, op1=ALU.mult),
                 reads=["nsin"], writes=[pk(bank), f"t2{it % 2}"])
            dk = "KCVk" if nm == "kc" else nm
            if nm == "ks":
                for dst_, RR in ((KSA, slice(0, 64)), (KSB, slice(64, 128))):
                    S.op("pool", lambda e, RR=RR, t1=t1, t2=t2, dst_=dst_, ts=ts: e.tensor_tensor(out=dst_[RR, ts], in0=t1[RR, :], in1=t2[RR, :], op=ALU.add),
                         reads=[f"t1{it % 2}", f"t2{it % 2}"], writes=[dk])
            else:
                S.op("pool", lambda e, R=R, t1=t1, t2=t2, dst=dst, ts=ts: e.tensor_tensor(out=dst[R, ts], in0=t1[R, :], in1=t2[R, :], op=ALU.add),
                     reads=[f"t1{it % 2}", f"t2{it % 2}"], writes=[dk])
            it += 1
    S.barrier()
    kb.release(m1)
    KCV = kb.alloc(N)
    BLK = kb.alloc(32 * 512, BF16).rearrange("p (l n) -> p l n", l=32)
    W1 = kb.alloc(32 * 256, BF16).rearrange("p (l h) -> p l h", l=32)
    pos2 = kb.alloc(32)
    b1 = kb.alloc(4)
    w2 = kb.alloc(2 * 2 * 64, BF16).rearrange("p (k m d) -> p k m d", k=2, m=2)
    HID = kb.alloc(2 * 2 * 512, BF16).rearrange("p (k m n) -> p k m n", k=2, m=2)
    zt = kb.alloc(512)
    ut = kb.alloc(512)
    stw = kb.alloc(1024).rearrange("p (l h) -> p l h", l=4)
    S.dma("sp", lambda e: e.dma_start(out=pos2, in_=D["nsa_pos"][:, :]), writes=["pos2"])
    S.dma("sp", lambda e: e.dma_start(out=b1, in_=D["nsa_b1"][:, :]), writes=["b1"])
    for kv in range(2):
        load_cast(kb, D["nsa_w2"][kv].rearrange("(m p) d -> p m d", p=128), w2[:, kv, :, :], st[:, 0:128].rearrange("p (m d) -> p m d", m=2), "st", "w2", "act")
    for l0 in range(0, 32, 4):
        for kv in range(2):
            src = D["nsa_w1"][kv].rearrange("(l d) h -> d l h", d=64)[:, l0:l0 + 4, :]
            S.dma("sp", lambda e, src=src, kv=kv: e.dma_start(out=stw[kv * 64:(kv + 1) * 64, :, :], in_=src), writes=["stw"])
        if (l0 // 4) % 2 == 0:
            S.op("act", lambda e, l0=l0: e.copy(out=W1[:, l0:l0 + 4, :], in_=stw), reads=["stw"], writes=["W1"])
        else:
            S.op("pool", lambda e, l0=l0: e.tensor_copy(out=W1[:, l0:l0 + 4, :], in_=stw), reads=["stw"], writes=["W1"])
    S.op("pool", lambda e: e.memset(BLK[:, :, 511:512], 0.0), writes=["BLKpad"])
    K3 = KCV.rearrange("p (g r) -> p g r", r=16)
    for l in range(32):
        src = K3[:, 0:511, l] if l < 16 else K3[:, 1:512, l - 16]
        eng = "dve" if l % 2 == 0 else "pool"
        S.op(eng, lambda e, l=l, src=src: e.tensor_scalar(out=BLK[:, l, 0:511], in0=src, scalar1=pos2[:, l:l + 1], scalar2=None, op0=ALU.add),
             reads=["pos2"], writes=[f"BLK{l}"])
    for kv in range(2):
        R = slice(kv * 64, kv * 64 + 64)
        for m in range(2):
            bank = 2 * kv + m
            for l in range(32):
                S.op("pe", lambda e, l=l, R=R, m=m, bank=bank: e.matmul(P[bank][:, :], lhsT=W1[R, l, m * 128:(m + 1) * 128], rhs=BLK[R, l, :], start=(l == 0), stop=(l == 31)),
                     reads=["W1", f"BLK{l}", "BLKpad"], writes=[pk(bank)])
            S.op("act", lambda e, kv=kv, m=m, bank=bank: e.activation(out=zt, in_=P[bank][:, :], func=AF.Identity, bias=b1[:, kv * 2 + m:kv * 2 + m + 1]),
                 reads=["b1"], writes=[pk(bank), "zt"])
            gelu_tanh(kb, zt, ut, HID[:, kv, m, :], "zt", "ut", f"HID{kv}")
    for m in range(2):
        S.op("pe", lambda e, m=m: e.matmul(P[4][0:64, :], lhsT=w2[:, 0, m, :], rhs=HID[:, 0, m, :], start=(m == 0), stop=(m == 1)), reads=["w2", "HID0"], writes=[pk(4)])
    S.op("act", lambda e: e.copy(out=KCMP2[0:64, :], in_=P[4][0:64, :]), writes=[pk(4), "KCMP2"])
    S.op("dve", lambda e: e.tensor_copy(out=KCMP2[64:128, :], in_=P[4][0:64, :]), writes=[pk(4), "KCMP2"])
    for nb in range(4):
        for m in range(2):
            S.op("pe", lambda e, m=m, nb=nb: e.matmul(P[5][:, 0:64], lhsT=HID[:, 1, m, nb * 128:(nb + 1) * 128], rhs=w2[:, 1, m, :], start=(m == 0), stop=(m == 1)),
                 reads=["w2", "HID1"], writes=[pk(5)])
        S.op("act", lambda e, nb=nb: e.copy(out=VCMP[:, nb, 0:64], in_=P[5][:, 0:64]), writes=[pk(5), "VCMP"])
    S.barrier()
    kb.release(m1)
    VS = kb.alloc(64 * 128, BF16).rearrange("p (b d) -> p b d", b=64)
    VW = kb.alloc(64 * 128, BF16).rearrange("p (b d) -> p b d", b=64)
    for vt_, vk_ in ((VS, "VS"), (VW, "VW")):
        S.op("pool", lambda e, vt_=vt_: e.memset(vt_, 0.0), writes=[vk_])
        S.op("pool", lambda e, vt_=vt_: e.memset(vt_[:, :, 64:65], 1.0), writes=[vk_])
    for nm, dst, coff in (("nsa_vs", VS, 0), ("nsa_vw", VW, 64)):
        for b0 in range(0, 64, 8):
            load_cast(kb, (lambda b0=b0, coff=coff: L.pv(coff, 0, 64, b0 * 128, (b0 + 8) * 128).rearrange("(b p) d -> p b d", p=128)),
                      dst[:, b0:b0 + 8, 0:64], st3[:, 0:8, :], "st", nm[-2:].upper(), "act" if (b0 // 8) % 2 == 0 else "pool")

    NST = kb.alloc(N, BF16)
    EALL = kb.alloc(N, BF16)
    for j in range(16):
        load_cast(kb, D["eall"][:, j * 512:(j + 1) * 512], EALL[:, j * 512:(j + 1) * 512], st[:, 0:512], "st", "EALL", "act" if j % 2 == 0 else "pool")
    ET = [kb.alloc(512, BF16) for _ in range(4)]
    IMP = kb.alloc(512).rearrange("p (a s) -> p a s", a=4)
    scr = kb.alloc(128)
    mr = kb.alloc(128)
    nsb = kb.alloc(128)
    mx = kb.alloc(16)
    thr = kb.alloc(2)
    rsum = kb.alloc(2)
    g6 = kb.alloc(512)
    gs = [[kb.alloc(512) for _ in range(3)] for _ in range(2)]
    acc = [kb.alloc(512), kb.alloc(512)]
    ctmp = kb.alloc(512)
    otmp = kb.alloc(512)
    rowbuf = ctmp
    bcs = kb.alloc(512)
    att = Attn(kb, sbanks=(0, 1))
    oi = 0

    def combine(ob, hA, br, first):
        norm_coef(kb, c, ob, rowbuf, 4, bcs)
        S.op("dve", lambda e: e.tensor_tensor(out=ctmp[0:64, :], in0=bcs[0:64, :], in1=gs[hA][br][0:64, :], op=ALU.mult),
             reads=["bcs", f"gs{hA}{br}"], writes=["ctmp"])
        if first:
            S.op("dve", lambda e: e.tensor_tensor(out=acc[hA][0:64, :], in0=P[ob][0:64, :], in1=ctmp[0:64, :], op=ALU.mult),
                 reads=["ctmp"], writes=[pk(ob), f"acc{hA}"])
        else:
            S.op("dve", lambda e: e.tensor_tensor(out=otmp[0:64, :], in0=P[ob][0:64, :], in1=ctmp[0:64, :], op=ALU.mult),
                 reads=["ctmp"], writes=[pk(ob), "otmp"])
            S.op("pool", lambda e: e.tensor_tensor(out=acc[hA][0:64, :], in0=acc[hA][0:64, :], in1=otmp[0:64, :], op=ALU.add),
                 reads=["otmp"], writes=[f"acc{hA}"])

    pending = []
    for qt in range(NQT):
        qs = slice(qt * TQ, (qt + 1) * TQ)
        for f in pending:
            f()
        del pending[:]
        S.dma("sp", lambda e, qs=qs: e.dma_start(out=g6[0:6, :], in_=L.pt(OFF["b_gate"], 6, 6, qs.start, qs.stop)), writes=["g6"])
        for hA in range(2):
            for br in range(3):
                r = hA * 3 + br
                S.op("pe", lambda e, r=r: e.matmul(P[5][0:64, :], lhsT=selg[0:6, r * 64:(r + 1) * 64], rhs=g6[0:6, :], start=True, stop=True),
                     reads=["g6", "selg"], writes=[pk(5)])
                S.op("act", lambda e, hA=hA, br=br: e.activation(out=gs[hA][br][0:64, :], in_=P[5][0:64, :], func=AF.Sigmoid), writes=[pk(5), f"gs{hA}{br}"])
        nbm = (512 * qt + 480) // 2048
        for hh in range(4):
            Qt = (Qm if hh < 2 else Qo)
            qk = "q0" if hh < 2 else "q1"
            R = slice(64 * (hh % 2), 64 * (hh % 2) + 64)
            ob = 2 + oi % 2
            for nb in range(nbm + 1):
                bank = nb % 2
                dl = 512 * qt - 2048 * nb
                mms = [(KCMP2[R, nb * 128:(nb + 1) * 128], Qt[R, qs], ["KCMP2", qk])]
                if dl < 2560:
                    mms.append((c.ident_bf, cmpm[:, dl // 512, :], ["ident_bf", "cmpm"]))
                for j, (l_, r_, ks) in enumerate(mms):
                    S.op("pe", lambda e, l_=l_, r_=r_, j=j, bank=bank, nm=len(mms): e.matmul(P[bank][:, :], lhsT=l_, rhs=r_, start=(j == 0), stop=(j == nm - 1)),
                         reads=ks, writes=[pk(bank)])
                S.op("act", lambda e, nb=nb, bank=bank: e.activation(out=ET[nb], in_=P[bank][:, :], func=AF.Exp), writes=[pk(bank), f"ET{nb}"])
                if hh < 2:
                    S.op("pe", lambda e, nb=nb, ob=ob, nbm=nbm: e.matmul(P[ob][0:128, :], lhsT=VCMP[:, nb, :], rhs=ET[nb], start=(nb == 0), stop=(nb == nbm)),
                         reads=[f"ET{nb}", "VCMP"], writes=[pk(ob)])
            for s4 in range(4):
                for nb in range(nbm + 1):
                    S.op("pe", lambda e, nb=nb, s4=s4, nbm=nbm: e.matmul(P[6][:, 0:129], lhsT=ET[nb][:, s4 * 128:(s4 + 1) * 128], rhs=ovl[:, nb, 0:129], start=(nb == 0), stop=(nb == nbm)),
                         reads=[f"ET{nb}", "ovl"], writes=[pk(6)])
                S.op("dve", lambda e: e.tensor_scalar_max(out=rsum[:, 0:1], in0=P[6][:, 128:129], scalar1=1e-30), writes=[pk(6), "rsum"])
                S.op("dve", lambda e: e.reciprocal(out=rsum[:, 0:1], in_=rsum[:, 0:1]), writes=["rsum"])
                if hh == 0:
                    S.op("dve", lambda e, s4=s4: e.tensor_scalar(out=IMP[:, s4, :], in0=P[6][:, 0:128], scalar1=rsum[:, 0:1], scalar2=None, op0=ALU.mult),
                         reads=["rsum"], writes=[pk(6), f"IMP{s4}"])
                else:
                    S.op("dve", lambda e, s4=s4: e.scalar_tensor_tensor(out=IMP[:, s4, :], in0=P[6][:, 0:128], scalar=rsum[:, 0:1], in1=IMP[:, s4, :], op0=ALU.mult, op1=ALU.add),
                         reads=["rsum"], writes=[pk(6), f"IMP{s4}"])
            if hh < 2:
                combine(ob, hh, 0, True)
                oi += 1
        for s4 in range(4):
            qb = 4 * qt + s4
            o0 = 126 - 2 * qb
            S.op("dve", lambda e, s4=s4, o0=o0: e.tensor_tensor(out=scr, in0=IMP[:, s4, :], in1=bv[:, o0:o0 + 128], op=ALU.mult), reads=[f"IMP{s4}", "bv"], writes=["scr"])
            S.op("dve", lambda e, o0=o0: e.tensor_tensor(out=scr, in0=scr, in1=bf[:, o0:o0 + 128], op=ALU.add), reads=["bf"], writes=["scr"])
            S.op("dve", lambda e: e.memset(scr[:, 0:1], 1e9), writes=["scr"])
            S.op("dve", lambda e: e.max(out=mx[:, 0:8], in_=scr), reads=["scr"], writes=["mx"])
            S.op("dve", lambda e: e.match_replace(out=mr, in_to_replace=mx[:, 0:8], in_values=scr, imm_value=-2.0), reads=["scr", "mx"], writes=["mr"])
            S.op("dve", lambda e: e.max(out=mx[:, 8:16], in_=mr), reads=["mr"], writes=["mx"])
            S.op("dve", lambda e: e.tensor_reduce(out=thr[:, 0:1], in_=mx[:, 8:16], axis=AX.X, op=ALU.min), reads=["mx"], writes=["thr"])
            S.op("dve", lambda e: e.tensor_scalar(out=nsb, in0=scr, scalar1=thr[:, 0:1], scalar2=-NEG, op0=ALU.is_ge, op1=ALU.mult), reads=["scr", "thr"], writes=["nsb"])
            S.op("pool", lambda e: e.tensor_scalar(out=nsb, in0=nsb, scalar1=NEG, scalar2=None, op0=ALU.add), writes=["nsb"])
            S.op("pe", lambda e: e.transpose(P[7][:, 0:128], nsb, c.ident), reads=["nsb", "ident"], writes=[pk(7)])
            S.op("act", lambda e, qb=qb: e.copy(out=NST[:, qb * 128:(qb + 1) * 128], in_=P[7][:, 0:128]), writes=[pk(7), "NST"])
        for hA in range(2):
            R = slice(64 * hA, 64 * hA + 64)
            blocks = []
            for kbk in range(4 * qt + 4):
                mms = [((KSA if hA == 0 else KSB)[:, kbk * 128:(kbk + 1) * 128], Qm[:, qs], ["ks", "q0", "ksz"]),
                       (EALL[:, kbk * 128:(kbk + 1) * 128], NST[:, qs], ["EALL", "NST"])]
                if kbk >= 4 * qt:
                    mms.append((c.ident_bf, tri[:, kbk - 4 * qt, :], ["ident_bf", "tri"]))
                blocks.append((mms, VS[:, kbk, :], ["VS"]))
            ob = 2 + oi % 2
            oi += 1
            att.run(blocks, ob, defer=pending)
            pending.append(lambda ob=ob, hA=hA: combine(ob, hA, 1, False))
            blocks = []
            for kbk in range(max(0, 4 * qt - 4), 4 * qt + 4):
                mms = [(KW2[R, kbk * 128:(kbk + 1) * 128], Qm[R, qs], ["kw", "q0"]),
                       (c.ident_bf, win[:, kbk - 4 * qt + 4, :], ["ident_bf", "win"])]
                blocks.append((mms, VW[:, kbk, :], ["VW"]))
            ob = 2 + oi % 2
            oi += 1
            att.run(blocks, ob, defer=pending)

            def fin(ob=ob, hA=hA, qs=qs):
                combine(ob, hA, 2, False)
                S.dma("sp", lambda e: e.dma_start(out=yT[128 + hA * 64:128 + (hA + 1) * 64, qs], in_=acc[hA][0:64, :]), reads=[f"acc{hA}"])
            pending.append(fin)
    for f in pending:
        f()
    S.barrier()
    kb.release(m0)


def part_gla(kb, c, D, yT, L):
    STAGE = 9
    S = kb.S
    P = kb.psb
    m0 = kb.mark()
    QS = float(32 ** -0.5)
    A = kb.alloc(N)
    B = kb.alloc(N)
    C = kb.alloc(2048)
    QG = kb.alloc(N, BF16)
    KG = kb.alloc(N, BF16)
    KHT = kb.alloc(128 * 64, BF16).rearrange("p (b d) -> p b d", b=128)
    V = kb.alloc(128 * 128, BF16).rearrange("p (b d) -> p b d", b=128)
    SB = kb.alloc(128 * 64, BF16).rearrange("p (c v) -> p c v", c=128)
    DC = kb.alloc(128)
    wg2 = kb.alloc(64)
    glr = kb.alloc(512)
    v2 = kb.alloc(4)
    tri64 = kb.alloc(64)
    st = kb.alloc(1024)
    S.dma("sp", lambda e: e.dma_start(out=wg2[0:16, :], in_=D["gla_wg2"][:, :]), writes=["wg2"])
    S.dma("sp", lambda e: e.dma_start(out=v2[:, 0:2], in_=D["gla_v2"][:, :]), writes=["v2"])
    S.dma("sp", lambda e: e.dma_start(out=tri64[0:64, :], in_=D["tri64"][:, :]), writes=["tri64"])
    S.dma("sp", lambda e: e.dma_start(out=tri64[64:128, :], in_=D["tri64"][:, :]), writes=["tri64"])
    S.op("dve", lambda e: e.tensor_scalar(out=v2[:, 2:3], in0=v2[:, 0:1], scalar1=-1.0, scalar2=None, op0=ALU.mult), reads=["v2"], writes=["v2n"])
    R = slice(0, 64)
    st3 = st.rearrange("p (b d) -> p b d", d=128)
    for b0 in range(0, 128, 8):
        load_cast(kb, (lambda b0=b0: L.pv(128, 128, 128, b0 * 64, (b0 + 8) * 64).rearrange("(b p) d -> p b d", p=64)),
                  V[R, b0:b0 + 8, :], st3[R, :, :], "st", "V", "act" if (b0 // 8) % 2 == 0 else "pool")
    for t in range(NQT):
        ts = slice(t * TQ, (t + 1) * TQ)
        bank = t % 2
        S.dma("sp", lambda e, ts=ts: e.dma_start(out=glr[0:16, :], in_=L.pt(OFF["c_glr"], 0, 16, ts.start, ts.stop)), writes=["glr"])
        S.op("pe", lambda e, bank=bank: e.matmul(P[bank][R, :], lhsT=wg2[0:16, 0:64], rhs=glr[0:16, :], start=True, stop=True), reads=["glr", "wg2"], writes=[pk(bank)])
        S.op("act", lambda e, bank=bank, ts=ts: e.activation(out=A[R, ts], in_=P[bank][R, :], func=AF.Exp, bias=v2[R, 2:3], scale=-1.0),
             reads=["v2n"], writes=[pk(bank), "A"])
    S.op("act", lambda e: e.activation(out=A[R, :], in_=A[R, :], func=AF.Ln, bias=1.0), writes=["A"])
    for ch in range(128):
        cs = slice(ch * 64, (ch + 1) * 64)
        S.op("dve", lambda e, cs=cs: e.tensor_tensor_scan(out=B[R, cs], data0=c.ones_f[R, 0:64], data1=A[R, cs], initial=0.0, op0=ALU.mult, op1=ALU.add),
             reads=["A", "ones_f"], writes=["B"])
    if STAGE <= 1:
        S.barrier(); kb.release(m0); return
    B3 = B.rearrange("p (c j) -> p c j", j=64)
    A3 = A.rearrange("p (c j) -> p c j", j=64)
    S.op("act", lambda e: e.activation(out=DC[R, :], in_=B3[R, :, 63], func=AF.Exp, scale=-1.0 / 16.0), reads=["B"], writes=["DC"])
    S.op("act", lambda e: e.activation(out=A[R, :], in_=B[R, :], func=AF.Exp, scale=-1.0 / 16.0), reads=["B"], writes=["A"])
    for pc in range(4):
        ps_ = slice(pc * 2048, (pc + 1) * 2048)
        S.dma("sp", lambda e, ps_=ps_: e.dma_start(out=C[R, :], in_=L.pt(OFF["c_q"], 64, 64, ps_.start, ps_.stop)), writes=["C"])
        S.op("dve", lambda e, ps_=ps_: e.scalar_tensor_tensor(out=QG[R, ps_], in0=C[R, :], scalar=QS, in1=A[R, ps_], op0=ALU.mult, op1=ALU.mult),
             reads=["C", "A"], writes=["QG"])
    S.op("act", lambda e: e.activation(out=A[R, :], in_=B[R, :], func=AF.Exp, scale=1.0 / 16.0), reads=["B", "QG"], writes=["A"])
    for pc in range(4):
        ps_ = slice(pc * 2048, (pc + 1) * 2048)
        S.dma("sp", lambda e, ps_=ps_: e.dma_start(out=C[R, :], in_=L.pt(OFF["c_k"], 64, 64, ps_.start, ps_.stop)), writes=["C"])
        S.op("dve", lambda e, ps_=ps_: e.tensor_tensor(out=KG[R, ps_], in0=C[R, :], in1=A[R, ps_], op=ALU.mult), reads=["C", "A"], writes=["KG"])
    S.op("dve", lambda e: e.tensor_tensor(out=A3[R, :, :], in0=B3[R, :, :], in1=B3[R, :, 63:64].to_broadcast([64, 128, 64]), op=ALU.subtract),
         reads=["B", "KG"], writes=["A"])
    S.op("act", lambda e: e.activation(out=A[R, :], in_=A[R, :], func=AF.Exp, scale=1.0 / 16.0), writes=["A"])
    for pc in range(4):
        ps_ = slice(pc * 2048, (pc + 1) * 2048)
        S.dma("sp", lambda e, ps_=ps_: e.dma_start(out=C[R, :], in_=L.pt(OFF["c_k"], 64, 64, ps_.start, ps_.stop)), writes=["C"])
        S.op("dve", lambda e, ps_=ps_: e.tensor_tensor(out=A[R, ps_], in0=C[R, :], in1=A[R, ps_], op=ALU.mult), reads=["C"], writes=["A"])
    if STAGE <= 2:
        S.barrier(); kb.release(m0); return
    for blk in range(64):
        bank = blk % 2
        S.op("pe", lambda e, blk=blk, bank=bank: e.transpose(P[bank][:, 0:64], A[R, blk * 128:(blk + 1) * 128], c.ident[0:64, 0:64]),
             reads=["A", "ident"], writes=[pk(bank)])
        S.op("act", lambda e, blk=blk, bank=bank: e.copy(out=KHT[R, 2 * blk, :], in_=P[bank][0:64, 0:64]), writes=[pk(bank), "KHT"])
        S.op("dve", lambda e, blk=blk, bank=bank: e.tensor_copy(out=KHT[R, 2 * blk + 1, :], in_=P[bank][64:128, 0:64]), writes=[pk(bank), "KHT"])
    if STAGE <= 3:
        S.barrier(); kb.release(m0); return
    U3 = B.rearrange("p (v c) -> p v c", c=128)
    uev = [kb.alloc(512), kb.alloc(512)]
    S3 = A.rearrange("p (v c) -> p v c", c=128)
    for g in range(32):
        bank = 2 + g % 2
        for j in range(4):
            ch = 4 * g + j
            S.op("pe", lambda e, ch=ch, j=j, bank=bank: e.matmul(P[bank][R, j * 128:(j + 1) * 128], lhsT=KHT[R, ch, :], rhs=V[R, ch, :], start=True, stop=True),
                 reads=["KHT", "V"], writes=[pk(bank)])
        ue = uev[g % 2]
        S.op("act", lambda e, bank=bank, ue=ue: e.copy(out=ue[R, :], in_=P[bank][R, :]), writes=[pk(bank), f"uev{g % 2}"])
        for h in range(2):
            hr = slice(32 * h, 32 * h + 32)
            for j in range(4):
                eng = "dve" if j % 2 == 0 else "pool"
                S.op(eng, lambda e, hr=hr, ue=ue, g=g, j=j, h=h: e.tensor_copy(out=U3[hr, :, 4 * g + j], in_=ue[hr, j * 128 + h * 64:j * 128 + (h + 1) * 64]),
                     reads=[f"uev{g % 2}"], writes=["U3", "B"])
    if STAGE <= 4:
        S.barrier(); kb.release(m0); return
    for v in range(64):
        S.op("dve", lambda e, v=v: e.tensor_tensor_scan(out=S3[R, v, :], data0=DC[R, :], data1=U3[R, v, :], initial=0.0, op0=ALU.mult, op1=ALU.add),
             reads=["U3", "DC", "KHT"], writes=["S3", "A"])
    S.op("pool", lambda e: e.memset(SB[R, 0, :], 0.0), writes=["SB0"])
    S.op("act", lambda e: e.copy(out=SB[R, 1:128, :], in_=S3[R, :, 0:127].rearrange("p v c -> p c v")), reads=["S3"], writes=["SB"])
    if STAGE <= 5:
        S.barrier(); kb.release(m0); return
    osb = kb.alloc(512)
    sqb = kb.alloc(512, BF16)
    rstd = kb.alloc(512)
    og = kb.alloc(512)
    at = [kb.alloc(64, BF16), kb.alloc(64, BF16)]
    ai = 0
    for t in range(NQT):
        ts = slice(t * TQ, (t + 1) * TQ)
        for h in range(2):
            hr = slice(32 * h, 32 * h + 32)
            ob = 4 + (2 * t + h) % 2
            for j in range(8):
                ch = 8 * t + j
                cs = slice(ch * 64, (ch + 1) * 64)
                rr = R
                sbk = ai % 2
                a_t = at[ai % 2]
                ak = f"at{ai % 2}"
                ai += 1
                S.op("pe", lambda e, hr=hr, cs=cs, rr=rr, sbk=sbk: e.matmul(P[sbk][rr, 0:64], lhsT=KG[hr, cs], rhs=QG[hr, cs], start=True, stop=True),
                     reads=["KG", "QG"], writes=[pk(sbk)])
                S.op("dve", lambda e, rr=rr, sbk=sbk, a_t=a_t: e.tensor_tensor(out=a_t[rr, :], in0=P[sbk][rr, 0:64], in1=tri64[rr, :], op=ALU.mult),
                     reads=["tri64"], writes=[pk(sbk), ak])
                S.op("pe", lambda e, rr=rr, ch=ch, h=h, a_t=a_t, ob=ob, j=j: e.matmul(P[ob][R, j * 64:(j + 1) * 64], lhsT=V[rr, ch, h * 64:(h + 1) * 64], rhs=a_t[rr, :], start=True, stop=False),
                     reads=[ak, "V"], writes=[pk(ob)])
                S.op("pe", lambda e, hr=hr, ch=ch, cs=cs, ob=ob, j=j: e.matmul(P[ob][R, j * 64:(j + 1) * 64], lhsT=SB[hr, ch, :], rhs=QG[hr, cs], start=False, stop=True),
                     reads=["SB", "SB0", "QG"], writes=[pk(ob)])
            S.op("act", lambda e, ob=ob: e.copy(out=osb[R, :], in_=P[ob][R, :]), writes=[pk(ob), "osb"])
            S.op("act", lambda e: e.activation(out=sqb[R, :], in_=osb[R, :], func=AF.Square), reads=["osb"], writes=["sqb"])
            S.op("pe", lambda e: e.matmul(P[6][R, :], lhsT=c.ones_bf[R, 0:64], rhs=sqb[R, :], start=True, stop=True), reads=["sqb", "ones_bf"], writes=[pk(6)])
            S.op("act", lambda e: e.activation(out=rstd[R, :], in_=P[6][R, :], func=AF.Sqrt, bias=EPS, scale=1.0 / 64.0), writes=[pk(6), "rstd"])
            S.op("dve", lambda e: e.reciprocal(out=rstd[R, :], in_=rstd[R, :]), writes=["rstd"])
            S.op("dve", lambda e: e.scalar_tensor_tensor(out=osb[R, :], in0=osb[R, :], scalar=v2[R, 1:2], in1=rstd[R, :], op0=ALU.mult, op1=ALU.mult),
                 reads=["rstd", "v2"], writes=["osb"])
            S.dma("sp", lambda e, h=h, ts=ts: e.dma_start(out=og[R, :], in_=L.pt(OFF["c_og"] + h * 64, 128, 64, ts.start, ts.stop)), writes=["og"])
            S.op("act", lambda e: e.activation(out=og[R, :], in_=og[R, :], func=AF.Silu), writes=["og"])
            S.op("dve", lambda e: e.tensor_tensor(out=osb[R, :], in0=osb[R, :], in1=og[R, :], op=ALU.mult), reads=["og"], writes=["osb"])
            S.dma("sp", lambda e, h=h, ts=ts: e.dma_start(out=yT[256 + h * 64:256 + (h + 1) * 64, ts], in_=osb[R, :]), reads=["osb"])
    S.barrier()
    kb.release(m0)


WB = ["lru_w", "lru_v", "mla_wuq", "mla_wk", "mla_wv", "mla_v", "nsa_pos", "nsa_w1", "nsa_b1", "nsa_w2", "gla_wg2", "gla_v2"]
WAC = (("gain", [128, 8]), ("w_in", [1024, NCOLW]), ("gng", [128, 8]), ("nfg", [128, 8]), ("w_out", [1024, 1024]),
       ("w_gu", [1024, 2 * DFF]), ("w_dn", [DFF, 1024]))
RG = [[0, 1], [2, 3], [4, 5], [6, 7]]


def build_fused():
    kb = KB(arena_cols=53100)
    S = kb.S
    D = {k: kb.din(k, shp) for k, shp in CONST_SHAPES.items()}
    D["pos"] = kb.din("pos", [1, N], I32)
    xT = kb.din("xT", [1024, 4096])
    out = kb.dout("out", [1024, 4096])
    LW = []
    for l in range(2):
        d = {k: kb.din(f"{k}_{l}", IN_SHAPES[k][0]) for k in WB}
        for k, shp in WAC:
            d[k] = kb.din(f"{k}_{l}", shp)
        LW.append(d)
    fing = kb.din("fing", [128, 8])
    XA = kb.dint("XA", [NCOLP, 4096])
    XV = kb.dint("XV", [4096, NV])
    GP = kb.dint("GP", [2 * NCOLP, 4096])
    GV = kb.dint("GV", [N, NV])
    YB = kb.dint("YB", [1024, 4096])
    YV = YView(YB)
    GY = kb.dint("GY", [2048, 4096])
    XO = kb.dint("XO", [1024, 4096])
    MYP = kb.dint("MYP", [SELROWS, N])
    MYV = kb.dint("MYV", [N, 128])
    MYY = kb.dint("MYY", [1024, 4096])
    c = common_setup(kb, D)
    L = Loader(S, GP, GV, MYP, MYV)
    xs = xT
    for l in range(2):
        W = LW[l]
        ag = lambda i_, o_: (lambda e: e.collective_compute("AllGather", ALU.bypass, replica_groups=RG, ins=[i_], outs=[o_]))
        phase_A(kb, xs, W["gain"], W["w_in"], XA, XV, GP, GV, ag)
        L.select()
        Dl = dict(D)
        Dl.update({k: W[k] for k in WB})
        for part, g in ((part_lru, 0), (part_gla, 2), (part_mla, 3), (part_nsa, 1)):
            part(kb, c, Dl, YV, L)
            for ch in range(2):
                S.collective_async(ag(YB[ch * 512 + g * 128:ch * 512 + (g + 1) * 128, :], GY[(g * 2 + ch) * 256:(g * 2 + ch + 1) * 256, :]))
        S.cc_wait_all()
        GY4 = GY.rearrange("(g ch q) t -> g ch q t", g=4, ch=2)
        S.dma("act", lambda e: e.dma_start(out=MYY.rearrange("(g q) t -> g q t", g=4), in_=GY4[:, bass.ds(S.rt["hp_act"], 1), :, :].rearrange("g o q t -> g (o q) t")),
              writes=["MYY"])
        S.barrier()
        phase_C(kb, l == 1, xs, MYY, W["gng"], W["nfg"], fing, W["w_out"], W["w_gu"], W["w_dn"], out if l == 1 else XO)
        xs = XO
    return kb.close()


def prep_W(W, l, hp):
    A = np.ascontiguousarray
    o = {}
    ch = slice(hp * 128, hp * 128 + 128)
    o["lru_w"] = A(np.stack([W["lru_wa"][l][2 * hp:2 * hp + 2], W["lru_wx"][l][2 * hp:2 * hp + 2]]))
    o["lru_v"] = A(np.stack([W["conv_w"][l][0, ch], W["conv_w"][l][1, ch], W["conv_w"][l][2, ch], W["conv_w"][l][3, ch],
                             W["conv_b"][l][ch], W["lru_ba"][l][ch], W["lru_bx"][l][ch], W["lru_lambda"][l][ch]], axis=1))
    o["mla_wuq"] = A(W["mla_w_uq"][l][:, hp * 192:(hp + 1) * 192])
    wkv = W["mla_w_ukv"][l].reshape(128, 4, 128)
    o["mla_wk"] = A(wkv[:, 2 * hp:2 * hp + 2, 0:64].reshape(128, 128))
    o["mla_wv"] = A(wkv[:, 2 * hp:2 * hp + 2, 64:128].reshape(128, 128))
    mv = np.zeros((128, 3), np.float32)
    mv[:, 0] = W["mla_q_norm"][l][0:128]
    mv[0:64, 1] = W["mla_q_norm"][l][128:192]
    mv[:, 2] = W["mla_kv_norm"][l]
    o["mla_v"] = mv
    o["nsa_pos"] = A(np.concatenate([W["cmp_pos"][l][0].T, W["cmp_pos"][l][1].T], axis=0))
    o["nsa_w1"] = A(W["cmp_w1"][l])
    o["nsa_b1"] = A(W["cmp_b1"][l].reshape(2, 2, 128).transpose(2, 0, 1).reshape(128, 4))
    o["nsa_w2"] = A(W["cmp_w2"][l])
    o["gla_wg2"] = A(W["gla_wg2"][l][:, hp * 64:(hp + 1) * 64])
    g2 = np.zeros((128, 2), np.float32)
    g2[0:64, 0] = W["gla_bg2"][l][hp * 64:(hp + 1) * 64]
    g2[:, 1] = np.tile(W["gla_norm"][l], 2)
    o["gla_v2"] = g2
    return o


_PROG = {}


def _arr8(g):
    return np.ascontiguousarray(np.asarray(g, np.float32).reshape(8, 128).T)


def kernel(**inp):
    W = {k: np.asarray(v) for k, v in inp.items()}
    x = W["x"]
    Bn, Sn, Dm = x.shape
    HT = Sn // 2
    cores = [(b, r) for b in range(Bn) for r in range(2)]
    if "F" not in _PROG:
        _PROG["F"] = build_fused()
    nc = _PROG["F"]
    consts = host_consts()
    pc = perm_cols()
    ins = []
    for (b, r) in cores:
        d = dict(consts)
        d["pos"] = np.ascontiguousarray(W["positions"][b][None, :].astype(np.int32))
        d["xT"] = np.ascontiguousarray(x[b, r * HT:(r + 1) * HT].T)
        d["fing"] = _arr8(W["final_norm"])
        for l in range(2):
            for k, v in prep_W(W, l, r).items():
                d[f"{k}_{l}"] = v
            d[f"gain_{l}"] = _arr8(W["norm_mix"][l])
            d[f"w_in_{l}"] = np.ascontiguousarray(W["w_in"][l][:, pc])
            d[f"gng_{l}"] = _arr8(W["group_norm"][l])
            d[f"nfg_{l}"] = _arr8(W["norm_ffn"][l])
            d[f"w_out_{l}"] = np.ascontiguousarray(W["w_out"][l])
            d[f"w_gu_{l}"] = np.ascontiguousarray(W["w_gate_up"][l])
            d[f"w_dn_{l}"] = np.ascontiguousarray(W["w_down"][l])
        ins.append(d)
    res = run_bass_kernel_spmd(nc, ins, core_ids=list(range(8))).results
    out = np.empty((Bn, Sn, Dm), np.float32)
    for ci, (b, r) in enumerate(cores):
        out[b, r * HT:(r + 1) * HT] = res[ci]["out"].T
    return out
```

```python
import numpy as np
from contextlib import ExitStack
import concourse.bass as bass
import concourse.mybir as mybir
from concourse.bass_utils import run_bass_kernel_spmd

F32 = mybir.dt.float32
BF16 = mybir.dt.bfloat16
I32 = mybir.dt.int32
AF = mybir.ActivationFunctionType
ALU = mybir.AluOpType
AX = mybir.AxisListType

ENGS = ("pe", "act", "dve", "pool", "sp")
NDMA_SEMS = 8
EPS = 1e-6


class Sched:
    def __init__(self, nc, es):
        self.nc = nc
        self.sem = {e: es.enter_context(nc.semaphore("s_" + e)) for e in ENGS}
        self.cnt = {e: 0 for e in ENGS}
        self.dsem = {e: [es.enter_context(nc.semaphore(f"d_{e}{i}")) for i in range(NDMA_SEMS)]
                     for e in ("sp", "pool", "act")}
        self.dval = {e: [0] * NDMA_SEMS for e in self.dsem}
        self.drr = {e: 0 for e in self.dsem}
        self.ops = {e: [] for e in ENGS}
        self.known = {e: {} for e in ENGS}
        self.semobj = {}
        self.last_w = {}
        self.readers = {}
        self.ccsem = es.enter_context(nc.semaphore("s_cc"))
        self.semobj["cc"] = self.ccsem
        self.ccn = 0
        self.rt = {}
        for e in ENGS:
            self.semobj["c_" + e] = self.sem[e]
        for e in self.dsem:
            for i in range(NDMA_SEMS):
                self.semobj[f"d_{e}{i}"] = self.dsem[e][i]

    def _need(self, eng, tok, waits):
        if tok is None:
            return
        sk, val, teng = tok
        if teng == "pe" and eng == "pe" and sk == "c_pe":
            return
        if self.known[eng].get(sk, 0) >= val:
            return
        self.known[eng][sk] = val
        waits[sk] = max(waits.get(sk, 0), val)

    def _deps(self, eng, reads, writes):
        waits = {}
        for k in reads:
            self._need(eng, self.last_w.get(k), waits)
        for k in writes:
            self._need(eng, self.last_w.get(k), waits)
            for t in self.readers.get(k, ()):
                self._need(eng, t, waits)
        return waits

    def _commit(self, tok, reads, writes):
        for k in reads:
            self.readers.setdefault(k, []).append(tok)
        for k in writes:
            self.last_w[k] = tok
            self.readers[k] = []

    def op(self, eng, emit, reads=(), writes=()):
        waits = self._deps(eng, reads, writes)
        self.cnt[eng] += 1
        tok = ("c_" + eng, self.cnt[eng], eng)
        self.ops[eng].append((waits, emit, (self.sem[eng], 1)))
        self._commit(tok, reads, writes)
        return tok

    def dma(self, eng, emit, reads=(), writes=()):
        waits = self._deps(eng, reads, writes)
        i = self.drr[eng]
        self.drr[eng] = (i + 1) % NDMA_SEMS
        sk = f"d_{eng}{i}"
        prev = self.dval[eng][i]
        if prev and self.known[eng].get(sk, 0) < prev:
            self.known[eng][sk] = prev
            waits[sk] = prev
        self.dval[eng][i] = prev + 16
        tok = (sk, prev + 16, eng)
        self.ops[eng].append((waits, emit, (self.dsem[eng][i], 16)))
        self._commit(tok, reads, writes)
        return tok

    def barrier(self):
        for eng in ENGS:
            waits = {}
            for e in ENGS:
                if e != eng and self.cnt[e]:
                    self._need(eng, ("c_" + e, self.cnt[e], e), waits)
            for e in self.dsem:
                for i in range(NDMA_SEMS):
                    if self.dval[e][i]:
                        self._need(eng, (f"d_{e}{i}", self.dval[e][i], "dma"), waits)
            if eng != "pe" and self.cnt[eng]:
                self._need(eng, ("c_" + eng, self.cnt[eng], eng), waits)
            if waits:
                self.ops[eng].append((waits, None, None))
        self.last_w = {}
        self.readers = {}

    def collective(self, emits):
        self.barrier()
        for emit in emits:
            w = {"cc": self.ccn} if self.ccn else {}
            self.ccn += 1
            self.ops["pool"].append((w, emit, (self.ccsem, 1)))
        for eng in ENGS:
            self.known[eng]["cc"] = self.ccn
            self.ops[eng].append(({"cc": self.ccn}, None, None))

    def collective_async(self, emit, reads=()):
        waits = {}
        for k in reads:
            self._need("pool", self.last_w.get(k), waits)
        if self.ccn:
            waits["cc"] = self.ccn
        self.ccn += 1
        self.ops["pool"].append((waits, emit, (self.ccsem, 1)))

    def cc_wait_all(self):
        self.barrier()
        for eng in ENGS:
            if self.known[eng].get("cc", 0) < self.ccn:
                self.known[eng]["cc"] = self.ccn
                self.ops[eng].append(({"cc": self.ccn}, None, None))

    def finish(self):
        self.barrier()
        semobj = self.semobj

        def run(engobj, lst):
            for waits, emit, inc in lst:
                for sk, v in waits.items():
                    engobj.wait_ge(semobj[sk], v)
                if emit is not None:
                    emit(engobj).then_inc(inc[0], inc[1])

        with self.nc.Block() as block:
            @block.tensor
            def _(e):
                run(e, self.ops["pe"])

            @block.scalar
            def _(e):
                self.rt["hp_act"] = e.partition_id() % 2
                run(e, self.ops["act"])

            @block.vector
            def _(e):
                run(e, self.ops["dve"])

            @block.gpsimd
            def _(e):
                run(e, self.ops["pool"])

            @block.sync
            def _(e):
                self.rt["hp_sp"] = e.partition_id() % 2
                run(e, self.ops["sp"])


class KB:
    def __init__(self, arena_cols=50000):
        self.nc = bass.Bass("TRN2", target_bir_lowering=False)
        self.es = ExitStack()
        self.S = Sched(self.nc, self.es)
        self.arena = self.es.enter_context(self.nc.sbuf_tensor("arena", [128, arena_cols], F32))
        self.acols = arena_cols
        self.top = 0
        self.psb = [self.es.enter_context(self.nc.psum_tensor(f"psb{i}", [128, 512], F32)) for i in range(8)]
        self.uid = 0

    def dint(self, name, shape, dt=F32):
        return self.nc.dram_tensor(name, list(shape), dt).ap()

    def din(self, name, shape, dt=F32):
        return self.nc.dram_tensor(name, list(shape), dt, kind="ExternalInput").ap()

    def dout(self, name, shape, dt=F32):
        return self.nc.dram_tensor(name, list(shape), dt, kind="ExternalOutput").ap()

    def alloc(self, cols, dt=F32):
        n32 = cols if dt != BF16 else (cols + 1) // 2
        a = self.top
        self.top += n32
        assert self.top <= self.acols, f"arena overflow {self.top}"
        v = self.arena[:, a:a + n32]
        if dt == BF16:
            v = v.bitcast(BF16)
        elif dt == I32:
            v = v.bitcast(I32)
        return v

    def mark(self):
        return self.top

    def release(self, m):
        self.top = m

    def key(self, base="k"):
        self.uid += 1
        return f"{base}{self.uid}"

    def close(self):
        self.S.finish()
        self.es.close()
        return self.nc


NCOL = 2300
NCOLP = 1920
NCOLW = NCOLP + 384
VCOLS = [(NCOLP, NCOLW)]
NV = 384
OFF = dict(a_x=0, a_gate=256, b_q=512, b_kv=768, c_q=1024, c_k=1152, c_og=1280, d_cq=1536, d_kr=1728, c_glr=1760,
           b_gate=1776, d_ckv=1792)


def perm_cols():
    o = dict(a_x=0, a_gate=256, b_q=512, b_kv=768, b_gate=1152, c_q=1164, c_k=1292, c_v=1420, c_glr=1676, c_og=1692,
             d_cq=1948, d_ckv=2140, d_kr=2268)
    r = lambda a, n: list(range(a, a + n))
    p = (r(o["a_x"], 256) + r(o["a_gate"], 256) + r(o["b_q"], 256)
         + r(o["b_kv"], 64) + r(o["b_kv"] + 64, 64) + r(o["b_kv"] + 128, 64) + r(o["b_kv"] + 256, 64)
         + r(o["c_q"], 128) + r(o["c_k"], 128) + r(o["c_og"], 256) + r(o["d_cq"], 192) + r(o["d_kr"], 32)
         + r(o["c_glr"], 16) + r(o["b_gate"], 12) + r(o["b_gate"], 4) + r(o["d_ckv"], 128))
    assert len(p) == NCOLP
    p += r(o["b_kv"] + 192, 64) + r(o["b_kv"] + 320, 64) + r(o["c_v"], 256)
    assert len(p) == NCOLW
    return np.array(p)
DFF = 2816


def load_cast_weight(kb, w_dram, wsb, kchunks, ncols, stage, key, piece=1024):
    S = kb.S
    i = 0
    for c in range(kchunks):
        for c0 in range(0, ncols, piece):
            c1 = min(ncols, c0 + piece)
            st = stage[i % 2]
            sk = f"wstage{i % 2}"
            S.dma("sp", lambda e, st=st, c=c, c0=c0, c1=c1: e.dma_start(out=st[:, 0:c1 - c0], in_=w_dram[c * 128:(c + 1) * 128, c0:c1]),
                  writes=[sk])
            eng = "act" if i % 2 == 0 else "pool"
            if eng == "act":
                S.op("act", lambda e, st=st, c=c, c0=c0, c1=c1: e.copy(out=wsb[:, c, c0:c1], in_=st[:, 0:c1 - c0]), reads=[sk], writes=[key])
            else:
                S.op("pool", lambda e, st=st, c=c, c0=c0, c1=c1: e.tensor_copy(out=wsb[:, c, c0:c1], in_=st[:, 0:c1 - c0]), reads=[sk], writes=[key])
            i += 1


def rms_stats(kb, src3, nch, T, sq, ones_bf, ps_ap, rstd, denom, keys_in, key_sq, key_ps, key_rstd):
    S = kb.S
    S.op("act", lambda e: e.activation(out=sq, in_=src3, func=AF.Square), reads=keys_in, writes=[key_sq])
    for c in range(nch):
        S.op("pe", lambda e, c=c: e.matmul(ps_ap, lhsT=ones_bf, rhs=sq[:, c, :], start=(c == 0), stop=(c == nch - 1)),
             reads=[key_sq, "ones_bf"], writes=[key_ps])
    S.op("act", lambda e: e.activation(out=rstd, in_=ps_ap, func=AF.Sqrt, bias=EPS, scale=1.0 / denom), writes=[key_ps, key_rstd])
    S.op("dve", lambda e: e.reciprocal(out=rstd, in_=rstd), reads=[key_rstd], writes=[key_rstd])


def phase_A(kb, xT, gain, w, pT, pV, GP, GV, ag):
    S = kb.S
    m0 = kb.mark()
    T = 512
    NT = 8
    xv = xT.rearrange("(c p) t -> p c t", p=128)

    wsb = kb.alloc(8 * NCOLW, BF16).rearrange("p (c n) -> p c n", c=8)
    gsb = kb.alloc(8)
    ones_bf = kb.alloc(128, BF16)
    stage = [kb.alloc(1024), kb.alloc(1024)]
    xt = [kb.alloc(8 * T).rearrange("p (c t) -> p c t", c=8) for _ in range(2)]
    sq = kb.alloc(8 * T, BF16).rearrange("p (c t) -> p c t", c=8)
    hb = kb.alloc(8 * 4096, BF16).rearrange("p (c t) -> p c t", c=8)
    rstd = kb.alloc(T)
    ost = [kb.alloc(T) for _ in range(4)]
    P = [p[:] for p in kb.psb]

    S.dma("sp", lambda e: e.dma_start(out=gsb, in_=gain[:, :]), writes=["gsb"])
    S.op("pool", lambda e: e.memset(ones_bf, 1.0), writes=["ones_bf"])
    S.dma("sp", lambda e: e.dma_start(out=xt[0], in_=xv[:, :, 0:T]), writes=["xt0"])
    load_cast_weight(kb, w, wsb, 8, NCOLW, stage, "wsb")
    for t in range(NT):
        b = t % 2
        if t + 1 < NT:
            S.dma("sp", lambda e, t=t: e.dma_start(out=xt[(t + 1) % 2], in_=xv[:, :, (t + 1) * T:(t + 2) * T]),
                  writes=[f"xt{(t + 1) % 2}"])
        rms_stats(kb, xt[b], 8, T, sq, ones_bf, P[0], rstd, 1024.0, [f"xt{b}"], "sq", "psb0", "rstd")
        for c in range(8):
            S.op("dve", lambda e, c=c, b=b, t=t: e.scalar_tensor_tensor(out=hb[:, c, t * T:(t + 1) * T], in0=xt[b][:, c, :], scalar=gsb[:, c:c + 1], in1=rstd,
                                                                   op0=ALU.mult, op1=ALU.mult),
                 reads=[f"xt{b}", "rstd", "gsb"], writes=[f"hb{t}"])
    oi = 0
    for t in range(NT):
        for tb in range(4):
            pb = 5 + (tb % 2)
            for c in range(8):
                S.op("pe", lambda e, c=c, t=t, tb=tb, pb=pb: e.matmul(
                    P[pb][:, 0:NV], lhsT=hb[:, c, t * T + tb * 128:t * T + (tb + 1) * 128], rhs=wsb[:, c, NCOLP:NCOLW], start=(c == 0), stop=(c == 7)),
                    reads=[f"hb{t}", "wsb"], writes=[f"psb{pb}"])
            o = oi % 4
            oi += 1
            S.op("act", lambda e, o=o, pb=pb: e.copy(out=ost[o][:, 0:NV], in_=P[pb][:, 0:NV]), writes=[f"psb{pb}", f"ost{o}"])
            S.dma("sp", lambda e, o=o, t=t, tb=tb: e.dma_start(out=pV[t * T + tb * 128: t * T + (tb + 1) * 128, :], in_=ost[o][:, 0:NV]),
                  reads=[f"ost{o}"], writes=[f"XV{t}_{tb}"])
        if t % 2 == 1:
            j = t // 2
            S.collective_async(ag(pV[j * 1024:(j + 1) * 1024, :], GV[j * 2048:(j + 1) * 2048, :]),
                               reads=[f"XV{tt}_{tb}" for tt in (t - 1, t) for tb in range(4)])
    for k in range(NCOLP // 128):
        c0, c1 = k * 128, (k + 1) * 128
        for t in range(NT):
            pb = 1 + (oi % 4)
            for c in range(8):
                S.op("pe", lambda e, c=c, t=t, c0=c0, c1=c1, pb=pb: e.matmul(P[pb][:, :], lhsT=wsb[:, c, c0:c1], rhs=hb[:, c, t * T:(t + 1) * T],
                                                                             start=(c == 0), stop=(c == 7)),
                     reads=[f"hb{t}", "wsb"], writes=[f"psb{pb}"])
            o = oi % 4
            oi += 1
            if t % 2 == 0:
                S.op("act", lambda e, o=o, pb=pb: e.copy(out=ost[o], in_=P[pb][:, :]), writes=[f"psb{pb}", f"ost{o}"])
            else:
                S.op("dve", lambda e, o=o, pb=pb: e.tensor_copy(out=ost[o], in_=P[pb][:, :]), writes=[f"psb{pb}", f"ost{o}"])
            S.dma("sp", lambda e, o=o, c0=c0, c1=c1, t=t: e.dma_start(out=pT[c0:c1, t * T:(t + 1) * T], in_=ost[o]),
                  reads=[f"ost{o}"], writes=[f"XA{k}_{t}"])
        S.collective_async(ag(pT[c0:c1, :], GP[k * 256:(k + 1) * 256, :]), reads=[f"XA{k}_{t}" for t in range(NT)])
    S.cc_wait_all()
    kb.release(m0)


def phase_C(kb, final, xT, GY, gng, nfg, fing, w_out, w_gu, w_dn, xo, NT=16):
    S = kb.S
    m0 = kb.mark()
    T = 256
    xv = xT.rearrange("(c p) t -> p c t", p=128)
    ov = xo.rearrange("(c p) t -> p c t", p=128)

    wo = kb.alloc(8 * 1024, BF16).rearrange("p (c n) -> p c n", c=8)
    wgu = kb.alloc(8 * 2 * DFF, BF16).rearrange("p (c n) -> p c n", c=8)
    wdn = kb.alloc(22 * 1024, BF16).rearrange("p (c n) -> p c n", c=22)
    g3 = kb.alloc(24)
    ones_bf = kb.alloc(128, BF16)
    xts = [kb.alloc(8 * T).rearrange("p (c t) -> p c t", c=8) for _ in range(2)]
    yt = kb.alloc(8 * T).rearrange("p (c t) -> p c t", c=8)
    ytf = yt.rearrange("p c t -> p (c t)")
    stage = [ytf[:, 0:1024], ytf[:, 1024:2048]]
    sq = kb.alloc(8 * T, BF16).rearrange("p (c t) -> p c t", c=8)
    hb = kb.alloc(8 * T, BF16).rearrange("p (c t) -> p c t", c=8)
    aT = kb.alloc(22 * T, BF16).rearrange("p (c t) -> p c t", c=22)
    rstd4 = kb.alloc(4 * T).rearrange("p (c t) -> p c t", c=4)
    rstd = kb.alloc(T)
    sg = [kb.alloc(T), kb.alloc(T)]
    P = [p[:] for p in kb.psb]

    S.dma("sp", lambda e: e.dma_start(out=g3[:, 0:8], in_=gng[:, :]), writes=["g3"])
    S.dma("sp", lambda e: e.dma_start(out=g3[:, 8:16], in_=nfg[:, :]), writes=["g3"])
    S.dma("sp", lambda e: e.dma_start(out=g3[:, 16:24], in_=fing[:, :]), writes=["g3"])
    S.op("pool", lambda e: e.memset(ones_bf, 1.0), writes=["ones_bf"])
    load_cast_weight(kb, w_out, wo, 8, 1024, stage, "wo")
    load_cast_weight(kb, w_gu, wgu, 8, 2 * DFF, stage, "wgu")
    load_cast_weight(kb, w_dn, wdn, 22, 1024, stage, "wdn")
    S.barrier()

    def load_y(t):
        for cc_ in range(8):
            r0 = cc_ * 128
            S.dma("sp", lambda e, cc_=cc_, r0=r0, t=t: e.dma_start(out=yt[:, cc_, :], in_=GY[r0:r0 + 128, t * T:(t + 1) * T]),
                  writes=["yt"])

    S.dma("sp", lambda e: e.dma_start(out=xts[0], in_=xv[:, :, 0:T]), writes=["xt0"])
    load_y(0)
    for t in range(NT):
        ts = slice(t * T, (t + 1) * T)
        xt = xts[t % 2]
        XK = f"xt{t % 2}"
        if t + 1 < NT:
            S.dma("sp", lambda e, t=t: e.dma_start(out=xts[(t + 1) % 2], in_=xv[:, :, (t + 1) * T:(t + 2) * T]), writes=[f"xt{(t + 1) % 2}"])
        S.op("act", lambda e: e.activation(out=sq, in_=yt, func=AF.Square), reads=["yt"], writes=["sq"])
        for g in range(4):
            pa = P[g][:, 0:T]
            for j in range(2):
                S.op("pe", lambda e, g=g, j=j, pa=pa: e.matmul(pa, lhsT=ones_bf, rhs=sq[:, 2 * g + j, :], start=(j == 0), stop=(j == 1)),
                     reads=["sq", "ones_bf"], writes=[f"psb{g}"])
            S.op("act", lambda e, g=g, pa=pa: e.activation(out=rstd4[:, g, :], in_=pa, func=AF.Sqrt, bias=EPS, scale=1.0 / 256.0),
                 writes=[f"psb{g}", f"rstd4_{g}"])
            S.op("dve", lambda e, g=g: e.reciprocal(out=rstd4[:, g, :], in_=rstd4[:, g, :]), reads=[f"rstd4_{g}"], writes=[f"rstd4_{g}"])
        for c in range(8):
            eng = "dve"
            S.op(eng, lambda e, c=c: e.scalar_tensor_tensor(out=hb[:, c, :], in0=yt[:, c, :], scalar=g3[:, c:c + 1], in1=rstd4[:, c // 2, :],
                                                         op0=ALU.mult, op1=ALU.mult),
                 reads=["yt", f"rstd4_{c // 2}", "g3"], writes=[f"hb{c}"])
        if t + 1 < NT:
            load_y(t + 1)
        for m in range(8):
            pa = P[4 + m % 4][:, 0:T]
            for c in range(8):
                S.op("pe", lambda e, m=m, c=c, pa=pa: e.matmul(pa, lhsT=wo[:, c, m * 128:(m + 1) * 128], rhs=hb[:, c, :], start=(c == 0), stop=(c == 7)),
                     reads=[f"hb{c}", "wo"], writes=[f"psb{4 + m % 4}"])
            S.op("dve", lambda e, m=m, pa=pa, xt=xt: e.tensor_tensor(out=xt[:, m, :], in0=xt[:, m, :], in1=pa, op=ALU.add),
                 reads=[XK], writes=[XK, f"psb{4 + m % 4}"])
        rms_stats(kb, xt, 8, T, sq, ones_bf, P[0][:, 0:T], rstd, 1024.0, [XK], "sq", "psb0", "rstd")
        for c in range(8):
            eng = "dve"
            S.op(eng, lambda e, c=c, xt=xt: e.scalar_tensor_tensor(out=hb[:, c, :], in0=xt[:, c, :], scalar=g3[:, 8 + c:9 + c], in1=rstd,
                                                         op0=ALU.mult, op1=ALU.mult),
                 reads=[XK, "rstd", "g3"], writes=[f"hb{c}"])
        for j in range(22):
            pg = P[j % 2][:, 0:T]
            pu = P[2 + j % 2][:, 0:T]
            for c in range(8):
                S.op("pe", lambda e, j=j, c=c, pg=pg: e.matmul(pg, lhsT=wgu[:, c, j * 128:(j + 1) * 128], rhs=hb[:, c, :], start=(c == 0), stop=(c == 7)),
                     reads=[f"hb{c}", "wgu"], writes=[f"psb{j % 2}"])
            for c in range(8):
                S.op("pe", lambda e, j=j, c=c, pu=pu: e.matmul(pu, lhsT=wgu[:, c, DFF + j * 128:DFF + (j + 1) * 128], rhs=hb[:, c, :], start=(c == 0), stop=(c == 7)),
                     reads=[f"hb{c}", "wgu"], writes=[f"psb{2 + j % 2}"])
            S.op("act", lambda e, j=j, pg=pg: e.activation(out=sg[j % 2], in_=pg, func=AF.Silu), writes=[f"psb{j % 2}", f"sg{j % 2}"])
            S.op("dve", lambda e, j=j, pu=pu: e.tensor_tensor(out=aT[:, j, :], in0=sg[j % 2], in1=pu, op=ALU.mult),
                 reads=[f"sg{j % 2}"], writes=[f"aT{j}", f"psb{2 + j % 2}"])
        for m in range(8):
            pa = P[4 + m % 4][:, 0:T]
            for k in range(22):
                S.op("pe", lambda e, m=m, k=k, pa=pa: e.matmul(pa, lhsT=wdn[:, k, m * 128:(m + 1) * 128], rhs=aT[:, k, :], start=(k == 0), stop=(k == 21)),
                     reads=[f"aT{k}", "wdn"], writes=[f"psb{4 + m % 4}"])
            S.op("dve", lambda e, m=m, pa=pa, xt=xt: e.tensor_tensor(out=xt[:, m, :], in0=xt[:, m, :], in1=pa, op=ALU.add),
                 reads=[XK], writes=[XK, f"psb{4 + m % 4}"])
        if final:
            rms_stats(kb, xt, 8, T, sq, ones_bf, P[0][:, 0:T], rstd, 1024.0, [XK], "sq", "psb0", "rstd")
            for c in range(8):
                eng = "dve"
                S.op(eng, lambda e, c=c, xt=xt: e.scalar_tensor_tensor(out=xt[:, c, :], in0=xt[:, c, :], scalar=g3[:, 16 + c:17 + c], in1=rstd,
                                                                    op0=ALU.mult, op1=ALU.mult),
                     reads=["rstd", "g3"], writes=[XK])
            S.dma("sp", lambda e, ts=ts, xt=xt: e.dma_start(out=ov[:, :, ts], in_=xt), reads=[XK])
        else:
            S.dma("sp", lambda e, ts=ts, xt=xt: e.dma_start(out=ov[:, :, ts], in_=xt), reads=[XK])
    S.barrier()
    kb.release(m0)


N = 8192
TQ = 512
NQT = N // TQ
NEG = -30000.0
PI = float(np.pi)
TWO_PI = float(2 * np.pi)
THETA = 10000.0


def pk(b):
    return f"psb{b}"


def host_consts():
    c = {}
    c["ident"] = np.eye(128, dtype=np.float32)
    R = np.zeros((128, 128), np.float32)
    for blk in range(2):
        for m in range(64):
            if m < 32:
                R[blk * 64 + m + 32, blk * 64 + m] = -1.0
            else:
                R[blk * 64 + m - 32, blk * 64 + m] = 1.0
    c["rbd"] = R
    R32 = np.zeros((128, 32), np.float32)
    for m in range(32):
        if m < 16:
            R32[64 + m + 16, m] = -1.0
        else:
            R32[64 + m - 16, m] = 1.0
    c["r32"] = R32
    invf = np.zeros((128, 2), np.float32)
    for p in range(128):
        invf[p, 0] = np.float32(THETA) ** np.float32(-(2.0 * ((p % 64) % 32)) / 64.0)
    for p in range(64, 96):
        invf[p, 1] = np.float32(THETA) ** np.float32(-(2.0 * ((p - 64) % 16)) / 32.0)
    c["invf"] = invf
    k = np.arange(128)[:, None]
    q = np.arange(512)[None, :]
    c["tri"] = np.stack([np.where(k + i * 128 <= q, 0.0, NEG) for i in range(4)]).astype(np.float32)
    c["win"] = np.stack([np.where((k + (i - 4) * 128 <= q) & (k + (i - 4) * 128 > q - 512), 0.0, NEG) for i in range(8)]).astype(np.float32)
    c["cmpm"] = np.stack([np.where(16 * k + 31 <= i * 512 + q, 0.0, NEG) for i in range(5)]).astype(np.float32)
    n = np.arange(512)
    s = np.arange(128)
    ov = ((n[:, None] * 16 < s[None, :] * 64 + 64) & (n[:, None] * 16 + 32 > s[None, :] * 64)).astype(np.float32)
    ov[511] = 0.0
    ovl = np.zeros((4, 128, 129), np.float32)
    ovl[:, :, :128] = ov.reshape(4, 128, 128)
    ovl[:, :, 128] = 1.0
    c["ovl"] = ovl
    c["eall"] = (np.arange(N)[None, :] // 64 == np.arange(128)[:, None]).astype(np.float32)
    ql = np.arange(128)[:, None] // 64
    j = np.arange(254)[None, :] - 126
    c["bv"] = (j <= ql - 2).astype(np.float32)
    c["bf"] = (np.where((j == ql) | (j == ql - 1), 1e9, 0.0) + np.where(j > ql, -1.0, 0.0)).astype(np.float32)
    selg = np.zeros((8, 6 * 64), np.float32)
    for r in range(6):
        selg[r, r * 64:(r + 1) * 64] = 1.0
    c["selg"] = selg
    c["tri64"] = (np.arange(64)[:, None] <= np.arange(64)[None, :]).astype(np.float32)
    return c


CONST_SHAPES = {"ident": [128, 128], "rbd": [128, 128], "r32": [128, 32], "invf": [128, 2], "tri": [4, 128, 512],
                "win": [8, 128, 512], "cmpm": [5, 128, 512], "ovl": [4, 128, 129], "eall": [128, N], "bv": [128, 254],
                "bf": [128, 254], "selg": [8, 384], "tri64": [64, 64]}

IN_SHAPES = {
    "pos": ([1, N], I32),
    "lru_x": ([2, 128, N], F32), "lru_w": ([2, 2, 64, 64], F32), "lru_v": ([128, 8], F32),
    "mla_cq": ([192, N], F32), "mla_ckv": ([128, N], F32), "mla_kr": ([32, N], F32),
    "mla_wuq": ([192, 192], F32), "mla_wk": ([128, 128], F32), "mla_wv": ([128, 128], F32), "mla_v": ([128, 3], F32),
    "nsa_q": ([2, 128, N], F32), "nsa_k": ([3, 64, N], F32), "nsa_vc": ([64, N], F32), "nsa_vs": ([N, 64], F32),
    "nsa_vw": ([N, 64], F32), "nsa_g": ([6, N], F32), "nsa_pos": ([128, 32], F32), "nsa_w1": ([2, 2048, 256], F32),
    "nsa_b1": ([128, 4], F32), "nsa_w2": ([2, 256, 64], F32),
    "gla_q": ([64, N], F32), "gla_k": ([64, N], F32), "gla_v": ([N, 128], F32), "gla_glr": ([16, N], F32),
    "gla_og": ([128, N], F32), "gla_wg2": ([16, 64], F32), "gla_v2": ([128, 2], F32),
}


class Ctx:
    pass


class YView:
    def __init__(self, ap):
        self.ap = ap

    def __getitem__(self, key):
        rs, cs = key
        half = cs.start // 4096
        assert (cs.stop - 1) // 4096 == half
        return self.ap[half * 512 + rs.start:half * 512 + rs.stop, cs.start - half * 4096:cs.stop - half * 4096]


SEL = [("a_x", 128, False, 128), ("a_gate", 128, False, 128), ("b_q", 128, False, 128), ("b_q", 128, True, 128),
       ("b_gate", 6, False, 6), ("c_q", 64, False, 64), ("c_k", 64, False, 64), ("c_og", 128, False, 128)]
SELROWS = sum(x[3] for x in SEL)


class Loader:
    def __init__(self, S, GP, GV, MYP, MYV):
        self.S = S
        self.GP, self.GV, self.MYP, self.MYV = GP, GV, MYP, MYV
        self.row0 = {}
        r = 0
        for nm, hpm, inv, n in SEL:
            self.row0[(nm, inv)] = r
            r += n

    @staticmethod
    def gprow(r, half):
        return (r // 128) * 256 + half * 128 + r % 128

    @staticmethod
    def gvrow(t):
        half, tl = t // 4096, t % 4096
        return (tl // 1024) * 2048 + half * 1024 + tl % 1024

    def select(self):
        S = self.S
        i = 0
        for nm, hpm, inv, n in SEL:
            r0 = self.row0[(nm, inv)]
            mult = 256 if hpm == 128 else hpm
            for hf in range(2):
                q = "sp" if i % 2 == 0 else "act"
                i += 1

                def emit(e, nm=nm, mult=mult, inv=inv, n=n, r0=r0, hf=hf, q=q):
                    hp = S.rt["hp_" + q]
                    start = ((1 - hp) if inv else hp) * mult + self.gprow(OFF[nm], hf)
                    return e.dma_start(out=self.MYP[r0:r0 + n, hf * 4096:(hf + 1) * 4096], in_=self.GP[bass.ds(start, n), :])
                S.dma(q, emit, writes=[f"MYP{i}"])
        for j in range(4):
            S.dma("sp", lambda e, j=j: e.dma_start(out=self.MYV[j * 2048:(j + 1) * 2048, :],
                                                   in_=self.GV[:, bass.ds(S.rt["hp_sp"] * 128 + 128, 128)][j * 2048:(j + 1) * 2048, :]), writes=[f"MYV{j}"])
        S.barrier()

    def pt(self, off, hpm, n, c0, c1, inv=False):
        if hpm == 0:
            half = c0 // 4096
            assert off // 128 == (off + n - 1) // 128
            base = self.gprow(off, half)
            return self.GP[base:base + n, c0 - half * 4096:c1 - half * 4096]
        for nm, hm, iv, nn in SEL:
            if hm == hpm and iv == inv and OFF[nm] <= off and (off - OFF[nm]) + n <= nn:
                r0 = self.row0[(nm, inv)] + (off - OFF[nm])
                return self.MYP[r0:r0 + n, c0:c1]
        raise KeyError((off, hpm, n, inv))

    def pv(self, coff, hpm, n, t0, t1):
        assert t0 // 1024 == (t1 - 1) // 1024
        g0 = self.gvrow(t0)
        if hpm == 0:
            return self.GV[g0:g0 + (t1 - t0), coff:coff + n]
        assert coff == 128 and n == 128
        return self.MYV[g0:g0 + (t1 - t0), :]


def common_setup(kb, D):
    S = kb.S
    c = Ctx()
    c.ident = kb.alloc(128)
    c.ones_f = kb.alloc(128)
    c.ones_bf = kb.alloc(128, BF16)
    c.ident_bf = kb.alloc(128, BF16)
    S.dma("sp", lambda e: e.dma_start(out=c.ident, in_=D["ident"][:, :]), writes=["ident"])
    S.op("pool", lambda e: e.memset(c.ones_f, 1.0), writes=["ones_f"])
    S.op("pool", lambda e: e.memset(c.ones_bf, 1.0), writes=["ones_bf"])
    S.op("act", lambda e: e.copy(out=c.ident_bf, in_=c.ident), reads=["ident"], writes=["ident_bf"])
    c.invf = kb.alloc(2)
    S.dma("sp", lambda e: e.dma_start(out=c.invf, in_=D["invf"][:, :]), writes=["invf"])
    return c


def rope_tables(kb, c, posf, posk, r0, r1, col, T, bank, tag):
    S = kb.S
    P = kb.psb[bank]
    n = r1 - r0
    rs = slice(r0, r1)
    a, kf, ki, sn, cs = T["ang"], T["kf"], T["ki"], T["sin"], T["cos"]
    S.op("pe", lambda e: e.matmul(P[rs, :], lhsT=c.ones_f[0:1, 0:n], rhs=posf[0:1, :], start=True, stop=True),
         reads=["ones_f", posk], writes=[pk(bank)])
    S.op("dve", lambda e: e.tensor_scalar(out=a[rs, :], in0=P[rs, :], scalar1=c.invf[rs, col:col + 1], scalar2=None, op0=ALU.mult),
         reads=["invf"], writes=[pk(bank), tag + "ang"])
    S.op("dve", lambda e: e.tensor_scalar(out=ki[rs, :], in0=a[rs, :], scalar1=1.0 / TWO_PI, scalar2=None, op0=ALU.mult),
         reads=[tag + "ang"], writes=[tag + "ki"])
    S.op("dve", lambda e: e.tensor_copy(out=kf[rs, :], in_=ki[rs, :]), reads=[tag + "ki"], writes=[tag + "kf"])
    S.op("dve", lambda e: e.scalar_tensor_tensor(out=a[rs, :], in0=kf[rs, :], scalar=-TWO_PI, in1=a[rs, :], op0=ALU.mult, op1=ALU.add),
         reads=[tag + "kf"], writes=[tag + "ang"])
    S.op("dve", lambda e: e.tensor_scalar(out=kf[rs, :], in0=a[rs, :], scalar1=PI, scalar2=-TWO_PI, op0=ALU.is_gt, op1=ALU.mult),
         reads=[tag + "ang"], writes=[tag + "kf"])
    S.op("dve", lambda e: e.tensor_tensor(out=sn[rs, :], in0=a[rs, :], in1=kf[rs, :], op=ALU.add),
         reads=[tag + "ang", tag + "kf"], writes=[tag + "sin"])
    S.op("dve", lambda e: e.tensor_scalar(out=a[rs, :], in0=a[rs, :], scalar1=PI / 2, scalar2=None, op0=ALU.add),
         reads=[], writes=[tag + "ang"])
    S.op("dve", lambda e: e.tensor_scalar(out=kf[rs, :], in0=a[rs, :], scalar1=PI, scalar2=-TWO_PI, op0=ALU.is_gt, op1=ALU.mult),
         reads=[tag + "ang"], writes=[tag + "kf"])
    S.op("dve", lambda e: e.tensor_tensor(out=cs[rs, :], in0=a[rs, :], in1=kf[rs, :], op=ALU.add),
         reads=[tag + "ang", tag + "kf"], writes=[tag + "cos"])
    S.op("act", lambda e: e.activation(out=sn[rs, :], in_=sn[rs, :], func=AF.Sin), writes=[tag + "sin"])
    S.op("act", lambda e: e.activation(out=cs[rs, :], in_=cs[rs, :], func=AF.Sin), writes=[tag + "cos"])


def alloc_tables(kb):
    return {"ang": kb.alloc(512), "kf": kb.alloc(512), "ki": kb.alloc(512, I32), "sin": kb.alloc(512), "cos": kb.alloc(512)}


def load_pos(kb, D, posi, posf, t):
    S = kb.S
    S.dma("sp", lambda e: e.dma_start(out=posi[0:1, :], in_=D["pos"][0:1, t * TQ:(t + 1) * TQ]), writes=["posi"])
    S.op("dve", lambda e: e.tensor_copy(out=posf[0:1, :], in_=posi[0:1, :]), reads=["posi"], writes=["posf"])


class Attn:
    def __init__(self, kb, sbanks=(0, 1), npt=3):
        self.kb = kb
        self.sbanks = sbanks
        self.PT = [kb.alloc(512, BF16) for _ in range(npt)]
        self.pti = 0
        self.si = 0

    def run(self, blocks, obank, defer=None):
        kb = self.kb
        S = kb.S
        n = len(blocks)
        O = kb.psb[obank]
        banks = []

        def scores(i):
            bank = self.sbanks[self.si % len(self.sbanks)]
            self.si += 1
            banks.append(bank)
            mms = blocks[i][0]
            for j, (l, r, ks) in enumerate(mms):
                S.op("pe", lambda e, l=l, r=r, j=j, bank=bank, nm=len(mms): e.matmul(kb.psb[bank][:, :], lhsT=l, rhs=r, start=(j == 0), stop=(j == nm - 1)),
                     reads=ks, writes=[pk(bank)])

        scores(0)
        for i in range(n):
            if i + 1 < n:
                scores(i + 1)
            bank = banks[i]
            pi_ = self.pti % len(self.PT)
            self.pti += 1
            pt = self.PT[pi_]
            S.op("act", lambda e, pt=pt, bank=bank: e.activation(out=pt, in_=kb.psb[bank][:, :], func=AF.Exp), writes=[pk(bank), f"PT{pi_}"])
            v, vk = blocks[i][1], blocks[i][2]
            S.op("pe", lambda e, v=v, pt=pt, i=i: e.matmul(O[0:128, :], lhsT=v, rhs=pt, start=(i == 0), stop=(i == n - 1)),
                 reads=[f"PT{pi_}"] + vk, writes=[pk(obank)])
            if defer and i == min(2, n - 1):
                for f in defer:
                    f()
                del defer[:]


def norm_coef(kb, c, obank, rowbuf, bcbank, bcs):
    S = kb.S
    O = kb.psb[obank]
    B = kb.psb[bcbank]
    S.op("dve", lambda e: e.tensor_scalar_max(out=rowbuf[64:65, :], in0=O[64:65, :], scalar1=1e-30), writes=[pk(obank), "rowbuf"])
    S.op("dve", lambda e: e.reciprocal(out=rowbuf[64:65, :], in_=rowbuf[64:65, :]), writes=["rowbuf"])
    S.op("pe", lambda e: e.matmul(B[0:64, :], lhsT=c.ones_f[64:65, 0:64], rhs=rowbuf[64:65, :], start=True, stop=True),
         reads=["rowbuf", "ones_f"], writes=[pk(bcbank)])
    S.op("act", lambda e: e.copy(out=bcs[0:64, :], in_=B[0:64, :]), writes=[pk(bcbank), "bcs"])


def part_lru(kb, c, D, yT, L):
    S = kb.S
    m0 = kb.mark()
    xa = kb.alloc(N + 4)
    xc = kb.alloc(N)
    A = kb.alloc(N)
    U = kb.alloc(N)
    G = kb.alloc(N)
    xcb = kb.alloc(N, BF16)
    vec = kb.alloc(16)
    wtmp = kb.alloc(256)
    wbd = kb.alloc(256, BF16)
    T1 = xa[:, 0:N]
    P = kb.psb
    S.op("pool", lambda e: e.memset(xa[:, 0:3], 0.0), writes=["xa_pad"])
    for hf in range(2):
        S.dma("sp", lambda e, hf=hf: e.dma_start(out=xa[:, 3 + hf * 4096:3 + (hf + 1) * 4096], in_=L.pt(OFF["a_x"], 128, 128, hf * 4096, (hf + 1) * 4096)), writes=["xa"])
        S.dma("sp", lambda e, hf=hf: e.dma_start(out=G[:, hf * 4096:(hf + 1) * 4096], in_=L.pt(OFF["a_gate"], 128, 128, hf * 4096, (hf + 1) * 4096)), writes=["G"])
    S.dma("sp", lambda e: e.dma_start(out=vec[:, 0:8], in_=D["lru_v"][:, :]), writes=["vec"])
    S.op("pool", lambda e: e.memset(wtmp, 0.0), writes=["wtmp"])
    for a in range(2):
        for b in range(2):
            S.dma("sp", lambda e, a=a, b=b: e.dma_start(out=wtmp[b * 64:(b + 1) * 64, a * 128 + b * 64:a * 128 + (b + 1) * 64], in_=D["lru_w"][a, b]),
                  writes=["wtmp"])
    S.op("act", lambda e: e.copy(out=wbd, in_=wtmp), reads=["wtmp"], writes=["wbd"])
    S.op("act", lambda e: e.activation(out=vec[:, 8:9], in_=vec[:, 7:8], func=AF.Exp, scale=-1.0), reads=["vec"], writes=["vec8"])
    S.op("act", lambda e: e.activation(out=vec[:, 8:9], in_=vec[:, 8:9], func=AF.Ln, bias=1.0), writes=["vec8"])
    S.op("dve", lambda e: e.tensor_scalar(out=vec[:, 9:10], in0=vec[:, 8:9], scalar1=-8.0, scalar2=None, op0=ALU.mult), reads=["vec8"], writes=["vec9"])
    S.op("dve", lambda e: e.tensor_scalar(out=xc, in0=xa[:, 0:N], scalar1=vec[:, 0:1], scalar2=vec[:, 4:5], op0=ALU.mult, op1=ALU.add),
         reads=["xa", "xa_pad", "vec"], writes=["xc"])
    for j in range(1, 4):
        S.op("dve", lambda e, j=j: e.scalar_tensor_tensor(out=xc, in0=xa[:, j:j + N], scalar=vec[:, j:j + 1], in1=xc, op0=ALU.mult, op1=ALU.add),
             reads=["xa", "xa_pad", "vec"], writes=["xc"])
    S.op("act", lambda e: e.copy(out=xcb, in_=xc), reads=["xc"], writes=["xcb"])
    allA = [f"A{t}" for t in range(16)]
    allU = [f"U{t}" for t in range(16)]
    for t in range(16):
        ts = slice(t * 512, (t + 1) * 512)
        b0, b1 = 2 * (t % 2), 2 * (t % 2) + 1
        S.op("pe", lambda e, ts=ts, b0=b0: e.matmul(P[b0][:, :], lhsT=wbd[:, 0:128], rhs=xcb[:, ts], start=True, stop=True),
             reads=["xcb", "wbd"], writes=[pk(b0)])
        S.op("pe", lambda e, ts=ts, b1=b1: e.matmul(P[b1][:, :], lhsT=wbd[:, 128:256], rhs=xcb[:, ts], start=True, stop=True),
             reads=["xcb", "wbd"], writes=[pk(b1)])
        S.op("act", lambda e, ts=ts, b0=b0: e.activation(out=A[:, ts], in_=P[b0][:, :], func=AF.Sigmoid, bias=vec[:, 5:6]),
             reads=["vec"], writes=[pk(b0), f"A{t}"])
        S.op("act", lambda e, ts=ts, b1=b1: e.activation(out=U[:, ts], in_=P[b1][:, :], func=AF.Sigmoid, bias=vec[:, 6:7]),
             reads=["vec"], writes=[pk(b1), f"U{t}"])
    NP = 4
    W_ = N // NP
    for p_ in range(NP):
        cs = slice(p_ * W_, (p_ + 1) * W_)
        tA = [f"A{t}" for t in range(16) if p_ * W_ <= t * 512 < (p_ + 1) * W_]
        tU = [f"U{t}" for t in range(16) if p_ * W_ <= t * 512 < (p_ + 1) * W_]
        kA, kU, kT, kG, kH = f"Ap{p_}", f"Up{p_}", f"Tp{p_}", f"Gp{p_}", f"Hp{p_}"
        S.op("act", lambda e, cs=cs: e.activation(out=A[:, cs], in_=A[:, cs], func=AF.Exp, scale=vec[:, 9:10]), reads=["vec9"], writes=[kA] + tA)
        S.op("pool", lambda e, cs=cs: e.tensor_tensor(out=T1[:, cs], in0=A[:, cs], in1=A[:, cs], op=ALU.mult), reads=[kA, "xc"], writes=[kT, "xa", "xa_pad"])
        S.op("act", lambda e, cs=cs: e.activation(out=T1[:, cs], in_=T1[:, cs], func=AF.Sqrt, bias=1.0, scale=-1.0), writes=[kT])
        S.op("dve", lambda e, cs=cs: e.tensor_tensor(out=U[:, cs], in0=U[:, cs], in1=xc[:, cs], op=ALU.mult), reads=["xc"], writes=[kU] + tU)
        S.op("dve", lambda e, cs=cs: e.tensor_tensor(out=U[:, cs], in0=U[:, cs], in1=T1[:, cs], op=ALU.mult), reads=[kT], writes=[kU])
    for p_ in range(NP):
        cs = slice(p_ * W_, (p_ + 1) * W_)
        init = 0.0 if p_ == 0 else xc[:, p_ * W_ - 1:p_ * W_]
        S.op("dve", lambda e, cs=cs, init=init: e.tensor_tensor_scan(out=xc[:, cs], data0=A[:, cs], data1=U[:, cs], initial=init, op0=ALU.mult, op1=ALU.add),
             reads=[f"Ap{p_}", f"Up{p_}"] + [f"Up{q_}" for q_ in range(NP)], writes=["xc", f"Hp{p_}"])
    for p_ in range(NP):
        cs = slice(p_ * W_, (p_ + 1) * W_)
        kT, kG = f"Tp{p_}", f"Gp{p_}"
        S.op("pool", lambda e, cs=cs: e.tensor_tensor(out=T1[:, cs], in0=G[:, cs], in1=G[:, cs], op=ALU.mult), reads=["G", f"Up{p_}"], writes=[kT])
        S.op("pool", lambda e, cs=cs: e.tensor_scalar(out=T1[:, cs], in0=T1[:, cs], scalar1=0.044715, scalar2=1.0, op0=ALU.mult, op1=ALU.add), writes=[kT])
        S.op("pool", lambda e, cs=cs: e.tensor_tensor(out=T1[:, cs], in0=T1[:, cs], in1=G[:, cs], op=ALU.mult), reads=["G"], writes=[kT])
        S.op("act", lambda e, cs=cs: e.activation(out=T1[:, cs], in_=T1[:, cs], func=AF.Sigmoid, scale=1.5957691216057308), writes=[kT])
        S.op("dve", lambda e, cs=cs: e.tensor_tensor(out=G[:, cs], in0=G[:, cs], in1=T1[:, cs], op=ALU.mult), reads=[kT], writes=[kG])
        S.op("dve", lambda e, cs=cs: e.tensor_tensor(out=A[:, cs], in0=xc[:, cs], in1=G[:, cs], op=ALU.mult), reads=[f"Hp{p_}", kG], writes=[f"Ap{p_}"])
        for hf in range(2):
            if hf * 4096 >= p_ * W_ and (hf + 1) * 4096 <= (p_ + 1) * W_ or (p_ * W_ >= hf * 4096 and (p_ + 1) * W_ <= (hf + 1) * 4096):
                c0, c1 = max(hf * 4096, p_ * W_), min((hf + 1) * 4096, (p_ + 1) * W_)
                S.dma("sp", lambda e, c0=c0, c1=c1: e.dma_start(out=yT[0:128, c0:c1], in_=A[:, c0:c1]), reads=[f"Ap{p_}"])
    S.barrier()
    kb.release(m0)


def part_mla(kb, c, D, yT, L):
    S = kb.S
    m0 = kb.mark()
    P = kb.psb
    SC = float(96 ** -0.5)
    QD = [kb.alloc(N, BF16) for _ in range(2)]
    KD = [kb.alloc(N, BF16) for _ in range(2)]
    VD = kb.alloc(64 * 2 * 128, BF16).rearrange("p (b h d) -> p b h d", b=64, h=2)
    tri = kb.alloc(4 * 512, BF16).rearrange("p (i q) -> p i q", i=4)
    wuq = kb.alloc(2 * 192, BF16).rearrange("p (c n) -> p c n", c=2)
    wk = kb.alloc(128, BF16)
    wv = kb.alloc(128, BF16)
    vec = kb.alloc(4)
    r32 = kb.alloc(32)
    st = kb.alloc(512)
    S.op("pool", lambda e: e.memset(VD, 0.0), writes=["VD"])
    S.op("pool", lambda e: e.memset(VD[:, :, :, 64:65], 1.0), writes=["VD"])
    S.dma("sp", lambda e: e.dma_start(out=vec[:, 0:3], in_=D["mla_v"][:, :]), writes=["mvec"])
    S.dma("sp", lambda e: e.dma_start(out=r32, in_=D["r32"][:, :]), writes=["r32"])
    for i in range(4):
        S.dma("sp", lambda e, i=i: e.dma_start(out=st, in_=D["tri"][i]), writes=["st"])
        S.op("act", lambda e, i=i: e.copy(out=tri[:, i, :], in_=st), reads=["st"], writes=["tri"])
    S.dma("sp", lambda e: e.dma_start(out=st[:, 0:192], in_=D["mla_wuq"][0:128, :]), writes=["st"])
    S.op("act", lambda e: e.copy(out=wuq[:, 0, :], in_=st[:, 0:192]), reads=["st"], writes=["wuq"])
    S.dma("sp", lambda e: e.dma_start(out=st[0:64, 0:192], in_=D["mla_wuq"][128:192, :]), writes=["st"])
    S.op("act", lambda e: e.copy(out=wuq[0:64, 1, :], in_=st[0:64, 0:192]), reads=["st"], writes=["wuq"])
    S.dma("sp", lambda e: e.dma_start(out=st[:, 0:128], in_=D["mla_wk"][:, :]), writes=["st"])
    S.op("act", lambda e: e.copy(out=wk, in_=st[:, 0:128]), reads=["st"], writes=["wk"])
    S.dma("sp", lambda e: e.dma_start(out=st[:, 0:128], in_=D["mla_wv"][:, :]), writes=["st"])
    S.op("act", lambda e: e.copy(out=wv, in_=st[:, 0:128]), reads=["st"], writes=["wv"])

    m1 = kb.mark()
    cq0 = kb.alloc(512)
    cq1 = kb.alloc(512)
    ckv = kb.alloc(512)
    krt = kb.alloc(512)
    sq0 = kb.alloc(512, BF16)
    sq1 = kb.alloc(512, BF16)
    cn0 = kb.alloc(512, BF16)
    cn1 = kb.alloc(512, BF16)
    ckn = kb.alloc(512, BF16)
    rstd = kb.alloc(512)
    qr = kb.alloc(512)
    t1 = kb.alloc(512)
    t2 = kb.alloc(512)
    posi = kb.alloc(512, I32)
    posf = kb.alloc(512)
    T = alloc_tables(kb)
    R = slice(64, 96)
    for t in range(NQT):
        ts = slice(t * TQ, (t + 1) * TQ)
        load_pos(kb, D, posi, posf, t)
        S.dma("sp", lambda e, ts=ts: e.dma_start(out=cq0, in_=L.pt(OFF["d_cq"], 0, 128, ts.start, ts.stop)), writes=["cq0"])
        S.dma("sp", lambda e, ts=ts: e.dma_start(out=cq1[0:64, :], in_=L.pt(OFF["d_cq"] + 128, 0, 64, ts.start, ts.stop)), writes=["cq1"])
        S.dma("sp", lambda e, ts=ts: e.dma_start(out=ckv, in_=L.pt(OFF["d_ckv"], 0, 128, ts.start, ts.stop)), writes=["ckv"])
        S.dma("sp", lambda e, ts=ts: e.dma_start(out=krt[R, :], in_=L.pt(OFF["d_kr"], 0, 32, ts.start, ts.stop)), writes=["krt"])
        rope_tables(kb, c, posf, "posf", 64, 96, 1, T, 6, "m")
        S.op("act", lambda e: e.activation(out=sq0, in_=cq0, func=AF.Square), reads=["cq0"], writes=["sq0"])
        S.op("act", lambda e: e.activation(out=sq1[0:64, :], in_=cq1[0:64, :], func=AF.Square), reads=["cq1"], writes=["sq1"])
        S.op("pe", lambda e: e.matmul(P[0][:, :], lhsT=c.ones_bf, rhs=sq0, start=True, stop=False), reads=["sq0", "ones_bf"], writes=[pk(0)])
        S.op("pe", lambda e: e.matmul(P[0][:, :], lhsT=c.ones_bf[0:64, :], rhs=sq1[0:64, :], start=False, stop=True), reads=["sq1", "ones_bf"], writes=[pk(0)])
        S.op("act", lambda e: e.activation(out=rstd, in_=P[0][:, :], func=AF.Sqrt, bias=EPS, scale=1.0 / 192.0), writes=[pk(0), "rstd"])
        S.op("dve", lambda e: e.reciprocal(out=rstd, in_=rstd), writes=["rstd"])
        S.op("dve", lambda e: e.scalar_tensor_tensor(out=cn0, in0=cq0, scalar=vec[:, 0:1], in1=rstd, op0=ALU.mult, op1=ALU.mult),
             reads=["cq0", "rstd", "mvec"], writes=["cn0"])
        S.op("dve", lambda e: e.scalar_tensor_tensor(out=cn1[0:64, :], in0=cq1[0:64, :], scalar=vec[0:64, 1:2], in1=rstd[0:64, :], op0=ALU.mult, op1=ALU.mult),
             reads=["cq1", "rstd", "mvec"], writes=["cn1"])
        for h in range(2):
            hs = slice(h * 96, (h + 1) * 96)
            S.op("pe", lambda e, hs=hs: e.matmul(P[1][0:96, :], lhsT=wuq[:, 0, hs], rhs=cn0, start=True, stop=False), reads=["cn0", "wuq"], writes=[pk(1)])
            S.op("pe", lambda e, hs=hs: e.matmul(P[1][0:96, :], lhsT=wuq[0:64, 1, hs], rhs=cn1[0:64, :], start=False, stop=True), reads=["cn1", "wuq"], writes=[pk(1)])
            S.op("act", lambda e, h=h, ts=ts: e.mul(out=QD[h][0:64, ts], in_=P[1][0:64, :], mul=SC), writes=[pk(1), f"QD{h}"])
            S.op("dve", lambda e: e.tensor_copy(out=qr[R, :], in_=P[1][R, :]), writes=[pk(1), "qr"])
            S.op("pe", lambda e: e.matmul(P[2][R, :], lhsT=r32[R, 0:32], rhs=qr[R, :], start=True, stop=True), reads=["qr", "r32"], writes=[pk(2)])
            S.op("dve", lambda e: e.scalar_tensor_tensor(out=t1[R, :], in0=qr[R, :], scalar=SC, in1=T["cos"][R, :], op0=ALU.mult, op1=ALU.mult),
                 reads=["qr", "mcos"], writes=["t1"])
            S.op("dve", lambda e: e.scalar_tensor_tensor(out=t2[R, :], in0=P[2][R, :], scalar=SC, in1=T["sin"][R, :], op0=ALU.mult, op1=ALU.mult),
                 reads=["msin"], writes=[pk(2), "t2"])
            S.op("dve", lambda e, h=h, ts=ts: e.tensor_tensor(out=QD[h][R, ts], in0=t1[R, :], in1=t2[R, :], op=ALU.add), reads=["t1", "t2"], writes=[f"QD{h}"])
        S.op("act", lambda e: e.activation(out=sq0, in_=ckv, func=AF.Square), reads=["ckv"], writes=["sq0"])
        S.op("pe", lambda e: e.matmul(P[7][:, :], lhsT=c.ones_bf, rhs=sq0, start=True, stop=True), reads=["sq0", "ones_bf"], writes=[pk(7)])
        S.op("act", lambda e: e.activation(out=rstd, in_=P[7][:, :], func=AF.Sqrt, bias=EPS, scale=1.0 / 128.0), writes=[pk(7), "rstd"])
        S.op("dve", lambda e: e.reciprocal(out=rstd, in_=rstd), writes=["rstd"])
        S.op("dve", lambda e: e.scalar_tensor_tensor(out=ckn, in0=ckv, scalar=vec[:, 2:3], in1=rstd, op0=ALU.mult, op1=ALU.mult),
             reads=["ckv", "rstd", "mvec"], writes=["ckn"])
        for h in range(2):
            S.op("pe", lambda e, h=h: e.matmul(P[3][0:64, :], lhsT=wk[:, h * 64:(h + 1) * 64], rhs=ckn, start=True, stop=True), reads=["ckn", "wk"], writes=[pk(3)])
            S.op("act", lambda e, h=h, ts=ts: e.copy(out=KD[h][0:64, ts], in_=P[3][0:64, :]), writes=[pk(3), f"KD{h}"])
        for tb in range(4):
            S.op("pe", lambda e, tb=tb: e.matmul(P[4][:, 0:128], lhsT=ckn[:, tb * 128:(tb + 1) * 128], rhs=wv, start=True, stop=True), reads=["ckn", "wv"], writes=[pk(4)])
            S.op("act", lambda e, tb=tb, t=t: e.copy(out=VD[:, 4 * t + tb, :, 0:64], in_=P[4][:, 0:128].rearrange("p (h d) -> p h d", h=2)),
                 writes=[pk(4), "VD"])
        S.op("pe", lambda e: e.matmul(P[5][R, :], lhsT=r32[R, 0:32], rhs=krt[R, :], start=True, stop=True), reads=["krt", "r32"], writes=[pk(5)])
        S.op("dve", lambda e: e.tensor_tensor(out=t1[R, :], in0=krt[R, :], in1=T["cos"][R, :], op=ALU.mult), reads=["krt", "mcos"], writes=["t1"])
        S.op("dve", lambda e: e.tensor_tensor(out=t2[R, :], in0=P[5][R, :], in1=T["sin"][R, :], op=ALU.mult), reads=["msin"], writes=[pk(5), "t2"])
        for h in range(2):
            S.op("dve", lambda e, h=h, ts=ts: e.tensor_tensor(out=KD[h][R, ts], in0=t1[R, :], in1=t2[R, :], op=ALU.add), reads=["t1", "t2"], writes=[f"KD{h}"])
    S.barrier()
    kb.release(m1)
    att = Attn(kb, sbanks=(0, 1))
    rowbuf = kb.alloc(512)
    bcs = kb.alloc(512)
    yst = [kb.alloc(512), kb.alloc(512)]
    it = 0
    pending = []
    for qt in range(NQT):
        qs = slice(qt * TQ, (qt + 1) * TQ)
        for h in range(2):
            blocks = []
            for kbk in range(4 * qt + 4):
                mms = [(KD[h][0:96, kbk * 128:(kbk + 1) * 128], QD[h][0:96, qs], [f"KD{h}", f"QD{h}"])]
                if kbk >= 4 * qt:
                    mms.append((c.ident_bf, tri[:, kbk - 4 * qt, :], ["ident_bf", "tri"]))
                blocks.append((mms, VD[:, kbk, h, :], ["VD"]))
            ob = 2 + it % 2
            att.run(blocks, ob, defer=pending)

            def fin(ob=ob, ys=yst[it % 2], yk=f"yst{it % 2}", h=h, qs=qs):
                norm_coef(kb, c, ob, rowbuf, 4, bcs)
                S.op("dve", lambda e: e.tensor_tensor(out=ys[0:64, :], in0=P[ob][0:64, :], in1=bcs[0:64, :], op=ALU.mult),
                     reads=["bcs"], writes=[pk(ob), yk])
                S.dma("sp", lambda e: e.dma_start(out=yT[384 + h * 64:384 + (h + 1) * 64, qs], in_=ys[0:64, :]), reads=[yk])
            pending.append(fin)
            it += 1
    for f in pending:
        f()
    S.barrier()
    kb.release(m0)


def load_cast(kb, src_ap, dst_ap, stage, skey, dkey, eng="act", rows=slice(0, 128)):
    S = kb.S
    S.dma("sp", lambda e: e.dma_start(out=stage, in_=(src_ap() if callable(src_ap) else src_ap)), writes=[skey])
    if eng == "act":
        S.op("act", lambda e: e.copy(out=dst_ap, in_=stage), reads=[skey], writes=[dkey])
    else:
        S.op("pool", lambda e: e.tensor_copy(out=dst_ap, in_=stage), reads=[skey], writes=[dkey])


def gelu_tanh(kb, z, u, out, zk, uk, outk):
    S = kb.S
    S.op("pool", lambda e: e.tensor_tensor(out=u, in0=z, in1=z, op=ALU.mult), reads=[zk], writes=[uk])
    S.op("pool", lambda e: e.tensor_scalar(out=u, in0=u, scalar1=0.044715, scalar2=1.0, op0=ALU.mult, op1=ALU.add), writes=[uk])
    S.op("pool", lambda e: e.tensor_tensor(out=u, in0=u, in1=z, op=ALU.mult), reads=[zk], writes=[uk])
    S.op("act", lambda e: e.activation(out=u, in_=u, func=AF.Sigmoid, scale=1.5957691216057308), writes=[uk])
    S.op("dve", lambda e: e.tensor_tensor(out=out, in0=z, in1=u, op=ALU.mult), reads=[zk, uk], writes=[outk])


def part_nsa(kb, c, D, yT, L):
    S = kb.S
    P = kb.psb
    m0 = kb.mark()
    Qm = kb.alloc(N, BF16)
    Qo = kb.alloc(N, BF16)
    KSA = kb.alloc(N, BF16)
    KSB = kb.alloc(N, BF16)
    KW2 = kb.alloc(N, BF16)
    tri = kb.alloc(4 * 512, BF16).rearrange("p (i q) -> p i q", i=4)
    win = kb.alloc(8 * 512, BF16).rearrange("p (i q) -> p i q", i=8)
    cmpm = kb.alloc(5 * 512, BF16).rearrange("p (i q) -> p i q", i=5)
    rbd = kb.alloc(128)
    ovl = kb.alloc(4 * 130, BF16).rearrange("p (i q) -> p i q", i=4)
    bv = kb.alloc(254)
    bf = kb.alloc(254)
    selg = kb.alloc(384)
    KCMP2 = kb.alloc(512, BF16)
    VCMP = kb.alloc(4 * 128, BF16).rearrange("p (b d) -> p b d", b=4)
    st = kb.alloc(512)
    st3 = st.rearrange("p (b d) -> p b d", d=64)
    S.op("pool", lambda e: e.memset(KSA[64:128, :], 0.0), writes=["ksz"])
    S.op("pool", lambda e: e.memset(KSB[0:64, :], 0.0), writes=["ksz"])
    S.op("pool", lambda e: e.memset(VCMP, 0.0), writes=["VCMP"])
    S.op("pool", lambda e: e.memset(VCMP[:, :, 64:65], 1.0), writes=["VCMP"])
    S.dma("sp", lambda e: e.dma_start(out=rbd, in_=D["rbd"][:, :]), writes=["rbd"])
    S.dma("sp", lambda e: e.dma_start(out=bv, in_=D["bv"][:, :]), writes=["bv"])
    S.dma("sp", lambda e: e.dma_start(out=bf, in_=D["bf"][:, :]), writes=["bf"])
    S.dma("sp", lambda e: e.dma_start(out=selg[0:8, :], in_=D["selg"][:, :]), writes=["selg"])
    i = 0
    for nm, dst, cnt in (("tri", tri, 4), ("win", win, 8), ("cmpm", cmpm, 5)):
        for j in range(cnt):
            load_cast(kb, D[nm][j], dst[:, j, :], st[:, 0:512], "st", nm, "act" if i % 2 == 0 else "pool")
            i += 1
    for j in range(4):
        load_cast(kb, D["ovl"][j], ovl[:, j, 0:129], st[:, 0:129], "st", "ovl", "act")

    m1 = kb.mark()
    KCV = kb.alloc(N)
    xs = [kb.alloc(512), kb.alloc(512)]
    t1s = [kb.alloc(512), kb.alloc(512)]
    t2s = [kb.alloc(512), kb.alloc(512)]
    posi = kb.alloc(512, I32)
    posf = kb.alloc(512)
    T = alloc_tables(kb)
    for hf in range(2):
        S.dma("sp", lambda e, hf=hf: e.dma_start(out=KCV[64:128, hf * 4096:(hf + 1) * 4096], in_=L.pt(OFF["b_kv"] + 64, 0, 64, hf * 4096, (hf + 1) * 4096)), writes=["KCVv"])
    flat = []
    for t in range(NQT):
        ts = slice(t * TQ, (t + 1) * TQ)
        a0, a1 = ts.start, ts.stop
        flat += [(t, ts, "q0", [lambda a0=a0, a1=a1: L.pt(OFF["b_q"], 128, 128, a0, a1)], Qm, 0.125, 128),
                 (t, ts, "q1", [lambda a0=a0, a1=a1: L.pt(OFF["b_q"], 128, 128, a0, a1, inv=True)], Qo, 0.125, 128),
                 (t, ts, "ks", [lambda a0=a0, a1=a1: L.pt(OFF["b_kv"] + 128, 0, 64, a0, a1)] * 2, None, 1.0, 128),
                 (t, ts, "kw", [lambda a0=a0, a1=a1: L.pt(OFF["b_kv"] + 192, 0, 64, a0, a1)] * 2, KW2, 1.0, 128),
                 (t, ts, "kc", [lambda a0=a0, a1=a1: L.pt(OFF["b_kv"], 0, 64, a0, a1)], KCV, 1.0, 64)]

    def emit_load(i):
        t, ts, nm, srcs, dst, sc, rows = flat[i]
        x = xs[i % 2]
        xk = f"xs{i % 2}"
        if len(srcs) == 2:
            S.dma("sp", lambda e: e.dma_start(out=x[0:64, :], in_=srcs[0]()), writes=[xk])
            S.dma("sp", lambda e: e.dma_start(out=x[64:128, :], in_=srcs[1]()), writes=[xk])
        else:
            S.dma("sp", lambda e: e.dma_start(out=x[0:rows, :], in_=srcs[0]()), writes=[xk])

    def emit_compute(i):
        t, ts, nm, srcs, dst, sc, rows = flat[i]
        x = xs[i % 2]
        xk = f"xs{i % 2}"
        t1 = t1s[i % 2]
        t2 = t2s[i % 2]
        bank = 4 + i % 2
        R = slice(0, rows)
        S.op("pe", lambda e: e.matmul(P[bank][R, :], lhsT=rbd[R, 0:rows], rhs=x[R, :], start=True, stop=True),
             reads=[xk, "rbd"], writes=[pk(bank)])
        S.op("dve", lambda e: e.scalar_tensor_tensor(out=t1[R, :], in0=x[R, :], scalar=sc, in1=T["cos"][R, :], op0=ALU.mult, op1=ALU.mult),
             reads=[xk, "ncos"], writes=[f"t1{i % 2}"])
        S.op("dve", lambda e: e.scalar_tensor_tensor(out=t2[R, :], in0=P[bank][R, :], scalar=sc, in1=T["sin"][R, :], op0=ALU.mult, op1=ALU.mult),
             reads=["nsin"], writes=[pk(bank), f"t2{i % 2}"])
        dk = "KCVk" if nm == "kc" else nm
        if nm == "ks":
            for dst_, RR in ((KSA, slice(0, 64)), (KSB, slice(64, 128))):
                S.op("pool", lambda e, RR=RR, dst_=dst_: e.tensor_tensor(out=dst_[RR, ts], in0=t1[RR, :], in1=t2[RR, :], op=ALU.add),
                     reads=[f"t1{i % 2}", f"t2{i % 2}"], writes=[dk])
        else:
            S.op("pool", lambda e: e.tensor_tensor(out=dst[R, ts], in0=t1[R, :], in1=t2[R, :], op=ALU.add),
                 reads=[f"t1{i % 2}", f"t2{i % 2}"], writes=[dk])

    emit_load(0)
    for i in range(len(flat)):
        if i % 5 == 0:
            load_pos(kb, D, posi, posf, flat[i][0])
            rope_tables(kb, c, posf, "posf", 0, 128, 0, T, 6, "n")
        if i + 1 < len(flat):
            emit_load(i + 1)
        emit_compute(i)
    S.barrier()
    kb.release(m1)
    KCV = kb.alloc(N)
    BLK = kb.alloc(32 * 512, BF16).rearrange("p (l n) -> p l n", l=32)
    W1 = kb.alloc(32 * 256, BF16).rearrange("p (l h) -> p l h", l=32)
    pos2 = kb.alloc(32)
    b1 = kb.alloc(4)
    w2 = kb.alloc(2 * 2 * 64, BF16).rearrange("p (k m d) -> p k m d", k=2, m=2)
    HID = kb.alloc(2 * 2 * 512, BF16).rearrange("p (k m n) -> p k m n", k=2, m=2)
    zt = kb.alloc(512)
    ut = kb.alloc(512)
    stw = kb.alloc(1024).rearrange("p (l h) -> p l h", l=4)
    S.dma("sp", lambda e: e.dma_start(out=pos2, in_=D["nsa_pos"][:, :]), writes=["pos2"])
    S.dma("sp", lambda e: e.dma_start(out=b1, in_=D["nsa_b1"][:, :]), writes=["b1"])
    for kv in range(2):
        load_cast(kb, D["nsa_w2"][kv].rearrange("(m p) d -> p m d", p=128), w2[:, kv, :, :], st[:, 0:128].rearrange("p (m d) -> p m d", m=2), "st", "w2", "act")
    for l0 in range(0, 32, 4):
        for kv in range(2):
            src = D["nsa_w1"][kv].rearrange("(l d) h -> d l h", d=64)[:, l0:l0 + 4, :]
            S.dma("sp", lambda e, src=src, kv=kv: e.dma_start(out=stw[kv * 64:(kv + 1) * 64, :, :], in_=src), writes=["stw"])
        if (l0 // 4) % 2 == 0:
            S.op("act", lambda e, l0=l0: e.copy(out=W1[:, l0:l0 + 4, :], in_=stw), reads=["stw"], writes=["W1"])
        else:
            S.op("pool", lambda e, l0=l0: e.tensor_copy(out=W1[:, l0:l0 + 4, :], in_=stw), reads=["stw"], writes=["W1"])
    S.op("pool", lambda e: e.memset(BLK[:, :, 511:512], 0.0), writes=["BLKpad"])
    K3 = KCV.rearrange("p (g r) -> p g r", r=16)
    for l in range(32):
        src = K3[:, 0:511, l] if l < 16 else K3[:, 1:512, l - 16]
        eng = "dve" if l % 2 == 0 else "pool"
        S.op(eng, lambda e, l=l, src=src: e.tensor_scalar(out=BLK[:, l, 0:511], in0=src, scalar1=pos2[:, l:l + 1], scalar2=None, op0=ALU.add),
             reads=["pos2"], writes=[f"BLK{l}"])
    for kv in range(2):
        R = slice(kv * 64, kv * 64 + 64)
        for m in range(2):
            bank = 2 * kv + m
            for l in range(32):
                S.op("pe", lambda e, l=l, R=R, m=m, bank=bank: e.matmul(P[bank][:, :], lhsT=W1[R, l, m * 128:(m + 1) * 128], rhs=BLK[R, l, :], start=(l == 0), stop=(l == 31)),
                     reads=["W1", f"BLK{l}", "BLKpad"], writes=[pk(bank)])
            S.op("act", lambda e, kv=kv, m=m, bank=bank: e.activation(out=zt, in_=P[bank][:, :], func=AF.Identity, bias=b1[:, kv * 2 + m:kv * 2 + m + 1]),
                 reads=["b1"], writes=[pk(bank), "zt"])
            gelu_tanh(kb, zt, ut, HID[:, kv, m, :], "zt", "ut", f"HID{kv}")
    for m in range(2):
        S.op("pe", lambda e, m=m: e.matmul(P[4][0:64, :], lhsT=w2[:, 0, m, :], rhs=HID[:, 0, m, :], start=(m == 0), stop=(m == 1)), reads=["w2", "HID0"], writes=[pk(4)])
    S.op("act", lambda e: e.copy(out=KCMP2[0:64, :], in_=P[4][0:64, :]), writes=[pk(4), "KCMP2"])
    S.op("dve", lambda e: e.tensor_copy(out=KCMP2[64:128, :], in_=P[4][0:64, :]), writes=[pk(4), "KCMP2"])
    for nb in range(4):
        for m in range(2):
            S.op("pe", lambda e, m=m, nb=nb: e.matmul(P[5][:, 0:64], lhsT=HID[:, 1, m, nb * 128:(nb + 1) * 128], rhs=w2[:, 1, m, :], start=(m == 0), stop=(m == 1)),
                 reads=["w2", "HID1"], writes=[pk(5)])
        S.op("act", lambda e, nb=nb: e.copy(out=VCMP[:, nb, 0:64], in_=P[5][:, 0:64]), writes=[pk(5), "VCMP"])
    S.barrier()
    kb.release(m1)
    VS = kb.alloc(64 * 128, BF16).rearrange("p (b d) -> p b d", b=64)
    VW = kb.alloc(64 * 128, BF16).rearrange("p (b d) -> p b d", b=64)
    for vt_, vk_ in ((VS, "VS"), (VW, "VW")):
        S.op("pool", lambda e, vt_=vt_: e.memset(vt_, 0.0), writes=[vk_])
        S.op("pool", lambda e, vt_=vt_: e.memset(vt_[:, :, 64:65], 1.0), writes=[vk_])
    for nm, dst, coff in (("nsa_vs", VS, 0), ("nsa_vw", VW, 64)):
        for b0 in range(0, 64, 8):
            load_cast(kb, (lambda b0=b0, coff=coff: L.pv(coff, 0, 64, b0 * 128, (b0 + 8) * 128).rearrange("(b p) d -> p b d", p=128)),
                      dst[:, b0:b0 + 8, 0:64], st3[:, 0:8, :], "st", nm[-2:].upper(), "act" if (b0 // 8) % 2 == 0 else "pool")

    NST = kb.alloc(N, BF16)
    EALL = kb.alloc(N, BF16)
    for j in range(16):
        load_cast(kb, D["eall"][:, j * 512:(j + 1) * 512], EALL[:, j * 512:(j + 1) * 512], st[:, 0:512], "st", "EALL", "act" if j % 2 == 0 else "pool")
    ET = [kb.alloc(512, BF16) for _ in range(4)]
    IMP = kb.alloc(512).rearrange("p (a s) -> p a s", a=4)
    scr = kb.alloc(128)
    mr = kb.alloc(128)
    nsb = kb.alloc(128)
    mx = kb.alloc(16)
    thr = kb.alloc(2)
    rsum = kb.alloc(2)
    g6 = kb.alloc(512)
    gs = [[kb.alloc(512) for _ in range(3)] for _ in range(2)]
    acc = [kb.alloc(512), kb.alloc(512)]
    ctmp = kb.alloc(512)
    otmp = kb.alloc(512)
    rowbuf = ctmp
    bcs = kb.alloc(512)
    att = Attn(kb, sbanks=(0, 1))
    oi = 0

    def combine(ob, hA, br, first):
        norm_coef(kb, c, ob, rowbuf, 4, bcs)
        S.op("dve", lambda e: e.tensor_tensor(out=ctmp[0:64, :], in0=bcs[0:64, :], in1=gs[hA][br][0:64, :], op=ALU.mult),
             reads=["bcs", f"gs{hA}{br}"], writes=["ctmp"])
        if first:
            S.op("dve", lambda e: e.tensor_tensor(out=acc[hA][0:64, :], in0=P[ob][0:64, :], in1=ctmp[0:64, :], op=ALU.mult),
                 reads=["ctmp"], writes=[pk(ob), f"acc{hA}"])
        else:
            S.op("dve", lambda e: e.tensor_tensor(out=otmp[0:64, :], in0=P[ob][0:64, :], in1=ctmp[0:64, :], op=ALU.mult),
                 reads=["ctmp"], writes=[pk(ob), "otmp"])
            S.op("pool", lambda e: e.tensor_tensor(out=acc[hA][0:64, :], in0=acc[hA][0:64, :], in1=otmp[0:64, :], op=ALU.add),
                 reads=["otmp"], writes=[f"acc{hA}"])

    pending = []
    for qt in range(NQT):
        qs = slice(qt * TQ, (qt + 1) * TQ)
        for f in pending:
            f()
        del pending[:]
        S.dma("sp", lambda e, qs=qs: e.dma_start(out=g6[0:6, :], in_=L.pt(OFF["b_gate"], 6, 6, qs.start, qs.stop)), writes=["g6"])
        for hA in range(2):
            for br in range(3):
                r = hA * 3 + br
                S.op("pe", lambda e, r=r: e.matmul(P[5][0:64, :], lhsT=selg[0:6, r * 64:(r + 1) * 64], rhs=g6[0:6, :], start=True, stop=True),
                     reads=["g6", "selg"], writes=[pk(5)])
                S.op("act", lambda e, hA=hA, br=br: e.activation(out=gs[hA][br][0:64, :], in_=P[5][0:64, :], func=AF.Sigmoid), writes=[pk(5), f"gs{hA}{br}"])
        nbm = (512 * qt + 480) // 2048
        for hh in range(4):
            Qt = (Qm if hh < 2 else Qo)
            qk = "q0" if hh < 2 else "q1"
            R = slice(64 * (hh % 2), 64 * (hh % 2) + 64)
            ob = 2 + oi % 2
            for nb in range(nbm + 1):
                bank = nb % 2
                dl = 512 * qt - 2048 * nb
                mms = [(KCMP2[R, nb * 128:(nb + 1) * 128], Qt[R, qs], ["KCMP2", qk])]
                if dl < 2560:
                    mms.append((c.ident_bf, cmpm[:, dl // 512, :], ["ident_bf", "cmpm"]))
                for j, (l_, r_, ks) in enumerate(mms):
                    S.op("pe", lambda e, l_=l_, r_=r_, j=j, bank=bank, nm=len(mms): e.matmul(P[bank][:, :], lhsT=l_, rhs=r_, start=(j == 0), stop=(j == nm - 1)),
                         reads=ks, writes=[pk(bank)])
                S.op("act", lambda e, nb=nb, bank=bank: e.activation(out=ET[nb], in_=P[bank][:, :], func=AF.Exp), writes=[pk(bank), f"ET{nb}"])
                if hh < 2:
                    S.op("pe", lambda e, nb=nb, ob=ob, nbm=nbm: e.matmul(P[ob][0:128, :], lhsT=VCMP[:, nb, :], rhs=ET[nb], start=(nb == 0), stop=(nb == nbm)),
                         reads=[f"ET{nb}", "VCMP"], writes=[pk(ob)])
            for s4 in range(4):
                for nb in range(nbm + 1):
                    S.op("pe", lambda e, nb=nb, s4=s4, nbm=nbm: e.matmul(P[6][:, 0:129], lhsT=ET[nb][:, s4 * 128:(s4 + 1) * 128], rhs=ovl[:, nb, 0:129], start=(nb == 0), stop=(nb == nbm)),
                         reads=[f"ET{nb}", "ovl"], writes=[pk(6)])
                S.op("dve", lambda e: e.tensor_scalar_max(out=rsum[:, 0:1], in0=P[6][:, 128:129], scalar1=1e-30), writes=[pk(6), "rsum"])
                S.op("dve", lambda e: e.reciprocal(out=rsum[:, 0:1], in_=rsum[:, 0:1]), writes=["rsum"])
                if hh == 0:
                    S.op("dve", lambda e, s4=s4: e.tensor_scalar(out=IMP[:, s4, :], in0=P[6][:, 0:128], scalar1=rsum[:, 0:1], scalar2=None, op0=ALU.mult),
                         reads=["rsum"], writes=[pk(6), f"IMP{s4}"])
                else:
                    S.op("dve", lambda e, s4=s4: e.scalar_tensor_tensor(out=IMP[:, s4, :], in0=P[6][:, 0:128], scalar=rsum[:, 0:1], in1=IMP[:, s4, :], op0=ALU.mult, op1=ALU.add),
                         reads=["rsum"], writes=[pk(6), f"IMP{s4}"])
            if hh < 2:
                combine(ob, hh, 0, True)
                oi += 1
        for s4 in range(4):
            qb = 4 * qt + s4
            o0 = 126 - 2 * qb
            S.op("dve", lambda e, s4=s4, o0=o0: e.tensor_tensor(out=scr, in0=IMP[:, s4, :], in1=bv[:, o0:o0 + 128], op=ALU.mult), reads=[f"IMP{s4}", "bv"], writes=["scr"])
            S.op("dve", lambda e, o0=o0: e.tensor_tensor(out=scr, in0=scr, in1=bf[:, o0:o0 + 128], op=ALU.add), reads=["bf"], writes=["scr"])
            S.op("dve", lambda e: e.memset(scr[:, 0:1], 1e9), writes=["scr"])
            S.op("dve", lambda e: e.max(out=mx[:, 0:8], in_=scr), reads=["scr"], writes=["mx"])
            S.op("dve", lambda e: e.match_replace(out=mr, in_to_replace=mx[:, 0:8], in_values=scr, imm_value=-2.0), reads=["scr", "mx"], writes=["mr"])
            S.op("dve", lambda e: e.max(out=mx[:, 8:16], in_=mr), reads=["mr"], writes=["mx"])
            S.op("dve", lambda e: e.tensor_reduce(out=thr[:, 0:1], in_=mx[:, 8:16], axis=AX.X, op=ALU.min), reads=["mx"], writes=["thr"])
            S.op("dve", lambda e: e.tensor_scalar(out=nsb, in0=scr, scalar1=thr[:, 0:1], scalar2=-NEG, op0=ALU.is_ge, op1=ALU.mult), reads=["scr", "thr"], writes=["nsb"])
            S.op("pool", lambda e: e.tensor_scalar(out=nsb, in0=nsb, scalar1=NEG, scalar2=None, op0=ALU.add), writes=["nsb"])
            S.op("pe", lambda e: e.transpose(P[7][:, 0:128], nsb, c.ident), reads=["nsb", "ident"], writes=[pk(7)])
            S.op("act", lambda e, qb=qb: e.copy(out=NST[:, qb * 128:(qb + 1) * 128], in_=P[7][:, 0:128]), writes=[pk(7), "NST"])
        for hA in range(2):
            R = slice(64 * hA, 64 * hA + 64)
            blocks = []
            for kbk in range(4 * qt + 4):
                mms = [((KSA if hA == 0 else KSB)[:, kbk * 128:(kbk + 1) * 128], Qm[:, qs], ["ks", "q0", "ksz"]),
                       (EALL[:, kbk * 128:(kbk + 1) * 128], NST[:, qs], ["EALL", "NST"])]
                if kbk >= 4 * qt:
                    mms.append((c.ident_bf, tri[:, kbk - 4 * qt, :], ["ident_bf", "tri"]))
                blocks.append((mms, VS[:, kbk, :], ["VS"]))
            ob = 2 + oi % 2
            oi += 1
            att.run(blocks, ob, defer=pending)
            pending.append(lambda ob=ob, hA=hA: combine(ob, hA, 1, False))
            blocks = []
            for kbk in range(max(0, 4 * qt - 4), 4 * qt + 4):
                mms = [(KW2[R, kbk * 128:(kbk + 1) * 128], Qm[R, qs], ["kw", "q0"]),
                       (c.ident_bf, win[:, kbk - 4 * qt + 4, :], ["ident_bf", "win"])]
                blocks.append((mms, VW[:, kbk, :], ["VW"]))
            ob = 2 + oi % 2
            oi += 1
            att.run(blocks, ob, defer=pending)

            def fin(ob=ob, hA=hA, qs=qs):
                combine(ob, hA, 2, False)
                S.dma("sp", lambda e: e.dma_start(out=yT[128 + hA * 64:128 + (hA + 1) * 64, qs], in_=acc[hA][0:64, :]), reads=[f"acc{hA}"])
            pending.append(fin)
    for f in pending:
        f()
    S.barrier()
    kb.release(m0)


def part_gla(kb, c, D, yT, L):
    STAGE = 9
    S = kb.S
    P = kb.psb
    m0 = kb.mark()
    QS = float(32 ** -0.5)
    A = kb.alloc(N)
    B = kb.alloc(N)
    C = kb.alloc(2048)
    QG = kb.alloc(N, BF16)
    KG = kb.alloc(N, BF16)
    KHT = kb.alloc(128 * 64, BF16).rearrange("p (b d) -> p b d", b=128)
    V = kb.alloc(128 * 128, BF16).rearrange("p (b d) -> p b d", b=128)
    SB = kb.alloc(128 * 64, BF16).rearrange("p (c v) -> p c v", c=128)
    DC = kb.alloc(128)
    wg2 = kb.alloc(64)
    glr = kb.alloc(512)
    v2 = kb.alloc(4)
    tri64 = kb.alloc(64)
    st = kb.alloc(1024)
    S.dma("sp", lambda e: e.dma_start(out=wg2[0:16, :], in_=D["gla_wg2"][:, :]), writes=["wg2"])
    S.dma("sp", lambda e: e.dma_start(out=v2[:, 0:2], in_=D["gla_v2"][:, :]), writes=["v2"])
    S.dma("sp", lambda e: e.dma_start(out=tri64[0:64, :], in_=D["tri64"][:, :]), writes=["tri64"])
    S.dma("sp", lambda e: e.dma_start(out=tri64[64:128, :], in_=D["tri64"][:, :]), writes=["tri64"])
    S.op("dve", lambda e: e.tensor_scalar(out=v2[:, 2:3], in0=v2[:, 0:1], scalar1=-1.0, scalar2=None, op0=ALU.mult), reads=["v2"], writes=["v2n"])
    R = slice(0, 64)
    st3 = st.rearrange("p (b d) -> p b d", d=128)
    for b0 in range(0, 128, 8):
        load_cast(kb, (lambda b0=b0: L.pv(128, 128, 128, b0 * 64, (b0 + 8) * 64).rearrange("(b p) d -> p b d", p=64)),
                  V[R, b0:b0 + 8, :], st3[R, :, :], "st", "V", "act" if (b0 // 8) % 2 == 0 else "pool")
    for t in range(NQT):
        ts = slice(t * TQ, (t + 1) * TQ)
        bank = t % 2
        S.dma("sp", lambda e, ts=ts: e.dma_start(out=glr[0:16, :], in_=L.pt(OFF["c_glr"], 0, 16, ts.start, ts.stop)), writes=["glr"])
        S.op("pe", lambda e, bank=bank: e.matmul(P[bank][R, :], lhsT=wg2[0:16, 0:64], rhs=glr[0:16, :], start=True, stop=True), reads=["glr", "wg2"], writes=[pk(bank)])
        S.op("act", lambda e, bank=bank, ts=ts: e.activation(out=A[R, ts], in_=P[bank][R, :], func=AF.Exp, bias=v2[R, 2:3], scale=-1.0),
             reads=["v2n"], writes=[pk(bank), "A"])
    S.op("act", lambda e: e.activation(out=A[R, :], in_=A[R, :], func=AF.Ln, bias=1.0), writes=["A"])
    for ch in range(128):
        cs = slice(ch * 64, (ch + 1) * 64)
        S.op("dve", lambda e, cs=cs: e.tensor_tensor_scan(out=B[R, cs], data0=c.ones_f[R, 0:64], data1=A[R, cs], initial=0.0, op0=ALU.mult, op1=ALU.add),
             reads=["A", "ones_f"], writes=["B"])
    if STAGE <= 1:
        S.barrier(); kb.release(m0); return
    B3 = B.rearrange("p (c j) -> p c j", j=64)
    A3 = A.rearrange("p (c j) -> p c j", j=64)
    S.op("act", lambda e: e.activation(out=DC[R, :], in_=B3[R, :, 63], func=AF.Exp, scale=-1.0 / 16.0), reads=["B"], writes=["DC"])
    S.op("act", lambda e: e.activation(out=A[R, :], in_=B[R, :], func=AF.Exp, scale=-1.0 / 16.0), reads=["B"], writes=["A"])
    for pc in range(4):
        ps_ = slice(pc * 2048, (pc + 1) * 2048)
        S.dma("sp", lambda e, ps_=ps_: e.dma_start(out=C[R, :], in_=L.pt(OFF["c_q"], 64, 64, ps_.start, ps_.stop)), writes=["C"])
        S.op("dve", lambda e, ps_=ps_: e.scalar_tensor_tensor(out=QG[R, ps_], in0=C[R, :], scalar=QS, in1=A[R, ps_], op0=ALU.mult, op1=ALU.mult),
             reads=["C", "A"], writes=["QG"])
    S.op("act", lambda e: e.activation(out=A[R, :], in_=B[R, :], func=AF.Exp, scale=1.0 / 16.0), reads=["B", "QG"], writes=["A"])
    for pc in range(4):
        ps_ = slice(pc * 2048, (pc + 1) * 2048)
        S.dma("sp", lambda e, ps_=ps_: e.dma_start(out=C[R, :], in_=L.pt(OFF["c_k"], 64, 64, ps_.start, ps_.stop)), writes=["C"])
        S.op("dve", lambda e, ps_=ps_: e.tensor_tensor(out=KG[R, ps_], in0=C[R, :], in1=A[R, ps_], op=ALU.mult), reads=["C", "A"], writes=["KG"])
    S.op("dve", lambda e: e.tensor_tensor(out=A3[R, :, :], in0=B3[R, :, :], in1=B3[R, :, 63:64].to_broadcast([64, 128, 64]), op=ALU.subtract),
         reads=["B", "KG"], writes=["A"])
    S.op("act", lambda e: e.activation(out=A[R, :], in_=A[R, :], func=AF.Exp, scale=1.0 / 16.0), writes=["A"])
    for pc in range(4):
        ps_ = slice(pc * 2048, (pc + 1) * 2048)
        S.dma("sp", lambda e, ps_=ps_: e.dma_start(out=C[R, :], in_=L.pt(OFF["c_k"], 64, 64, ps_.start, ps_.stop)), writes=["C"])
        S.op("dve", lambda e, ps_=ps_: e.tensor_tensor(out=A[R, ps_], in0=C[R, :], in1=A[R, ps_], op=ALU.mult), reads=["C"], writes=["A"])
    if STAGE <= 2:
        S.barrier(); kb.release(m0); return
    for blk in range(64):
        bank = blk % 2
        S.op("pe", lambda e, blk=blk, bank=bank: e.transpose(P[bank][:, 0:64], A[R, blk * 128:(blk + 1) * 128], c.ident[0:64, 0:64]),
             reads=["A", "ident"], writes=[pk(bank)])
        S.op("act", lambda e, blk=blk, bank=bank: e.copy(out=KHT[R, 2 * blk, :], in_=P[bank][0:64, 0:64]), writes=[pk(bank), "KHT"])
        S.op("dve", lambda e, blk=blk, bank=bank: e.tensor_copy(out=KHT[R, 2 * blk + 1, :], in_=P[bank][64:128, 0:64]), writes=[pk(bank), "KHT"])
    if STAGE <= 3:
        S.barrier(); kb.release(m0); return
    U3 = B.rearrange("p (v c) -> p v c", c=128)
    uev = [kb.alloc(512), kb.alloc(512)]
    S3 = A.rearrange("p (v c) -> p v c", c=128)
    for g in range(32):
        bank = 2 + g % 2
        for j in range(4):
            ch = 4 * g + j
            S.op("pe", lambda e, ch=ch, j=j, bank=bank: e.matmul(P[bank][R, j * 128:(j + 1) * 128], lhsT=KHT[R, ch, :], rhs=V[R, ch, :], start=True, stop=True),
                 reads=["KHT", "V"], writes=[pk(bank)])
        ue = uev[g % 2]
        S.op("act", lambda e, bank=bank, ue=ue: e.copy(out=ue[R, :], in_=P[bank][R, :]), writes=[pk(bank), f"uev{g % 2}"])
        for h in range(2):
            hr = slice(32 * h, 32 * h + 32)
            for j in range(4):
                eng = "dve" if j % 2 == 0 else "pool"
                S.op(eng, lambda e, hr=hr, ue=ue, g=g, j=j, h=h: e.tensor_copy(out=U3[hr, :, 4 * g + j], in_=ue[hr, j * 128 + h * 64:j * 128 + (h + 1) * 64]),
                     reads=[f"uev{g % 2}"], writes=["U3", "B"])
    if STAGE <= 4:
        S.barrier(); kb.release(m0); return
    for v in range(64):
        S.op("dve", lambda e, v=v: e.tensor_tensor_scan(out=S3[R, v, :], data0=DC[R, :], data1=U3[R, v, :], initial=0.0, op0=ALU.mult, op1=ALU.add),
             reads=["U3", "DC", "KHT"], writes=["S3", "A"])
    S.op("pool", lambda e: e.memset(SB[R, 0, :], 0.0), writes=["SB0"])
    S.op("act", lambda e: e.copy(out=SB[R, 1:128, :], in_=S3[R, :, 0:127].rearrange("p v c -> p c v")), reads=["S3"], writes=["SB"])
    if STAGE <= 5:
        S.barrier(); kb.release(m0); return
    osb = kb.alloc(512)
    sqb = kb.alloc(512, BF16)
    rstd = kb.alloc(512)
    og = kb.alloc(512)
    at = [kb.alloc(64, BF16), kb.alloc(64, BF16)]
    ai = 0
    for t in range(NQT):
        ts = slice(t * TQ, (t + 1) * TQ)
        for h in range(2):
            hr = slice(32 * h, 32 * h + 32)
            ob = 4 + (2 * t + h) % 2
            for j in range(8):
                ch = 8 * t + j
                cs = slice(ch * 64, (ch + 1) * 64)
                rr = R
                sbk = ai % 2
                a_t = at[ai % 2]
                ak = f"at{ai % 2}"
                ai += 1
                S.op("pe", lambda e, hr=hr, cs=cs, rr=rr, sbk=sbk: e.matmul(P[sbk][rr, 0:64], lhsT=KG[hr, cs], rhs=QG[hr, cs], start=True, stop=True),
                     reads=["KG", "QG"], writes=[pk(sbk)])
                S.op("dve", lambda e, rr=rr, sbk=sbk, a_t=a_t: e.tensor_tensor(out=a_t[rr, :], in0=P[sbk][rr, 0:64], in1=tri64[rr, :], op=ALU.mult),
                     reads=["tri64"], writes=[pk(sbk), ak])
                S.op("pe", lambda e, rr=rr, ch=ch, h=h, a_t=a_t, ob=ob, j=j: e.matmul(P[ob][R, j * 64:(j + 1) * 64], lhsT=V[rr, ch, h * 64:(h + 1) * 64], rhs=a_t[rr, :], start=True, stop=False),
                     reads=[ak, "V"], writes=[pk(ob)])
                S.op("pe", lambda e, hr=hr, ch=ch, cs=cs, ob=ob, j=j: e.matmul(P[ob][R, j * 64:(j + 1) * 64], lhsT=SB[hr, ch, :], rhs=QG[hr, cs], start=False, stop=True),
                     reads=["SB", "SB0", "QG"], writes=[pk(ob)])
            S.op("act", lambda e, ob=ob: e.copy(out=osb[R, :], in_=P[ob][R, :]), writes=[pk(ob), "osb"])
            S.op("act", lambda e: e.activation(out=sqb[R, :], in_=osb[R, :], func=AF.Square), reads=["osb"], writes=["sqb"])
            S.op("pe", lambda e: e.matmul(P[6][R, :], lhsT=c.ones_bf[R, 0:64], rhs=sqb[R, :], start=True, stop=True), reads=["sqb", "ones_bf"], writes=[pk(6)])
            S.op("act", lambda e: e.activation(out=rstd[R, :], in_=P[6][R, :], func=AF.Sqrt, bias=EPS, scale=1.0 / 64.0), writes=[pk(6), "rstd"])
            S.op("dve", lambda e: e.reciprocal(out=rstd[R, :], in_=rstd[R, :]), writes=["rstd"])
            S.op("dve", lambda e: e.scalar_tensor_tensor(out=osb[R, :], in0=osb[R, :], scalar=v2[R, 1:2], in1=rstd[R, :], op0=ALU.mult, op1=ALU.mult),
                 reads=["rstd", "v2"], writes=["osb"])
            S.dma("sp", lambda e, h=h, ts=ts: e.dma_start(out=og[R, :], in_=L.pt(OFF["c_og"] + h * 64, 128, 64, ts.start, ts.stop)), writes=["og"])
            S.op("act", lambda e: e.activation(out=og[R, :], in_=og[R, :], func=AF.Silu), writes=["og"])
            S.op("dve", lambda e: e.tensor_tensor(out=osb[R, :], in0=osb[R, :], in1=og[R, :], op=ALU.mult), reads=["og"], writes=["osb"])
            S.dma("sp", lambda e, h=h, ts=ts: e.dma_start(out=yT[256 + h * 64:256 + (h + 1) * 64, ts], in_=osb[R, :]), reads=["osb"])
    S.barrier()
    kb.release(m0)


WB = ["lru_w", "lru_v", "mla_wuq", "mla_wk", "mla_wv", "mla_v", "nsa_pos", "nsa_w1", "nsa_b1", "nsa_w2", "gla_wg2", "gla_v2"]
WAC = (("gain", [128, 8]), ("w_in", [1024, NCOLW]), ("gng", [128, 8]), ("nfg", [128, 8]), ("w_out", [1024, 1024]),
       ("w_gu", [1024, 2 * DFF]), ("w_dn", [DFF, 1024]))
RG = [[0, 1], [2, 3], [4, 5], [6, 7]]


def build_fused():
    kb = KB(arena_cols=53100)
    S = kb.S
    D = {k: kb.din(k, shp) for k, shp in CONST_SHAPES.items()}
    D["pos"] = kb.din("pos", [1, N], I32)
    xT = kb.din("xT", [1024, 4096])
    out = kb.dout("out", [1024, 4096])
    LW = []
    for l in range(2):
        d = {k: kb.din(f"{k}_{l}", IN_SHAPES[k][0]) for k in WB}
        for k, shp in WAC:
            d[k] = kb.din(f"{k}_{l}", shp)
        LW.append(d)
    fing = kb.din("fing", [128, 8])
    XA = kb.dint("XA", [NCOLP, 4096])
    XV = kb.dint("XV", [4096, NV])
    GP = kb.dint("GP", [2 * NCOLP, 4096])
    GV = kb.dint("GV", [N, NV])
    YB = kb.dint("YB", [1024, 4096])
    YV = YView(YB)
    GY = kb.dint("GY", [2048, 4096])
    XO = kb.dint("XO", [1024, 4096])
    MYP = kb.dint("MYP", [SELROWS, N])
    MYV = kb.dint("MYV", [N, 128])
    MYY = kb.dint("MYY", [1024, 4096])
    c = common_setup(kb, D)
    L = Loader(S, GP, GV, MYP, MYV)
    xs = xT
    for l in range(2):
        W = LW[l]
        ag = lambda i_, o_: (lambda e: e.collective_compute("AllGather", ALU.bypass, replica_groups=RG, ins=[i_], outs=[o_]))
        phase_A(kb, xs, W["gain"], W["w_in"], XA, XV, GP, GV, ag)
        L.select()
        Dl = dict(D)
        Dl.update({k: W[k] for k in WB})
        for part, g in ((part_lru, 0), (part_gla, 2), (part_mla, 3), (part_nsa, 1)):
            part(kb, c, Dl, YV, L)
            for ch in range(2):
                S.collective_async(ag(YB[ch * 512 + g * 128:ch * 512 + (g + 1) * 128, :], GY[(g * 2 + ch) * 256:(g * 2 + ch + 1) * 256, :]))
        S.cc_wait_all()
        GY4 = GY.rearrange("(g ch q) t -> g ch q t", g=4, ch=2)
        S.dma("act", lambda e: e.dma_start(out=MYY.rearrange("(g q) t -> g q t", g=4), in_=GY4[:, bass.ds(S.rt["hp_act"], 1), :, :].rearrange("g o q t -> g (o q) t")),
              writes=["MYY"])
        S.barrier()
        phase_C(kb, l == 1, xs, MYY, W["gng"], W["nfg"], fing, W["w_out"], W["w_gu"], W["w_dn"], out if l == 1 else XO)
        xs = XO
    return kb.close()


def prep_W(W, l, hp):
    A = np.ascontiguousarray
    o = {}
    ch = slice(hp * 128, hp * 128 + 128)
    o["lru_w"] = A(np.stack([W["lru_wa"][l][2 * hp:2 * hp + 2], W["lru_wx"][l][2 * hp:2 * hp + 2]]))
    o["lru_v"] = A(np.stack([W["conv_w"][l][0, ch], W["conv_w"][l][1, ch], W["conv_w"][l][2, ch], W["conv_w"][l][3, ch],
                             W["conv_b"][l][ch], W["lru_ba"][l][ch], W["lru_bx"][l][ch], W["lru_lambda"][l][ch]], axis=1))
    o["mla_wuq"] = A(W["mla_w_uq"][l][:, hp * 192:(hp + 1) * 192])
    wkv = W["mla_w_ukv"][l].reshape(128, 4, 128)
    o["mla_wk"] = A(wkv[:, 2 * hp:2 * hp + 2, 0:64].reshape(128, 128))
    o["mla_wv"] = A(wkv[:, 2 * hp:2 * hp + 2, 64:128].reshape(128, 128))
    mv = np.zeros((128, 3), np.float32)
    mv[:, 0] = W["mla_q_norm"][l][0:128]
    mv[0:64, 1] = W["mla_q_norm"][l][128:192]
    mv[:, 2] = W["mla_kv_norm"][l]
    o["mla_v"] = mv
    o["nsa_pos"] = A(np.concatenate([W["cmp_pos"][l][0].T, W["cmp_pos"][l][1].T], axis=0))
    o["nsa_w1"] = A(W["cmp_w1"][l])
    o["nsa_b1"] = A(W["cmp_b1"][l].reshape(2, 2, 128).transpose(2, 0, 1).reshape(128, 4))
    o["nsa_w2"] = A(W["cmp_w2"][l])
    o["gla_wg2"] = A(W["gla_wg2"][l][:, hp * 64:(hp + 1) * 64])
    g2 = np.zeros((128, 2), np.float32)
    g2[0:64, 0] = W["gla_bg2"][l][hp * 64:(hp + 1) * 64]
    g2[:, 1] = np.tile(W["gla_norm"][l], 2)
    o["gla_v2"] = g2
    return o


_PROG = {}


def _arr8(g):
    return np.ascontiguousarray(np.asarray(g, np.float32).reshape(8, 128).T)


def kernel(**inp):
    W = {k: np.asarray(v) for k, v in inp.items()}
    x = W["x"]
    Bn, Sn, Dm = x.shape
    HT = Sn // 2
    cores = [(b, r) for b in range(Bn) for r in range(2)]
    if "F" not in _PROG:
        _PROG["F"] = build_fused()
    nc = _PROG["F"]
    consts = host_consts()
    pc = perm_cols()
    ins = []
    for (b, r) in cores:
        d = dict(consts)
        d["pos"] = np.ascontiguousarray(W["positions"][b][None, :].astype(np.int32))
        d["xT"] = np.ascontiguousarray(x[b, r * HT:(r + 1) * HT].T)
        d["fing"] = _arr8(W["final_norm"])
        for l in range(2):
            for k, v in prep_W(W, l, r).items():
                d[f"{k}_{l}"] = v
            d[f"gain_{l}"] = _arr8(W["norm_mix"][l])
            d[f"w_in_{l}"] = np.ascontiguousarray(W["w_in"][l][:, pc])
            d[f"gng_{l}"] = _arr8(W["group_norm"][l])
            d[f"nfg_{l}"] = _arr8(W["norm_ffn"][l])
            d[f"w_out_{l}"] = np.ascontiguousarray(W["w_out"][l])
            d[f"w_gu_{l}"] = np.ascontiguousarray(W["w_gate_up"][l])
            d[f"w_dn_{l}"] = np.ascontiguousarray(W["w_down"][l])
        ins.append(d)
    res = run_bass_kernel_spmd(nc, ins, core_ids=list(range(8))).results
    out = np.empty((Bn, Sn, Dm), np.float32)
    for ci, (b, r) in enumerate(cores):
        out[b, r * HT:(r + 1) * HT] = res[ci]["out"].T
    return out
```

```python
import numpy as np
from contextlib import ExitStack
import concourse.bass as bass
import concourse.mybir as mybir
from concourse.bass_utils import run_bass_kernel_spmd

F32 = mybir.dt.float32
BF16 = mybir.dt.bfloat16
I32 = mybir.dt.int32
AF = mybir.ActivationFunctionType
ALU = mybir.AluOpType
AX = mybir.AxisListType

ENGS = ("pe", "act", "dve", "pool", "sp")
NDMA_SEMS = 8
EPS = 1e-6


class Sched:
    def __init__(self, nc, es):
        self.nc = nc
        self.sem = {e: es.enter_context(nc.semaphore("s_" + e)) for e in ENGS}
        self.cnt = {e: 0 for e in ENGS}
        self.dsem = {e: [es.enter_context(nc.semaphore(f"d_{e}{i}")) for i in range(NDMA_SEMS)]
                     for e in ("sp", "pool", "act")}
        self.dval = {e: [0] * NDMA_SEMS for e in self.dsem}
        self.drr = {e: 0 for e in self.dsem}
        self.ops = {e: [] for e in ENGS}
        self.known = {e: {} for e in ENGS}
        self.semobj = {}
        self.last_w = {}
        self.readers = {}
        self.ccsem = es.enter_context(nc.semaphore("s_cc"))
        self.semobj["cc"] = self.ccsem
        self.ccn = 0
        self.rt = {}
        for e in ENGS:
            self.semobj["c_" + e] = self.sem[e]
        for e in self.dsem:
            for i in range(NDMA_SEMS):
                self.semobj[f"d_{e}{i}"] = self.dsem[e][i]

    def _need(self, eng, tok, waits):
        if tok is None:
            return
        sk, val, teng = tok
        if teng == "pe" and eng == "pe" and sk == "c_pe":
            return
        if self.known[eng].get(sk, 0) >= val:
            return
        self.known[eng][sk] = val
        waits[sk] = max(waits.get(sk, 0), val)

    def _deps(self, eng, reads, writes):
        waits = {}
        for k in reads:
            self._need(eng, self.last_w.get(k), waits)
        for k in writes:
            self._need(eng, self.last_w.get(k), waits)
            for t in self.readers.get(k, ()):
                self._need(eng, t, waits)
        return waits

    def _commit(self, tok, reads, writes):
        for k in reads:
            self.readers.setdefault(k, []).append(tok)
        for k in writes:
            self.last_w[k] = tok
            self.readers[k] = []

    def op(self, eng, emit, reads=(), writes=()):
        waits = self._deps(eng, reads, writes)
        self.cnt[eng] += 1
        tok = ("c_" + eng, self.cnt[eng], eng)
        self.ops[eng].append((waits, emit, (self.sem[eng], 1)))
        self._commit(tok, reads, writes)
        return tok

    def dma(self, eng, emit, reads=(), writes=()):
        waits = self._deps(eng, reads, writes)
        i = self.drr[eng]
        self.drr[eng] = (i + 1) % NDMA_SEMS
        sk = f"d_{eng}{i}"
        prev = self.dval[eng][i]
        if prev and self.known[eng].get(sk, 0) < prev:
            self.known[eng][sk] = prev
            waits[sk] = prev
        self.dval[eng][i] = prev + 16
        tok = (sk, prev + 16, eng)
        self.ops[eng].append((waits, emit, (self.dsem[eng][i], 16)))
        self._commit(tok, reads, writes)
        return tok

    def barrier(self):
        for eng in ENGS:
            waits = {}
            for e in ENGS:
                if e != eng and self.cnt[e]:
                    self._need(eng, ("c_" + e, self.cnt[e], e), waits)
            for e in self.dsem:
                for i in range(NDMA_SEMS):
                    if self.dval[e][i]:
                        self._need(eng, (f"d_{e}{i}", self.dval[e][i], "dma"), waits)
            if eng != "pe" and self.cnt[eng]:
                self._need(eng, ("c_" + eng, self.cnt[eng], eng), waits)
            if waits:
                self.ops[eng].append((waits, None, None))
        self.last_w = {}
        self.readers = {}

    def collective(self, emits):
        self.barrier()
        for emit in emits:
            w = {"cc": self.ccn} if self.ccn else {}
            self.ccn += 1
            self.ops["pool"].append((w, emit, (self.ccsem, 1)))
        for eng in ENGS:
            self.known[eng]["cc"] = self.ccn
            self.ops[eng].append(({"cc": self.ccn}, None, None))

    def collective_async(self, emit, reads=()):
        waits = {}
        for k in reads:
            self._need("pool", self.last_w.get(k), waits)
        if self.ccn:
            waits["cc"] = self.ccn
        self.ccn += 1
        self.ops["pool"].append((waits, emit, (self.ccsem, 1)))

    def cc_wait(self, n):
        self.barrier()
        for eng in ENGS:
            if self.known[eng].get("cc", 0) < n:
                self.known[eng]["cc"] = n
                self.ops[eng].append(({"cc": n}, None, None))

    def cc_wait_all(self):
        self.barrier()
        for eng in ENGS:
            if self.known[eng].get("cc", 0) < self.ccn:
                self.known[eng]["cc"] = self.ccn
                self.ops[eng].append(({"cc": self.ccn}, None, None))

    def finish(self):
        self.barrier()
        semobj = self.semobj

        def run(engobj, lst):
            for waits, emit, inc in lst:
                for sk, v in waits.items():
                    engobj.wait_ge(semobj[sk], v)
                if emit is not None:
                    emit(engobj).then_inc(inc[0], inc[1])

        with self.nc.Block() as block:
            @block.tensor
            def _(e):
                run(e, self.ops["pe"])

            @block.scalar
            def _(e):
                self.rt["hp_act"] = e.partition_id() % 2
                run(e, self.ops["act"])

            @block.vector
            def _(e):
                run(e, self.ops["dve"])

            @block.gpsimd
            def _(e):
                run(e, self.ops["pool"])

            @block.sync
            def _(e):
                self.rt["hp_sp"] = e.partition_id() % 2
                run(e, self.ops["sp"])


class KB:
    def __init__(self, arena_cols=50000):
        self.nc = bass.Bass("TRN2", target_bir_lowering=False)
        self.es = ExitStack()
        self.S = Sched(self.nc, self.es)
        self.arena = self.es.enter_context(self.nc.sbuf_tensor("arena", [128, arena_cols], F32))
        self.acols = arena_cols
        self.top = 0
        self.psb = [self.es.enter_context(self.nc.psum_tensor(f"psb{i}", [128, 512], F32)) for i in range(8)]
        self.uid = 0

    def dint(self, name, shape, dt=F32):
        return self.nc.dram_tensor(name, list(shape), dt).ap()

    def din(self, name, shape, dt=F32):
        return self.nc.dram_tensor(name, list(shape), dt, kind="ExternalInput").ap()

    def dout(self, name, shape, dt=F32):
        return self.nc.dram_tensor(name, list(shape), dt, kind="ExternalOutput").ap()

    def alloc(self, cols, dt=F32):
        n32 = cols if dt != BF16 else (cols + 1) // 2
        a = self.top
        self.top += n32
        assert self.top <= self.acols, f"arena overflow {self.top}"
        v = self.arena[:, a:a + n32]
        if dt == BF16:
            v = v.bitcast(BF16)
        elif dt == I32:
            v = v.bitcast(I32)
        return v

    def mark(self):
        return self.top

    def release(self, m):
        self.top = m

    def key(self, base="k"):
        self.uid += 1
        return f"{base}{self.uid}"

    def close(self):
        self.S.finish()
        self.es.close()
        return self.nc


NCOL = 2300
NCOLP = 1920
NCOLW = NCOLP + 384
VCOLS = [(NCOLP, NCOLW)]
NV = 384
OFF = dict(a_x=0, a_gate=256, b_q=512, b_kv=768, c_q=1024, c_k=1152, c_og=1280, d_cq=1536, d_kr=1728, c_glr=1760,
           b_gate=1776, d_ckv=1792)


def perm_cols():
    o = dict(a_x=0, a_gate=256, b_q=512, b_kv=768, b_gate=1152, c_q=1164, c_k=1292, c_v=1420, c_glr=1676, c_og=1692,
             d_cq=1948, d_ckv=2140, d_kr=2268)
    r = lambda a, n: list(range(a, a + n))
    p = (r(o["a_x"], 256) + r(o["a_gate"], 256) + r(o["b_q"], 256)
         + r(o["b_kv"], 64) + r(o["b_kv"] + 64, 64) + r(o["b_kv"] + 128, 64) + r(o["b_kv"] + 256, 64)
         + r(o["c_q"], 128) + r(o["c_k"], 128) + r(o["c_og"], 256) + r(o["d_cq"], 192) + r(o["d_kr"], 32)
         + r(o["c_glr"], 16) + r(o["b_gate"], 12) + r(o["b_gate"], 4) + r(o["d_ckv"], 128))
    assert len(p) == NCOLP
    p += r(o["b_kv"] + 192, 64) + r(o["b_kv"] + 320, 64) + r(o["c_v"], 256)
    assert len(p) == NCOLW
    return np.array(p)
DFF = 2816


def load_cast_weight(kb, w_dram, wsb, kchunks, ncols, stage, key, piece=1024):
    S = kb.S
    i = 0
    for c in range(kchunks):
        for c0 in range(0, ncols, piece):
            c1 = min(ncols, c0 + piece)
            st = stage[i % 2]
            sk = f"wstage{i % 2}"
            S.dma("sp", lambda e, st=st, c=c, c0=c0, c1=c1: e.dma_start(out=st[:, 0:c1 - c0], in_=w_dram[c * 128:(c + 1) * 128, c0:c1]),
                  writes=[sk])
            eng = "act" if i % 2 == 0 else "pool"
            if eng == "act":
                S.op("act", lambda e, st=st, c=c, c0=c0, c1=c1: e.copy(out=wsb[:, c, c0:c1], in_=st[:, 0:c1 - c0]), reads=[sk], writes=[key])
            else:
                S.op("pool", lambda e, st=st, c=c, c0=c0, c1=c1: e.tensor_copy(out=wsb[:, c, c0:c1], in_=st[:, 0:c1 - c0]), reads=[sk], writes=[key])
            i += 1


def rms_stats(kb, src3, nch, T, sq, ones_bf, ps_ap, rstd, denom, keys_in, key_sq, key_ps, key_rstd):
    S = kb.S
    S.op("act", lambda e: e.activation(out=sq, in_=src3, func=AF.Square), reads=keys_in, writes=[key_sq])
    for c in range(nch):
        S.op("pe", lambda e, c=c: e.matmul(ps_ap, lhsT=ones_bf, rhs=sq[:, c, :], start=(c == 0), stop=(c == nch - 1)),
             reads=[key_sq, "ones_bf"], writes=[key_ps])
    S.op("act", lambda e: e.activation(out=rstd, in_=ps_ap, func=AF.Sqrt, bias=EPS, scale=1.0 / denom), writes=[key_ps, key_rstd])
    S.op("dve", lambda e: e.reciprocal(out=rstd, in_=rstd), reads=[key_rstd], writes=[key_rstd])


def phase_A(kb, xT, gain, w, pT, pV, GP, GV, ag):
    S = kb.S
    m0 = kb.mark()
    T = 512
    NT = 8
    xv = xT.rearrange("(c p) t -> p c t", p=128)

    wsb = kb.alloc(8 * NCOLW, BF16).rearrange("p (c n) -> p c n", c=8)
    gsb = kb.alloc(8)
    ones_bf = kb.alloc(128, BF16)
    stage = [kb.alloc(1024), kb.alloc(1024)]
    xt = [kb.alloc(8 * T).rearrange("p (c t) -> p c t", c=8) for _ in range(2)]
    sq = kb.alloc(8 * T, BF16).rearrange("p (c t) -> p c t", c=8)
    hb = kb.alloc(8 * 4096, BF16).rearrange("p (c t) -> p c t", c=8)
    rstd = kb.alloc(T)
    ost = [kb.alloc(T) for _ in range(4)]
    P = [p[:] for p in kb.psb]

    S.dma("sp", lambda e: e.dma_start(out=gsb, in_=gain[:, :]), writes=["gsb"])
    S.op("pool", lambda e: e.memset(ones_bf, 1.0), writes=["ones_bf"])
    S.dma("sp", lambda e: e.dma_start(out=xt[0], in_=xv[:, :, 0:T]), writes=["xt0"])
    load_cast_weight(kb, w, wsb, 8, NCOLW, stage, "wsb")
    for t in range(NT):
        b = t % 2
        if t + 1 < NT:
            S.dma("sp", lambda e, t=t: e.dma_start(out=xt[(t + 1) % 2], in_=xv[:, :, (t + 1) * T:(t + 2) * T]),
                  writes=[f"xt{(t + 1) % 2}"])
        rms_stats(kb, xt[b], 8, T, sq, ones_bf, P[0], rstd, 1024.0, [f"xt{b}"], "sq", "psb0", "rstd")
        for c in range(8):
            S.op("dve", lambda e, c=c, b=b, t=t: e.scalar_tensor_tensor(out=hb[:, c, t * T:(t + 1) * T], in0=xt[b][:, c, :], scalar=gsb[:, c:c + 1], in1=rstd,
                                                                   op0=ALU.mult, op1=ALU.mult),
                 reads=[f"xt{b}", "rstd", "gsb"], writes=[f"hb{t}"])
    oi = 0
    for t in range(NT):
        for tb in range(4):
            pb = 5 + (tb % 2)
            for c in range(8):
                S.op("pe", lambda e, c=c, t=t, tb=tb, pb=pb: e.matmul(
                    P[pb][:, 0:NV], lhsT=hb[:, c, t * T + tb * 128:t * T + (tb + 1) * 128], rhs=wsb[:, c, NCOLP:NCOLW], start=(c == 0), stop=(c == 7)),
                    reads=[f"hb{t}", "wsb"], writes=[f"psb{pb}"])
            o = oi % 4
            oi += 1
            S.op("act", lambda e, o=o, pb=pb: e.copy(out=ost[o][:, 0:NV], in_=P[pb][:, 0:NV]), writes=[f"psb{pb}", f"ost{o}"])
            S.dma("sp", lambda e, o=o, t=t, tb=tb: e.dma_start(out=pV[t * T + tb * 128: t * T + (tb + 1) * 128, :], in_=ost[o][:, 0:NV]),
                  reads=[f"ost{o}"], writes=[f"XV{t}_{tb}"])
        if t % 2 == 1:
            j = t // 2
            S.collective_async(ag(pV[j * 1024:(j + 1) * 1024, :], GV[j * 2048:(j + 1) * 2048, :]),
                               reads=[f"XV{tt}_{tb}" for tt in (t - 1, t) for tb in range(4)])
    for k in range(NCOLP // 128):
        c0, c1 = k * 128, (k + 1) * 128
        for t in range(NT):
            pb = 1 + (oi % 4)
            for c in range(8):
                S.op("pe", lambda e, c=c, t=t, c0=c0, c1=c1, pb=pb: e.matmul(P[pb][:, :], lhsT=wsb[:, c, c0:c1], rhs=hb[:, c, t * T:(t + 1) * T],
                                                                             start=(c == 0), stop=(c == 7)),
                     reads=[f"hb{t}", "wsb"], writes=[f"psb{pb}"])
            o = oi % 4
            oi += 1
            if t % 2 == 0:
                S.op("act", lambda e, o=o, pb=pb: e.copy(out=ost[o], in_=P[pb][:, :]), writes=[f"psb{pb}", f"ost{o}"])
            else:
                S.op("dve", lambda e, o=o, pb=pb: e.tensor_copy(out=ost[o], in_=P[pb][:, :]), writes=[f"psb{pb}", f"ost{o}"])
            S.dma("sp", lambda e, o=o, c0=c0, c1=c1, t=t: e.dma_start(out=pT[c0:c1, t * T:(t + 1) * T], in_=ost[o]),
                  reads=[f"ost{o}"], writes=[f"XA{k}_{t}"])
        S.collective_async(ag(pT[c0:c1, :], GP[k * 256:(k + 1) * 256, :]), reads=[f"XA{k}_{t}" for t in range(NT)])
        if k == 3:
            kb.cc_lru = S.ccn
    S.barrier()
    kb.release(m0)


def phase_C(kb, final, xT, GY, gng, nfg, fing, w_out, w_gu, w_dn, xo, NT=16):
    S = kb.S
    m0 = kb.mark()
    T = 256
    xv = xT.rearrange("(c p) t -> p c t", p=128)
    ov = xo.rearrange("(c p) t -> p c t", p=128)

    wo = kb.alloc(8 * 1024, BF16).rearrange("p (c n) -> p c n", c=8)
    wgu = kb.alloc(8 * 2 * DFF, BF16).rearrange("p (c n) -> p c n", c=8)
    wdn = kb.alloc(22 * 1024, BF16).rearrange("p (c n) -> p c n", c=22)
    g3 = kb.alloc(24)
    ones_bf = kb.alloc(128, BF16)
    xts = [kb.alloc(8 * T).rearrange("p (c t) -> p c t", c=8) for _ in range(2)]
    yt = kb.alloc(8 * T).rearrange("p (c t) -> p c t", c=8)
    ytf = yt.rearrange("p c t -> p (c t)")
    stage = [ytf[:, 0:1024], ytf[:, 1024:2048]]
    sq = kb.alloc(8 * T, BF16).rearrange("p (c t) -> p c t", c=8)
    hb = kb.alloc(8 * T, BF16).rearrange("p (c t) -> p c t", c=8)
    aT = kb.alloc(22 * T, BF16).rearrange("p (c t) -> p c t", c=22)
    rstd4 = kb.alloc(4 * T).rearrange("p (c t) -> p c t", c=4)
    rstd = kb.alloc(T)
    sg = [kb.alloc(T), kb.alloc(T)]
    P = [p[:] for p in kb.psb]

    S.dma("sp", lambda e: e.dma_start(out=g3[:, 0:8], in_=gng[:, :]), writes=["g3"])
    S.dma("sp", lambda e: e.dma_start(out=g3[:, 8:16], in_=nfg[:, :]), writes=["g3"])
    S.dma("sp", lambda e: e.dma_start(out=g3[:, 16:24], in_=fing[:, :]), writes=["g3"])
    S.op("pool", lambda e: e.memset(ones_bf, 1.0), writes=["ones_bf"])
    load_cast_weight(kb, w_out, wo, 8, 1024, stage, "wo")
    load_cast_weight(kb, w_gu, wgu, 8, 2 * DFF, stage, "wgu")
    load_cast_weight(kb, w_dn, wdn, 22, 1024, stage, "wdn")
    S.barrier()

    def load_y(t):
        for cc_ in range(8):
            r0 = cc_ * 128
            S.dma("sp", lambda e, cc_=cc_, r0=r0, t=t: e.dma_start(out=yt[:, cc_, :], in_=GY[r0:r0 + 128, t * T:(t + 1) * T]),
                  writes=["yt"])

    S.dma("sp", lambda e: e.dma_start(out=xts[0], in_=xv[:, :, 0:T]), writes=["xt0"])
    load_y(0)
    for t in range(NT):
        ts = slice(t * T, (t + 1) * T)
        xt = xts[t % 2]
        XK = f"xt{t % 2}"
        if t + 1 < NT:
            S.dma("sp", lambda e, t=t: e.dma_start(out=xts[(t + 1) % 2], in_=xv[:, :, (t + 1) * T:(t + 2) * T]), writes=[f"xt{(t + 1) % 2}"])
        S.op("act", lambda e: e.activation(out=sq, in_=yt, func=AF.Square), reads=["yt"], writes=["sq"])
        for g in range(4):
            pa = P[g][:, 0:T]
            for j in range(2):
                S.op("pe", lambda e, g=g, j=j, pa=pa: e.matmul(pa, lhsT=ones_bf, rhs=sq[:, 2 * g + j, :], start=(j == 0), stop=(j == 1)),
                     reads=["sq", "ones_bf"], writes=[f"psb{g}"])
            S.op("act", lambda e, g=g, pa=pa: e.activation(out=rstd4[:, g, :], in_=pa, func=AF.Sqrt, bias=EPS, scale=1.0 / 256.0),
                 writes=[f"psb{g}", f"rstd4_{g}"])
            S.op("dve", lambda e, g=g: e.reciprocal(out=rstd4[:, g, :], in_=rstd4[:, g, :]), reads=[f"rstd4_{g}"], writes=[f"rstd4_{g}"])
        for c in range(8):
            eng = "dve"
            S.op(eng, lambda e, c=c: e.scalar_tensor_tensor(out=hb[:, c, :], in0=yt[:, c, :], scalar=g3[:, c:c + 1], in1=rstd4[:, c // 2, :],
                                                         op0=ALU.mult, op1=ALU.mult),
                 reads=["yt", f"rstd4_{c // 2}", "g3"], writes=[f"hb{c}"])
        if t + 1 < NT:
            load_y(t + 1)
        for m in range(8):
            pa = P[4 + m % 4][:, 0:T]
            for c in range(8):
                S.op("pe", lambda e, m=m, c=c, pa=pa: e.matmul(pa, lhsT=wo[:, c, m * 128:(m + 1) * 128], rhs=hb[:, c, :], start=(c == 0), stop=(c == 7)),
                     reads=[f"hb{c}", "wo"], writes=[f"psb{4 + m % 4}"])
            S.op("dve", lambda e, m=m, pa=pa, xt=xt: e.tensor_tensor(out=xt[:, m, :], in0=xt[:, m, :], in1=pa, op=ALU.add),
                 reads=[XK], writes=[XK, f"psb{4 + m % 4}"])
        rms_stats(kb, xt, 8, T, sq, ones_bf, P[0][:, 0:T], rstd, 1024.0, [XK], "sq", "psb0", "rstd")
        for c in range(8):
            eng = "dve"
            S.op(eng, lambda e, c=c, xt=xt: e.scalar_tensor_tensor(out=hb[:, c, :], in0=xt[:, c, :], scalar=g3[:, 8 + c:9 + c], in1=rstd,
                                                         op0=ALU.mult, op1=ALU.mult),
                 reads=[XK, "rstd", "g3"], writes=[f"hb{c}"])
        for j in range(22):
            pg = P[j % 2][:, 0:T]
            pu = P[2 + j % 2][:, 0:T]
            for c in range(8):
                S.op("pe", lambda e, j=j, c=c, pg=pg: e.matmul(pg, lhsT=wgu[:, c, j * 128:(j + 1) * 128], rhs=hb[:, c, :], start=(c == 0), stop=(c == 7)),
                     reads=[f"hb{c}", "wgu"], writes=[f"psb{j % 2}"])
            for c in range(8):
                S.op("pe", lambda e, j=j, c=c, pu=pu: e.matmul(pu, lhsT=wgu[:, c, DFF + j * 128:DFF + (j + 1) * 128], rhs=hb[:, c, :], start=(c == 0), stop=(c == 7)),
                     reads=[f"hb{c}", "wgu"], writes=[f"psb{2 + j % 2}"])
            S.op("act", lambda e, j=j, pg=pg: e.activation(out=sg[j % 2], in_=pg, func=AF.Silu), writes=[f"psb{j % 2}", f"sg{j % 2}"])
            S.op("dve", lambda e, j=j, pu=pu: e.tensor_tensor(out=aT[:, j, :], in0=sg[j % 2], in1=pu, op=ALU.mult),
                 reads=[f"sg{j % 2}"], writes=[f"aT{j}", f"psb{2 + j % 2}"])
        for m in range(8):
            pa = P[4 + m % 4][:, 0:T]
            for k in range(22):
                S.op("pe", lambda e, m=m, k=k, pa=pa: e.matmul(pa, lhsT=wdn[:, k, m * 128:(m + 1) * 128], rhs=aT[:, k, :], start=(k == 0), stop=(k == 21)),
                     reads=[f"aT{k}", "wdn"], writes=[f"psb{4 + m % 4}"])
            S.op("dve", lambda e, m=m, pa=pa, xt=xt: e.tensor_tensor(out=xt[:, m, :], in0=xt[:, m, :], in1=pa, op=ALU.add),
                 reads=[XK], writes=[XK, f"psb{4 + m % 4}"])
        if final:
            rms_stats(kb, xt, 8, T, sq, ones_bf, P[0][:, 0:T], rstd, 1024.0, [XK], "sq", "psb0", "rstd")
            for c in range(8):
                eng = "dve"
                S.op(eng, lambda e, c=c, xt=xt: e.scalar_tensor_tensor(out=xt[:, c, :], in0=xt[:, c, :], scalar=g3[:, 16 + c:17 + c], in1=rstd,
                                                                    op0=ALU.mult, op1=ALU.mult),
                     reads=["rstd", "g3"], writes=[XK])
            S.dma("sp", lambda e, ts=ts, xt=xt: e.dma_start(out=ov[:, :, ts], in_=xt), reads=[XK])
        else:
            S.dma("sp", lambda e, ts=ts, xt=xt: e.dma_start(out=ov[:, :, ts], in_=xt), reads=[XK])
    S.barrier()
    kb.release(m0)


N = 8192
TQ = 512
NQT = N // TQ
NEG = -30000.0
PI = float(np.pi)
TWO_PI = float(2 * np.pi)
THETA = 10000.0


def pk(b):
    return f"psb{b}"


def host_consts():
    c = {}
    c["ident"] = np.eye(128, dtype=np.float32)
    R = np.zeros((128, 128), np.float32)
    for blk in range(2):
        for m in range(64):
            if m < 32:
                R[blk * 64 + m + 32, blk * 64 + m] = -1.0
            else:
                R[blk * 64 + m - 32, blk * 64 + m] = 1.0
    c["rbd"] = R
    R32 = np.zeros((128, 32), np.float32)
    for m in range(32):
        if m < 16:
            R32[64 + m + 16, m] = -1.0
        else:
            R32[64 + m - 16, m] = 1.0
    c["r32"] = R32
    invf = np.zeros((128, 2), np.float32)
    for p in range(128):
        invf[p, 0] = np.float32(THETA) ** np.float32(-(2.0 * ((p % 64) % 32)) / 64.0)
    for p in range(64, 96):
        invf[p, 1] = np.float32(THETA) ** np.float32(-(2.0 * ((p - 64) % 16)) / 32.0)
    c["invf"] = invf
    k = np.arange(128)[:, None]
    q = np.arange(512)[None, :]
    c["tri"] = np.stack([np.where(k + i * 128 <= q, 0.0, NEG) for i in range(4)]).astype(np.float32)
    c["win"] = np.stack([np.where((k + (i - 4) * 128 <= q) & (k + (i - 4) * 128 > q - 512), 0.0, NEG) for i in range(8)]).astype(np.float32)
    c["cmpm"] = np.stack([np.where(16 * k + 31 <= i * 512 + q, 0.0, NEG) for i in range(5)]).astype(np.float32)
    n = np.arange(512)
    s = np.arange(128)
    ov = ((n[:, None] * 16 < s[None, :] * 64 + 64) & (n[:, None] * 16 + 32 > s[None, :] * 64)).astype(np.float32)
    ov[511] = 0.0
    ovl = np.zeros((4, 128, 129), np.float32)
    ovl[:, :, :128] = ov.reshape(4, 128, 128)
    ovl[:, :, 128] = 1.0
    c["ovl"] = ovl
    c["eall"] = (np.arange(N)[None, :] // 64 == np.arange(128)[:, None]).astype(np.float32)
    ql = np.arange(128)[:, None] // 64
    j = np.arange(254)[None, :] - 126
    c["bv"] = (j <= ql - 2).astype(np.float32)
    c["bf"] = (np.where((j == ql) | (j == ql - 1), 1e9, 0.0) + np.where(j > ql, -1.0, 0.0)).astype(np.float32)
    selg = np.zeros((8, 6 * 64), np.float32)
    for r in range(6):
        selg[r, r * 64:(r + 1) * 64] = 1.0
    c["selg"] = selg
    c["tri64"] = (np.arange(64)[:, None] <= np.arange(64)[None, :]).astype(np.float32)
    return c


CONST_SHAPES = {"ident": [128, 128], "rbd": [128, 128], "r32": [128, 32], "invf": [128, 2], "tri": [4, 128, 512],
                "win": [8, 128, 512], "cmpm": [5, 128, 512], "ovl": [4, 128, 129], "eall": [128, N], "bv": [128, 254],
                "bf": [128, 254], "selg": [8, 384], "tri64": [64, 64]}

IN_SHAPES = {
    "pos": ([1, N], I32),
    "lru_x": ([2, 128, N], F32), "lru_w": ([2, 2, 64, 64], F32), "lru_v": ([128, 8], F32),
    "mla_cq": ([192, N], F32), "mla_ckv": ([128, N], F32), "mla_kr": ([32, N], F32),
    "mla_wuq": ([192, 192], F32), "mla_wk": ([128, 128], F32), "mla_wv": ([128, 128], F32), "mla_v": ([128, 3], F32),
    "nsa_q": ([2, 128, N], F32), "nsa_k": ([3, 64, N], F32), "nsa_vc": ([64, N], F32), "nsa_vs": ([N, 64], F32),
    "nsa_vw": ([N, 64], F32), "nsa_g": ([6, N], F32), "nsa_pos": ([128, 32], F32), "nsa_w1": ([2, 2048, 256], F32),
    "nsa_b1": ([128, 4], F32), "nsa_w2": ([2, 256, 64], F32),
    "gla_q": ([64, N], F32), "gla_k": ([64, N], F32), "gla_v": ([N, 128], F32), "gla_glr": ([16, N], F32),
    "gla_og": ([128, N], F32), "gla_wg2": ([16, 64], F32), "gla_v2": ([128, 2], F32),
}


class Ctx:
    pass


class YView:
    def __init__(self, ap):
        self.ap = ap

    def __getitem__(self, key):
        rs, cs = key
        half = cs.start // 4096
        assert (cs.stop - 1) // 4096 == half
        return self.ap[half * 512 + rs.start:half * 512 + rs.stop, cs.start - half * 4096:cs.stop - half * 4096]


SEL = [("a_x", 128, False, 128), ("a_gate", 128, False, 128), ("b_q", 128, False, 128), ("b_q", 128, True, 128),
       ("b_gate", 6, False, 6), ("c_q", 64, False, 64), ("c_k", 64, False, 64), ("c_og", 128, False, 128)]
SELROWS = sum(x[3] for x in SEL)


class Loader:
    def __init__(self, S, GP, GV, MYP, MYV):
        self.S = S
        self.GP, self.GV, self.MYP, self.MYV = GP, GV, MYP, MYV
        self.row0 = {}
        r = 0
        for nm, hpm, inv, n in SEL:
            self.row0[(nm, inv)] = r
            r += n

    @staticmethod
    def gprow(r, half):
        return (r // 128) * 256 + half * 128 + r % 128

    @staticmethod
    def gvrow(t):
        half, tl = t // 4096, t % 4096
        return (tl // 1024) * 2048 + half * 1024 + tl % 1024

    def select(self, names=None, with_v=True):
        S = self.S
        i = 0
        for nm, hpm, inv, n in SEL:
            if names is not None and nm not in names:
                i += 2
                continue
            r0 = self.row0[(nm, inv)]
            mult = 256 if hpm == 128 else hpm
            for hf in range(2):
                q = "sp" if i % 2 == 0 else "act"
                i += 1

                def emit(e, nm=nm, mult=mult, inv=inv, n=n, r0=r0, hf=hf, q=q):
                    hp = S.rt["hp_" + q]
                    start = ((1 - hp) if inv else hp) * mult + self.gprow(OFF[nm], hf)
                    return e.dma_start(out=self.MYP[r0:r0 + n, hf * 4096:(hf + 1) * 4096], in_=self.GP[bass.ds(start, n), :])
                S.dma(q, emit, writes=[f"MYP{i}"])
        for j in (range(4) if with_v else ()):
            S.dma("sp", lambda e, j=j: e.dma_start(out=self.MYV[j * 2048:(j + 1) * 2048, :],
                                                   in_=self.GV[:, bass.ds(S.rt["hp_sp"] * 128 + 128, 128)][j * 2048:(j + 1) * 2048, :]), writes=[f"MYV{j}"])
        S.barrier()

    def pt(self, off, hpm, n, c0, c1, inv=False):
        if hpm == 0:
            half = c0 // 4096
            assert off // 128 == (off + n - 1) // 128
            base = self.gprow(off, half)
            return self.GP[base:base + n, c0 - half * 4096:c1 - half * 4096]
        for nm, hm, iv, nn in SEL:
            if hm == hpm and iv == inv and OFF[nm] <= off and (off - OFF[nm]) + n <= nn:
                r0 = self.row0[(nm, inv)] + (off - OFF[nm])
                return self.MYP[r0:r0 + n, c0:c1]
        raise KeyError((off, hpm, n, inv))

    def pv(self, coff, hpm, n, t0, t1):
        assert t0 // 1024 == (t1 - 1) // 1024
        g0 = self.gvrow(t0)
        if hpm == 0:
            return self.GV[g0:g0 + (t1 - t0), coff:coff + n]
        assert coff == 128 and n == 128
        return self.MYV[g0:g0 + (t1 - t0), :]


def common_setup(kb, D):
    S = kb.S
    c = Ctx()
    c.ident = kb.alloc(128)
    c.ones_f = kb.alloc(128)
    c.ones_bf = kb.alloc(128, BF16)
    c.ident_bf = kb.alloc(128, BF16)
    S.dma("sp", lambda e: e.dma_start(out=c.ident, in_=D["ident"][:, :]), writes=["ident"])
    S.op("pool", lambda e: e.memset(c.ones_f, 1.0), writes=["ones_f"])
    S.op("pool", lambda e: e.memset(c.ones_bf, 1.0), writes=["ones_bf"])
    S.op("act", lambda e: e.copy(out=c.ident_bf, in_=c.ident), reads=["ident"], writes=["ident_bf"])
    c.invf = kb.alloc(2)
    S.dma("sp", lambda e: e.dma_start(out=c.invf, in_=D["invf"][:, :]), writes=["invf"])
    return c


def rope_tables(kb, c, posf, posk, r0, r1, col, T, bank, tag):
    S = kb.S
    P = kb.psb[bank]
    n = r1 - r0
    rs = slice(r0, r1)
    a, kf, ki, sn, cs = T["ang"], T["kf"], T["ki"], T["sin"], T["cos"]
    S.op("pe", lambda e: e.matmul(P[rs, :], lhsT=c.ones_f[0:1, 0:n], rhs=posf[0:1, :], start=True, stop=True),
         reads=["ones_f", posk], writes=[pk(bank)])
    S.op("dve", lambda e: e.tensor_scalar(out=a[rs, :], in0=P[rs, :], scalar1=c.invf[rs, col:col + 1], scalar2=None, op0=ALU.mult),
         reads=["invf"], writes=[pk(bank), tag + "ang"])
    S.op("dve", lambda e: e.tensor_scalar(out=ki[rs, :], in0=a[rs, :], scalar1=1.0 / TWO_PI, scalar2=None, op0=ALU.mult),
         reads=[tag + "ang"], writes=[tag + "ki"])
    S.op("dve", lambda e: e.tensor_copy(out=kf[rs, :], in_=ki[rs, :]), reads=[tag + "ki"], writes=[tag + "kf"])
    S.op("dve", lambda e: e.scalar_tensor_tensor(out=a[rs, :], in0=kf[rs, :], scalar=-TWO_PI, in1=a[rs, :], op0=ALU.mult, op1=ALU.add),
         reads=[tag + "kf"], writes=[tag + "ang"])
    S.op("dve", lambda e: e.tensor_scalar(out=kf[rs, :], in0=a[rs, :], scalar1=PI, scalar2=-TWO_PI, op0=ALU.is_gt, op1=ALU.mult),
         reads=[tag + "ang"], writes=[tag + "kf"])
    S.op("dve", lambda e: e.tensor_tensor(out=sn[rs, :], in0=a[rs, :], in1=kf[rs, :], op=ALU.add),
         reads=[tag + "ang", tag + "kf"], writes=[tag + "sin"])
    S.op("dve", lambda e: e.tensor_scalar(out=a[rs, :], in0=a[rs, :], scalar1=PI / 2, scalar2=None, op0=ALU.add),
         reads=[], writes=[tag + "ang"])
    S.op("dve", lambda e: e.tensor_scalar(out=kf[rs, :], in0=a[rs, :], scalar1=PI, scalar2=-TWO_PI, op0=ALU.is_gt, op1=ALU.mult),
         reads=[tag + "ang"], writes=[tag + "kf"])
    S.op("dve", lambda e: e.tensor_tensor(out=cs[rs, :], in0=a[rs, :], in1=kf[rs, :], op=ALU.add),
         reads=[tag + "ang", tag + "kf"], writes=[tag + "cos"])
    S.op("act", lambda e: e.activation(out=sn[rs, :], in_=sn[rs, :], func=AF.Sin), writes=[tag + "sin"])
    S.op("act", lambda e: e.activation(out=cs[rs, :], in_=cs[rs, :], func=AF.Sin), writes=[tag + "cos"])


def alloc_tables(kb):
    return {"ang": kb.alloc(512), "kf": kb.alloc(512), "ki": kb.alloc(512, I32), "sin": kb.alloc(512), "cos": kb.alloc(512)}


def load_pos(kb, D, posi, posf, t):
    S = kb.S
    S.dma("sp", lambda e: e.dma_start(out=posi[0:1, :], in_=D["pos"][0:1, t * TQ:(t + 1) * TQ]), writes=["posi"])
    S.op("dve", lambda e: e.tensor_copy(out=posf[0:1, :], in_=posi[0:1, :]), reads=["posi"], writes=["posf"])


class Attn:
    def __init__(self, kb, sbanks=(0, 1), npt=3):
        self.kb = kb
        self.sbanks = sbanks
        self.PT = [kb.alloc(512, BF16) for _ in range(npt)]
        self.pti = 0
        self.si = 0

    def run(self, blocks, obank, defer=None):
        kb = self.kb
        S = kb.S
        n = len(blocks)
        O = kb.psb[obank]
        banks = []

        def scores(i):
            bank = self.sbanks[self.si % len(self.sbanks)]
            self.si += 1
            banks.append(bank)
            mms = blocks[i][0]
            for j, (l, r, ks) in enumerate(mms):
                S.op("pe", lambda e, l=l, r=r, j=j, bank=bank, nm=len(mms): e.matmul(kb.psb[bank][:, :], lhsT=l, rhs=r, start=(j == 0), stop=(j == nm - 1)),
                     reads=ks, writes=[pk(bank)])

        scores(0)
        for i in range(n):
            if i + 1 < n:
                scores(i + 1)
            bank = banks[i]
            pi_ = self.pti % len(self.PT)
            self.pti += 1
            pt = self.PT[pi_]
            S.op("act", lambda e, pt=pt, bank=bank: e.activation(out=pt, in_=kb.psb[bank][:, :], func=AF.Exp), writes=[pk(bank), f"PT{pi_}"])
            v, vk = blocks[i][1], blocks[i][2]
            S.op("pe", lambda e, v=v, pt=pt, i=i: e.matmul(O[0:128, :], lhsT=v, rhs=pt, start=(i == 0), stop=(i == n - 1)),
                 reads=[f"PT{pi_}"] + vk, writes=[pk(obank)])
            if defer and i == min(2, n - 1):
                for f in defer:
                    f()
                del defer[:]


def norm_coef(kb, c, obank, rowbuf, bcbank, bcs):
    S = kb.S
    O = kb.psb[obank]
    B = kb.psb[bcbank]
    S.op("dve", lambda e: e.tensor_scalar_max(out=rowbuf[64:65, :], in0=O[64:65, :], scalar1=1e-30), writes=[pk(obank), "rowbuf"])
    S.op("dve", lambda e: e.reciprocal(out=rowbuf[64:65, :], in_=rowbuf[64:65, :]), writes=["rowbuf"])
    S.op("pe", lambda e: e.matmul(B[0:64, :], lhsT=c.ones_f[64:65, 0:64], rhs=rowbuf[64:65, :], start=True, stop=True),
         reads=["rowbuf", "ones_f"], writes=[pk(bcbank)])
    S.op("act", lambda e: e.copy(out=bcs[0:64, :], in_=B[0:64, :]), writes=[pk(bcbank), "bcs"])


def part_lru(kb, c, D, yT, L):
    S = kb.S
    m0 = kb.mark()
    xa = kb.alloc(N + 4)
    xc = kb.alloc(N)
    A = kb.alloc(N)
    U = kb.alloc(N)
    G = kb.alloc(N)
    xcb = kb.alloc(N, BF16)
    vec = kb.alloc(16)
    wtmp = kb.alloc(256)
    wbd = kb.alloc(256, BF16)
    T1 = xa[:, 0:N]
    P = kb.psb
    S.op("dve", lambda e: e.memset(xa[:, 0:3], 0.0), writes=["xa_pad"])
    for hf in range(2):
        S.dma("sp", lambda e, hf=hf: e.dma_start(out=xa[:, 3 + hf * 4096:3 + (hf + 1) * 4096], in_=L.pt(OFF["a_x"], 128, 128, hf * 4096, (hf + 1) * 4096)), writes=["xa"])
        S.dma("sp", lambda e, hf=hf: e.dma_start(out=G[:, hf * 4096:(hf + 1) * 4096], in_=L.pt(OFF["a_gate"], 128, 128, hf * 4096, (hf + 1) * 4096)), writes=["G"])
    S.dma("sp", lambda e: e.dma_start(out=vec[:, 0:8], in_=D["lru_v"][:, :]), writes=["vec"])
    S.op("dve", lambda e: e.memset(wtmp, 0.0), writes=["wtmp"])
    for a in range(2):
        for b in range(2):
            S.dma("sp", lambda e, a=a, b=b: e.dma_start(out=wtmp[b * 64:(b + 1) * 64, a * 128 + b * 64:a * 128 + (b + 1) * 64], in_=D["lru_w"][a, b]),
                  writes=["wtmp"])
    S.op("act", lambda e: e.copy(out=wbd, in_=wtmp), reads=["wtmp"], writes=["wbd"])
    S.op("act", lambda e: e.activation(out=vec[:, 8:9], in_=vec[:, 7:8], func=AF.Exp, scale=-1.0), reads=["vec"], writes=["vec8"])
    S.op("act", lambda e: e.activation(out=vec[:, 8:9], in_=vec[:, 8:9], func=AF.Ln, bias=1.0), writes=["vec8"])
    S.op("dve", lambda e: e.tensor_scalar(out=vec[:, 9:10], in0=vec[:, 8:9], scalar1=-8.0, scalar2=None, op0=ALU.mult), reads=["vec8"], writes=["vec9"])
    S.op("dve", lambda e: e.tensor_scalar(out=xc, in0=xa[:, 0:N], scalar1=vec[:, 0:1], scalar2=vec[:, 4:5], op0=ALU.mult, op1=ALU.add),
         reads=["xa", "xa_pad", "vec"], writes=["xc"])
    for j in range(1, 4):
        S.op("dve", lambda e, j=j: e.scalar_tensor_tensor(out=xc, in0=xa[:, j:j + N], scalar=vec[:, j:j + 1], in1=xc, op0=ALU.mult, op1=ALU.add),
             reads=["xa", "xa_pad", "vec"], writes=["xc"])
    S.op("act", lambda e: e.copy(out=xcb, in_=xc), reads=["xc"], writes=["xcb"])
    allA = [f"A{t}" for t in range(16)]
    allU = [f"U{t}" for t in range(16)]
    for t in range(16):
        ts = slice(t * 512, (t + 1) * 512)
        b0, b1 = 2 * (t % 2), 2 * (t % 2) + 1
        S.op("pe", lambda e, ts=ts, b0=b0: e.matmul(P[b0][:, :], lhsT=wbd[:, 0:128], rhs=xcb[:, ts], start=True, stop=True),
             reads=["xcb", "wbd"], writes=[pk(b0)])
        S.op("pe", lambda e, ts=ts, b1=b1: e.matmul(P[b1][:, :], lhsT=wbd[:, 128:256], rhs=xcb[:, ts], start=True, stop=True),
             reads=["xcb", "wbd"], writes=[pk(b1)])
        S.op("act", lambda e, ts=ts, b0=b0: e.activation(out=A[:, ts], in_=P[b0][:, :], func=AF.Sigmoid, bias=vec[:, 5:6]),
             reads=["vec"], writes=[pk(b0), f"A{t}"])
        S.op("act", lambda e, ts=ts, b1=b1: e.activation(out=U[:, ts], in_=P[b1][:, :], func=AF.Sigmoid, bias=vec[:, 6:7]),
             reads=["vec"], writes=[pk(b1), f"U{t}"])
    NP = 4
    W_ = N // NP
    for p_ in range(NP):
        cs = slice(p_ * W_, (p_ + 1) * W_)
        tA = [f"A{t}" for t in range(16) if p_ * W_ <= t * 512 < (p_ + 1) * W_]
        tU = [f"U{t}" for t in range(16) if p_ * W_ <= t * 512 < (p_ + 1) * W_]
        kA, kU, kT, kG, kH = f"Ap{p_}", f"Up{p_}", f"Tp{p_}", f"Gp{p_}", f"Hp{p_}"
        S.op("act", lambda e, cs=cs: e.activation(out=A[:, cs], in_=A[:, cs], func=AF.Exp, scale=vec[:, 9:10]), reads=["vec9"], writes=[kA] + tA)
        S.op("act", lambda e, cs=cs: e.activation(out=T1[:, cs], in_=A[:, cs], func=AF.Square), reads=[kA, "xc"], writes=[kT, "xa", "xa_pad"])
        S.op("act", lambda e, cs=cs: e.activation(out=T1[:, cs], in_=T1[:, cs], func=AF.Sqrt, bias=1.0, scale=-1.0), writes=[kT])
        S.op("dve", lambda e, cs=cs: e.tensor_tensor(out=U[:, cs], in0=U[:, cs], in1=xc[:, cs], op=ALU.mult), reads=["xc"], writes=[kU] + tU)
        S.op("dve", lambda e, cs=cs: e.tensor_tensor(out=U[:, cs], in0=U[:, cs], in1=T1[:, cs], op=ALU.mult), reads=[kT], writes=[kU])
    for p_ in range(NP):
        cs = slice(p_ * W_, (p_ + 1) * W_)
        init = 0.0 if p_ == 0 else xc[:, p_ * W_ - 1:p_ * W_]
        S.op("dve", lambda e, cs=cs, init=init: e.tensor_tensor_scan(out=xc[:, cs], data0=A[:, cs], data1=U[:, cs], initial=init, op0=ALU.mult, op1=ALU.add),
             reads=[f"Ap{p_}", f"Up{p_}"] + [f"Up{q_}" for q_ in range(NP)], writes=["xc", f"Hp{p_}"])
    for p_ in range(NP):
        cs = slice(p_ * W_, (p_ + 1) * W_)
        kT, kG = f"Tp{p_}", f"Gp{p_}"
        S.op("act", lambda e, cs=cs: e.activation(out=T1[:, cs], in_=G[:, cs], func=AF.Square), reads=["G", f"Up{p_}"], writes=[kT])
        S.op("dve", lambda e, cs=cs: e.tensor_scalar(out=T1[:, cs], in0=T1[:, cs], scalar1=0.044715, scalar2=1.0, op0=ALU.mult, op1=ALU.add), writes=[kT])
        S.op("dve", lambda e, cs=cs: e.tensor_tensor(out=T1[:, cs], in0=T1[:, cs], in1=G[:, cs], op=ALU.mult), reads=["G"], writes=[kT])
        S.op("act", lambda e, cs=cs: e.activation(out=T1[:, cs], in_=T1[:, cs], func=AF.Sigmoid, scale=1.5957691216057308), writes=[kT])
        S.op("dve", lambda e, cs=cs: e.tensor_tensor(out=G[:, cs], in0=G[:, cs], in1=T1[:, cs], op=ALU.mult), reads=[kT], writes=[kG])
        S.op("dve", lambda e, cs=cs: e.tensor_tensor(out=A[:, cs], in0=xc[:, cs], in1=G[:, cs], op=ALU.mult), reads=[f"Hp{p_}", kG], writes=[f"Ap{p_}"])
        for hf in range(2):
            if hf * 4096 >= p_ * W_ and (hf + 1) * 4096 <= (p_ + 1) * W_ or (p_ * W_ >= hf * 4096 and (p_ + 1) * W_ <= (hf + 1) * 4096):
                c0, c1 = max(hf * 4096, p_ * W_), min((hf + 1) * 4096, (p_ + 1) * W_)
                S.dma("sp", lambda e, c0=c0, c1=c1: e.dma_start(out=yT[0:128, c0:c1], in_=A[:, c0:c1]), reads=[f"Ap{p_}"])
    S.barrier()
    kb.release(m0)


def part_mla(kb, c, D, yT, L):
    S = kb.S
    m0 = kb.mark()
    P = kb.psb
    SC = float(96 ** -0.5)
    QD = [kb.alloc(N, BF16) for _ in range(2)]
    KD = [kb.alloc(N, BF16) for _ in range(2)]
    VD = kb.alloc(64 * 2 * 128, BF16).rearrange("p (b h d) -> p b h d", b=64, h=2)
    tri = kb.alloc(4 * 512, BF16).rearrange("p (i q) -> p i q", i=4)
    wuq = kb.alloc(2 * 192, BF16).rearrange("p (c n) -> p c n", c=2)
    wk = kb.alloc(128, BF16)
    wv = kb.alloc(128, BF16)
    vec = kb.alloc(4)
    r32 = kb.alloc(32)
    st = kb.alloc(512)
    S.op("pool", lambda e: e.memset(VD, 0.0), writes=["VD"])
    S.op("pool", lambda e: e.memset(VD[:, :, :, 64:65], 1.0), writes=["VD"])
    S.dma("sp", lambda e: e.dma_start(out=vec[:, 0:3], in_=D["mla_v"][:, :]), writes=["mvec"])
    S.dma("sp", lambda e: e.dma_start(out=r32, in_=D["r32"][:, :]), writes=["r32"])
    for i in range(4):
        S.dma("sp", lambda e, i=i: e.dma_start(out=st, in_=D["tri"][i]), writes=["st"])
        S.op("act", lambda e, i=i: e.copy(out=tri[:, i, :], in_=st), reads=["st"], writes=["tri"])
    S.dma("sp", lambda e: e.dma_start(out=st[:, 0:192], in_=D["mla_wuq"][0:128, :]), writes=["st"])
    S.op("act", lambda e: e.copy(out=wuq[:, 0, :], in_=st[:, 0:192]), reads=["st"], writes=["wuq"])
    S.dma("sp", lambda e: e.dma_start(out=st[0:64, 0:192], in_=D["mla_wuq"][128:192, :]), writes=["st"])
    S.op("act", lambda e: e.copy(out=wuq[0:64, 1, :], in_=st[0:64, 0:192]), reads=["st"], writes=["wuq"])
    S.dma("sp", lambda e: e.dma_start(out=st[:, 0:128], in_=D["mla_wk"][:, :]), writes=["st"])
    S.op("act", lambda e: e.copy(out=wk, in_=st[:, 0:128]), reads=["st"], writes=["wk"])
    S.dma("sp", lambda e: e.dma_start(out=st[:, 0:128], in_=D["mla_wv"][:, :]), writes=["st"])
    S.op("act", lambda e: e.copy(out=wv, in_=st[:, 0:128]), reads=["st"], writes=["wv"])

    m1 = kb.mark()
    cq0 = kb.alloc(512)
    cq1 = kb.alloc(512)
    ckv = kb.alloc(512)
    krt = kb.alloc(512)
    sq0 = kb.alloc(512, BF16)
    sq1 = kb.alloc(512, BF16)
    cn0 = kb.alloc(512, BF16)
    cn1 = kb.alloc(512, BF16)
    ckn = kb.alloc(512, BF16)
    rstd = kb.alloc(512)
    qr = kb.alloc(512)
    t1 = kb.alloc(512)
    t2 = kb.alloc(512)
    posi = kb.alloc(512, I32)
    posf = kb.alloc(512)
    T = alloc_tables(kb)
    R = slice(64, 96)
    for t in range(NQT):
        ts = slice(t * TQ, (t + 1) * TQ)
        load_pos(kb, D, posi, posf, t)
        S.dma("sp", lambda e, ts=ts: e.dma_start(out=cq0, in_=L.pt(OFF["d_cq"], 0, 128, ts.start, ts.stop)), writes=["cq0"])
        S.dma("sp", lambda e, ts=ts: e.dma_start(out=cq1[0:64, :], in_=L.pt(OFF["d_cq"] + 128, 0, 64, ts.start, ts.stop)), writes=["cq1"])
        S.dma("sp", lambda e, ts=ts: e.dma_start(out=ckv, in_=L.pt(OFF["d_ckv"], 0, 128, ts.start, ts.stop)), writes=["ckv"])
        S.dma("sp", lambda e, ts=ts: e.dma_start(out=krt[R, :], in_=L.pt(OFF["d_kr"], 0, 32, ts.start, ts.stop)), writes=["krt"])
        rope_tables(kb, c, posf, "posf", 64, 96, 1, T, 6, "m")
        S.op("act", lambda e: e.activation(out=sq0, in_=cq0, func=AF.Square), reads=["cq0"], writes=["sq0"])
        S.op("act", lambda e: e.activation(out=sq1[0:64, :], in_=cq1[0:64, :], func=AF.Square), reads=["cq1"], writes=["sq1"])
        S.op("pe", lambda e: e.matmul(P[0][:, :], lhsT=c.ones_bf, rhs=sq0, start=True, stop=False), reads=["sq0", "ones_bf"], writes=[pk(0)])
        S.op("pe", lambda e: e.matmul(P[0][:, :], lhsT=c.ones_bf[0:64, :], rhs=sq1[0:64, :], start=False, stop=True), reads=["sq1", "ones_bf"], writes=[pk(0)])
        S.op("act", lambda e: e.activation(out=rstd, in_=P[0][:, :], func=AF.Sqrt, bias=EPS, scale=1.0 / 192.0), writes=[pk(0), "rstd"])
        S.op("dve", lambda e: e.reciprocal(out=rstd, in_=rstd), writes=["rstd"])
        S.op("dve", lambda e: e.scalar_tensor_tensor(out=cn0, in0=cq0, scalar=vec[:, 0:1], in1=rstd, op0=ALU.mult, op1=ALU.mult),
             reads=["cq0", "rstd", "mvec"], writes=["cn0"])
        S.op("dve", lambda e: e.scalar_tensor_tensor(out=cn1[0:64, :], in0=cq1[0:64, :], scalar=vec[0:64, 1:2], in1=rstd[0:64, :], op0=ALU.mult, op1=ALU.mult),
             reads=["cq1", "rstd", "mvec"], writes=["cn1"])
        for h in range(2):
            hs = slice(h * 96, (h + 1) * 96)
            S.op("pe", lambda e, hs=hs: e.matmul(P[1][0:96, :], lhsT=wuq[:, 0, hs], rhs=cn0, start=True, stop=False), reads=["cn0", "wuq"], writes=[pk(1)])
            S.op("pe", lambda e, hs=hs: e.matmul(P[1][0:96, :], lhsT=wuq[0:64, 1, hs], rhs=cn1[0:64, :], start=False, stop=True), reads=["cn1", "wuq"], writes=[pk(1)])
            S.op("act", lambda e, h=h, ts=ts: e.mul(out=QD[h][0:64, ts], in_=P[1][0:64, :], mul=SC), writes=[pk(1), f"QD{h}"])
            S.op("dve", lambda e: e.tensor_copy(out=qr[R, :], in_=P[1][R, :]), writes=[pk(1), "qr"])
            S.op("pe", lambda e: e.matmul(P[2][R, :], lhsT=r32[R, 0:32], rhs=qr[R, :], start=True, stop=True), reads=["qr", "r32"], writes=[pk(2)])
            S.op("dve", lambda e: e.scalar_tensor_tensor(out=t1[R, :], in0=qr[R, :], scalar=SC, in1=T["cos"][R, :], op0=ALU.mult, op1=ALU.mult),
                 reads=["qr", "mcos"], writes=["t1"])
            S.op("dve", lambda e: e.scalar_tensor_tensor(out=t2[R, :], in0=P[2][R, :], scalar=SC, in1=T["sin"][R, :], op0=ALU.mult, op1=ALU.mult),
                 reads=["msin"], writes=[pk(2), "t2"])
            S.op("dve", lambda e, h=h, ts=ts: e.tensor_tensor(out=QD[h][R, ts], in0=t1[R, :], in1=t2[R, :], op=ALU.add), reads=["t1", "t2"], writes=[f"QD{h}"])
        S.op("act", lambda e: e.activation(out=sq0, in_=ckv, func=AF.Square), reads=["ckv"], writes=["sq0"])
        S.op("pe", lambda e: e.matmul(P[7][:, :], lhsT=c.ones_bf, rhs=sq0, start=True, stop=True), reads=["sq0", "ones_bf"], writes=[pk(7)])
        S.op("act", lambda e: e.activation(out=rstd, in_=P[7][:, :], func=AF.Sqrt, bias=EPS, scale=1.0 / 128.0), writes=[pk(7), "rstd"])
        S.op("dve", lambda e: e.reciprocal(out=rstd, in_=rstd), writes=["rstd"])
        S.op("dve", lambda e: e.scalar_tensor_tensor(out=ckn, in0=ckv, scalar=vec[:, 2:3], in1=rstd, op0=ALU.mult, op1=ALU.mult),
             reads=["ckv", "rstd", "mvec"], writes=["ckn"])
        for h in range(2):
            S.op("pe", lambda e, h=h: e.matmul(P[3][0:64, :], lhsT=wk[:, h * 64:(h + 1) * 64], rhs=ckn, start=True, stop=True), reads=["ckn", "wk"], writes=[pk(3)])
            S.op("act", lambda e, h=h, ts=ts: e.copy(out=KD[h][0:64, ts], in_=P[3][0:64, :]), writes=[pk(3), f"KD{h}"])
        for tb in range(4):
            S.op("pe", lambda e, tb=tb: e.matmul(P[4][:, 0:128], lhsT=ckn[:, tb * 128:(tb + 1) * 128], rhs=wv, start=True, stop=True), reads=["ckn", "wv"], writes=[pk(4)])
            S.op("act", lambda e, tb=tb, t=t: e.copy(out=VD[:, 4 * t + tb, :, 0:64], in_=P[4][:, 0:128].rearrange("p (h d) -> p h d", h=2)),
                 writes=[pk(4), "VD"])
        S.op("pe", lambda e: e.matmul(P[5][R, :], lhsT=r32[R, 0:32], rhs=krt[R, :], start=True, stop=True), reads=["krt", "r32"], writes=[pk(5)])
        S.op("dve", lambda e: e.tensor_tensor(out=t1[R, :], in0=krt[R, :], in1=T["cos"][R, :], op=ALU.mult), reads=["krt", "mcos"], writes=["t1"])
        S.op("dve", lambda e: e.tensor_tensor(out=t2[R, :], in0=P[5][R, :], in1=T["sin"][R, :], op=ALU.mult), reads=["msin"], writes=[pk(5), "t2"])
        for h in range(2):
            S.op("dve", lambda e, h=h, ts=ts: e.tensor_tensor(out=KD[h][R, ts], in0=t1[R, :], in1=t2[R, :], op=ALU.add), reads=["t1", "t2"], writes=[f"KD{h}"])
    S.barrier()
    kb.release(m1)
    att = Attn(kb, sbanks=(0, 1))
    rowbuf = kb.alloc(512)
    bcs = kb.alloc(512)
    yst = [kb.alloc(512), kb.alloc(512)]
    it = 0
    pending = []
    for qt in range(NQT):
        qs = slice(qt * TQ, (qt + 1) * TQ)
        for h in range(2):
            blocks = []
            for kbk in range(4 * qt + 4):
                mms = [(KD[h][0:96, kbk * 128:(kbk + 1) * 128], QD[h][0:96, qs], [f"KD{h}", f"QD{h}"])]
                if kbk >= 4 * qt:
                    mms.append((c.ident_bf, tri[:, kbk - 4 * qt, :], ["ident_bf", "tri"]))
                blocks.append((mms, VD[:, kbk, h, :], ["VD"]))
            ob = 2 + it % 2
            att.run(blocks, ob, defer=pending)

            def fin(ob=ob, ys=yst[it % 2], yk=f"yst{it % 2}", h=h, qs=qs):
                norm_coef(kb, c, ob, rowbuf, 4, bcs)
                S.op("dve", lambda e: e.tensor_tensor(out=ys[0:64, :], in0=P[ob][0:64, :], in1=bcs[0:64, :], op=ALU.mult),
                     reads=["bcs"], writes=[pk(ob), yk])
                S.dma("sp", lambda e: e.dma_start(out=yT[384 + h * 64:384 + (h + 1) * 64, qs], in_=ys[0:64, :]), reads=[yk])
            pending.append(fin)
            it += 1
    for f in pending:
        f()
    S.barrier()
    kb.release(m0)


def load_cast(kb, src_ap, dst_ap, stage, skey, dkey, eng="act", rows=slice(0, 128)):
    S = kb.S
    S.dma("sp", lambda e: e.dma_start(out=stage, in_=(src_ap() if callable(src_ap) else src_ap)), writes=[skey])
    if eng == "act":
        S.op("act", lambda e: e.copy(out=dst_ap, in_=stage), reads=[skey], writes=[dkey])
    else:
        S.op("pool", lambda e: e.tensor_copy(out=dst_ap, in_=stage), reads=[skey], writes=[dkey])


def gelu_tanh(kb, z, u, out, zk, uk, outk):
    S = kb.S
    S.op("pool", lambda e: e.tensor_tensor(out=u, in0=z, in1=z, op=ALU.mult), reads=[zk], writes=[uk])
    S.op("pool", lambda e: e.tensor_scalar(out=u, in0=u, scalar1=0.044715, scalar2=1.0, op0=ALU.mult, op1=ALU.add), writes=[uk])
    S.op("pool", lambda e: e.tensor_tensor(out=u, in0=u, in1=z, op=ALU.mult), reads=[zk], writes=[uk])
    S.op("act", lambda e: e.activation(out=u, in_=u, func=AF.Sigmoid, scale=1.5957691216057308), writes=[uk])
    S.op("dve", lambda e: e.tensor_tensor(out=out, in0=z, in1=u, op=ALU.mult), reads=[zk, uk], writes=[outk])


def part_nsa(kb, c, D, yT, L):
    S = kb.S
    P = kb.psb
    m0 = kb.mark()
    Qm = kb.alloc(N, BF16)
    Qo = kb.alloc(N, BF16)
    KSA = kb.alloc(N, BF16)
    KSB = kb.alloc(N, BF16)
    KW2 = kb.alloc(N, BF16)
    tri = kb.alloc(4 * 512, BF16).rearrange("p (i q) -> p i q", i=4)
    win = kb.alloc(8 * 512, BF16).rearrange("p (i q) -> p i q", i=8)
    cmpm = kb.alloc(5 * 512, BF16).rearrange("p (i q) -> p i q", i=5)
    rbd = kb.alloc(128)
    ovl = kb.alloc(4 * 130, BF16).rearrange("p (i q) -> p i q", i=4)
    bv = kb.alloc(254)
    bf = kb.alloc(254)
    selg = kb.alloc(384)
    KCMP2 = kb.alloc(512, BF16)
    VCMP = kb.alloc(4 * 128, BF16).rearrange("p (b d) -> p b d", b=4)
    st = kb.alloc(512)
    st3 = st.rearrange("p (b d) -> p b d", d=64)
    S.op("pool", lambda e: e.memset(KSA[64:128, :], 0.0), writes=["ksz"])
    S.op("pool", lambda e: e.memset(KSB[0:64, :], 0.0), writes=["ksz"])
    S.op("pool", lambda e: e.memset(VCMP, 0.0), writes=["VCMP"])
    S.op("pool", lambda e: e.memset(VCMP[:, :, 64:65], 1.0), writes=["VCMP"])
    S.dma("sp", lambda e: e.dma_start(out=rbd, in_=D["rbd"][:, :]), writes=["rbd"])
    S.dma("sp", lambda e: e.dma_start(out=bv, in_=D["bv"][:, :]), writes=["bv"])
    S.dma("sp", lambda e: e.dma_start(out=bf, in_=D["bf"][:, :]), writes=["bf"])
    S.dma("sp", lambda e: e.dma_start(out=selg[0:8, :], in_=D["selg"][:, :]), writes=["selg"])
    i = 0
    for nm, dst, cnt in (("tri", tri, 4), ("win", win, 8), ("cmpm", cmpm, 5)):
        for j in range(cnt):
            load_cast(kb, D[nm][j], dst[:, j, :], st[:, 0:512], "st", nm, "act" if i % 2 == 0 else "pool")
            i += 1
    for j in range(4):
        load_cast(kb, D["ovl"][j], ovl[:, j, 0:129], st[:, 0:129], "st", "ovl", "act")

    m1 = kb.mark()
    KCV = kb.alloc(N)
    xs = [kb.alloc(512), kb.alloc(512)]
    t1s = [kb.alloc(512), kb.alloc(512)]
    t2s = [kb.alloc(512), kb.alloc(512)]
    posi = kb.alloc(512, I32)
    posf = kb.alloc(512)
    T = alloc_tables(kb)
    for hf in range(2):
        S.dma("sp", lambda e, hf=hf: e.dma_start(out=KCV[64:128, hf * 4096:(hf + 1) * 4096], in_=L.pt(OFF["b_kv"] + 64, 0, 64, hf * 4096, (hf + 1) * 4096)), writes=["KCVv"])
    flat = []
    for t in range(NQT):
        ts = slice(t * TQ, (t + 1) * TQ)
        a0, a1 = ts.start, ts.stop
        flat += [(t, ts, "q0", [lambda a0=a0, a1=a1: L.pt(OFF["b_q"], 128, 128, a0, a1)], Qm, 0.125, 128),
                 (t, ts, "q1", [lambda a0=a0, a1=a1: L.pt(OFF["b_q"], 128, 128, a0, a1, inv=True)], Qo, 0.125, 128),
                 (t, ts, "ks", [lambda a0=a0, a1=a1: L.pt(OFF["b_kv"] + 128, 0, 64, a0, a1)] * 2, None, 1.0, 128),
                 (t, ts, "kw", [lambda a0=a0, a1=a1: L.pt(OFF["b_kv"] + 192, 0, 64, a0, a1)] * 2, KW2, 1.0, 128),
                 (t, ts, "kc", [lambda a0=a0, a1=a1: L.pt(OFF["b_kv"], 0, 64, a0, a1)], KCV, 1.0, 64)]

    def emit_load(i):
        t, ts, nm, srcs, dst, sc, rows = flat[i]
        x = xs[i % 2]
        xk = f"xs{i % 2}"
        if len(srcs) == 2:
            S.dma("sp", lambda e: e.dma_start(out=x[0:64, :], in_=srcs[0]()), writes=[xk])
            S.dma("sp", lambda e: e.dma_start(out=x[64:128, :], in_=srcs[1]()), writes=[xk])
        else:
            S.dma("sp", lambda e: e.dma_start(out=x[0:rows, :], in_=srcs[0]()), writes=[xk])

    def emit_compute(i):
        t, ts, nm, srcs, dst, sc, rows = flat[i]
        x = xs[i % 2]
        xk = f"xs{i % 2}"
        t1 = t1s[i % 2]
        t2 = t2s[i % 2]
        bank = 4 + i % 2
        R = slice(0, rows)
        S.op("pe", lambda e: e.matmul(P[bank][R, :], lhsT=rbd[R, 0:rows], rhs=x[R, :], start=True, stop=True),
             reads=[xk, "rbd"], writes=[pk(bank)])
        S.op("dve", lambda e: e.scalar_tensor_tensor(out=t1[R, :], in0=x[R, :], scalar=sc, in1=T["cos"][R, :], op0=ALU.mult, op1=ALU.mult),
             reads=[xk, "ncos"], writes=[f"t1{i % 2}"])
        S.op("dve", lambda e: e.scalar_tensor_tensor(out=t2[R, :], in0=P[bank][R, :], scalar=sc, in1=T["sin"][R, :], op0=ALU.mult, op1=ALU.mult),
             reads=["nsin"], writes=[pk(bank), f"t2{i % 2}"])
        dk = "KCVk" if nm == "kc" else nm
        if nm == "ks":
            for dst_, RR in ((KSA, slice(0, 64)), (KSB, slice(64, 128))):
                S.op("pool", lambda e, RR=RR, dst_=dst_: e.tensor_tensor(out=dst_[RR, ts], in0=t1[RR, :], in1=t2[RR, :], op=ALU.add),
                     reads=[f"t1{i % 2}", f"t2{i % 2}"], writes=[dk])
        else:
            S.op("pool", lambda e: e.tensor_tensor(out=dst[R, ts], in0=t1[R, :], in1=t2[R, :], op=ALU.add),
                 reads=[f"t1{i % 2}", f"t2{i % 2}"], writes=[dk])

    emit_load(0)
    for i in range(len(flat)):
        if i % 5 == 0:
            load_pos(kb, D, posi, posf, flat[i][0])
            rope_tables(kb, c, posf, "posf", 0, 128, 0, T, 6, "n")
        if i + 1 < len(flat):
            emit_load(i + 1)
        emit_compute(i)
    S.barrier()
    kb.release(m1)
    KCV = kb.alloc(N)
    BLK = kb.alloc(32 * 512, BF16).rearrange("p (l n) -> p l n", l=32)
    W1 = kb.alloc(32 * 256, BF16).rearrange("p (l h) -> p l h", l=32)
    pos2 = kb.alloc(32)
    b1 = kb.alloc(4)
    w2 = kb.alloc(2 * 2 * 64, BF16).rearrange("p (k m d) -> p k m d", k=2, m=2)
    HID = kb.alloc(2 * 2 * 512, BF16).rearrange("p (k m n) -> p k m n", k=2, m=2)
    zt = kb.alloc(512)
    ut = kb.alloc(512)
    stw = kb.alloc(1024).rearrange("p (l h) -> p l h", l=4)
    S.dma("sp", lambda e: e.dma_start(out=pos2, in_=D["nsa_pos"][:, :]), writes=["pos2"])
    S.dma("sp", lambda e: e.dma_start(out=b1, in_=D["nsa_b1"][:, :]), writes=["b1"])
    for kv in range(2):
        load_cast(kb, D["nsa_w2"][kv].rearrange("(m p) d -> p m d", p=128), w2[:, kv, :, :], st[:, 0:128].rearrange("p (m d) -> p m d", m=2), "st", "w2", "act")
    for l0 in range(0, 32, 4):
        for kv in range(2):
            src = D["nsa_w1"][kv].rearrange("(l d) h -> d l h", d=64)[:, l0:l0 + 4, :]
            S.dma("sp", lambda e, src=src, kv=kv: e.dma_start(out=stw[kv * 64:(kv + 1) * 64, :, :], in_=src), writes=["stw"])
        if (l0 // 4) % 2 == 0:
            S.op("act", lambda e, l0=l0: e.copy(out=W1[:, l0:l0 + 4, :], in_=stw), reads=["stw"], writes=["W1"])
        else:
            S.op("pool", lambda e, l0=l0: e.tensor_copy(out=W1[:, l0:l0 + 4, :], in_=stw), reads=["stw"], writes=["W1"])
    S.op("pool", lambda e: e.memset(BLK[:, :, 511:512], 0.0), writes=["BLKpad"])
    K3 = KCV.rearrange("p (g r) -> p g r", r=16)
    for l in range(32):
        src = K3[:, 0:511, l] if l < 16 else K3[:, 1:512, l - 16]
        eng = "dve" if l % 2 == 0 else "pool"
        S.op(eng, lambda e, l=l, src=src: e.tensor_scalar(out=BLK[:, l, 0:511], in0=src, scalar1=pos2[:, l:l + 1], scalar2=None, op0=ALU.add),
             reads=["pos2"], writes=[f"BLK{l}"])
    for kv in range(2):
        R = slice(kv * 64, kv * 64 + 64)
        for m in range(2):
            bank = 2 * kv + m
            for l in range(32):
                S.op("pe", lambda e, l=l, R=R, m=m, bank=bank: e.matmul(P[bank][:, :], lhsT=W1[R, l, m * 128:(m + 1) * 128], rhs=BLK[R, l, :], start=(l == 0), stop=(l == 31)),
                     reads=["W1", f"BLK{l}", "BLKpad"], writes=[pk(bank)])
            S.op("act", lambda e, kv=kv, m=m, bank=bank: e.activation(out=zt, in_=P[bank][:, :], func=AF.Identity, bias=b1[:, kv * 2 + m:kv * 2 + m + 1]),
                 reads=["b1"], writes=[pk(bank), "zt"])
            gelu_tanh(kb, zt, ut, HID[:, kv, m, :], "zt", "ut", f"HID{kv}")
    for m in range(2):
        S.op("pe", lambda e, m=m: e.matmul(P[4][0:64, :], lhsT=w2[:, 0, m, :], rhs=HID[:, 0, m, :], start=(m == 0), stop=(m == 1)), reads=["w2", "HID0"], writes=[pk(4)])
    S.op("act", lambda e: e.copy(out=KCMP2[0:64, :], in_=P[4][0:64, :]), writes=[pk(4), "KCMP2"])
    S.op("dve", lambda e: e.tensor_copy(out=KCMP2[64:128, :], in_=P[4][0:64, :]), writes=[pk(4), "KCMP2"])
    for nb in range(4):
        for m in range(2):
            S.op("pe", lambda e, m=m, nb=nb: e.matmul(P[5][:, 0:64], lhsT=HID[:, 1, m, nb * 128:(nb + 1) * 128], rhs=w2[:, 1, m, :], start=(m == 0), stop=(m == 1)),
                 reads=["w2", "HID1"], writes=[pk(5)])
        S.op("act", lambda e, nb=nb: e.copy(out=VCMP[:, nb, 0:64], in_=P[5][:, 0:64]), writes=[pk(5), "VCMP"])
    S.barrier()
    kb.release(m1)
    VS = kb.alloc(64 * 128, BF16).rearrange("p (b d) -> p b d", b=64)
    VW = kb.alloc(64 * 128, BF16).rearrange("p (b d) -> p b d", b=64)
    for vt_, vk_ in ((VS, "VS"), (VW, "VW")):
        S.op("pool", lambda e, vt_=vt_: e.memset(vt_, 0.0), writes=[vk_])
        S.op("pool", lambda e, vt_=vt_: e.memset(vt_[:, :, 64:65], 1.0), writes=[vk_])
    for nm, dst, coff in (("nsa_vs", VS, 0), ("nsa_vw", VW, 64)):
        for b0 in range(0, 64, 8):
            load_cast(kb, (lambda b0=b0, coff=coff: L.pv(coff, 0, 64, b0 * 128, (b0 + 8) * 128).rearrange("(b p) d -> p b d", p=128)),
                      dst[:, b0:b0 + 8, 0:64], st3[:, 0:8, :], "st", nm[-2:].upper(), "act" if (b0 // 8) % 2 == 0 else "pool")

    NST = kb.alloc(N, BF16)
    EALL = kb.alloc(N, BF16)
    for j in range(16):
        load_cast(kb, D["eall"][:, j * 512:(j + 1) * 512], EALL[:, j * 512:(j + 1) * 512], st[:, 0:512], "st", "EALL", "act" if j % 2 == 0 else "pool")
    ET = [kb.alloc(512, BF16) for _ in range(4)]
    IMP = kb.alloc(512).rearrange("p (a s) -> p a s", a=4)
    scr = kb.alloc(128)
    mr = kb.alloc(128)
    nsb = kb.alloc(128)
    mx = kb.alloc(16)
    thr = kb.alloc(2)
    rsum = kb.alloc(2)
    g6 = kb.alloc(512)
    gs = [[kb.alloc(512) for _ in range(3)] for _ in range(2)]
    acc = [kb.alloc(512), kb.alloc(512)]
    ctmp = kb.alloc(512)
    otmp = kb.alloc(512)
    rowbuf = ctmp
    bcs = kb.alloc(512)
    att = Attn(kb, sbanks=(0, 1))
    oi = 0

    def combine(ob, hA, br, first):
        norm_coef(kb, c, ob, rowbuf, 4, bcs)
        S.op("dve", lambda e: e.tensor_tensor(out=ctmp[0:64, :], in0=bcs[0:64, :], in1=gs[hA][br][0:64, :], op=ALU.mult),
             reads=["bcs", f"gs{hA}{br}"], writes=["ctmp"])
        if first:
            S.op("dve", lambda e: e.tensor_tensor(out=acc[hA][0:64, :], in0=P[ob][0:64, :], in1=ctmp[0:64, :], op=ALU.mult),
                 reads=["ctmp"], writes=[pk(ob), f"acc{hA}"])
        else:
            S.op("dve", lambda e: e.tensor_tensor(out=otmp[0:64, :], in0=P[ob][0:64, :], in1=ctmp[0:64, :], op=ALU.mult),
                 reads=["ctmp"], writes=[pk(ob), "otmp"])
            S.op("pool", lambda e: e.tensor_tensor(out=acc[hA][0:64, :], in0=acc[hA][0:64, :], in1=otmp[0:64, :], op=ALU.add),
                 reads=["otmp"], writes=[f"acc{hA}"])

    pending = []
    for qt in range(NQT):
        qs = slice(qt * TQ, (qt + 1) * TQ)
        for f in pending:
            f()
        del pending[:]
        S.dma("sp", lambda e, qs=qs: e.dma_start(out=g6[0:6, :], in_=L.pt(OFF["b_gate"], 6, 6, qs.start, qs.stop)), writes=["g6"])
        for hA in range(2):
            for br in range(3):
                r = hA * 3 + br
                S.op("pe", lambda e, r=r: e.matmul(P[5][0:64, :], lhsT=selg[0:6, r * 64:(r + 1) * 64], rhs=g6[0:6, :], start=True, stop=True),
                     reads=["g6", "selg"], writes=[pk(5)])
                S.op("act", lambda e, hA=hA, br=br: e.activation(out=gs[hA][br][0:64, :], in_=P[5][0:64, :], func=AF.Sigmoid), writes=[pk(5), f"gs{hA}{br}"])
        nbm = (512 * qt + 480) // 2048
        for hh in range(4):
            Qt = (Qm if hh < 2 else Qo)
            qk = "q0" if hh < 2 else "q1"
            R = slice(64 * (hh % 2), 64 * (hh % 2) + 64)
            ob = 2 + oi % 2
            for nb in range(nbm + 1):
                bank = nb % 2
                dl = 512 * qt - 2048 * nb
                mms = [(KCMP2[R, nb * 128:(nb + 1) * 128], Qt[R, qs], ["KCMP2", qk])]
                if dl < 2560:
                    mms.append((c.ident_bf, cmpm[:, dl // 512, :], ["ident_bf", "cmpm"]))
                for j, (l_, r_, ks) in enumerate(mms):
                    S.op("pe", lambda e, l_=l_, r_=r_, j=j, bank=bank, nm=len(mms): e.matmul(P[bank][:, :], lhsT=l_, rhs=r_, start=(j == 0), stop=(j == nm - 1)),
                         reads=ks, writes=[pk(bank)])
                S.op("act", lambda e, nb=nb, bank=bank: e.activation(out=ET[nb], in_=P[bank][:, :], func=AF.Exp), writes=[pk(bank), f"ET{nb}"])
                if hh < 2:
                    S.op("pe", lambda e, nb=nb, ob=ob, nbm=nbm: e.matmul(P[ob][0:128, :], lhsT=VCMP[:, nb, :], rhs=ET[nb], start=(nb == 0), stop=(nb == nbm)),
                         reads=[f"ET{nb}", "VCMP"], writes=[pk(ob)])
            for s4 in range(4):
                for nb in range(nbm + 1):
                    S.op("pe", lambda e, nb=nb, s4=s4, nbm=nbm: e.matmul(P[6][:, 0:129], lhsT=ET[nb][:, s4 * 128:(s4 + 1) * 128], rhs=ovl[:, nb, 0:129], start=(nb == 0), stop=(nb == nbm)),
                         reads=[f"ET{nb}", "ovl"], writes=[pk(6)])
                S.op("dve", lambda e: e.tensor_scalar_max(out=rsum[:, 0:1], in0=P[6][:, 128:129], scalar1=1e-30), writes=[pk(6), "rsum"])
                S.op("dve", lambda e: e.reciprocal(out=rsum[:, 0:1], in_=rsum[:, 0:1]), writes=["rsum"])
                if hh == 0:
                    S.op("dve", lambda e, s4=s4: e.tensor_scalar(out=IMP[:, s4, :], in0=P[6][:, 0:128], scalar1=rsum[:, 0:1], scalar2=None, op0=ALU.mult),
                         reads=["rsum"], writes=[pk(6), f"IMP{s4}"])
                else:
                    S.op("dve", lambda e, s4=s4: e.scalar_tensor_tensor(out=IMP[:, s4, :], in0=P[6][:, 0:128], scalar=rsum[:, 0:1], in1=IMP[:, s4, :], op0=ALU.mult, op1=ALU.add),
                         reads=["rsum"], writes=[pk(6), f"IMP{s4}"])
            if hh < 2:
                combine(ob, hh, 0, True)
                oi += 1
        for s4 in range(4):
            qb = 4 * qt + s4
            o0 = 126 - 2 * qb
            S.op("dve", lambda e, s4=s4, o0=o0: e.tensor_tensor(out=scr, in0=IMP[:, s4, :], in1=bv[:, o0:o0 + 128], op=ALU.mult), reads=[f"IMP{s4}", "bv"], writes=["scr"])
            S.op("dve", lambda e, o0=o0: e.tensor_tensor(out=scr, in0=scr, in1=bf[:, o0:o0 + 128], op=ALU.add), reads=["bf"], writes=["scr"])
            S.op("dve", lambda e: e.memset(scr[:, 0:1], 1e9), writes=["scr"])
            S.op("dve", lambda e: e.max(out=mx[:, 0:8], in_=scr), reads=["scr"], writes=["mx"])
            S.op("dve", lambda e: e.match_replace(out=mr, in_to_replace=mx[:, 0:8], in_values=scr, imm_value=-2.0), reads=["scr", "mx"], writes=["mr"])
            S.op("dve", lambda e: e.max(out=mx[:, 8:16], in_=mr), reads=["mr"], writes=["mx"])
            S.op("dve", lambda e: e.tensor_reduce(out=thr[:, 0:1], in_=mx[:, 8:16], axis=AX.X, op=ALU.min), reads=["mx"], writes=["thr"])
            S.op("dve", lambda e: e.tensor_scalar(out=nsb, in0=scr, scalar1=thr[:, 0:1], scalar2=-NEG, op0=ALU.is_ge, op1=ALU.mult), reads=["scr", "thr"], writes=["nsb"])
            S.op("pool", lambda e: e.tensor_scalar(out=nsb, in0=nsb, scalar1=NEG, scalar2=None, op0=ALU.add), writes=["nsb"])
            S.op("pe", lambda e: e.transpose(P[7][:, 0:128], nsb, c.ident), reads=["nsb", "ident"], writes=[pk(7)])
            S.op("act", lambda e, qb=qb: e.copy(out=NST[:, qb * 128:(qb + 1) * 128], in_=P[7][:, 0:128]), writes=[pk(7), "NST"])
        for hA in range(2):
            R = slice(64 * hA, 64 * hA + 64)
            blocks = []
            for kbk in range(4 * qt + 4):
                mms = [((KSA if hA == 0 else KSB)[:, kbk * 128:(kbk + 1) * 128], Qm[:, qs], ["ks", "q0", "ksz"]),
                       (EALL[:, kbk * 128:(kbk + 1) * 128], NST[:, qs], ["EALL", "NST"])]
                if kbk >= 4 * qt:
                    mms.append((c.ident_bf, tri[:, kbk - 4 * qt, :], ["ident_bf", "tri"]))
                blocks.append((mms, VS[:, kbk, :], ["VS"]))
            ob = 2 + oi % 2
            oi += 1
            att.run(blocks, ob, defer=pending)
            pending.append(lambda ob=ob, hA=hA: combine(ob, hA, 1, False))
            blocks = []
            for kbk in range(max(0, 4 * qt - 4), 4 * qt + 4):
                mms = [(KW2[R, kbk * 128:(kbk + 1) * 128], Qm[R, qs], ["kw", "q0"]),
                       (c.ident_bf, win[:, kbk - 4 * qt + 4, :], ["ident_bf", "win"])]
                blocks.append((mms, VW[:, kbk, :], ["VW"]))
            ob = 2 + oi % 2
            oi += 1
            att.run(blocks, ob, defer=pending)

            def fin(ob=ob, hA=hA, qs=qs):
                combine(ob, hA, 2, False)
                S.dma("sp", lambda e: e.dma_start(out=yT[128 + hA * 64:128 + (hA + 1) * 64, qs], in_=acc[hA][0:64, :]), reads=[f"acc{hA}"])
            pending.append(fin)
    for f in pending:
        f()
    S.barrier()
    kb.release(m0)


def part_gla(kb, c, D, yT, L):
    STAGE = 9
    S = kb.S
    P = kb.psb
    m0 = kb.mark()
    QS = float(32 ** -0.5)
    A = kb.alloc(N)
    B = kb.alloc(N)
    C = kb.alloc(2048)
    QG = kb.alloc(N, BF16)
    KG = kb.alloc(N, BF16)
    KHT = kb.alloc(128 * 64, BF16).rearrange("p (b d) -> p b d", b=128)
    V = kb.alloc(128 * 128, BF16).rearrange("p (b d) -> p b d", b=128)
    SB = kb.alloc(128 * 64, BF16).rearrange("p (c v) -> p c v", c=128)
    DC = kb.alloc(128)
    wg2 = kb.alloc(64)
    glr = kb.alloc(512)
    v2 = kb.alloc(4)
    tri64 = kb.alloc(64)
    st = kb.alloc(1024)
    S.dma("sp", lambda e: e.dma_start(out=wg2[0:16, :], in_=D["gla_wg2"][:, :]), writes=["wg2"])
    S.dma("sp", lambda e: e.dma_start(out=v2[:, 0:2], in_=D["gla_v2"][:, :]), writes=["v2"])
    S.dma("sp", lambda e: e.dma_start(out=tri64[0:64, :], in_=D["tri64"][:, :]), writes=["tri64"])
    S.dma("sp", lambda e: e.dma_start(out=tri64[64:128, :], in_=D["tri64"][:, :]), writes=["tri64"])
    S.op("dve", lambda e: e.tensor_scalar(out=v2[:, 2:3], in0=v2[:, 0:1], scalar1=-1.0, scalar2=None, op0=ALU.mult), reads=["v2"], writes=["v2n"])
    R = slice(0, 64)
    st3 = st.rearrange("p (b d) -> p b d", d=128)
    for b0 in range(0, 128, 8):
        load_cast(kb, (lambda b0=b0: L.pv(128, 128, 128, b0 * 64, (b0 + 8) * 64).rearrange("(b p) d -> p b d", p=64)),
                  V[R, b0:b0 + 8, :], st3[R, :, :], "st", "V", "act" if (b0 // 8) % 2 == 0 else "pool")
    for t in range(NQT):
        ts = slice(t * TQ, (t + 1) * TQ)
        bank = t % 2
        S.dma("sp", lambda e, ts=ts: e.dma_start(out=glr[0:16, :], in_=L.pt(OFF["c_glr"], 0, 16, ts.start, ts.stop)), writes=["glr"])
        S.op("pe", lambda e, bank=bank: e.matmul(P[bank][R, :], lhsT=wg2[0:16, 0:64], rhs=glr[0:16, :], start=True, stop=True), reads=["glr", "wg2"], writes=[pk(bank)])
        S.op("act", lambda e, bank=bank, ts=ts: e.activation(out=A[R, ts], in_=P[bank][R, :], func=AF.Exp, bias=v2[R, 2:3], scale=-1.0),
             reads=["v2n"], writes=[pk(bank), "A"])
    S.op("act", lambda e: e.activation(out=A[R, :], in_=A[R, :], func=AF.Ln, bias=1.0), writes=["A"])
    for ch in range(128):
        cs = slice(ch * 64, (ch + 1) * 64)
        S.op("dve", lambda e, cs=cs: e.tensor_tensor_scan(out=B[R, cs], data0=c.ones_f[R, 0:64], data1=A[R, cs], initial=0.0, op0=ALU.mult, op1=ALU.add),
             reads=["A", "ones_f"], writes=["B"])
    if STAGE <= 1:
        S.barrier(); kb.release(m0); return
    B3 = B.rearrange("p (c j) -> p c j", j=64)
    A3 = A.rearrange("p (c j) -> p c j", j=64)
    S.op("act", lambda e: e.activation(out=DC[R, :], in_=B3[R, :, 63], func=AF.Exp, scale=-1.0 / 16.0), reads=["B"], writes=["DC"])
    S.op("act", lambda e: e.activation(out=A[R, :], in_=B[R, :], func=AF.Exp, scale=-1.0 / 16.0), reads=["B"], writes=["A"])
    for pc in range(4):
        ps_ = slice(pc * 2048, (pc + 1) * 2048)
        S.dma("sp", lambda e, ps_=ps_: e.dma_start(out=C[R, :], in_=L.pt(OFF["c_q"], 64, 64, ps_.start, ps_.stop)), writes=["C"])
        S.op("dve", lambda e, ps_=ps_: e.scalar_tensor_tensor(out=QG[R, ps_], in0=C[R, :], scalar=QS, in1=A[R, ps_], op0=ALU.mult, op1=ALU.mult),
             reads=["C", "A"], writes=["QG"])
    S.op("act", lambda e: e.activation(out=A[R, :], in_=B[R, :], func=AF.Exp, scale=1.0 / 16.0), reads=["B", "QG"], writes=["A"])
    for pc in range(4):
        ps_ = slice(pc * 2048, (pc + 1) * 2048)
        S.dma("sp", lambda e, ps_=ps_: e.dma_start(out=C[R, :], in_=L.pt(OFF["c_k"], 64, 64, ps_.start, ps_.stop)), writes=["C"])
        S.op("dve", lambda e, ps_=ps_: e.tensor_tensor(out=KG[R, ps_], in0=C[R, :], in1=A[R, ps_], op=ALU.mult), reads=["C", "A"], writes=["KG"])
    S.op("dve", lambda e: e.tensor_tensor(out=A3[R, :, :], in0=B3[R, :, :], in1=B3[R, :, 63:64].to_broadcast([64, 128, 64]), op=ALU.subtract),
         reads=["B", "KG"], writes=["A"])
    S.op("act", lambda e: e.activation(out=A[R, :], in_=A[R, :], func=AF.Exp, scale=1.0 / 16.0), writes=["A"])
    for pc in range(4):
        ps_ = slice(pc * 2048, (pc + 1) * 2048)
        S.dma("sp", lambda e, ps_=ps_: e.dma_start(out=C[R, :], in_=L.pt(OFF["c_k"], 64, 64, ps_.start, ps_.stop)), writes=["C"])
        S.op("dve", lambda e, ps_=ps_: e.tensor_tensor(out=A[R, ps_], in0=C[R, :], in1=A[R, ps_], op=ALU.mult), reads=["C"], writes=["A"])
    if STAGE <= 2:
        S.barrier(); kb.release(m0); return
    for blk in range(64):
        bank = blk % 2
        S.op("pe", lambda e, blk=blk, bank=bank: e.transpose(P[bank][:, 0:64], A[R, blk * 128:(blk + 1) * 128], c.ident[0:64, 0:64]),
             reads=["A", "ident"], writes=[pk(bank)])
        S.op("act", lambda e, blk=blk, bank=bank: e.copy(out=KHT[R, 2 * blk, :], in_=P[bank][0:64, 0:64]), writes=[pk(bank), "KHT"])
        S.op("dve", lambda e, blk=blk, bank=bank: e.tensor_copy(out=KHT[R, 2 * blk + 1, :], in_=P[bank][64:128, 0:64]), writes=[pk(bank), "KHT"])
    if STAGE <= 3:
        S.barrier(); kb.release(m0); return
    U3 = B.rearrange("p (v c) -> p v c", c=128)
    uev = [kb.alloc(512), kb.alloc(512)]
    S3 = A.rearrange("p (v c) -> p v c", c=128)
    for g in range(32):
        bank = 2 + g % 2
        for j in range(4):
            ch = 4 * g + j
            S.op("pe", lambda e, ch=ch, j=j, bank=bank: e.matmul(P[bank][R, j * 128:(j + 1) * 128], lhsT=KHT[R, ch, :], rhs=V[R, ch, :], start=True, stop=True),
                 reads=["KHT", "V"], writes=[pk(bank)])
        ue = uev[g % 2]
        S.op("act", lambda e, bank=bank, ue=ue: e.copy(out=ue[R, :], in_=P[bank][R, :]), writes=[pk(bank), f"uev{g % 2}"])
        for h in range(2):
            hr = slice(32 * h, 32 * h + 32)
            for j in range(4):
                eng = "dve" if j % 2 == 0 else "pool"
                S.op(eng, lambda e, hr=hr, ue=ue, g=g, j=j, h=h: e.tensor_copy(out=U3[hr, :, 4 * g + j], in_=ue[hr, j * 128 + h * 64:j * 128 + (h + 1) * 64]),
                     reads=[f"uev{g % 2}"], writes=["U3", "B"])
    if STAGE <= 4:
        S.barrier(); kb.release(m0); return
    for v in range(64):
        S.op("dve", lambda e, v=v: e.tensor_tensor_scan(out=S3[R, v, :], data0=DC[R, :], data1=U3[R, v, :], initial=0.0, op0=ALU.mult, op1=ALU.add),
             reads=["U3", "DC", "KHT"], writes=["S3", "A"])
    S.op("pool", lambda e: e.memset(SB[R, 0, :], 0.0), writes=["SB0"])
    S.op("act", lambda e: e.copy(out=SB[R, 1:128, :], in_=S3[R, :, 0:127].rearrange("p v c -> p c v")), reads=["S3"], writes=["SB"])
    if STAGE <= 5:
        S.barrier(); kb.release(m0); return
    osb = kb.alloc(512)
    sqb = kb.alloc(512, BF16)
    rstd = kb.alloc(512)
    og = kb.alloc(512)
    at = [kb.alloc(64, BF16), kb.alloc(64, BF16)]
    ai = 0
    for t in range(NQT):
        ts = slice(t * TQ, (t + 1) * TQ)
        for h in range(2):
            hr = slice(32 * h, 32 * h + 32)
            ob = 4 + (2 * t + h) % 2
            for j in range(8):
                ch = 8 * t + j
                cs = slice(ch * 64, (ch + 1) * 64)
                rr = R
                sbk = ai % 2
                a_t = at[ai % 2]
                ak = f"at{ai % 2}"
                ai += 1
                S.op("pe", lambda e, hr=hr, cs=cs, rr=rr, sbk=sbk: e.matmul(P[sbk][rr, 0:64], lhsT=KG[hr, cs], rhs=QG[hr, cs], start=True, stop=True),
                     reads=["KG", "QG"], writes=[pk(sbk)])
                S.op("dve", lambda e, rr=rr, sbk=sbk, a_t=a_t: e.tensor_tensor(out=a_t[rr, :], in0=P[sbk][rr, 0:64], in1=tri64[rr, :], op=ALU.mult),
                     reads=["tri64"], writes=[pk(sbk), ak])
                S.op("pe", lambda e, rr=rr, ch=ch, h=h, a_t=a_t, ob=ob, j=j: e.matmul(P[ob][R, j * 64:(j + 1) * 64], lhsT=V[rr, ch, h * 64:(h + 1) * 64], rhs=a_t[rr, :], start=True, stop=False),
                     reads=[ak, "V"], writes=[pk(ob)])
                S.op("pe", lambda e, hr=hr, ch=ch, cs=cs, ob=ob, j=j: e.matmul(P[ob][R, j * 64:(j + 1) * 64], lhsT=SB[hr, ch, :], rhs=QG[hr, cs], start=False, stop=True),
                     reads=["SB", "SB0", "QG"], writes=[pk(ob)])
            S.op("act", lambda e, ob=ob: e.copy(out=osb[R, :], in_=P[ob][R, :]), writes=[pk(ob), "osb"])
            S.op("act", lambda e: e.activation(out=sqb[R, :], in_=osb[R, :], func=AF.Square), reads=["osb"], writes=["sqb"])
            S.op("pe", lambda e: e.matmul(P[6][R, :], lhsT=c.ones_bf[R, 0:64], rhs=sqb[R, :], start=True, stop=True), reads=["sqb", "ones_bf"], writes=[pk(6)])
            S.op("act", lambda e: e.activation(out=rstd[R, :], in_=P[6][R, :], func=AF.Sqrt, bias=EPS, scale=1.0 / 64.0), writes=[pk(6), "rstd"])
            S.op("dve", lambda e: e.reciprocal(out=rstd[R, :], in_=rstd[R, :]), writes=["rstd"])
            S.op("dve", lambda e: e.scalar_tensor_tensor(out=osb[R, :], in0=osb[R, :], scalar=v2[R, 1:2], in1=rstd[R, :], op0=ALU.mult, op1=ALU.mult),
                 reads=["rstd", "v2"], writes=["osb"])
            S.dma("sp", lambda e, h=h, ts=ts: e.dma_start(out=og[R, :], in_=L.pt(OFF["c_og"] + h * 64, 128, 64, ts.start, ts.stop)), writes=["og"])
            S.op("act", lambda e: e.activation(out=og[R, :], in_=og[R, :], func=AF.Silu), writes=["og"])
            S.op("dve", lambda e: e.tensor_tensor(out=osb[R, :], in0=osb[R, :], in1=og[R, :], op=ALU.mult), reads=["og"], writes=["osb"])
            S.dma("sp", lambda e, h=h, ts=ts: e.dma_start(out=yT[256 + h * 64:256 + (h + 1) * 64, ts], in_=osb[R, :]), reads=["osb"])
    S.barrier()
    kb.release(m0)


WB = ["lru_w", "lru_v", "mla_wuq", "mla_wk", "mla_wv", "mla_v", "nsa_pos", "nsa_w1", "nsa_b1", "nsa_w2", "gla_wg2", "gla_v2"]
WAC = (("gain", [128, 8]), ("w_in", [1024, NCOLW]), ("gng", [128, 8]), ("nfg", [128, 8]), ("w_out", [1024, 1024]),
       ("w_gu", [1024, 2 * DFF]), ("w_dn", [DFF, 1024]))
RG = [[0, 1], [2, 3], [4, 5], [6, 7]]


def build_fused():
    kb = KB(arena_cols=53100)
    S = kb.S
    D = {k: kb.din(k, shp) for k, shp in CONST_SHAPES.items()}
    D["pos"] = kb.din("pos", [1, N], I32)
    xT = kb.din("xT", [1024, 4096])
    out = kb.dout("out", [1024, 4096])
    LW = []
    for l in range(2):
        d = {k: kb.din(f"{k}_{l}", IN_SHAPES[k][0]) for k in WB}
        for k, shp in WAC:
            d[k] = kb.din(f"{k}_{l}", shp)
        LW.append(d)
    fing = kb.din("fing", [128, 8])
    XA = kb.dint("XA", [NCOLP, 4096])
    XV = kb.dint("XV", [4096, NV])
    GP = kb.dint("GP", [2 * NCOLP, 4096])
    GV = kb.dint("GV", [N, NV])
    YB = kb.dint("YB", [1024, 4096])
    YV = YView(YB)
    GY = kb.dint("GY", [2048, 4096])
    XO = kb.dint("XO", [1024, 4096])
    MYP = kb.dint("MYP", [SELROWS, N])
    MYV = kb.dint("MYV", [N, 128])
    MYY = kb.dint("MYY", [1024, 4096])
    c = common_setup(kb, D)
    L = Loader(S, GP, GV, MYP, MYV)
    xs = xT
    for l in range(2):
        W = LW[l]
        ag = lambda i_, o_: (lambda e: e.collective_compute("AllGather", ALU.bypass, replica_groups=RG, ins=[i_], outs=[o_]))
        phase_A(kb, xs, W["gain"], W["w_in"], XA, XV, GP, GV, ag)
        ccA = S.ccn
        Dl = dict(D)
        Dl.update({k: W[k] for k in WB})
        S.cc_wait(kb.cc_lru)
        L.select(names=("a_x", "a_gate"), with_v=False)
        for part, g in ((part_lru, 0), (part_gla, 2), (part_mla, 3), (part_nsa, 1)):
            if g == 2:
                S.cc_wait(ccA)
                L.select(names=("b_q", "b_gate", "c_q", "c_k", "c_og"), with_v=True)
            part(kb, c, Dl, YV, L)
            for ch in range(2):
                S.collective_async(ag(YB[ch * 512 + g * 128:ch * 512 + (g + 1) * 128, :], GY[(g * 2 + ch) * 256:(g * 2 + ch + 1) * 256, :]))
        S.cc_wait_all()
        GY4 = GY.rearrange("(g ch q) t -> g ch q t", g=4, ch=2)
        S.dma("act", lambda e: e.dma_start(out=MYY.rearrange("(g q) t -> g q t", g=4), in_=GY4[:, bass.ds(S.rt["hp_act"], 1), :, :].rearrange("g o q t -> g (o q) t")),
              writes=["MYY"])
        S.barrier()
        phase_C(kb, l == 1, xs, MYY, W["gng"], W["nfg"], fing, W["w_out"], W["w_gu"], W["w_dn"], out if l == 1 else XO)
        xs = XO
    return kb.close()


def prep_W(W, l, hp):
    A = np.ascontiguousarray
    o = {}
    ch = slice(hp * 128, hp * 128 + 128)
    o["lru_w"] = A(np.stack([W["lru_wa"][l][2 * hp:2 * hp + 2], W["lru_wx"][l][2 * hp:2 * hp + 2]]))
    o["lru_v"] = A(np.stack([W["conv_w"][l][0, ch], W["conv_w"][l][1, ch], W["conv_w"][l][2, ch], W["conv_w"][l][3, ch],
                             W["conv_b"][l][ch], W["lru_ba"][l][ch], W["lru_bx"][l][ch], W["lru_lambda"][l][ch]], axis=1))
    o["mla_wuq"] = A(W["mla_w_uq"][l][:, hp * 192:(hp + 1) * 192])
    wkv = W["mla_w_ukv"][l].reshape(128, 4, 128)
    o["mla_wk"] = A(wkv[:, 2 * hp:2 * hp + 2, 0:64].reshape(128, 128))
    o["mla_wv"] = A(wkv[:, 2 * hp:2 * hp + 2, 64:128].reshape(128, 128))
    mv = np.zeros((128, 3), np.float32)
    mv[:, 0] = W["mla_q_norm"][l][0:128]
    mv[0:64, 1] = W["mla_q_norm"][l][128:192]
    mv[:, 2] = W["mla_kv_norm"][l]
    o["mla_v"] = mv
    o["nsa_pos"] = A(np.concatenate([W["cmp_pos"][l][0].T, W["cmp_pos"][l][1].T], axis=0))
    o["nsa_w1"] = A(W["cmp_w1"][l])
    o["nsa_b1"] = A(W["cmp_b1"][l].reshape(2, 2, 128).transpose(2, 0, 1).reshape(128, 4))
    o["nsa_w2"] = A(W["cmp_w2"][l])
    o["gla_wg2"] = A(W["gla_wg2"][l][:, hp * 64:(hp + 1) * 64])
    g2 = np.zeros((128, 2), np.float32)
    g2[0:64, 0] = W["gla_bg2"][l][hp * 64:(hp + 1) * 64]
    g2[:, 1] = np.tile(W["gla_norm"][l], 2)
    o["gla_v2"] = g2
    return o


_PROG = {}


def _arr8(g):
    return np.ascontiguousarray(np.asarray(g, np.float32).reshape(8, 128).T)


def kernel(**inp):
    W = {k: np.asarray(v) for k, v in inp.items()}
    x = W["x"]
    Bn, Sn, Dm = x.shape
    HT = Sn // 2
    cores = [(b, r) for b in range(Bn) for r in range(2)]
    if "F" not in _PROG:
        _PROG["F"] = build_fused()
    nc = _PROG["F"]
    consts = host_consts()
    pc = perm_cols()
    ins = []
    for (b, r) in cores:
        d = dict(consts)
        d["pos"] = np.ascontiguousarray(W["positions"][b][None, :].astype(np.int32))
        d["xT"] = np.ascontiguousarray(x[b, r * HT:(r + 1) * HT].T)
        d["fing"] = _arr8(W["final_norm"])
        for l in range(2):
            for k, v in prep_W(W, l, r).items():
                d[f"{k}_{l}"] = v
            d[f"gain_{l}"] = _arr8(W["norm_mix"][l])
            d[f"w_in_{l}"] = np.ascontiguousarray(W["w_in"][l][:, pc])
            d[f"gng_{l}"] = _arr8(W["group_norm"][l])
            d[f"nfg_{l}"] = _arr8(W["norm_ffn"][l])
            d[f"w_out_{l}"] = np.ascontiguousarray(W["w_out"][l])
            d[f"w_gu_{l}"] = np.ascontiguousarray(W["w_gate_up"][l])
            d[f"w_dn_{l}"] = np.ascontiguousarray(W["w_down"][l])
        ins.append(d)
    res = run_bass_kernel_spmd(nc, ins, core_ids=list(range(8))).results
    out = np.empty((Bn, Sn, Dm), np.float32)
    for ci, (b, r) in enumerate(cores):
        out[b, r * HT:(r + 1) * HT] = res[ci]["out"].T
    return out
```

```python
import numpy as np
from contextlib import ExitStack
import concourse.bass as bass
import concourse.mybir as mybir
from concourse.bass_utils import run_bass_kernel_spmd

F32 = mybir.dt.float32
BF16 = mybir.dt.bfloat16
I32 = mybir.dt.int32
AF = mybir.ActivationFunctionType
ALU = mybir.AluOpType
AX = mybir.AxisListType

ENGS = ("pe", "act", "dve", "pool", "sp")
NDMA_SEMS = 8
EPS = 1e-6


class Sched:
    def __init__(self, nc, es):
        self.nc = nc
        self.sem = {e: es.enter_context(nc.semaphore("s_" + e)) for e in ENGS}
        self.cnt = {e: 0 for e in ENGS}
        self.dsem = {e: [es.enter_context(nc.semaphore(f"d_{e}{i}")) for i in range(NDMA_SEMS)]
                     for e in ("sp", "pool", "act")}
        self.dval = {e: [0] * NDMA_SEMS for e in self.dsem}
        self.drr = {e: 0 for e in self.dsem}
        self.ops = {e: [] for e in ENGS}
        self.known = {e: {} for e in ENGS}
        self.semobj = {}
        self.last_w = {}
        self.readers = {}
        self.ccsem = es.enter_context(nc.semaphore("s_cc"))
        self.semobj["cc"] = self.ccsem
        self.ccn = 0
        self.rt = {}
        for e in ENGS:
            self.semobj["c_" + e] = self.sem[e]
        for e in self.dsem:
            for i in range(NDMA_SEMS):
                self.semobj[f"d_{e}{i}"] = self.dsem[e][i]

    def _need(self, eng, tok, waits):
        if tok is None:
            return
        sk, val, teng = tok
        if teng == "pe" and eng == "pe" and sk == "c_pe":
            return
        if self.known[eng].get(sk, 0) >= val:
            return
        self.known[eng][sk] = val
        waits[sk] = max(waits.get(sk, 0), val)

    def _deps(self, eng, reads, writes):
        waits = {}
        for k in reads:
            self._need(eng, self.last_w.get(k), waits)
        for k in writes:
            self._need(eng, self.last_w.get(k), waits)
            for t in self.readers.get(k, ()):
                self._need(eng, t, waits)
        return waits

    def _commit(self, tok, reads, writes):
        for k in reads:
            self.readers.setdefault(k, []).append(tok)
        for k in writes:
            self.last_w[k] = tok
            self.readers[k] = []

    def op(self, eng, emit, reads=(), writes=()):
        waits = self._deps(eng, reads, writes)
        self.cnt[eng] += 1
        tok = ("c_" + eng, self.cnt[eng], eng)
        self.ops[eng].append((waits, emit, (self.sem[eng], 1)))
        self._commit(tok, reads, writes)
        return tok

    def dma(self, eng, emit, reads=(), writes=()):
        waits = self._deps(eng, reads, writes)
        i = self.drr[eng]
        self.drr[eng] = (i + 1) % NDMA_SEMS
        sk = f"d_{eng}{i}"
        prev = self.dval[eng][i]
        if prev and self.known[eng].get(sk, 0) < prev:
            self.known[eng][sk] = prev
            waits[sk] = prev
        self.dval[eng][i] = prev + 16
        tok = (sk, prev + 16, eng)
        self.ops[eng].append((waits, emit, (self.dsem[eng][i], 16)))
        self._commit(tok, reads, writes)
        return tok

    def barrier(self):
        for eng in ENGS:
            waits = {}
            for e in ENGS:
                if e != eng and self.cnt[e]:
                    self._need(eng, ("c_" + e, self.cnt[e], e), waits)
            for e in self.dsem:
                for i in range(NDMA_SEMS):
                    if self.dval[e][i]:
                        self._need(eng, (f"d_{e}{i}", self.dval[e][i], "dma"), waits)
            if eng != "pe" and self.cnt[eng]:
                self._need(eng, ("c_" + eng, self.cnt[eng], eng), waits)
            if waits:
                self.ops[eng].append((waits, None, None))
        self.last_w = {}
        self.readers = {}

    def collective(self, emits):
        self.barrier()
        for emit in emits:
            w = {"cc": self.ccn} if self.ccn else {}
            self.ccn += 1
            self.ops["pool"].append((w, emit, (self.ccsem, 1)))
        for eng in ENGS:
            self.known[eng]["cc"] = self.ccn
            self.ops[eng].append(({"cc": self.ccn}, None, None))

    def collective_async(self, emit, reads=()):
        waits = {}
        for k in reads:
            self._need("pool", self.last_w.get(k), waits)
        if self.ccn:
            waits["cc"] = self.ccn
        self.ccn += 1
        self.ops["pool"].append((waits, emit, (self.ccsem, 1)))

    def cc_wait(self, n):
        self.barrier()
        for eng in ENGS:
            if self.known[eng].get("cc", 0) < n:
                self.known[eng]["cc"] = n
                self.ops[eng].append(({"cc": n}, None, None))

    def cc_wait_all(self):
        self.barrier()
        for eng in ENGS:
            if self.known[eng].get("cc", 0) < self.ccn:
                self.known[eng]["cc"] = self.ccn
                self.ops[eng].append(({"cc": self.ccn}, None, None))

    def finish(self):
        self.barrier()
        semobj = self.semobj

        def run(engobj, lst):
            for waits, emit, inc in lst:
                for sk, v in waits.items():
                    engobj.wait_ge(semobj[sk], v)
                if emit is not None:
                    emit(engobj).then_inc(inc[0], inc[1])

        with self.nc.Block() as block:
            @block.tensor
            def _(e):
                run(e, self.ops["pe"])

            @block.scalar
            def _(e):
                self.rt["hp_act"] = e.partition_id() % 2
                run(e, self.ops["act"])

            @block.vector
            def _(e):
                run(e, self.ops["dve"])

            @block.gpsimd
            def _(e):
                run(e, self.ops["pool"])

            @block.sync
            def _(e):
                self.rt["hp_sp"] = e.partition_id() % 2
                run(e, self.ops["sp"])


class KB:
    def __init__(self, arena_cols=50000):
        self.nc = bass.Bass("TRN2", target_bir_lowering=False)
        self.es = ExitStack()
        self.S = Sched(self.nc, self.es)
        self.arena = self.es.enter_context(self.nc.sbuf_tensor("arena", [128, arena_cols], F32))
        self.acols = arena_cols
        self.top = 0
        self.psb = [self.es.enter_context(self.nc.psum_tensor(f"psb{i}", [128, 512], F32)) for i in range(8)]
        self.uid = 0

    def dint(self, name, shape, dt=F32):
        return self.nc.dram_tensor(name, list(shape), dt).ap()

    def din(self, name, shape, dt=F32):
        return self.nc.dram_tensor(name, list(shape), dt, kind="ExternalInput").ap()

    def dout(self, name, shape, dt=F32):
        return self.nc.dram_tensor(name, list(shape), dt, kind="ExternalOutput").ap()

    def alloc(self, cols, dt=F32):
        n32 = cols if dt != BF16 else (cols + 1) // 2
        a = self.top
        self.top += n32
        assert self.top <= self.acols, f"arena overflow {self.top}"
        v = self.arena[:, a:a + n32]
        if dt == BF16:
            v = v.bitcast(BF16)
        elif dt == I32:
            v = v.bitcast(I32)
        return v

    def mark(self):
        return self.top

    def release(self, m):
        self.top = m

    def key(self, base="k"):
        self.uid += 1
        return f"{base}{self.uid}"

    def close(self):
        self.S.finish()
        self.es.close()
        return self.nc


NCOL = 2300
NCOLP = 1920
NCOLW = NCOLP + 384
VCOLS = [(NCOLP, NCOLW)]
NV = 384
OFF = dict(a_x=0, a_gate=256, b_q=512, b_kv=768, c_q=1024, c_k=1152, c_og=1280, d_cq=1536, d_kr=1728, c_glr=1760,
           b_gate=1776, d_ckv=1792)


def perm_cols():
    o = dict(a_x=0, a_gate=256, b_q=512, b_kv=768, b_gate=1152, c_q=1164, c_k=1292, c_v=1420, c_glr=1676, c_og=1692,
             d_cq=1948, d_ckv=2140, d_kr=2268)
    r = lambda a, n: list(range(a, a + n))
    p = (r(o["a_x"], 256) + r(o["a_gate"], 256) + r(o["b_q"], 256)
         + r(o["b_kv"], 64) + r(o["b_kv"] + 64, 64) + r(o["b_kv"] + 128, 64) + r(o["b_kv"] + 256, 64)
         + r(o["c_q"], 128) + r(o["c_k"], 128) + r(o["c_og"], 256) + r(o["d_cq"], 192) + r(o["d_kr"], 32)
         + r(o["c_glr"], 16) + r(o["b_gate"], 12) + r(o["b_gate"], 4) + r(o["d_ckv"], 128))
    assert len(p) == NCOLP
    p += r(o["b_kv"] + 192, 64) + r(o["b_kv"] + 320, 64) + r(o["c_v"], 256)
    assert len(p) == NCOLW
    return np.array(p)
DFF = 2816


def load_cast_weight(kb, w_dram, wsb, kchunks, ncols, stage, key, piece=1024):
    S = kb.S
    i = 0
    for c in range(kchunks):
        for c0 in range(0, ncols, piece):
            c1 = min(ncols, c0 + piece)
            st = stage[i % len(stage)]
            sk = f"wstage{i % len(stage)}"
            S.dma("sp", lambda e, st=st, c=c, c0=c0, c1=c1: e.dma_start(out=st[:, 0:c1 - c0], in_=w_dram[c * 128:(c + 1) * 128, c0:c1]),
                  writes=[sk])
            eng = "act" if i % 2 == 0 else "pool"
            if eng == "act":
                S.op("act", lambda e, st=st, c=c, c0=c0, c1=c1: e.copy(out=wsb[:, c, c0:c1], in_=st[:, 0:c1 - c0]), reads=[sk], writes=[key])
            else:
                S.op("pool", lambda e, st=st, c=c, c0=c0, c1=c1: e.tensor_copy(out=wsb[:, c, c0:c1], in_=st[:, 0:c1 - c0]), reads=[sk], writes=[key])
            i += 1


def rms_stats(kb, src3, nch, T, sq, ones_bf, ps_ap, rstd, denom, keys_in, key_sq, key_ps, key_rstd):
    S = kb.S
    S.op("act", lambda e: e.activation(out=sq, in_=src3, func=AF.Square), reads=keys_in, writes=[key_sq])
    for c in range(nch):
        S.op("pe", lambda e, c=c: e.matmul(ps_ap, lhsT=ones_bf, rhs=sq[:, c, :], start=(c == 0), stop=(c == nch - 1)),
             reads=[key_sq, "ones_bf"], writes=[key_ps])
    S.op("act", lambda e: e.activation(out=rstd, in_=ps_ap, func=AF.Sqrt, bias=EPS, scale=1.0 / denom), writes=[key_ps, key_rstd])
    S.op("dve", lambda e: e.reciprocal(out=rstd, in_=rstd), reads=[key_rstd], writes=[key_rstd])


def phase_A(kb, xT, gain, w, pT, pV, GP, GV, ag):
    S = kb.S
    m0 = kb.mark()
    T = 512
    NT = 8
    xv = xT.rearrange("(c p) t -> p c t", p=128)

    wsb = kb.alloc(8 * NCOLW, BF16).rearrange("p (c n) -> p c n", c=8)
    gsb = kb.alloc(8)
    ones_bf = kb.alloc(128, BF16)
    stage = [kb.alloc(1024) for _ in range(4)]
    xt = [kb.alloc(8 * T).rearrange("p (c t) -> p c t", c=8) for _ in range(2)]
    sq = kb.alloc(8 * T, BF16).rearrange("p (c t) -> p c t", c=8)
    hb = kb.alloc(8 * 4096, BF16).rearrange("p (c t) -> p c t", c=8)
    rstd = kb.alloc(T)
    ost = [kb.alloc(T) for _ in range(4)]
    P = [p[:] for p in kb.psb]

    S.dma("sp", lambda e: e.dma_start(out=gsb, in_=gain[:, :]), writes=["gsb"])
    S.op("pool", lambda e: e.memset(ones_bf, 1.0), writes=["ones_bf"])
    S.dma("sp", lambda e: e.dma_start(out=xt[0], in_=xv[:, :, 0:T]), writes=["xt0"])
    load_cast_weight(kb, w, wsb, 8, NCOLW, stage, "wsb")
    for t in range(NT):
        b = t % 2
        if t + 1 < NT:
            S.dma("sp", lambda e, t=t: e.dma_start(out=xt[(t + 1) % 2], in_=xv[:, :, (t + 1) * T:(t + 2) * T]),
                  writes=[f"xt{(t + 1) % 2}"])
        rms_stats(kb, xt[b], 8, T, sq, ones_bf, P[0], rstd, 1024.0, [f"xt{b}"], "sq", "psb0", "rstd")
        for c in range(8):
            S.op("dve", lambda e, c=c, b=b, t=t: e.scalar_tensor_tensor(out=hb[:, c, t * T:(t + 1) * T], in0=xt[b][:, c, :], scalar=gsb[:, c:c + 1], in1=rstd,
                                                                   op0=ALU.mult, op1=ALU.mult),
                 reads=[f"xt{b}", "rstd", "gsb"], writes=[f"hb{t}"])
    oi = 0
    for t in range(NT):
        for tb in range(4):
            pb = 5 + (tb % 2)
            for c in range(8):
                S.op("pe", lambda e, c=c, t=t, tb=tb, pb=pb: e.matmul(
                    P[pb][:, 0:NV], lhsT=hb[:, c, t * T + tb * 128:t * T + (tb + 1) * 128], rhs=wsb[:, c, NCOLP:NCOLW], start=(c == 0), stop=(c == 7)),
                    reads=[f"hb{t}", "wsb"], writes=[f"psb{pb}"])
            o = oi % 4
            oi += 1
            S.op("act", lambda e, o=o, pb=pb: e.copy(out=ost[o][:, 0:NV], in_=P[pb][:, 0:NV]), writes=[f"psb{pb}", f"ost{o}"])
            S.dma("sp", lambda e, o=o, t=t, tb=tb: e.dma_start(out=pV[t * T + tb * 128: t * T + (tb + 1) * 128, :], in_=ost[o][:, 0:NV]),
                  reads=[f"ost{o}"], writes=[f"XV{t}_{tb}"])
        if t % 2 == 1:
            j = t // 2
            S.collective_async(ag(pV[j * 1024:(j + 1) * 1024, :], GV[j * 2048:(j + 1) * 2048, :]),
                               reads=[f"XV{tt}_{tb}" for tt in (t - 1, t) for tb in range(4)])
    for k in range(NCOLP // 128):
        c0, c1 = k * 128, (k + 1) * 128
        for t in range(NT):
            pb = 1 + (oi % 4)
            for c in range(8):
                S.op("pe", lambda e, c=c, t=t, c0=c0, c1=c1, pb=pb: e.matmul(P[pb][:, :], lhsT=wsb[:, c, c0:c1], rhs=hb[:, c, t * T:(t + 1) * T],
                                                                             start=(c == 0), stop=(c == 7)),
                     reads=[f"hb{t}", "wsb"], writes=[f"psb{pb}"])
            o = oi % 4
            oi += 1
            if t % 2 == 0:
                S.op("act", lambda e, o=o, pb=pb: e.copy(out=ost[o], in_=P[pb][:, :]), writes=[f"psb{pb}", f"ost{o}"])
            else:
                S.op("dve", lambda e, o=o, pb=pb: e.tensor_copy(out=ost[o], in_=P[pb][:, :]), writes=[f"psb{pb}", f"ost{o}"])
            S.dma("sp", lambda e, o=o, c0=c0, c1=c1, t=t: e.dma_start(out=pT[c0:c1, t * T:(t + 1) * T], in_=ost[o]),
                  reads=[f"ost{o}"], writes=[f"XA{k}_{t}"])
        S.collective_async(ag(pT[c0:c1, :], GP[k * 256:(k + 1) * 256, :]), reads=[f"XA{k}_{t}" for t in range(NT)])
        if k == 3:
            kb.cc_lru = S.ccn
    S.barrier()
    kb.release(m0)


def phase_C(kb, final, xT, GY, gng, nfg, fing, w_out, w_gu, w_dn, xo, NT=16):
    S = kb.S
    m0 = kb.mark()
    T = 256
    xv = xT.rearrange("(c p) t -> p c t", p=128)
    ov = xo.rearrange("(c p) t -> p c t", p=128)

    wo = kb.alloc(8 * 1024, BF16).rearrange("p (c n) -> p c n", c=8)
    wgu = kb.alloc(8 * 2 * DFF, BF16).rearrange("p (c n) -> p c n", c=8)
    wdn = kb.alloc(22 * 1024, BF16).rearrange("p (c n) -> p c n", c=22)
    g3 = kb.alloc(24)
    ones_bf = kb.alloc(128, BF16)
    xts = [kb.alloc(8 * T).rearrange("p (c t) -> p c t", c=8) for _ in range(2)]
    yt = kb.alloc(8 * T).rearrange("p (c t) -> p c t", c=8)
    ytf = yt.rearrange("p c t -> p (c t)")
    stage = [ytf[:, 0:1024], ytf[:, 1024:2048]]
    for xb_ in xts:
        xbf = xb_.rearrange("p c t -> p (c t)")
        stage += [xbf[:, 0:1024], xbf[:, 1024:2048]]
    sq = kb.alloc(8 * T, BF16).rearrange("p (c t) -> p c t", c=8)
    hb = kb.alloc(8 * T, BF16).rearrange("p (c t) -> p c t", c=8)
    aT = kb.alloc(22 * T, BF16).rearrange("p (c t) -> p c t", c=22)
    rstd4 = kb.alloc(4 * T).rearrange("p (c t) -> p c t", c=4)
    rstd = kb.alloc(T)
    sg = [kb.alloc(T), kb.alloc(T)]
    P = [p[:] for p in kb.psb]

    S.dma("sp", lambda e: e.dma_start(out=g3[:, 0:8], in_=gng[:, :]), writes=["g3"])
    S.dma("sp", lambda e: e.dma_start(out=g3[:, 8:16], in_=nfg[:, :]), writes=["g3"])
    S.dma("sp", lambda e: e.dma_start(out=g3[:, 16:24], in_=fing[:, :]), writes=["g3"])
    S.op("pool", lambda e: e.memset(ones_bf, 1.0), writes=["ones_bf"])
    load_cast_weight(kb, w_out, wo, 8, 1024, stage, "wo")
    load_cast_weight(kb, w_gu, wgu, 8, 2 * DFF, stage, "wgu")
    load_cast_weight(kb, w_dn, wdn, 22, 1024, stage, "wdn")
    S.barrier()

    def load_y(t):
        for cc_ in range(8):
            r0 = cc_ * 128
            S.dma("sp", lambda e, cc_=cc_, r0=r0, t=t: e.dma_start(out=yt[:, cc_, :], in_=GY[r0:r0 + 128, t * T:(t + 1) * T]),
                  writes=["yt"])

    S.dma("sp", lambda e: e.dma_start(out=xts[0], in_=xv[:, :, 0:T]), writes=["xt0"])
    load_y(0)
    for t in range(NT):
        ts = slice(t * T, (t + 1) * T)
        xt = xts[t % 2]
        XK = f"xt{t % 2}"
        if t + 1 < NT:
            S.dma("sp", lambda e, t=t: e.dma_start(out=xts[(t + 1) % 2], in_=xv[:, :, (t + 1) * T:(t + 2) * T]), writes=[f"xt{(t + 1) % 2}"])
        S.op("act", lambda e: e.activation(out=sq, in_=yt, func=AF.Square), reads=["yt"], writes=["sq"])
        for g in range(4):
            pa = P[g][:, 0:T]
            for j in range(2):
                S.op("pe", lambda e, g=g, j=j, pa=pa: e.matmul(pa, lhsT=ones_bf, rhs=sq[:, 2 * g + j, :], start=(j == 0), stop=(j == 1)),
                     reads=["sq", "ones_bf"], writes=[f"psb{g}"])
            S.op("act", lambda e, g=g, pa=pa: e.activation(out=rstd4[:, g, :], in_=pa, func=AF.Sqrt, bias=EPS, scale=1.0 / 256.0),
                 writes=[f"psb{g}", f"rstd4_{g}"])
            S.op("dve", lambda e, g=g: e.reciprocal(out=rstd4[:, g, :], in_=rstd4[:, g, :]), reads=[f"rstd4_{g}"], writes=[f"rstd4_{g}"])
        for c in range(8):
            eng = "dve"
            S.op(eng, lambda e, c=c: e.scalar_tensor_tensor(out=hb[:, c, :], in0=yt[:, c, :], scalar=g3[:, c:c + 1], in1=rstd4[:, c // 2, :],
                                                         op0=ALU.mult, op1=ALU.mult),
                 reads=["yt", f"rstd4_{c // 2}", "g3"], writes=[f"hb{c}"])
        if t + 1 < NT:
            load_y(t + 1)
        for m in range(8):
            pa = P[4 + m % 4][:, 0:T]
            for c in range(8):
                S.op("pe", lambda e, m=m, c=c, pa=pa: e.matmul(pa, lhsT=wo[:, c, m * 128:(m + 1) * 128], rhs=hb[:, c, :], start=(c == 0), stop=(c == 7)),
                     reads=[f"hb{c}", "wo"], writes=[f"psb{4 + m % 4}"])
            S.op("dve", lambda e, m=m, pa=pa, xt=xt: e.tensor_tensor(out=xt[:, m, :], in0=xt[:, m, :], in1=pa, op=ALU.add),
                 reads=[XK], writes=[XK, f"psb{4 + m % 4}"])
        rms_stats(kb, xt, 8, T, sq, ones_bf, P[0][:, 0:T], rstd, 1024.0, [XK], "sq", "psb0", "rstd")
        for c in range(8):
            eng = "dve"
            S.op(eng, lambda e, c=c, xt=xt: e.scalar_tensor_tensor(out=hb[:, c, :], in0=xt[:, c, :], scalar=g3[:, 8 + c:9 + c], in1=rstd,
                                                         op0=ALU.mult, op1=ALU.mult),
                 reads=[XK, "rstd", "g3"], writes=[f"hb{c}"])
        for j in range(22):
            pg = P[j % 2][:, 0:T]
            pu = P[2 + j % 2][:, 0:T]
            for c in range(8):
                S.op("pe", lambda e, j=j, c=c, pg=pg: e.matmul(pg, lhsT=wgu[:, c, j * 128:(j + 1) * 128], rhs=hb[:, c, :], start=(c == 0), stop=(c == 7)),
                     reads=[f"hb{c}", "wgu"], writes=[f"psb{j % 2}"])
            for c in range(8):
                S.op("pe", lambda e, j=j, c=c, pu=pu: e.matmul(pu, lhsT=wgu[:, c, DFF + j * 128:DFF + (j + 1) * 128], rhs=hb[:, c, :], start=(c == 0), stop=(c == 7)),
                     reads=[f"hb{c}", "wgu"], writes=[f"psb{2 + j % 2}"])
            S.op("act", lambda e, j=j, pg=pg: e.activation(out=sg[j % 2], in_=pg, func=AF.Silu), writes=[f"psb{j % 2}", f"sg{j % 2}"])
            S.op("dve", lambda e, j=j, pu=pu: e.tensor_tensor(out=aT[:, j, :], in0=sg[j % 2], in1=pu, op=ALU.mult),
                 reads=[f"sg{j % 2}"], writes=[f"aT{j}", f"psb{2 + j % 2}"])
        for m in range(8):
            pa = P[4 + m % 4][:, 0:T]
            for k in range(22):
                S.op("pe", lambda e, m=m, k=k, pa=pa: e.matmul(pa, lhsT=wdn[:, k, m * 128:(m + 1) * 128], rhs=aT[:, k, :], start=(k == 0), stop=(k == 21)),
                     reads=[f"aT{k}", "wdn"], writes=[f"psb{4 + m % 4}"])
            S.op("dve", lambda e, m=m, pa=pa, xt=xt: e.tensor_tensor(out=xt[:, m, :], in0=xt[:, m, :], in1=pa, op=ALU.add),
                 reads=[XK], writes=[XK, f"psb{4 + m % 4}"])
        if final:
            rms_stats(kb, xt, 8, T, sq, ones_bf, P[0][:, 0:T], rstd, 1024.0, [XK], "sq", "psb0", "rstd")
            for c in range(8):
                eng = "dve"
                S.op(eng, lambda e, c=c, xt=xt: e.scalar_tensor_tensor(out=xt[:, c, :], in0=xt[:, c, :], scalar=g3[:, 16 + c:17 + c], in1=rstd,
                                                                    op0=ALU.mult, op1=ALU.mult),
                     reads=["rstd", "g3"], writes=[XK])
            S.dma("sp", lambda e, ts=ts, xt=xt: e.dma_start(out=ov[:, :, ts], in_=xt), reads=[XK])
        else:
            S.dma("sp", lambda e, ts=ts, xt=xt: e.dma_start(out=ov[:, :, ts], in_=xt), reads=[XK])
    S.barrier()
    kb.release(m0)


N = 8192
TQ = 512
NQT = N // TQ
NEG = -30000.0
PI = float(np.pi)
TWO_PI = float(2 * np.pi)
THETA = 10000.0


def pk(b):
    return f"psb{b}"


def host_consts():
    c = {}
    c["ident"] = np.eye(128, dtype=np.float32)
    R = np.zeros((128, 128), np.float32)
    for blk in range(2):
        for m in range(64):
            if m < 32:
                R[blk * 64 + m + 32, blk * 64 + m] = -1.0
            else:
                R[blk * 64 + m - 32, blk * 64 + m] = 1.0
    c["rbd"] = R
    R32 = np.zeros((128, 32), np.float32)
    for m in range(32):
        if m < 16:
            R32[64 + m + 16, m] = -1.0
        else:
            R32[64 + m - 16, m] = 1.0
    c["r32"] = R32
    invf = np.zeros((128, 2), np.float32)
    for p in range(128):
        invf[p, 0] = np.float32(THETA) ** np.float32(-(2.0 * ((p % 64) % 32)) / 64.0)
    for p in range(64, 96):
        invf[p, 1] = np.float32(THETA) ** np.float32(-(2.0 * ((p - 64) % 16)) / 32.0)
    c["invf"] = invf
    k = np.arange(128)[:, None]
    q = np.arange(512)[None, :]
    c["tri"] = np.stack([np.where(k + i * 128 <= q, 0.0, NEG) for i in range(4)]).astype(np.float32)
    c["win"] = np.stack([np.where((k + (i - 4) * 128 <= q) & (k + (i - 4) * 128 > q - 512), 0.0, NEG) for i in range(8)]).astype(np.float32)
    c["cmpm"] = np.stack([np.where(16 * k + 31 <= i * 512 + q, 0.0, NEG) for i in range(5)]).astype(np.float32)
    n = np.arange(512)
    s = np.arange(128)
    ov = ((n[:, None] * 16 < s[None, :] * 64 + 64) & (n[:, None] * 16 + 32 > s[None, :] * 64)).astype(np.float32)
    ov[511] = 0.0
    ovl = np.zeros((4, 128, 129), np.float32)
    ovl[:, :, :128] = ov.reshape(4, 128, 128)
    ovl[:, :, 128] = 1.0
    c["ovl"] = ovl
    c["eall"] = (np.arange(N)[None, :] // 64 == np.arange(128)[:, None]).astype(np.float32)
    ql = np.arange(128)[:, None] // 64
    j = np.arange(254)[None, :] - 126
    c["bv"] = (j <= ql - 2).astype(np.float32)
    c["bf"] = (np.where((j == ql) | (j == ql - 1), 1e9, 0.0) + np.where(j > ql, -1.0, 0.0)).astype(np.float32)
    selg = np.zeros((8, 6 * 64), np.float32)
    for r in range(6):
        selg[r, r * 64:(r + 1) * 64] = 1.0
    c["selg"] = selg
    c["tri64"] = (np.arange(64)[:, None] <= np.arange(64)[None, :]).astype(np.float32)
    return c


CONST_SHAPES = {"ident": [128, 128], "rbd": [128, 128], "r32": [128, 32], "invf": [128, 2], "tri": [4, 128, 512],
                "win": [8, 128, 512], "cmpm": [5, 128, 512], "ovl": [4, 128, 129], "eall": [128, N], "bv": [128, 254],
                "bf": [128, 254], "selg": [8, 384], "tri64": [64, 64]}

IN_SHAPES = {
    "pos": ([1, N], I32),
    "lru_x": ([2, 128, N], F32), "lru_w": ([2, 2, 64, 64], F32), "lru_v": ([128, 8], F32),
    "mla_cq": ([192, N], F32), "mla_ckv": ([128, N], F32), "mla_kr": ([32, N], F32),
    "mla_wuq": ([192, 192], F32), "mla_wk": ([128, 128], F32), "mla_wv": ([128, 128], F32), "mla_v": ([128, 3], F32),
    "nsa_q": ([2, 128, N], F32), "nsa_k": ([3, 64, N], F32), "nsa_vc": ([64, N], F32), "nsa_vs": ([N, 64], F32),
    "nsa_vw": ([N, 64], F32), "nsa_g": ([6, N], F32), "nsa_pos": ([128, 32], F32), "nsa_w1": ([2, 2048, 256], F32),
    "nsa_b1": ([128, 4], F32), "nsa_w2": ([2, 256, 64], F32),
    "gla_q": ([64, N], F32), "gla_k": ([64, N], F32), "gla_v": ([N, 128], F32), "gla_glr": ([16, N], F32),
    "gla_og": ([128, N], F32), "gla_wg2": ([16, 64], F32), "gla_v2": ([128, 2], F32),
}


class Ctx:
    pass


class YView:
    def __init__(self, ap):
        self.ap = ap

    def __getitem__(self, key):
        rs, cs = key
        half = cs.start // 4096
        assert (cs.stop - 1) // 4096 == half
        return self.ap[half * 512 + rs.start:half * 512 + rs.stop, cs.start - half * 4096:cs.stop - half * 4096]


SEL = [("a_x", 128, False, 128), ("a_gate", 128, False, 128), ("b_q", 128, False, 128), ("b_q", 128, True, 128),
       ("b_gate", 6, False, 6), ("c_q", 64, False, 64), ("c_k", 64, False, 64), ("c_og", 128, False, 128)]
SELROWS = sum(x[3] for x in SEL)


class Loader:
    def __init__(self, S, GP, GV, MYP, MYV):
        self.S = S
        self.GP, self.GV, self.MYP, self.MYV = GP, GV, MYP, MYV
        self.row0 = {}
        r = 0
        for nm, hpm, inv, n in SEL:
            self.row0[(nm, inv)] = r
            r += n

    @staticmethod
    def gprow(r, half):
        return (r // 128) * 256 + half * 128 + r % 128

    @staticmethod
    def gvrow(t):
        half, tl = t // 4096, t % 4096
        return (tl // 1024) * 2048 + half * 1024 + tl % 1024

    def select(self, names=None, with_v=True):
        S = self.S
        i = 0
        for nm, hpm, inv, n in SEL:
            if names is not None and nm not in names:
                i += 2
                continue
            r0 = self.row0[(nm, inv)]
            mult = 256 if hpm == 128 else hpm
            for hf in range(2):
                q = "sp" if i % 2 == 0 else "act"
                i += 1

                def emit(e, nm=nm, mult=mult, inv=inv, n=n, r0=r0, hf=hf, q=q):
                    hp = S.rt["hp_" + q]
                    start = ((1 - hp) if inv else hp) * mult + self.gprow(OFF[nm], hf)
                    return e.dma_start(out=self.MYP[r0:r0 + n, hf * 4096:(hf + 1) * 4096], in_=self.GP[bass.ds(start, n), :])
                S.dma(q, emit, writes=[f"MYP{i}"])
        for j in (range(4) if with_v else ()):
            S.dma("sp", lambda e, j=j: e.dma_start(out=self.MYV[j * 2048:(j + 1) * 2048, :],
                                                   in_=self.GV[:, bass.ds(S.rt["hp_sp"] * 128 + 128, 128)][j * 2048:(j + 1) * 2048, :]), writes=[f"MYV{j}"])
        S.barrier()

    def pt(self, off, hpm, n, c0, c1, inv=False):
        if hpm == 0:
            half = c0 // 4096
            assert off // 128 == (off + n - 1) // 128
            base = self.gprow(off, half)
            return self.GP[base:base + n, c0 - half * 4096:c1 - half * 4096]
        for nm, hm, iv, nn in SEL:
            if hm == hpm and iv == inv and OFF[nm] <= off and (off - OFF[nm]) + n <= nn:
                r0 = self.row0[(nm, inv)] + (off - OFF[nm])
                return self.MYP[r0:r0 + n, c0:c1]
        raise KeyError((off, hpm, n, inv))

    def pv(self, coff, hpm, n, t0, t1):
        assert t0 // 1024 == (t1 - 1) // 1024
        g0 = self.gvrow(t0)
        if hpm == 0:
            return self.GV[g0:g0 + (t1 - t0), coff:coff + n]
        assert coff == 128 and n == 128
        return self.MYV[g0:g0 + (t1 - t0), :]


def common_setup(kb, D):
    S = kb.S
    c = Ctx()
    c.ident = kb.alloc(128)
    c.ones_f = kb.alloc(128)
    c.ones_bf = kb.alloc(128, BF16)
    c.ident_bf = kb.alloc(128, BF16)
    S.dma("sp", lambda e: e.dma_start(out=c.ident, in_=D["ident"][:, :]), writes=["ident"])
    S.op("pool", lambda e: e.memset(c.ones_f, 1.0), writes=["ones_f"])
    S.op("pool", lambda e: e.memset(c.ones_bf, 1.0), writes=["ones_bf"])
    S.op("act", lambda e: e.copy(out=c.ident_bf, in_=c.ident), reads=["ident"], writes=["ident_bf"])
    c.invf = kb.alloc(2)
    S.dma("sp", lambda e: e.dma_start(out=c.invf, in_=D["invf"][:, :]), writes=["invf"])
    return c


def rope_tables(kb, c, posf, posk, r0, r1, col, T, bank, tag):
    S = kb.S
    P = kb.psb[bank]
    n = r1 - r0
    rs = slice(r0, r1)
    a, kf, ki, sn, cs = T["ang"], T["kf"], T["ki"], T["sin"], T["cos"]
    S.op("pe", lambda e: e.matmul(P[rs, :], lhsT=c.ones_f[0:1, 0:n], rhs=posf[0:1, :], start=True, stop=True),
         reads=["ones_f", posk], writes=[pk(bank)])
    S.op("dve", lambda e: e.tensor_scalar(out=a[rs, :], in0=P[rs, :], scalar1=c.invf[rs, col:col + 1], scalar2=None, op0=ALU.mult),
         reads=["invf"], writes=[pk(bank), tag + "ang"])
    S.op("dve", lambda e: e.tensor_scalar(out=ki[rs, :], in0=a[rs, :], scalar1=1.0 / TWO_PI, scalar2=None, op0=ALU.mult),
         reads=[tag + "ang"], writes=[tag + "ki"])
    S.op("dve", lambda e: e.tensor_copy(out=kf[rs, :], in_=ki[rs, :]), reads=[tag + "ki"], writes=[tag + "kf"])
    S.op("dve", lambda e: e.scalar_tensor_tensor(out=a[rs, :], in0=kf[rs, :], scalar=-TWO_PI, in1=a[rs, :], op0=ALU.mult, op1=ALU.add),
         reads=[tag + "kf"], writes=[tag + "ang"])
    S.op("dve", lambda e: e.tensor_scalar(out=kf[rs, :], in0=a[rs, :], scalar1=PI, scalar2=-TWO_PI, op0=ALU.is_gt, op1=ALU.mult),
         reads=[tag + "ang"], writes=[tag + "kf"])
    S.op("dve", lambda e: e.tensor_tensor(out=sn[rs, :], in0=a[rs, :], in1=kf[rs, :], op=ALU.add),
         reads=[tag + "ang", tag + "kf"], writes=[tag + "sin"])
    S.op("dve", lambda e: e.tensor_scalar(out=a[rs, :], in0=a[rs, :], scalar1=PI / 2, scalar2=None, op0=ALU.add),
         reads=[], writes=[tag + "ang"])
    S.op("dve", lambda e: e.tensor_scalar(out=kf[rs, :], in0=a[rs, :], scalar1=PI, scalar2=-TWO_PI, op0=ALU.is_gt, op1=ALU.mult),
         reads=[tag + "ang"], writes=[tag + "kf"])
    S.op("dve", lambda e: e.tensor_tensor(out=cs[rs, :], in0=a[rs, :], in1=kf[rs, :], op=ALU.add),
         reads=[tag + "ang", tag + "kf"], writes=[tag + "cos"])
    S.op("act", lambda e: e.activation(out=sn[rs, :], in_=sn[rs, :], func=AF.Sin), writes=[tag + "sin"])
    S.op("act", lambda e: e.activation(out=cs[rs, :], in_=cs[rs, :], func=AF.Sin), writes=[tag + "cos"])


def alloc_tables(kb):
    return {"ang": kb.alloc(512), "kf": kb.alloc(512), "ki": kb.alloc(512, I32), "sin": kb.alloc(512), "cos": kb.alloc(512)}


def load_pos(kb, D, posi, posf, t):
    S = kb.S
    S.dma("sp", lambda e: e.dma_start(out=posi[0:1, :], in_=D["pos"][0:1, t * TQ:(t + 1) * TQ]), writes=["posi"])
    S.op("dve", lambda e: e.tensor_copy(out=posf[0:1, :], in_=posi[0:1, :]), reads=["posi"], writes=["posf"])


class Attn:
    def __init__(self, kb, sbanks=(0, 1), npt=3):
        self.kb = kb
        self.sbanks = sbanks
        self.PT = [kb.alloc(512, BF16) for _ in range(npt)]
        self.pti = 0
        self.si = 0

    def run(self, blocks, obank, defer=None):
        kb = self.kb
        S = kb.S
        n = len(blocks)
        O = kb.psb[obank]
        banks = []

        def scores(i):
            bank = self.sbanks[self.si % len(self.sbanks)]
            self.si += 1
            banks.append(bank)
            mms = blocks[i][0]
            for j, (l, r, ks) in enumerate(mms):
                S.op("pe", lambda e, l=l, r=r, j=j, bank=bank, nm=len(mms): e.matmul(kb.psb[bank][:, :], lhsT=l, rhs=r, start=(j == 0), stop=(j == nm - 1)),
                     reads=ks, writes=[pk(bank)])

        scores(0)
        for i in range(n):
            if i + 1 < n:
                scores(i + 1)
            bank = banks[i]
            pi_ = self.pti % len(self.PT)
            self.pti += 1
            pt = self.PT[pi_]
            S.op("act", lambda e, pt=pt, bank=bank: e.activation(out=pt, in_=kb.psb[bank][:, :], func=AF.Exp), writes=[pk(bank), f"PT{pi_}"])
            v, vk = blocks[i][1], blocks[i][2]
            S.op("pe", lambda e, v=v, pt=pt, i=i: e.matmul(O[0:128, :], lhsT=v, rhs=pt, start=(i == 0), stop=(i == n - 1)),
                 reads=[f"PT{pi_}"] + vk, writes=[pk(obank)])
            if defer and i == min(2, n - 1):
                for f in defer:
                    f()
                del defer[:]


def norm_coef(kb, c, obank, rowbuf, bcbank, bcs):
    S = kb.S
    O = kb.psb[obank]
    B = kb.psb[bcbank]
    S.op("dve", lambda e: e.tensor_scalar_max(out=rowbuf[64:65, :], in0=O[64:65, :], scalar1=1e-30), writes=[pk(obank), "rowbuf"])
    S.op("dve", lambda e: e.reciprocal(out=rowbuf[64:65, :], in_=rowbuf[64:65, :]), writes=["rowbuf"])
    S.op("pe", lambda e: e.matmul(B[0:64, :], lhsT=c.ones_f[64:65, 0:64], rhs=rowbuf[64:65, :], start=True, stop=True),
         reads=["rowbuf", "ones_f"], writes=[pk(bcbank)])
    S.op("act", lambda e: e.copy(out=bcs[0:64, :], in_=B[0:64, :]), writes=[pk(bcbank), "bcs"])


def part_lru(kb, c, D, yT, L):
    S = kb.S
    m0 = kb.mark()
    xa = kb.alloc(N + 4)
    xc = kb.alloc(N)
    A = kb.alloc(N)
    U = kb.alloc(N)
    G = kb.alloc(N)
    xcb = kb.alloc(N, BF16)
    vec = kb.alloc(16)
    wtmp = kb.alloc(256)
    wbd = kb.alloc(256, BF16)
    T1 = xa[:, 0:N]
    P = kb.psb
    S.op("dve", lambda e: e.memset(xa[:, 0:3], 0.0), writes=["xa_pad"])
    for hf in range(2):
        S.dma("sp", lambda e, hf=hf: e.dma_start(out=xa[:, 3 + hf * 4096:3 + (hf + 1) * 4096], in_=L.pt(OFF["a_x"], 128, 128, hf * 4096, (hf + 1) * 4096)), writes=["xa"])
        S.dma("sp", lambda e, hf=hf: e.dma_start(out=G[:, hf * 4096:(hf + 1) * 4096], in_=L.pt(OFF["a_gate"], 128, 128, hf * 4096, (hf + 1) * 4096)), writes=["G"])
    S.dma("sp", lambda e: e.dma_start(out=vec[:, 0:8], in_=D["lru_v"][:, :]), writes=["vec"])
    S.op("dve", lambda e: e.memset(wtmp, 0.0), writes=["wtmp"])
    for a in range(2):
        for b in range(2):
            S.dma("sp", lambda e, a=a, b=b: e.dma_start(out=wtmp[b * 64:(b + 1) * 64, a * 128 + b * 64:a * 128 + (b + 1) * 64], in_=D["lru_w"][a, b]),
                  writes=["wtmp"])
    S.op("act", lambda e: e.copy(out=wbd, in_=wtmp), reads=["wtmp"], writes=["wbd"])
    S.op("act", lambda e: e.activation(out=vec[:, 8:9], in_=vec[:, 7:8], func=AF.Exp, scale=-1.0), reads=["vec"], writes=["vec8"])
    S.op("act", lambda e: e.activation(out=vec[:, 8:9], in_=vec[:, 8:9], func=AF.Ln, bias=1.0), writes=["vec8"])
    S.op("dve", lambda e: e.tensor_scalar(out=vec[:, 9:10], in0=vec[:, 8:9], scalar1=-8.0, scalar2=None, op0=ALU.mult), reads=["vec8"], writes=["vec9"])
    S.op("dve", lambda e: e.tensor_scalar(out=xc, in0=xa[:, 0:N], scalar1=vec[:, 0:1], scalar2=vec[:, 4:5], op0=ALU.mult, op1=ALU.add),
         reads=["xa", "xa_pad", "vec"], writes=["xc"])
    for j in range(1, 4):
        S.op("dve", lambda e, j=j: e.scalar_tensor_tensor(out=xc, in0=xa[:, j:j + N], scalar=vec[:, j:j + 1], in1=xc, op0=ALU.mult, op1=ALU.add),
             reads=["xa", "xa_pad", "vec"], writes=["xc"])
    S.op("act", lambda e: e.copy(out=xcb, in_=xc), reads=["xc"], writes=["xcb"])
    allA = [f"A{t}" for t in range(16)]
    allU = [f"U{t}" for t in range(16)]
    for t in range(16):
        ts = slice(t * 512, (t + 1) * 512)
        b0, b1 = 2 * (t % 2), 2 * (t % 2) + 1
        S.op("pe", lambda e, ts=ts, b0=b0: e.matmul(P[b0][:, :], lhsT=wbd[:, 0:128], rhs=xcb[:, ts], start=True, stop=True),
             reads=["xcb", "wbd"], writes=[pk(b0)])
        S.op("pe", lambda e, ts=ts, b1=b1: e.matmul(P[b1][:, :], lhsT=wbd[:, 128:256], rhs=xcb[:, ts], start=True, stop=True),
             reads=["xcb", "wbd"], writes=[pk(b1)])
        S.op("act", lambda e, ts=ts, b0=b0: e.activation(out=A[:, ts], in_=P[b0][:, :], func=AF.Sigmoid, bias=vec[:, 5:6]),
             reads=["vec"], writes=[pk(b0), f"A{t}"])
        S.op("act", lambda e, ts=ts, b1=b1: e.activation(out=U[:, ts], in_=P[b1][:, :], func=AF.Sigmoid, bias=vec[:, 6:7]),
             reads=["vec"], writes=[pk(b1), f"U{t}"])
    NP = 4
    W_ = N // NP
    for p_ in range(NP):
        cs = slice(p_ * W_, (p_ + 1) * W_)
        tA = [f"A{t}" for t in range(16) if p_ * W_ <= t * 512 < (p_ + 1) * W_]
        tU = [f"U{t}" for t in range(16) if p_ * W_ <= t * 512 < (p_ + 1) * W_]
        kA, kU, kT, kG, kH = f"Ap{p_}", f"Up{p_}", f"Tp{p_}", f"Gp{p_}", f"Hp{p_}"
        S.op("act", lambda e, cs=cs: e.activation(out=A[:, cs], in_=A[:, cs], func=AF.Exp, scale=vec[:, 9:10]), reads=["vec9"], writes=[kA] + tA)
        S.op("act", lambda e, cs=cs: e.activation(out=T1[:, cs], in_=A[:, cs], func=AF.Square), reads=[kA, "xc"], writes=[kT, "xa", "xa_pad"])
        S.op("act", lambda e, cs=cs: e.activation(out=T1[:, cs], in_=T1[:, cs], func=AF.Sqrt, bias=1.0, scale=-1.0), writes=[kT])
        S.op("dve", lambda e, cs=cs: e.tensor_tensor(out=U[:, cs], in0=U[:, cs], in1=xc[:, cs], op=ALU.mult), reads=["xc"], writes=[kU] + tU)
        S.op("dve", lambda e, cs=cs: e.tensor_tensor(out=U[:, cs], in0=U[:, cs], in1=T1[:, cs], op=ALU.mult), reads=[kT], writes=[kU])
    for p_ in range(NP):
        cs = slice(p_ * W_, (p_ + 1) * W_)
        init = 0.0 if p_ == 0 else xc[:, p_ * W_ - 1:p_ * W_]
        S.op("dve", lambda e, cs=cs, init=init: e.tensor_tensor_scan(out=xc[:, cs], data0=A[:, cs], data1=U[:, cs], initial=init, op0=ALU.mult, op1=ALU.add),
             reads=[f"Ap{p_}", f"Up{p_}"] + [f"Up{q_}" for q_ in range(NP)], writes=["xc", f"Hp{p_}"])
    for p_ in range(NP):
        cs = slice(p_ * W_, (p_ + 1) * W_)
        kT, kG = f"Tp{p_}", f"Gp{p_}"
        S.op("act", lambda e, cs=cs: e.activation(out=T1[:, cs], in_=G[:, cs], func=AF.Square), reads=["G", f"Up{p_}"], writes=[kT])
        S.op("dve", lambda e, cs=cs: e.tensor_scalar(out=T1[:, cs], in0=T1[:, cs], scalar1=0.044715, scalar2=1.0, op0=ALU.mult, op1=ALU.add), writes=[kT])
        S.op("dve", lambda e, cs=cs: e.tensor_tensor(out=T1[:, cs], in0=T1[:, cs], in1=G[:, cs], op=ALU.mult), reads=["G"], writes=[kT])
        S.op("act", lambda e, cs=cs: e.activation(out=T1[:, cs], in_=T1[:, cs], func=AF.Sigmoid, scale=1.5957691216057308), writes=[kT])
        S.op("dve", lambda e, cs=cs: e.tensor_tensor(out=G[:, cs], in0=G[:, cs], in1=T1[:, cs], op=ALU.mult), reads=[kT], writes=[kG])
        S.op("dve", lambda e, cs=cs: e.tensor_tensor(out=A[:, cs], in0=xc[:, cs], in1=G[:, cs], op=ALU.mult), reads=[f"Hp{p_}", kG], writes=[f"Ap{p_}"])
        for hf in range(2):
            if hf * 4096 >= p_ * W_ and (hf + 1) * 4096 <= (p_ + 1) * W_ or (p_ * W_ >= hf * 4096 and (p_ + 1) * W_ <= (hf + 1) * 4096):
                c0, c1 = max(hf * 4096, p_ * W_), min((hf + 1) * 4096, (p_ + 1) * W_)
                S.dma("sp", lambda e, c0=c0, c1=c1: e.dma_start(out=yT[0:128, c0:c1], in_=A[:, c0:c1]), reads=[f"Ap{p_}"])
    S.barrier()
    kb.release(m0)


def part_mla(kb, c, D, yT, L):
    S = kb.S
    m0 = kb.mark()
    P = kb.psb
    SC = float(96 ** -0.5)
    QD = [kb.alloc(N, BF16) for _ in range(2)]
    KD = [kb.alloc(N, BF16) for _ in range(2)]
    VD = kb.alloc(64 * 2 * 128, BF16).rearrange("p (b h d) -> p b h d", b=64, h=2)
    tri = kb.alloc(4 * 512, BF16).rearrange("p (i q) -> p i q", i=4)
    wuq = kb.alloc(2 * 192, BF16).rearrange("p (c n) -> p c n", c=2)
    wk = kb.alloc(128, BF16)
    wv = kb.alloc(128, BF16)
    vec = kb.alloc(4)
    r32 = kb.alloc(32)
    st = kb.alloc(512)
    S.op("pool", lambda e: e.memset(VD, 0.0), writes=["VD"])
    S.op("pool", lambda e: e.memset(VD[:, :, :, 64:65], 1.0), writes=["VD"])
    S.dma("sp", lambda e: e.dma_start(out=vec[:, 0:3], in_=D["mla_v"][:, :]), writes=["mvec"])
    S.dma("sp", lambda e: e.dma_start(out=r32, in_=D["r32"][:, :]), writes=["r32"])
    for i in range(4):
        S.dma("sp", lambda e, i=i: e.dma_start(out=st, in_=D["tri"][i]), writes=["st"])
        S.op("act", lambda e, i=i: e.copy(out=tri[:, i, :], in_=st), reads=["st"], writes=["tri"])
    S.dma("sp", lambda e: e.dma_start(out=st[:, 0:192], in_=D["mla_wuq"][0:128, :]), writes=["st"])
    S.op("act", lambda e: e.copy(out=wuq[:, 0, :], in_=st[:, 0:192]), reads=["st"], writes=["wuq"])
    S.dma("sp", lambda e: e.dma_start(out=st[0:64, 0:192], in_=D["mla_wuq"][128:192, :]), writes=["st"])
    S.op("act", lambda e: e.copy(out=wuq[0:64, 1, :], in_=st[0:64, 0:192]), reads=["st"], writes=["wuq"])
    S.dma("sp", lambda e: e.dma_start(out=st[:, 0:128], in_=D["mla_wk"][:, :]), writes=["st"])
    S.op("act", lambda e: e.copy(out=wk, in_=st[:, 0:128]), reads=["st"], writes=["wk"])
    S.dma("sp", lambda e: e.dma_start(out=st[:, 0:128], in_=D["mla_wv"][:, :]), writes=["st"])
    S.op("act", lambda e: e.copy(out=wv, in_=st[:, 0:128]), reads=["st"], writes=["wv"])

    m1 = kb.mark()
    cq0 = kb.alloc(512)
    cq1 = kb.alloc(512)
    ckv = kb.alloc(512)
    krt = kb.alloc(512)
    sq0 = kb.alloc(512, BF16)
    sq1 = kb.alloc(512, BF16)
    cn0 = kb.alloc(512, BF16)
    cn1 = kb.alloc(512, BF16)
    ckn = kb.alloc(512, BF16)
    rstd = kb.alloc(512)
    qr = kb.alloc(512)
    t1 = kb.alloc(512)
    t2 = kb.alloc(512)
    posi = kb.alloc(512, I32)
    posf = kb.alloc(512)
    T = alloc_tables(kb)
    R = slice(64, 96)
    for t in range(NQT):
        ts = slice(t * TQ, (t + 1) * TQ)
        load_pos(kb, D, posi, posf, t)
        S.dma("sp", lambda e, ts=ts: e.dma_start(out=cq0, in_=L.pt(OFF["d_cq"], 0, 128, ts.start, ts.stop)), writes=["cq0"])
        S.dma("sp", lambda e, ts=ts: e.dma_start(out=cq1[0:64, :], in_=L.pt(OFF["d_cq"] + 128, 0, 64, ts.start, ts.stop)), writes=["cq1"])
        S.dma("sp", lambda e, ts=ts: e.dma_start(out=ckv, in_=L.pt(OFF["d_ckv"], 0, 128, ts.start, ts.stop)), writes=["ckv"])
        S.dma("sp", lambda e, ts=ts: e.dma_start(out=krt[R, :], in_=L.pt(OFF["d_kr"], 0, 32, ts.start, ts.stop)), writes=["krt"])
        rope_tables(kb, c, posf, "posf", 64, 96, 1, T, 6, "m")
        S.op("act", lambda e: e.activation(out=sq0, in_=cq0, func=AF.Square), reads=["cq0"], writes=["sq0"])
        S.op("act", lambda e: e.activation(out=sq1[0:64, :], in_=cq1[0:64, :], func=AF.Square), reads=["cq1"], writes=["sq1"])
        S.op("pe", lambda e: e.matmul(P[0][:, :], lhsT=c.ones_bf, rhs=sq0, start=True, stop=False), reads=["sq0", "ones_bf"], writes=[pk(0)])
        S.op("pe", lambda e: e.matmul(P[0][:, :], lhsT=c.ones_bf[0:64, :], rhs=sq1[0:64, :], start=False, stop=True), reads=["sq1", "ones_bf"], writes=[pk(0)])
        S.op("act", lambda e: e.activation(out=rstd, in_=P[0][:, :], func=AF.Sqrt, bias=EPS, scale=1.0 / 192.0), writes=[pk(0), "rstd"])
        S.op("dve", lambda e: e.reciprocal(out=rstd, in_=rstd), writes=["rstd"])
        S.op("dve", lambda e: e.scalar_tensor_tensor(out=cn0, in0=cq0, scalar=vec[:, 0:1], in1=rstd, op0=ALU.mult, op1=ALU.mult),
             reads=["cq0", "rstd", "mvec"], writes=["cn0"])
        S.op("dve", lambda e: e.scalar_tensor_tensor(out=cn1[0:64, :], in0=cq1[0:64, :], scalar=vec[0:64, 1:2], in1=rstd[0:64, :], op0=ALU.mult, op1=ALU.mult),
             reads=["cq1", "rstd", "mvec"], writes=["cn1"])
        for h in range(2):
            hs = slice(h * 96, (h + 1) * 96)
            S.op("pe", lambda e, hs=hs: e.matmul(P[1][0:96, :], lhsT=wuq[:, 0, hs], rhs=cn0, start=True, stop=False), reads=["cn0", "wuq"], writes=[pk(1)])
            S.op("pe", lambda e, hs=hs: e.matmul(P[1][0:96, :], lhsT=wuq[0:64, 1, hs], rhs=cn1[0:64, :], start=False, stop=True), reads=["cn1", "wuq"], writes=[pk(1)])
            S.op("act", lambda e, h=h, ts=ts: e.mul(out=QD[h][0:64, ts], in_=P[1][0:64, :], mul=SC), writes=[pk(1), f"QD{h}"])
            S.op("dve", lambda e: e.tensor_copy(out=qr[R, :], in_=P[1][R, :]), writes=[pk(1), "qr"])
            S.op("pe", lambda e: e.matmul(P[2][R, :], lhsT=r32[R, 0:32], rhs=qr[R, :], start=True, stop=True), reads=["qr", "r32"], writes=[pk(2)])
            S.op("dve", lambda e: e.scalar_tensor_tensor(out=t1[R, :], in0=qr[R, :], scalar=SC, in1=T["cos"][R, :], op0=ALU.mult, op1=ALU.mult),
                 reads=["qr", "mcos"], writes=["t1"])
            S.op("dve", lambda e: e.scalar_tensor_tensor(out=t2[R, :], in0=P[2][R, :], scalar=SC, in1=T["sin"][R, :], op0=ALU.mult, op1=ALU.mult),
                 reads=["msin"], writes=[pk(2), "t2"])
            S.op("dve", lambda e, h=h, ts=ts: e.tensor_tensor(out=QD[h][R, ts], in0=t1[R, :], in1=t2[R, :], op=ALU.add), reads=["t1", "t2"], writes=[f"QD{h}"])
        S.op("act", lambda e: e.activation(out=sq0, in_=ckv, func=AF.Square), reads=["ckv"], writes=["sq0"])
        S.op("pe", lambda e: e.matmul(P[7][:, :], lhsT=c.ones_bf, rhs=sq0, start=True, stop=True), reads=["sq0", "ones_bf"], writes=[pk(7)])
        S.op("act", lambda e: e.activation(out=rstd, in_=P[7][:, :], func=AF.Sqrt, bias=EPS, scale=1.0 / 128.0), writes=[pk(7), "rstd"])
        S.op("dve", lambda e: e.reciprocal(out=rstd, in_=rstd), writes=["rstd"])
        S.op("dve", lambda e: e.scalar_tensor_tensor(out=ckn, in0=ckv, scalar=vec[:, 2:3], in1=rstd, op0=ALU.mult, op1=ALU.mult),
             reads=["ckv", "rstd", "mvec"], writes=["ckn"])
        for h in range(2):
            S.op("pe", lambda e, h=h: e.matmul(P[3][0:64, :], lhsT=wk[:, h * 64:(h + 1) * 64], rhs=ckn, start=True, stop=True), reads=["ckn", "wk"], writes=[pk(3)])
            S.op("act", lambda e, h=h, ts=ts: e.copy(out=KD[h][0:64, ts], in_=P[3][0:64, :]), writes=[pk(3), f"KD{h}"])
        for tb in range(4):
            S.op("pe", lambda e, tb=tb: e.matmul(P[4][:, 0:128], lhsT=ckn[:, tb * 128:(tb + 1) * 128], rhs=wv, start=True, stop=True), reads=["ckn", "wv"], writes=[pk(4)])
            S.op("act", lambda e, tb=tb, t=t: e.copy(out=VD[:, 4 * t + tb, :, 0:64], in_=P[4][:, 0:128].rearrange("p (h d) -> p h d", h=2)),
                 writes=[pk(4), "VD"])
        S.op("pe", lambda e: e.matmul(P[5][R, :], lhsT=r32[R, 0:32], rhs=krt[R, :], start=True, stop=True), reads=["krt", "r32"], writes=[pk(5)])
        S.op("dve", lambda e: e.tensor_tensor(out=t1[R, :], in0=krt[R, :], in1=T["cos"][R, :], op=ALU.mult), reads=["krt", "mcos"], writes=["t1"])
        S.op("dve", lambda e: e.tensor_tensor(out=t2[R, :], in0=P[5][R, :], in1=T["sin"][R, :], op=ALU.mult), reads=["msin"], writes=[pk(5), "t2"])
        for h in range(2):
            S.op("dve", lambda e, h=h, ts=ts: e.tensor_tensor(out=KD[h][R, ts], in0=t1[R, :], in1=t2[R, :], op=ALU.add), reads=["t1", "t2"], writes=[f"KD{h}"])
    S.barrier()
    kb.release(m1)
    att = Attn(kb, sbanks=(0, 1))
    rowbuf = kb.alloc(512)
    bcs = kb.alloc(512)
    yst = [kb.alloc(512), kb.alloc(512)]
    it = 0
    pending = []
    for qt in range(NQT):
        qs = slice(qt * TQ, (qt + 1) * TQ)
        for h in range(2):
            blocks = []
            for kbk in range(4 * qt + 4):
                mms = [(KD[h][0:96, kbk * 128:(kbk + 1) * 128], QD[h][0:96, qs], [f"KD{h}", f"QD{h}"])]
                if kbk >= 4 * qt:
                    mms.append((c.ident_bf, tri[:, kbk - 4 * qt, :], ["ident_bf", "tri"]))
                blocks.append((mms, VD[:, kbk, h, :], ["VD"]))
            ob = 2 + it % 2
            att.run(blocks, ob, defer=pending)

            def fin(ob=ob, ys=yst[it % 2], yk=f"yst{it % 2}", h=h, qs=qs):
                norm_coef(kb, c, ob, rowbuf, 4, bcs)
                S.op("dve", lambda e: e.tensor_tensor(out=ys[0:64, :], in0=P[ob][0:64, :], in1=bcs[0:64, :], op=ALU.mult),
                     reads=["bcs"], writes=[pk(ob), yk])
                S.dma("sp", lambda e: e.dma_start(out=yT[384 + h * 64:384 + (h + 1) * 64, qs], in_=ys[0:64, :]), reads=[yk])
            pending.append(fin)
            it += 1
    for f in pending:
        f()
    S.barrier()
    kb.release(m0)


def load_cast(kb, src_ap, dst_ap, stage, skey, dkey, eng="act", rows=slice(0, 128)):
    S = kb.S
    S.dma("sp", lambda e: e.dma_start(out=stage, in_=(src_ap() if callable(src_ap) else src_ap)), writes=[skey])
    if eng == "act":
        S.op("act", lambda e: e.copy(out=dst_ap, in_=stage), reads=[skey], writes=[dkey])
    else:
        S.op("pool", lambda e: e.tensor_copy(out=dst_ap, in_=stage), reads=[skey], writes=[dkey])


def gelu_tanh(kb, z, u, out, zk, uk, outk):
    S = kb.S
    S.op("pool", lambda e: e.tensor_tensor(out=u, in0=z, in1=z, op=ALU.mult), reads=[zk], writes=[uk])
    S.op("pool", lambda e: e.tensor_scalar(out=u, in0=u, scalar1=0.044715, scalar2=1.0, op0=ALU.mult, op1=ALU.add), writes=[uk])
    S.op("pool", lambda e: e.tensor_tensor(out=u, in0=u, in1=z, op=ALU.mult), reads=[zk], writes=[uk])
    S.op("act", lambda e: e.activation(out=u, in_=u, func=AF.Sigmoid, scale=1.5957691216057308), writes=[uk])
    S.op("dve", lambda e: e.tensor_tensor(out=out, in0=z, in1=u, op=ALU.mult), reads=[zk, uk], writes=[outk])


def part_nsa(kb, c, D, yT, L):
    S = kb.S
    P = kb.psb
    m0 = kb.mark()
    Qm = kb.alloc(N, BF16)
    Qo = kb.alloc(N, BF16)
    KSA = kb.alloc(N, BF16)
    KSB = kb.alloc(N, BF16)
    KW2 = kb.alloc(N, BF16)
    tri = kb.alloc(4 * 512, BF16).rearrange("p (i q) -> p i q", i=4)
    win = kb.alloc(8 * 512, BF16).rearrange("p (i q) -> p i q", i=8)
    cmpm = kb.alloc(5 * 512, BF16).rearrange("p (i q) -> p i q", i=5)
    rbd = kb.alloc(128)
    ovl = kb.alloc(4 * 130, BF16).rearrange("p (i q) -> p i q", i=4)
    bv = kb.alloc(254)
    bf = kb.alloc(254)
    selg = kb.alloc(384)
    KCMP2 = kb.alloc(512, BF16)
    VCMP = kb.alloc(4 * 128, BF16).rearrange("p (b d) -> p b d", b=4)
    st = kb.alloc(512)
    st3 = st.rearrange("p (b d) -> p b d", d=64)
    S.op("pool", lambda e: e.memset(KSA[64:128, :], 0.0), writes=["ksz"])
    S.op("pool", lambda e: e.memset(KSB[0:64, :], 0.0), writes=["ksz"])
    S.op("pool", lambda e: e.memset(VCMP, 0.0), writes=["VCMP"])
    S.op("pool", lambda e: e.memset(VCMP[:, :, 64:65], 1.0), writes=["VCMP"])
    S.dma("sp", lambda e: e.dma_start(out=rbd, in_=D["rbd"][:, :]), writes=["rbd"])
    S.dma("sp", lambda e: e.dma_start(out=bv, in_=D["bv"][:, :]), writes=["bv"])
    S.dma("sp", lambda e: e.dma_start(out=bf, in_=D["bf"][:, :]), writes=["bf"])
    S.dma("sp", lambda e: e.dma_start(out=selg[0:8, :], in_=D["selg"][:, :]), writes=["selg"])
    i = 0
    for nm, dst, cnt in (("tri", tri, 4), ("win", win, 8), ("cmpm", cmpm, 5)):
        for j in range(cnt):
            load_cast(kb, D[nm][j], dst[:, j, :], st[:, 0:512], "st", nm, "act" if i % 2 == 0 else "pool")
            i += 1
    for j in range(4):
        load_cast(kb, D["ovl"][j], ovl[:, j, 0:129], st[:, 0:129], "st", "ovl", "act")

    m1 = kb.mark()
    KCV = kb.alloc(N)
    xs = [kb.alloc(512), kb.alloc(512)]
    t1s = [kb.alloc(512), kb.alloc(512)]
    t2s = [kb.alloc(512), kb.alloc(512)]
    posi = kb.alloc(512, I32)
    posf = kb.alloc(512)
    T = alloc_tables(kb)
    for hf in range(2):
        S.dma("sp", lambda e, hf=hf: e.dma_start(out=KCV[64:128, hf * 4096:(hf + 1) * 4096], in_=L.pt(OFF["b_kv"] + 64, 0, 64, hf * 4096, (hf + 1) * 4096)), writes=["KCVv"])
    flat = []
    for t in range(NQT):
        ts = slice(t * TQ, (t + 1) * TQ)
        a0, a1 = ts.start, ts.stop
        flat += [(t, ts, "q0", [lambda a0=a0, a1=a1: L.pt(OFF["b_q"], 128, 128, a0, a1)], Qm, 0.125, 128),
                 (t, ts, "q1", [lambda a0=a0, a1=a1: L.pt(OFF["b_q"], 128, 128, a0, a1, inv=True)], Qo, 0.125, 128),
                 (t, ts, "ks", [lambda a0=a0, a1=a1: L.pt(OFF["b_kv"] + 128, 0, 64, a0, a1)] * 2, None, 1.0, 128),
                 (t, ts, "kw", [lambda a0=a0, a1=a1: L.pt(OFF["b_kv"] + 192, 0, 64, a0, a1)] * 2, KW2, 1.0, 128),
                 (t, ts, "kc", [lambda a0=a0, a1=a1: L.pt(OFF["b_kv"], 0, 64, a0, a1)], KCV, 1.0, 64)]

    def emit_load(i):
        t, ts, nm, srcs, dst, sc, rows = flat[i]
        x = xs[i % 2]
        xk = f"xs{i % 2}"
        if len(srcs) == 2:
            S.dma("sp", lambda e: e.dma_start(out=x[0:64, :], in_=srcs[0]()), writes=[xk])
            S.dma("sp", lambda e: e.dma_start(out=x[64:128, :], in_=srcs[1]()), writes=[xk])
        else:
            S.dma("sp", lambda e: e.dma_start(out=x[0:rows, :], in_=srcs[0]()), writes=[xk])

    def emit_compute(i):
        t, ts, nm, srcs, dst, sc, rows = flat[i]
        x = xs[i % 2]
        xk = f"xs{i % 2}"
        t1 = t1s[i % 2]
        t2 = t2s[i % 2]
        bank = 4 + i % 2
        R = slice(0, rows)
        S.op("pe", lambda e: e.matmul(P[bank][R, :], lhsT=rbd[R, 0:rows], rhs=x[R, :], start=True, stop=True),
             reads=[xk, "rbd"], writes=[pk(bank)])
        S.op("dve", lambda e: e.scalar_tensor_tensor(out=t1[R, :], in0=x[R, :], scalar=sc, in1=T["cos"][R, :], op0=ALU.mult, op1=ALU.mult),
             reads=[xk, "ncos"], writes=[f"t1{i % 2}"])
        S.op("dve", lambda e: e.scalar_tensor_tensor(out=t2[R, :], in0=P[bank][R, :], scalar=sc, in1=T["sin"][R, :], op0=ALU.mult, op1=ALU.mult),
             reads=["nsin"], writes=[pk(bank), f"t2{i % 2}"])
        dk = "KCVk" if nm == "kc" else nm
        if nm == "ks":
            for dst_, RR in ((KSA, slice(0, 64)), (KSB, slice(64, 128))):
                S.op("pool", lambda e, RR=RR, dst_=dst_: e.tensor_tensor(out=dst_[RR, ts], in0=t1[RR, :], in1=t2[RR, :], op=ALU.add),
                     reads=[f"t1{i % 2}", f"t2{i % 2}"], writes=[dk])
        else:
            S.op("pool", lambda e: e.tensor_tensor(out=dst[R, ts], in0=t1[R, :], in1=t2[R, :], op=ALU.add),
                 reads=[f"t1{i % 2}", f"t2{i % 2}"], writes=[dk])

    emit_load(0)
    for i in range(len(flat)):
        if i % 5 == 0:
            load_pos(kb, D, posi, posf, flat[i][0])
            rope_tables(kb, c, posf, "posf", 0, 128, 0, T, 6, "n")
        if i + 1 < len(flat):
            emit_load(i + 1)
        emit_compute(i)
    S.barrier()
    kb.release(m1)
    KCV = kb.alloc(N)
    BLK = kb.alloc(32 * 512, BF16).rearrange("p (l n) -> p l n", l=32)
    W1 = kb.alloc(32 * 256, BF16).rearrange("p (l h) -> p l h", l=32)
    pos2 = kb.alloc(32)
    b1 = kb.alloc(4)
    w2 = kb.alloc(2 * 2 * 64, BF16).rearrange("p (k m d) -> p k m d", k=2, m=2)
    HID = kb.alloc(2 * 2 * 512, BF16).rearrange("p (k m n) -> p k m n", k=2, m=2)
    zt = kb.alloc(512)
    ut = kb.alloc(512)
    stw = kb.alloc(1024).rearrange("p (l h) -> p l h", l=4)
    S.dma("sp", lambda e: e.dma_start(out=pos2, in_=D["nsa_pos"][:, :]), writes=["pos2"])
    S.dma("sp", lambda e: e.dma_start(out=b1, in_=D["nsa_b1"][:, :]), writes=["b1"])
    for kv in range(2):
        load_cast(kb, D["nsa_w2"][kv].rearrange("(m p) d -> p m d", p=128), w2[:, kv, :, :], st[:, 0:128].rearrange("p (m d) -> p m d", m=2), "st", "w2", "act")
    for l0 in range(0, 32, 4):
        for kv in range(2):
            src = D["nsa_w1"][kv].rearrange("(l d) h -> d l h", d=64)[:, l0:l0 + 4, :]
            S.dma("sp", lambda e, src=src, kv=kv: e.dma_start(out=stw[kv * 64:(kv + 1) * 64, :, :], in_=src), writes=["stw"])
        if (l0 // 4) % 2 == 0:
            S.op("act", lambda e, l0=l0: e.copy(out=W1[:, l0:l0 + 4, :], in_=stw), reads=["stw"], writes=["W1"])
        else:
            S.op("pool", lambda e, l0=l0: e.tensor_copy(out=W1[:, l0:l0 + 4, :], in_=stw), reads=["stw"], writes=["W1"])
    S.op("pool", lambda e: e.memset(BLK[:, :, 511:512], 0.0), writes=["BLKpad"])
    K3 = KCV.rearrange("p (g r) -> p g r", r=16)
    for l in range(32):
        src = K3[:, 0:511, l] if l < 16 else K3[:, 1:512, l - 16]
        eng = "dve" if l % 2 == 0 else "pool"
        S.op(eng, lambda e, l=l, src=src: e.tensor_scalar(out=BLK[:, l, 0:511], in0=src, scalar1=pos2[:, l:l + 1], scalar2=None, op0=ALU.add),
             reads=["pos2"], writes=[f"BLK{l}"])
    for kv in range(2):
        R = slice(kv * 64, kv * 64 + 64)
        for m in range(2):
            bank = 2 * kv + m
            for l in range(32):
                S.op("pe", lambda e, l=l, R=R, m=m, bank=bank: e.matmul(P[bank][:, :], lhsT=W1[R, l, m * 128:(m + 1) * 128], rhs=BLK[R, l, :], start=(l == 0), stop=(l == 31)),
                     reads=["W1", f"BLK{l}", "BLKpad"], writes=[pk(bank)])
            S.op("act", lambda e, kv=kv, m=m, bank=bank: e.activation(out=zt, in_=P[bank][:, :], func=AF.Identity, bias=b1[:, kv * 2 + m:kv * 2 + m + 1]),
                 reads=["b1"], writes=[pk(bank), "zt"])
            gelu_tanh(kb, zt, ut, HID[:, kv, m, :], "zt", "ut", f"HID{kv}")
    for m in range(2):
        S.op("pe", lambda e, m=m: e.matmul(P[4][0:64, :], lhsT=w2[:, 0, m, :], rhs=HID[:, 0, m, :], start=(m == 0), stop=(m == 1)), reads=["w2", "HID0"], writes=[pk(4)])
    S.op("act", lambda e: e.copy(out=KCMP2[0:64, :], in_=P[4][0:64, :]), writes=[pk(4), "KCMP2"])
    S.op("dve", lambda e: e.tensor_copy(out=KCMP2[64:128, :], in_=P[4][0:64, :]), writes=[pk(4), "KCMP2"])
    for nb in range(4):
        for m in range(2):
            S.op("pe", lambda e, m=m, nb=nb: e.matmul(P[5][:, 0:64], lhsT=HID[:, 1, m, nb * 128:(nb + 1) * 128], rhs=w2[:, 1, m, :], start=(m == 0), stop=(m == 1)),
                 reads=["w2", "HID1"], writes=[pk(5)])
        S.op("act", lambda e, nb=nb: e.copy(out=VCMP[:, nb, 0:64], in_=P[5][:, 0:64]), writes=[pk(5), "VCMP"])
    S.barrier()
    kb.release(m1)
    VS = kb.alloc(64 * 128, BF16).rearrange("p (b d) -> p b d", b=64)
    VW = kb.alloc(64 * 128, BF16).rearrange("p (b d) -> p b d", b=64)
    for vt_, vk_ in ((VS, "VS"), (VW, "VW")):
        S.op("pool", lambda e, vt_=vt_: e.memset(vt_, 0.0), writes=[vk_])
        S.op("pool", lambda e, vt_=vt_: e.memset(vt_[:, :, 64:65], 1.0), writes=[vk_])
    for nm, dst, coff in (("nsa_vs", VS, 0), ("nsa_vw", VW, 64)):
        for b0 in range(0, 64, 8):
            load_cast(kb, (lambda b0=b0, coff=coff: L.pv(coff, 0, 64, b0 * 128, (b0 + 8) * 128).rearrange("(b p) d -> p b d", p=128)),
                      dst[:, b0:b0 + 8, 0:64], st3[:, 0:8, :], "st", nm[-2:].upper(), "act" if (b0 // 8) % 2 == 0 else "pool")

    NST = kb.alloc(N, BF16)
    EALL = kb.alloc(N, BF16)
    for j in range(16):
        load_cast(kb, D["eall"][:, j * 512:(j + 1) * 512], EALL[:, j * 512:(j + 1) * 512], st[:, 0:512], "st", "EALL", "act" if j % 2 == 0 else "pool")
    ET = [kb.alloc(512, BF16) for _ in range(4)]
    IMP = kb.alloc(512).rearrange("p (a s) -> p a s", a=4)
    scr = kb.alloc(128)
    mr = kb.alloc(128)
    nsb = kb.alloc(128)
    mx = kb.alloc(16)
    thr = kb.alloc(2)
    rsum = kb.alloc(2)
    g6 = kb.alloc(512)
    gs = [[kb.alloc(512) for _ in range(3)] for _ in range(2)]
    acc = [kb.alloc(512), kb.alloc(512)]
    ctmp = kb.alloc(512)
    otmp = kb.alloc(512)
    rowbuf = ctmp
    bcs = kb.alloc(512)
    att = Attn(kb, sbanks=(0, 1))
    oi = 0

    def combine(ob, hA, br, first):
        norm_coef(kb, c, ob, rowbuf, 4, bcs)
        S.op("dve", lambda e: e.tensor_tensor(out=ctmp[0:64, :], in0=bcs[0:64, :], in1=gs[hA][br][0:64, :], op=ALU.mult),
             reads=["bcs", f"gs{hA}{br}"], writes=["ctmp"])
        if first:
            S.op("dve", lambda e: e.tensor_tensor(out=acc[hA][0:64, :], in0=P[ob][0:64, :], in1=ctmp[0:64, :], op=ALU.mult),
                 reads=["ctmp"], writes=[pk(ob), f"acc{hA}"])
        else:
            S.op("dve", lambda e: e.tensor_tensor(out=otmp[0:64, :], in0=P[ob][0:64, :], in1=ctmp[0:64, :], op=ALU.mult),
                 reads=["ctmp"], writes=[pk(ob), "otmp"])
            S.op("pool", lambda e: e.tensor_tensor(out=acc[hA][0:64, :], in0=acc[hA][0:64, :], in1=otmp[0:64, :], op=ALU.add),
                 reads=["otmp"], writes=[f"acc{hA}"])

    pending = []
    for qt in range(NQT):
        qs = slice(qt * TQ, (qt + 1) * TQ)
        for f in pending:
            f()
        del pending[:]
        S.dma("sp", lambda e, qs=qs: e.dma_start(out=g6[0:6, :], in_=L.pt(OFF["b_gate"], 6, 6, qs.start, qs.stop)), writes=["g6"])
        for hA in range(2):
            for br in range(3):
                r = hA * 3 + br
                S.op("pe", lambda e, r=r: e.matmul(P[5][0:64, :], lhsT=selg[0:6, r * 64:(r + 1) * 64], rhs=g6[0:6, :], start=True, stop=True),
                     reads=["g6", "selg"], writes=[pk(5)])
                S.op("act", lambda e, hA=hA, br=br: e.activation(out=gs[hA][br][0:64, :], in_=P[5][0:64, :], func=AF.Sigmoid), writes=[pk(5), f"gs{hA}{br}"])
        nbm = (512 * qt + 480) // 2048
        for hh in range(4):
            Qt = (Qm if hh < 2 else Qo)
            qk = "q0" if hh < 2 else "q1"
            R = slice(64 * (hh % 2), 64 * (hh % 2) + 64)
            ob = 2 + oi % 2
            for nb in range(nbm + 1):
                bank = nb % 2
                dl = 512 * qt - 2048 * nb
                mms = [(KCMP2[R, nb * 128:(nb + 1) * 128], Qt[R, qs], ["KCMP2", qk])]
                if dl < 2560:
                    mms.append((c.ident_bf, cmpm[:, dl // 512, :], ["ident_bf", "cmpm"]))
                for j, (l_, r_, ks) in enumerate(mms):
                    S.op("pe", lambda e, l_=l_, r_=r_, j=j, bank=bank, nm=len(mms): e.matmul(P[bank][:, :], lhsT=l_, rhs=r_, start=(j == 0), stop=(j == nm - 1)),
                         reads=ks, writes=[pk(bank)])
                S.op("act", lambda e, nb=nb, bank=bank: e.activation(out=ET[nb], in_=P[bank][:, :], func=AF.Exp), writes=[pk(bank), f"ET{nb}"])
                if hh < 2:
                    S.op("pe", lambda e, nb=nb, ob=ob, nbm=nbm: e.matmul(P[ob][0:128, :], lhsT=VCMP[:, nb, :], rhs=ET[nb], start=(nb == 0), stop=(nb == nbm)),
                         reads=[f"ET{nb}", "VCMP"], writes=[pk(ob)])
            for s4 in range(4):
                for nb in range(nbm + 1):
                    S.op("pe", lambda e, nb=nb, s4=s4, nbm=nbm: e.matmul(P[6][:, 0:129], lhsT=ET[nb][:, s4 * 128:(s4 + 1) * 128], rhs=ovl[:, nb, 0:129], start=(nb == 0), stop=(nb == nbm)),
                         reads=[f"ET{nb}", "ovl"], writes=[pk(6)])
                S.op("dve", lambda e: e.tensor_scalar_max(out=rsum[:, 0:1], in0=P[6][:, 128:129], scalar1=1e-30), writes=[pk(6), "rsum"])
                S.op("dve", lambda e: e.reciprocal(out=rsum[:, 0:1], in_=rsum[:, 0:1]), writes=["rsum"])
                if hh == 0:
                    S.op("dve", lambda e, s4=s4: e.tensor_scalar(out=IMP[:, s4, :], in0=P[6][:, 0:128], scalar1=rsum[:, 0:1], scalar2=None, op0=ALU.mult),
                         reads=["rsum"], writes=[pk(6), f"IMP{s4}"])
                else:
                    S.op("dve", lambda e, s4=s4: e.scalar_tensor_tensor(out=IMP[:, s4, :], in0=P[6][:, 0:128], scalar=rsum[:, 0:1], in1=IMP[:, s4, :], op0=ALU.mult, op1=ALU.add),
                         reads=["rsum"], writes=[pk(6), f"IMP{s4}"])
            if hh < 2:
                combine(ob, hh, 0, True)
                oi += 1
        for s4 in range(4):
            qb = 4 * qt + s4
            o0 = 126 - 2 * qb
            S.op("dve", lambda e, s4=s4, o0=o0: e.tensor_tensor(out=scr, in0=IMP[:, s4, :], in1=bv[:, o0:o0 + 128], op=ALU.mult), reads=[f"IMP{s4}", "bv"], writes=["scr"])
            S.op("dve", lambda e, o0=o0: e.tensor_tensor(out=scr, in0=scr, in1=bf[:, o0:o0 + 128], op=ALU.add), reads=["bf"], writes=["scr"])
            S.op("dve", lambda e: e.memset(scr[:, 0:1], 1e9), writes=["scr"])
            S.op("dve", lambda e: e.max(out=mx[:, 0:8], in_=scr), reads=["scr"], writes=["mx"])
            S.op("dve", lambda e: e.match_replace(out=mr, in_to_replace=mx[:, 0:8], in_values=scr, imm_value=-2.0), reads=["scr", "mx"], writes=["mr"])
            S.op("dve", lambda e: e.max(out=mx[:, 8:16], in_=mr), reads=["mr"], writes=["mx"])
            S.op("dve", lambda e: e.tensor_reduce(out=thr[:, 0:1], in_=mx[:, 8:16], axis=AX.X, op=ALU.min), reads=["mx"], writes=["thr"])
            S.op("dve", lambda e: e.tensor_scalar(out=nsb, in0=scr, scalar1=thr[:, 0:1], scalar2=-NEG, op0=ALU.is_ge, op1=ALU.mult), reads=["scr", "thr"], writes=["nsb"])
            S.op("pool", lambda e: e.tensor_scalar(out=nsb, in0=nsb, scalar1=NEG, scalar2=None, op0=ALU.add), writes=["nsb"])
            S.op("pe", lambda e: e.transpose(P[7][:, 0:128], nsb, c.ident), reads=["nsb", "ident"], writes=[pk(7)])
            S.op("act", lambda e, qb=qb: e.copy(out=NST[:, qb * 128:(qb + 1) * 128], in_=P[7][:, 0:128]), writes=[pk(7), "NST"])
        for hA in range(2):
            R = slice(64 * hA, 64 * hA + 64)
            blocks = []
            for kbk in range(4 * qt + 4):
                mms = [((KSA if hA == 0 else KSB)[:, kbk * 128:(kbk + 1) * 128], Qm[:, qs], ["ks", "q0", "ksz"]),
                       (EALL[:, kbk * 128:(kbk + 1) * 128], NST[:, qs], ["EALL", "NST"])]
                if kbk >= 4 * qt:
                    mms.append((c.ident_bf, tri[:, kbk - 4 * qt, :], ["ident_bf", "tri"]))
                blocks.append((mms, VS[:, kbk, :], ["VS"]))
            ob = 2 + oi % 2
            oi += 1
            att.run(blocks, ob, defer=pending)
            pending.append(lambda ob=ob, hA=hA: combine(ob, hA, 1, False))
            blocks = []
            for kbk in range(max(0, 4 * qt - 4), 4 * qt + 4):
                mms = [(KW2[R, kbk * 128:(kbk + 1) * 128], Qm[R, qs], ["kw", "q0"]),
                       (c.ident_bf, win[:, kbk - 4 * qt + 4, :], ["ident_bf", "win"])]
                blocks.append((mms, VW[:, kbk, :], ["VW"]))
            ob = 2 + oi % 2
            oi += 1
            att.run(blocks, ob, defer=pending)

            def fin(ob=ob, hA=hA, qs=qs):
                combine(ob, hA, 2, False)
                S.dma("sp", lambda e: e.dma_start(out=yT[128 + hA * 64:128 + (hA + 1) * 64, qs], in_=acc[hA][0:64, :]), reads=[f"acc{hA}"])
            pending.append(fin)
    for f in pending:
        f()
    S.barrier()
    kb.release(m0)


def part_gla(kb, c, D, yT, L):
    STAGE = 9
    S = kb.S
    P = kb.psb
    m0 = kb.mark()
    QS = float(32 ** -0.5)
    A = kb.alloc(N)
    B = kb.alloc(N)
    C = kb.alloc(2048)
    QG = kb.alloc(N, BF16)
    KG = kb.alloc(N, BF16)
    KHT = kb.alloc(128 * 64, BF16).rearrange("p (b d) -> p b d", b=128)
    V = kb.alloc(128 * 128, BF16).rearrange("p (b d) -> p b d", b=128)
    SB = kb.alloc(128 * 64, BF16).rearrange("p (c v) -> p c v", c=128)
    DC = kb.alloc(128)
    wg2 = kb.alloc(64)
    glr = kb.alloc(512)
    v2 = kb.alloc(4)
    tri64 = kb.alloc(64)
    st = kb.alloc(1024)
    S.dma("sp", lambda e: e.dma_start(out=wg2[0:16, :], in_=D["gla_wg2"][:, :]), writes=["wg2"])
    S.dma("sp", lambda e: e.dma_start(out=v2[:, 0:2], in_=D["gla_v2"][:, :]), writes=["v2"])
    S.dma("sp", lambda e: e.dma_start(out=tri64[0:64, :], in_=D["tri64"][:, :]), writes=["tri64"])
    S.dma("sp", lambda e: e.dma_start(out=tri64[64:128, :], in_=D["tri64"][:, :]), writes=["tri64"])
    S.op("dve", lambda e: e.tensor_scalar(out=v2[:, 2:3], in0=v2[:, 0:1], scalar1=-1.0, scalar2=None, op0=ALU.mult), reads=["v2"], writes=["v2n"])
    R = slice(0, 64)
    st3 = st.rearrange("p (b d) -> p b d", d=128)
    for b0 in range(0, 128, 8):
        load_cast(kb, (lambda b0=b0: L.pv(128, 128, 128, b0 * 64, (b0 + 8) * 64).rearrange("(b p) d -> p b d", p=64)),
                  V[R, b0:b0 + 8, :], st3[R, :, :], "st", "V", "act" if (b0 // 8) % 2 == 0 else "pool")
    for t in range(NQT):
        ts = slice(t * TQ, (t + 1) * TQ)
        bank = t % 2
        S.dma("sp", lambda e, ts=ts: e.dma_start(out=glr[0:16, :], in_=L.pt(OFF["c_glr"], 0, 16, ts.start, ts.stop)), writes=["glr"])
        S.op("pe", lambda e, bank=bank: e.matmul(P[bank][R, :], lhsT=wg2[0:16, 0:64], rhs=glr[0:16, :], start=True, stop=True), reads=["glr", "wg2"], writes=[pk(bank)])
        S.op("act", lambda e, bank=bank, ts=ts: e.activation(out=A[R, ts], in_=P[bank][R, :], func=AF.Exp, bias=v2[R, 2:3], scale=-1.0),
             reads=["v2n"], writes=[pk(bank), "A"])
    S.op("act", lambda e: e.activation(out=A[R, :], in_=A[R, :], func=AF.Ln, bias=1.0), writes=["A"])
    for ch in range(128):
        cs = slice(ch * 64, (ch + 1) * 64)
        S.op("dve", lambda e, cs=cs: e.tensor_tensor_scan(out=B[R, cs], data0=c.ones_f[R, 0:64], data1=A[R, cs], initial=0.0, op0=ALU.mult, op1=ALU.add),
             reads=["A", "ones_f"], writes=["B"])
    if STAGE <= 1:
        S.barrier(); kb.release(m0); return
    B3 = B.rearrange("p (c j) -> p c j", j=64)
    A3 = A.rearrange("p (c j) -> p c j", j=64)
    S.op("act", lambda e: e.activation(out=DC[R, :], in_=B3[R, :, 63], func=AF.Exp, scale=-1.0 / 16.0), reads=["B"], writes=["DC"])
    S.op("act", lambda e: e.activation(out=A[R, :], in_=B[R, :], func=AF.Exp, scale=-1.0 / 16.0), reads=["B"], writes=["A"])
    for pc in range(4):
        ps_ = slice(pc * 2048, (pc + 1) * 2048)
        S.dma("sp", lambda e, ps_=ps_: e.dma_start(out=C[R, :], in_=L.pt(OFF["c_q"], 64, 64, ps_.start, ps_.stop)), writes=["C"])
        S.op("dve", lambda e, ps_=ps_: e.scalar_tensor_tensor(out=QG[R, ps_], in0=C[R, :], scalar=QS, in1=A[R, ps_], op0=ALU.mult, op1=ALU.mult),
             reads=["C", "A"], writes=["QG"])
    S.op("act", lambda e: e.activation(out=A[R, :], in_=B[R, :], func=AF.Exp, scale=1.0 / 16.0), reads=["B", "QG"], writes=["A"])
    for pc in range(4):
        ps_ = slice(pc * 2048, (pc + 1) * 2048)
        S.dma("sp", lambda e, ps_=ps_: e.dma_start(out=C[R, :], in_=L.pt(OFF["c_k"], 64, 64, ps_.start, ps_.stop)), writes=["C"])
        S.op("dve", lambda e, ps_=ps_: e.tensor_tensor(out=KG[R, ps_], in0=C[R, :], in1=A[R, ps_], op=ALU.mult), reads=["C", "A"], writes=["KG"])
    S.op("dve", lambda e: e.tensor_tensor(out=A3[R, :, :], in0=B3[R, :, :], in1=B3[R, :, 63:64].to_broadcast([64, 128, 64]), op=ALU.subtract),
         reads=["B", "KG"], writes=["A"])
    S.op("act", lambda e: e.activation(out=A[R, :], in_=A[R, :], func=AF.Exp, scale=1.0 / 16.0), writes=["A"])
    for pc in range(4):
        ps_ = slice(pc * 2048, (pc + 1) * 2048)
        S.dma("sp", lambda e, ps_=ps_: e.dma_start(out=C[R, :], in_=L.pt(OFF["c_k"], 64, 64, ps_.start, ps_.stop)), writes=["C"])
        S.op("dve", lambda e, ps_=ps_: e.tensor_tensor(out=A[R, ps_], in0=C[R, :], in1=A[R, ps_], op=ALU.mult), reads=["C"], writes=["A"])
    if STAGE <= 2:
        S.barrier(); kb.release(m0); return
    for blk in range(64):
        bank = blk % 2
        S.op("pe", lambda e, blk=blk, bank=bank: e.transpose(P[bank][:, 0:64], A[R, blk * 128:(blk + 1) * 128], c.ident[0:64, 0:64]),
             reads=["A", "ident"], writes=[pk(bank)])
        S.op("act", lambda e, blk=blk, bank=bank: e.copy(out=KHT[R, 2 * blk, :], in_=P[bank][0:64, 0:64]), writes=[pk(bank), "KHT"])
        S.op("dve", lambda e, blk=blk, bank=bank: e.tensor_copy(out=KHT[R, 2 * blk + 1, :], in_=P[bank][64:128, 0:64]), writes=[pk(bank), "KHT"])
    if STAGE <= 3:
        S.barrier(); kb.release(m0); return
    U3 = B.rearrange("p (v c) -> p v c", c=128)
    uev = [kb.alloc(512), kb.alloc(512)]
    S3 = A.rearrange("p (v c) -> p v c", c=128)
    for g in range(32):
        bank = 2 + g % 2
        for j in range(4):
            ch = 4 * g + j
            S.op("pe", lambda e, ch=ch, j=j, bank=bank: e.matmul(P[bank][R, j * 128:(j + 1) * 128], lhsT=KHT[R, ch, :], rhs=V[R, ch, :], start=True, stop=True),
                 reads=["KHT", "V"], writes=[pk(bank)])
        ue = uev[g % 2]
        S.op("act", lambda e, bank=bank, ue=ue: e.copy(out=ue[R, :], in_=P[bank][R, :]), writes=[pk(bank), f"uev{g % 2}"])
        for h in range(2):
            hr = slice(32 * h, 32 * h + 32)
            for j in range(4):
                eng = "dve" if j % 2 == 0 else "pool"
                S.op(eng, lambda e, hr=hr, ue=ue, g=g, j=j, h=h: e.tensor_copy(out=U3[hr, :, 4 * g + j], in_=ue[hr, j * 128 + h * 64:j * 128 + (h + 1) * 64]),
                     reads=[f"uev{g % 2}"], writes=["U3", "B"])
    if STAGE <= 4:
        S.barrier(); kb.release(m0); return
    for v in range(64):
        S.op("dve", lambda e, v=v: e.tensor_tensor_scan(out=S3[R, v, :], data0=DC[R, :], data1=U3[R, v, :], initial=0.0, op0=ALU.mult, op1=ALU.add),
             reads=["U3", "DC", "KHT"], writes=["S3", "A"])
    S.op("pool", lambda e: e.memset(SB[R, 0, :], 0.0), writes=["SB0"])
    S.op("act", lambda e: e.copy(out=SB[R, 1:128, :], in_=S3[R, :, 0:127].rearrange("p v c -> p c v")), reads=["S3"], writes=["SB"])
    if STAGE <= 5:
        S.barrier(); kb.release(m0); return
    osb = kb.alloc(512)
    sqb = kb.alloc(512, BF16)
    rstd = kb.alloc(512)
    og = kb.alloc(512)
    at = [kb.alloc(64, BF16), kb.alloc(64, BF16)]
    ai = 0
    for t in range(NQT):
        ts = slice(t * TQ, (t + 1) * TQ)
        for h in range(2):
            hr = slice(32 * h, 32 * h + 32)
            ob = 4 + (2 * t + h) % 2
            for j in range(8):
                ch = 8 * t + j
                cs = slice(ch * 64, (ch + 1) * 64)
                rr = R
                sbk = ai % 2
                a_t = at[ai % 2]
                ak = f"at{ai % 2}"
                ai += 1
                S.op("pe", lambda e, hr=hr, cs=cs, rr=rr, sbk=sbk: e.matmul(P[sbk][rr, 0:64], lhsT=KG[hr, cs], rhs=QG[hr, cs], start=True, stop=True),
                     reads=["KG", "QG"], writes=[pk(sbk)])
                S.op("dve", lambda e, rr=rr, sbk=sbk, a_t=a_t: e.tensor_tensor(out=a_t[rr, :], in0=P[sbk][rr, 0:64], in1=tri64[rr, :], op=ALU.mult),
                     reads=["tri64"], writes=[pk(sbk), ak])
                S.op("pe", lambda e, rr=rr, ch=ch, h=h, a_t=a_t, ob=ob, j=j: e.matmul(P[ob][R, j * 64:(j + 1) * 64], lhsT=V[rr, ch, h * 64:(h + 1) * 64], rhs=a_t[rr, :], start=True, stop=False),
                     reads=[ak, "V"], writes=[pk(ob)])
                S.op("pe", lambda e, hr=hr, ch=ch, cs=cs, ob=ob, j=j: e.matmul(P[ob][R, j * 64:(j + 1) * 64], lhsT=SB[hr, ch, :], rhs=QG[hr, cs], start=False, stop=True),
                     reads=["SB", "SB0", "QG"], writes=[pk(ob)])
            S.op("act", lambda e, ob=ob: e.copy(out=osb[R, :], in_=P[ob][R, :]), writes=[pk(ob), "osb"])
            S.op("act", lambda e: e.activation(out=sqb[R, :], in_=osb[R, :], func=AF.Square), reads=["osb"], writes=["sqb"])
            S.op("pe", lambda e: e.matmul(P[6][R, :], lhsT=c.ones_bf[R, 0:64], rhs=sqb[R, :], start=True, stop=True), reads=["sqb", "ones_bf"], writes=[pk(6)])
            S.op("act", lambda e: e.activation(out=rstd[R, :], in_=P[6][R, :], func=AF.Sqrt, bias=EPS, scale=1.0 / 64.0), writes=[pk(6), "rstd"])
            S.op("dve", lambda e: e.reciprocal(out=rstd[R, :], in_=rstd[R, :]), writes=["rstd"])
            S.op("dve", lambda e: e.scalar_tensor_tensor(out=osb[R, :], in0=osb[R, :], scalar=v2[R, 1:2], in1=rstd[R, :], op0=ALU.mult, op1=ALU.mult),
                 reads=["rstd", "v2"], writes=["osb"])
            S.dma("sp", lambda e, h=h, ts=ts: e.dma_start(out=og[R, :], in_=L.pt(OFF["c_og"] + h * 64, 128, 64, ts.start, ts.stop)), writes=["og"])
            S.op("act", lambda e: e.activation(out=og[R, :], in_=og[R, :], func=AF.Silu), writes=["og"])
            S.op("dve", lambda e: e.tensor_tensor(out=osb[R, :], in0=osb[R, :], in1=og[R, :], op=ALU.mult), reads=["og"], writes=["osb"])
            S.dma("sp", lambda e, h=h, ts=ts: e.dma_start(out=yT[256 + h * 64:256 + (h + 1) * 64, ts], in_=osb[R, :]), reads=["osb"])
    S.barrier()
    kb.release(m0)


WB = ["lru_w", "lru_v", "mla_wuq", "mla_wk", "mla_wv", "mla_v", "nsa_pos", "nsa_w1", "nsa_b1", "nsa_w2", "gla_wg2", "gla_v2"]
WAC = (("gain", [128, 8]), ("w_in", [1024, NCOLW]), ("gng", [128, 8]), ("nfg", [128, 8]), ("w_out", [1024, 1024]),
       ("w_gu", [1024, 2 * DFF]), ("w_dn", [DFF, 1024]))
RG = [[0, 1], [2, 3], [4, 5], [6, 7]]


def build_fused():
    kb = KB(arena_cols=53100)
    S = kb.S
    D = {k: kb.din(k, shp) for k, shp in CONST_SHAPES.items()}
    D["pos"] = kb.din("pos", [1, N], I32)
    xT = kb.din("xT", [1024, 4096])
    out = kb.dout("out", [1024, 4096])
    LW = []
    for l in range(2):
        d = {k: kb.din(f"{k}_{l}", IN_SHAPES[k][0]) for k in WB}
        for k, shp in WAC:
            d[k] = kb.din(f"{k}_{l}", shp)
        LW.append(d)
    fing = kb.din("fing", [128, 8])
    XA = kb.dint("XA", [NCOLP, 4096])
    XV = kb.dint("XV", [4096, NV])
    GP = kb.dint("GP", [2 * NCOLP, 4096])
    GV = kb.dint("GV", [N, NV])
    YB = kb.dint("YB", [1024, 4096])
    YV = YView(YB)
    GY = kb.dint("GY", [2048, 4096])
    XO = kb.dint("XO", [1024, 4096])
    MYP = kb.dint("MYP", [SELROWS, N])
    MYV = kb.dint("MYV", [N, 128])
    MYY = kb.dint("MYY", [1024, 4096])
    c = common_setup(kb, D)
    L = Loader(S, GP, GV, MYP, MYV)
    xs = xT
    for l in range(2):
        W = LW[l]
        ag = lambda i_, o_: (lambda e: e.collective_compute("AllGather", ALU.bypass, replica_groups=RG, ins=[i_], outs=[o_]))
        phase_A(kb, xs, W["gain"], W["w_in"], XA, XV, GP, GV, ag)
        ccA = S.ccn
        Dl = dict(D)
        Dl.update({k: W[k] for k in WB})
        S.cc_wait(kb.cc_lru)
        L.select(names=("a_x", "a_gate"), with_v=False)
        for part, g in ((part_lru, 0), (part_gla, 2), (part_mla, 3), (part_nsa, 1)):
            if g == 2:
                S.cc_wait(ccA)
                L.select(names=("b_q", "b_gate", "c_q", "c_k", "c_og"), with_v=True)
            part(kb, c, Dl, YV, L)
            for ch in range(2):
                S.collective_async(ag(YB[ch * 512 + g * 128:ch * 512 + (g + 1) * 128, :], GY[(g * 2 + ch) * 256:(g * 2 + ch + 1) * 256, :]))
        S.cc_wait_all()
        GY4 = GY.rearrange("(g ch q) t -> g ch q t", g=4, ch=2)
        S.dma("act", lambda e: e.dma_start(out=MYY.rearrange("(g q) t -> g q t", g=4), in_=GY4[:, bass.ds(S.rt["hp_act"], 1), :, :].rearrange("g o q t -> g (o q) t")),
              writes=["MYY"])
        S.barrier()
        phase_C(kb, l == 1, xs, MYY, W["gng"], W["nfg"], fing, W["w_out"], W["w_gu"], W["w_dn"], out if l == 1 else XO)
        xs = XO
    return kb.close()


def prep_W(W, l, hp):
    A = np.ascontiguousarray
    o = {}
    ch = slice(hp * 128, hp * 128 + 128)
    o["lru_w"] = A(np.stack([W["lru_wa"][l][2 * hp:2 * hp + 2], W["lru_wx"][l][2 * hp:2 * hp + 2]]))
    o["lru_v"] = A(np.stack([W["conv_w"][l][0, ch], W["conv_w"][l][1, ch], W["conv_w"][l][2, ch], W["conv_w"][l][3, ch],
                             W["conv_b"][l][ch], W["lru_ba"][l][ch], W["lru_bx"][l][ch], W["lru_lambda"][l][ch]], axis=1))
    o["mla_wuq"] = A(W["mla_w_uq"][l][:, hp * 192:(hp + 1) * 192])
    wkv = W["mla_w_ukv"][l].reshape(128, 4, 128)
    o["mla_wk"] = A(wkv[:, 2 * hp:2 * hp + 2, 0:64].reshape(128, 128))
    o["mla_wv"] = A(wkv[:, 2 * hp:2 * hp + 2, 64:128].reshape(128, 128))
    mv = np.zeros((128, 3), np.float32)
    mv[:, 0] = W["mla_q_norm"][l][0:128]
    mv[0:64, 1] = W["mla_q_norm"][l][128:192]
    mv[:, 2] = W["mla_kv_norm"][l]
    o["mla_v"] = mv
    o["nsa_pos"] = A(np.concatenate([W["cmp_pos"][l][0].T, W["cmp_pos"][l][1].T], axis=0))
    o["nsa_w1"] = A(W["cmp_w1"][l])
    o["nsa_b1"] = A(W["cmp_b1"][l].reshape(2, 2, 128).transpose(2, 0, 1).reshape(128, 4))
    o["nsa_w2"] = A(W["cmp_w2"][l])
    o["gla_wg2"] = A(W["gla_wg2"][l][:, hp * 64:(hp + 1) * 64])
    g2 = np.zeros((128, 2), np.float32)
    g2[0:64, 0] = W["gla_bg2"][l][hp * 64:(hp + 1) * 64]
    g2[:, 1] = np.tile(W["gla_norm"][l], 2)
    o["gla_v2"] = g2
    return o


_PROG = {}


def _arr8(g):
    return np.ascontiguousarray(np.asarray(g, np.float32).reshape(8, 128).T)


def kernel(**inp):
    W = {k: np.asarray(v) for k, v in inp.items()}
    x = W["x"]
    Bn, Sn, Dm = x.shape
    HT = Sn // 2
    cores = [(b, r) for b in range(Bn) for r in range(2)]
    if "F" not in _PROG:
        _PROG["F"] = build_fused()
    nc = _PROG["F"]
    consts = host_consts()
    pc = perm_cols()
    ins = []
    for (b, r) in cores:
        d = dict(consts)
        d["pos"] = np.ascontiguousarray(W["positions"][b][None, :].astype(np.int32))
        d["xT"] = np.ascontiguousarray(x[b, r * HT:(r + 1) * HT].T)
        d["fing"] = _arr8(W["final_norm"])
        for l in range(2):
            for k, v in prep_W(W, l, r).items():
                d[f"{k}_{l}"] = v
            d[f"gain_{l}"] = _arr8(W["norm_mix"][l])
            d[f"w_in_{l}"] = np.ascontiguousarray(W["w_in"][l][:, pc])
            d[f"gng_{l}"] = _arr8(W["group_norm"][l])
            d[f"nfg_{l}"] = _arr8(W["norm_ffn"][l])
            d[f"w_out_{l}"] = np.ascontiguousarray(W["w_out"][l])
            d[f"w_gu_{l}"] = np.ascontiguousarray(W["w_gate_up"][l])
            d[f"w_dn_{l}"] = np.ascontiguousarray(W["w_down"][l])
        ins.append(d)
    res = run_bass_kernel_spmd(nc, ins, core_ids=list(range(8))).results
    out = np.empty((Bn, Sn, Dm), np.float32)
    for ci, (b, r) in enumerate(cores):
        out[b, r * HT:(r + 1) * HT] = res[ci]["out"].T
    return out
```

```python
import numpy as np
from contextlib import ExitStack
import concourse.bass as bass
import concourse.mybir as mybir
from concourse.bass_utils import run_bass_kernel_spmd

F32 = mybir.dt.float32
BF16 = mybir.dt.bfloat16
I32 = mybir.dt.int32
AF = mybir.ActivationFunctionType
ALU = mybir.AluOpType
AX = mybir.AxisListType

ENGS = ("pe", "act", "dve", "pool", "sp")
NDMA_SEMS = 14
EPS = 1e-6


class Sched:
    def __init__(self, nc, es):
        self.nc = nc
        self.sem = {e: es.enter_context(nc.semaphore("s_" + e)) for e in ENGS}
        self.cnt = {e: 0 for e in ENGS}
        self.dsem = {e: [es.enter_context(nc.semaphore(f"d_{e}{i}")) for i in range(NDMA_SEMS)]
                     for e in ("sp", "pool", "act")}
        self.dval = {e: [0] * NDMA_SEMS for e in self.dsem}
        self.drr = {e: 0 for e in self.dsem}
        self.ops = {e: [] for e in ENGS}
        self.known = {e: {} for e in ENGS}
        self.semobj = {}
        self.last_w = {}
        self.readers = {}
        self.ccsem = es.enter_context(nc.semaphore("s_cc"))
        self.semobj["cc"] = self.ccsem
        self.ccn = 0
        self.rt = {}
        for e in ENGS:
            self.semobj["c_" + e] = self.sem[e]
        for e in self.dsem:
            for i in range(NDMA_SEMS):
                self.semobj[f"d_{e}{i}"] = self.dsem[e][i]

    def _need(self, eng, tok, waits):
        if tok is None:
            return
        sk, val, teng = tok
        if teng == "pe" and eng == "pe" and sk == "c_pe":
            return
        if self.known[eng].get(sk, 0) >= val:
            return
        self.known[eng][sk] = val
        waits[sk] = max(waits.get(sk, 0), val)

    def _deps(self, eng, reads, writes):
        waits = {}
        for k in reads:
            self._need(eng, self.last_w.get(k), waits)
        for k in writes:
            self._need(eng, self.last_w.get(k), waits)
            for t in self.readers.get(k, ()):
                self._need(eng, t, waits)
        return waits

    def _commit(self, tok, reads, writes):
        for k in reads:
            self.readers.setdefault(k, []).append(tok)
        for k in writes:
            self.last_w[k] = tok
            self.readers[k] = []

    def op(self, eng, emit, reads=(), writes=()):
        waits = self._deps(eng, reads, writes)
        self.cnt[eng] += 1
        tok = ("c_" + eng, self.cnt[eng], eng)
        self.ops[eng].append((waits, emit, (self.sem[eng], 1)))
        self._commit(tok, reads, writes)
        return tok

    def dma(self, eng, emit, reads=(), writes=()):
        waits = self._deps(eng, reads, writes)
        i = self.drr[eng]
        self.drr[eng] = (i + 1) % NDMA_SEMS
        sk = f"d_{eng}{i}"
        prev = self.dval[eng][i]
        if prev and self.known[eng].get(sk, 0) < prev:
            self.known[eng][sk] = prev
            waits[sk] = prev
        self.dval[eng][i] = prev + 16
        tok = (sk, prev + 16, eng)
        self.ops[eng].append((waits, emit, (self.dsem[eng][i], 16)))
        self._commit(tok, reads, writes)
        return tok

    def barrier(self):
        for eng in ENGS:
            waits = {}
            for e in ENGS:
                if e != eng and self.cnt[e]:
                    self._need(eng, ("c_" + e, self.cnt[e], e), waits)
            for e in self.dsem:
                for i in range(NDMA_SEMS):
                    if self.dval[e][i]:
                        self._need(eng, (f"d_{e}{i}", self.dval[e][i], "dma"), waits)
            if eng != "pe" and self.cnt[eng]:
                self._need(eng, ("c_" + eng, self.cnt[eng], eng), waits)
            if waits:
                self.ops[eng].append((waits, None, None))
        self.last_w = {}
        self.readers = {}

    def collective(self, emits):
        self.barrier()
        for emit in emits:
            w = {"cc": self.ccn} if self.ccn else {}
            self.ccn += 1
            self.ops["pool"].append((w, emit, (self.ccsem, 1)))
        for eng in ENGS:
            self.known[eng]["cc"] = self.ccn
            self.ops[eng].append(({"cc": self.ccn}, None, None))

    def collective_async(self, emit, reads=()):
        waits = {}
        for k in reads:
            self._need("pool", self.last_w.get(k), waits)
        if self.ccn:
            waits["cc"] = self.ccn
        self.ccn += 1
        self.ops["pool"].append((waits, emit, (self.ccsem, 1)))

    def cc_wait(self, n):
        self.barrier()
        for eng in ENGS:
            if self.known[eng].get("cc", 0) < n:
                self.known[eng]["cc"] = n
                self.ops[eng].append(({"cc": n}, None, None))

    def cc_wait_all(self):
        self.barrier()
        for eng in ENGS:
            if self.known[eng].get("cc", 0) < self.ccn:
                self.known[eng]["cc"] = self.ccn
                self.ops[eng].append(({"cc": self.ccn}, None, None))

    def finish(self):
        self.barrier()
        semobj = self.semobj

        def run(engobj, lst):
            for waits, emit, inc in lst:
                for sk, v in waits.items():
                    engobj.wait_ge(semobj[sk], v)
                if emit is not None:
                    emit(engobj).then_inc(inc[0], inc[1])

        with self.nc.Block() as block:
            @block.tensor
            def _(e):
                run(e, self.ops["pe"])

            @block.scalar
            def _(e):
                self.rt["hp_act"] = e.partition_id() % 2
                run(e, self.ops["act"])

            @block.vector
            def _(e):
                run(e, self.ops["dve"])

            @block.gpsimd
            def _(e):
                run(e, self.ops["pool"])

            @block.sync
            def _(e):
                self.rt["hp_sp"] = e.partition_id() % 2
                run(e, self.ops["sp"])


class KB:
    def __init__(self, arena_cols=50000):
        self.nc = bass.Bass("TRN2", target_bir_lowering=False)
        self.es = ExitStack()
        self.S = Sched(self.nc, self.es)
        self.arena = self.es.enter_context(self.nc.sbuf_tensor("arena", [128, arena_cols], F32))
        self.acols = arena_cols
        self.top = 0
        self.psb = [self.es.enter_context(self.nc.psum_tensor(f"psb{i}", [128, 512], F32)) for i in range(8)]
        self.uid = 0

    def dint(self, name, shape, dt=F32):
        return self.nc.dram_tensor(name, list(shape), dt).ap()

    def din(self, name, shape, dt=F32):
        return self.nc.dram_tensor(name, list(shape), dt, kind="ExternalInput").ap()

    def dout(self, name, shape, dt=F32):
        return self.nc.dram_tensor(name, list(shape), dt, kind="ExternalOutput").ap()

    def alloc(self, cols, dt=F32):
        n32 = cols if dt != BF16 else (cols + 1) // 2
        a = self.top
        self.top += n32
        assert self.top <= self.acols, f"arena overflow {self.top}"
        v = self.arena[:, a:a + n32]
        if dt == BF16:
            v = v.bitcast(BF16)
        elif dt == I32:
            v = v.bitcast(I32)
        return v

    def mark(self):
        return self.top

    def release(self, m):
        self.top = m

    def key(self, base="k"):
        self.uid += 1
        return f"{base}{self.uid}"

    def close(self):
        self.S.finish()
        self.es.close()
        return self.nc


NCOL = 2300
NCOLP = 1920
NCOLW = NCOLP + 384
VCOLS = [(NCOLP, NCOLW)]
NV = 384
OFF = dict(a_x=0, a_gate=256, b_q=512, b_kv=768, c_q=1024, c_k=1152, c_og=1280, d_cq=1536, d_kr=1728, c_glr=1760,
           b_gate=1776, d_ckv=1792)


def perm_cols():
    o = dict(a_x=0, a_gate=256, b_q=512, b_kv=768, b_gate=1152, c_q=1164, c_k=1292, c_v=1420, c_glr=1676, c_og=1692,
             d_cq=1948, d_ckv=2140, d_kr=2268)
    r = lambda a, n: list(range(a, a + n))
    p = (r(o["a_x"], 256) + r(o["a_gate"], 256) + r(o["b_q"], 256)
         + r(o["b_kv"], 64) + r(o["b_kv"] + 64, 64) + r(o["b_kv"] + 128, 64) + r(o["b_kv"] + 256, 64)
         + r(o["c_q"], 128) + r(o["c_k"], 128) + r(o["c_og"], 256) + r(o["d_cq"], 192) + r(o["d_kr"], 32)
         + r(o["c_glr"], 16) + r(o["b_gate"], 12) + r(o["b_gate"], 4) + r(o["d_ckv"], 128))
    assert len(p) == NCOLP
    p += r(o["b_kv"] + 192, 64) + r(o["b_kv"] + 320, 64) + r(o["c_v"], 256)
    assert len(p) == NCOLW
    return np.array(p)
DFF = 2816


def load_cast_weight(kb, w_dram, wsb, kchunks, ncols, stage, key, piece=1024):
    S = kb.S
    i = 0
    for c in range(kchunks):
        for c0 in range(0, ncols, piece):
            c1 = min(ncols, c0 + piece)
            st = stage[i % len(stage)]
            sk = f"wstage{i % len(stage)}"
            S.dma("sp", lambda e, st=st, c=c, c0=c0, c1=c1: e.dma_start(out=st[:, 0:c1 - c0], in_=w_dram[c * 128:(c + 1) * 128, c0:c1]),
                  writes=[sk])
            eng = ("act", "pool", "dve")[i % 3]
            if eng == "act":
                S.op("act", lambda e, st=st, c=c, c0=c0, c1=c1: e.copy(out=wsb[:, c, c0:c1], in_=st[:, 0:c1 - c0]), reads=[sk], writes=[key])
            else:
                S.op(eng, lambda e, st=st, c=c, c0=c0, c1=c1: e.tensor_copy(out=wsb[:, c, c0:c1], in_=st[:, 0:c1 - c0]), reads=[sk], writes=[key])
            i += 1


def rms_stats(kb, src3, nch, T, sq, ones_bf, ps_ap, rstd, denom, keys_in, key_sq, key_ps, key_rstd):
    S = kb.S
    S.op("act", lambda e: e.activation(out=sq, in_=src3, func=AF.Square), reads=keys_in, writes=[key_sq])
    for c in range(nch):
        S.op("pe", lambda e, c=c: e.matmul(ps_ap, lhsT=ones_bf, rhs=sq[:, c, :], start=(c == 0), stop=(c == nch - 1)),
             reads=[key_sq, "ones_bf"], writes=[key_ps])
    S.op("act", lambda e: e.activation(out=rstd, in_=ps_ap, func=AF.Sqrt, bias=EPS, scale=1.0 / denom), writes=[key_ps, key_rstd])
    S.op("dve", lambda e: e.reciprocal(out=rstd, in_=rstd), reads=[key_rstd], writes=[key_rstd])


def phase_A(kb, xT, gain, w, pT, pV, GP, GV, ag):
    S = kb.S
    m0 = kb.mark()
    T = 512
    NT = 8
    xv = xT.rearrange("(c p) t -> p c t", p=128)

    wsb = kb.alloc(8 * NCOLW, BF16).rearrange("p (c n) -> p c n", c=8)
    gsb = kb.alloc(8)
    ones_bf = kb.alloc(128, BF16)
    stage = [kb.alloc(1024) for _ in range(4)]
    xt = [kb.alloc(8 * T).rearrange("p (c t) -> p c t", c=8) for _ in range(2)]
    sq = kb.alloc(8 * T, BF16).rearrange("p (c t) -> p c t", c=8)
    hb = kb.alloc(8 * 4096, BF16).rearrange("p (c t) -> p c t", c=8)
    rstd = kb.alloc(T)
    ost = [kb.alloc(T) for _ in range(4)]
    P = [p[:] for p in kb.psb]

    S.dma("sp", lambda e: e.dma_start(out=gsb, in_=gain[:, :]), writes=["gsb"])
    S.op("pool", lambda e: e.memset(ones_bf, 1.0), writes=["ones_bf"])
    S.dma("sp", lambda e: e.dma_start(out=xt[0], in_=xv[:, :, 0:T]), writes=["xt0"])
    load_cast_weight(kb, w, wsb, 8, NCOLW, stage, "wsb")
    for t in range(NT):
        b = t % 2
        if t + 1 < NT:
            S.dma("sp", lambda e, t=t: e.dma_start(out=xt[(t + 1) % 2], in_=xv[:, :, (t + 1) * T:(t + 2) * T]),
                  writes=[f"xt{(t + 1) % 2}"])
        rms_stats(kb, xt[b], 8, T, sq, ones_bf, P[0], rstd, 1024.0, [f"xt{b}"], "sq", "psb0", "rstd")
        for c in range(8):
            S.op("dve", lambda e, c=c, b=b, t=t: e.scalar_tensor_tensor(out=hb[:, c, t * T:(t + 1) * T], in0=xt[b][:, c, :], scalar=gsb[:, c:c + 1], in1=rstd,
                                                                   op0=ALU.mult, op1=ALU.mult),
                 reads=[f"xt{b}", "rstd", "gsb"], writes=[f"hb{t}"])
    oi = 0
    for t in range(NT):
        for tb in range(4):
            pb = 5 + (tb % 2)
            for c in range(8):
                S.op("pe", lambda e, c=c, t=t, tb=tb, pb=pb: e.matmul(
                    P[pb][:, 0:NV], lhsT=hb[:, c, t * T + tb * 128:t * T + (tb + 1) * 128], rhs=wsb[:, c, NCOLP:NCOLW], start=(c == 0), stop=(c == 7)),
                    reads=[f"hb{t}", "wsb"], writes=[f"psb{pb}"])
            o = oi % 4
            oi += 1
            S.op("act", lambda e, o=o, pb=pb: e.copy(out=ost[o][:, 0:NV], in_=P[pb][:, 0:NV]), writes=[f"psb{pb}", f"ost{o}"])
            S.dma("sp", lambda e, o=o, t=t, tb=tb: e.dma_start(out=pV[t * T + tb * 128: t * T + (tb + 1) * 128, :], in_=ost[o][:, 0:NV]),
                  reads=[f"ost{o}"], writes=[f"XV{t}_{tb}"])
        if t % 2 == 1:
            j = t // 2
            S.collective_async(ag(pV[j * 1024:(j + 1) * 1024, :], GV[j * 2048:(j + 1) * 2048, :]),
                               reads=[f"XV{tt}_{tb}" for tt in (t - 1, t) for tb in range(4)])
    for k in range(NCOLP // 128):
        c0, c1 = k * 128, (k + 1) * 128
        for t in range(NT):
            pb = 1 + (oi % 4)
            for c in range(8):
                S.op("pe", lambda e, c=c, t=t, c0=c0, c1=c1, pb=pb: e.matmul(P[pb][:, :], lhsT=wsb[:, c, c0:c1], rhs=hb[:, c, t * T:(t + 1) * T],
                                                                             start=(c == 0), stop=(c == 7)),
                     reads=[f"hb{t}", "wsb"], writes=[f"psb{pb}"])
            o = oi % 4
            oi += 1
            if t % 2 == 0:
                S.op("act", lambda e, o=o, pb=pb: e.copy(out=ost[o], in_=P[pb][:, :]), writes=[f"psb{pb}", f"ost{o}"])
            else:
                S.op("dve", lambda e, o=o, pb=pb: e.tensor_copy(out=ost[o], in_=P[pb][:, :]), writes=[f"psb{pb}", f"ost{o}"])
            S.dma("sp", lambda e, o=o, c0=c0, c1=c1, t=t: e.dma_start(out=pT[c0:c1, t * T:(t + 1) * T], in_=ost[o]),
                  reads=[f"ost{o}"], writes=[f"XA{k}_{t}"])
        S.collective_async(ag(pT[c0:c1, :], GP[k * 256:(k + 1) * 256, :]), reads=[f"XA{k}_{t}" for t in range(NT)])
        if k == 3:
            kb.cc_lru = S.ccn
    S.barrier()
    kb.release(m0)


def phase_C(kb, final, xT, GY, gng, nfg, fing, w_out, w_gu, w_dn, xo, NT=16):
    S = kb.S
    m0 = kb.mark()
    T = 256
    xv = xT.rearrange("(c p) t -> p c t", p=128)
    ov = xo.rearrange("(c p) t -> p c t", p=128)

    wo = kb.alloc(8 * 1024, BF16).rearrange("p (c n) -> p c n", c=8)
    wgu = kb.alloc(8 * 2 * DFF, BF16).rearrange("p (c n) -> p c n", c=8)
    wdn = kb.alloc(22 * 1024, BF16).rearrange("p (c n) -> p c n", c=22)
    g3 = kb.alloc(24)
    ones_bf = kb.alloc(128, BF16)
    xts = [kb.alloc(8 * T).rearrange("p (c t) -> p c t", c=8) for _ in range(2)]
    yt = kb.alloc(8 * T).rearrange("p (c t) -> p c t", c=8)
    ytf = yt.rearrange("p c t -> p (c t)")
    stage = [ytf[:, 0:1024], ytf[:, 1024:2048]]
    for xb_ in xts:
        xbf = xb_.rearrange("p c t -> p (c t)")
        stage += [xbf[:, 0:1024], xbf[:, 1024:2048]]
    sq = kb.alloc(8 * T, BF16).rearrange("p (c t) -> p c t", c=8)
    hb = kb.alloc(8 * T, BF16).rearrange("p (c t) -> p c t", c=8)
    aT = kb.alloc(22 * T, BF16).rearrange("p (c t) -> p c t", c=22)
    rstd4 = kb.alloc(4 * T).rearrange("p (c t) -> p c t", c=4)
    rstd = kb.alloc(T)
    sg = [kb.alloc(T), kb.alloc(T)]
    P = [p[:] for p in kb.psb]

    S.dma("sp", lambda e: e.dma_start(out=g3[:, 0:8], in_=gng[:, :]), writes=["g3"])
    S.dma("sp", lambda e: e.dma_start(out=g3[:, 8:16], in_=nfg[:, :]), writes=["g3"])
    S.dma("sp", lambda e: e.dma_start(out=g3[:, 16:24], in_=fing[:, :]), writes=["g3"])
    S.op("pool", lambda e: e.memset(ones_bf, 1.0), writes=["ones_bf"])
    load_cast_weight(kb, w_out, wo, 8, 1024, stage, "wo")
    load_cast_weight(kb, w_gu, wgu, 8, 2 * DFF, stage, "wgu")
    load_cast_weight(kb, w_dn, wdn, 22, 1024, stage, "wdn")
    S.barrier()

    def load_y(t):
        for cc_ in range(8):
            r0 = cc_ * 128
            S.dma("sp", lambda e, cc_=cc_, r0=r0, t=t: e.dma_start(out=yt[:, cc_, :], in_=GY[r0:r0 + 128, t * T:(t + 1) * T]),
                  writes=["yt"])

    S.dma("sp", lambda e: e.dma_start(out=xts[0], in_=xv[:, :, 0:T]), writes=["xt0"])
    load_y(0)
    for t in range(NT):
        ts = slice(t * T, (t + 1) * T)
        xt = xts[t % 2]
        XK = f"xt{t % 2}"
        if t + 1 < NT:
            S.dma("sp", lambda e, t=t: e.dma_start(out=xts[(t + 1) % 2], in_=xv[:, :, (t + 1) * T:(t + 2) * T]), writes=[f"xt{(t + 1) % 2}"])
        S.op("act", lambda e: e.activation(out=sq, in_=yt, func=AF.Square), reads=["yt"], writes=["sq"])
        for g in range(4):
            pa = P[g][:, 0:T]
            for j in range(2):
                S.op("pe", lambda e, g=g, j=j, pa=pa: e.matmul(pa, lhsT=ones_bf, rhs=sq[:, 2 * g + j, :], start=(j == 0), stop=(j == 1)),
                     reads=["sq", "ones_bf"], writes=[f"psb{g}"])
            S.op("act", lambda e, g=g, pa=pa: e.activation(out=rstd4[:, g, :], in_=pa, func=AF.Sqrt, bias=EPS, scale=1.0 / 256.0),
                 writes=[f"psb{g}", f"rstd4_{g}"])
            S.op("dve", lambda e, g=g: e.reciprocal(out=rstd4[:, g, :], in_=rstd4[:, g, :]), reads=[f"rstd4_{g}"], writes=[f"rstd4_{g}"])
        for c in range(8):
            eng = "dve"
            S.op(eng, lambda e, c=c: e.scalar_tensor_tensor(out=hb[:, c, :], in0=yt[:, c, :], scalar=g3[:, c:c + 1], in1=rstd4[:, c // 2, :],
                                                         op0=ALU.mult, op1=ALU.mult),
                 reads=["yt", f"rstd4_{c // 2}", "g3"], writes=[f"hb{c}"])
        if t + 1 < NT:
            load_y(t + 1)
        for m in range(8):
            pa = P[4 + m % 4][:, 0:T]
            for c in range(8):
                S.op("pe", lambda e, m=m, c=c, pa=pa: e.matmul(pa, lhsT=wo[:, c, m * 128:(m + 1) * 128], rhs=hb[:, c, :], start=(c == 0), stop=(c == 7)),
                     reads=[f"hb{c}", "wo"], writes=[f"psb{4 + m % 4}"])
            S.op("dve", lambda e, m=m, pa=pa, xt=xt: e.tensor_tensor(out=xt[:, m, :], in0=xt[:, m, :], in1=pa, op=ALU.add),
                 reads=[XK], writes=[XK, f"psb{4 + m % 4}"])
        rms_stats(kb, xt, 8, T, sq, ones_bf, P[0][:, 0:T], rstd, 1024.0, [XK], "sq", "psb0", "rstd")
        for c in range(8):
            eng = "dve"
            S.op(eng, lambda e, c=c, xt=xt: e.scalar_tensor_tensor(out=hb[:, c, :], in0=xt[:, c, :], scalar=g3[:, 8 + c:9 + c], in1=rstd,
                                                         op0=ALU.mult, op1=ALU.mult),
                 reads=[XK, "rstd", "g3"], writes=[f"hb{c}"])
        for j in range(22):
            pg = P[j % 2][:, 0:T]
            pu = P[2 + j % 2][:, 0:T]
            for c in range(8):
                S.op("pe", lambda e, j=j, c=c, pg=pg: e.matmul(pg, lhsT=wgu[:, c, j * 128:(j + 1) * 128], rhs=hb[:, c, :], start=(c == 0), stop=(c == 7)),
                     reads=[f"hb{c}", "wgu"], writes=[f"psb{j % 2}"])
            for c in range(8):
                S.op("pe", lambda e, j=j, c=c, pu=pu: e.matmul(pu, lhsT=wgu[:, c, DFF + j * 128:DFF + (j + 1) * 128], rhs=hb[:, c, :], start=(c == 0), stop=(c == 7)),
                     reads=[f"hb{c}", "wgu"], writes=[f"psb{2 + j % 2}"])
            S.op("act", lambda e, j=j, pg=pg: e.activation(out=sg[j % 2], in_=pg, func=AF.Silu), writes=[f"psb{j % 2}", f"sg{j % 2}"])
            S.op("dve", lambda e, j=j, pu=pu: e.tensor_tensor(out=aT[:, j, :], in0=sg[j % 2], in1=pu, op=ALU.mult),
                 reads=[f"sg{j % 2}"], writes=[f"aT{j}", f"psb{2 + j % 2}"])
        for m in range(8):
            pa = P[4 + m % 4][:, 0:T]
            for k in range(22):
                S.op("pe", lambda e, m=m, k=k, pa=pa: e.matmul(pa, lhsT=wdn[:, k, m * 128:(m + 1) * 128], rhs=aT[:, k, :], start=(k == 0), stop=(k == 21)),
                     reads=[f"aT{k}", "wdn"], writes=[f"psb{4 + m % 4}"])
            S.op("dve", lambda e, m=m, pa=pa, xt=xt: e.tensor_tensor(out=xt[:, m, :], in0=xt[:, m, :], in1=pa, op=ALU.add),
                 reads=[XK], writes=[XK, f"psb{4 + m % 4}"])
        if final:
            rms_stats(kb, xt, 8, T, sq, ones_bf, P[0][:, 0:T], rstd, 1024.0, [XK], "sq", "psb0", "rstd")
            for c in range(8):
                eng = "dve"
                S.op(eng, lambda e, c=c, xt=xt: e.scalar_tensor_tensor(out=xt[:, c, :], in0=xt[:, c, :], scalar=g3[:, 16 + c:17 + c], in1=rstd,
                                                                    op0=ALU.mult, op1=ALU.mult),
                     reads=["rstd", "g3"], writes=[XK])
            S.dma("sp", lambda e, ts=ts, xt=xt: e.dma_start(out=ov[:, :, ts], in_=xt), reads=[XK])
        else:
            S.dma("sp", lambda e, ts=ts, xt=xt: e.dma_start(out=ov[:, :, ts], in_=xt), reads=[XK])
    S.barrier()
    kb.release(m0)


N = 8192
TQ = 512
NQT = N // TQ
NEG = -30000.0
PI = float(np.pi)
TWO_PI = float(2 * np.pi)
THETA = 10000.0


def pk(b):
    return f"psb{b}"


def host_consts():
    c = {}
    c["ident"] = np.eye(128, dtype=np.float32)
    R = np.zeros((128, 128), np.float32)
    for blk in range(2):
        for m in range(64):
            if m < 32:
                R[blk * 64 + m + 32, blk * 64 + m] = -1.0
            else:
                R[blk * 64 + m - 32, blk * 64 + m] = 1.0
    c["rbd"] = R
    R32 = np.zeros((128, 32), np.float32)
    for m in range(32):
        if m < 16:
            R32[64 + m + 16, m] = -1.0
        else:
            R32[64 + m - 16, m] = 1.0
    c["r32"] = R32
    invf = np.zeros((128, 2), np.float32)
    for p in range(128):
        invf[p, 0] = np.float32(THETA) ** np.float32(-(2.0 * ((p % 64) % 32)) / 64.0)
    for p in range(64, 96):
        invf[p, 1] = np.float32(THETA) ** np.float32(-(2.0 * ((p - 64) % 16)) / 32.0)
    c["invf"] = invf
    k = np.arange(128)[:, None]
    q = np.arange(512)[None, :]
    c["tri"] = np.stack([np.where(k + i * 128 <= q, 0.0, NEG) for i in range(4)]).astype(np.float32)
    c["win"] = np.stack([np.where((k + (i - 4) * 128 <= q) & (k + (i - 4) * 128 > q - 512), 0.0, NEG) for i in range(8)]).astype(np.float32)
    c["cmpm"] = np.stack([np.where(16 * k + 31 <= i * 512 + q, 0.0, NEG) for i in range(5)]).astype(np.float32)
    n = np.arange(512)
    s = np.arange(128)
    ov = ((n[:, None] * 16 < s[None, :] * 64 + 64) & (n[:, None] * 16 + 32 > s[None, :] * 64)).astype(np.float32)
    ov[511] = 0.0
    ovl = np.zeros((4, 128, 129), np.float32)
    ovl[:, :, :128] = ov.reshape(4, 128, 128)
    ovl[:, :, 128] = 1.0
    c["ovl"] = ovl
    c["eall"] = (np.arange(N)[None, :] // 64 == np.arange(128)[:, None]).astype(np.float32)
    ql = np.arange(128)[:, None] // 64
    j = np.arange(254)[None, :] - 126
    c["bv"] = (j <= ql - 2).astype(np.float32)
    c["bf"] = (np.where((j == ql) | (j == ql - 1), 1e9, 0.0) + np.where(j > ql, -1.0, 0.0)).astype(np.float32)
    selg = np.zeros((8, 6 * 64), np.float32)
    for r in range(6):
        selg[r, r * 64:(r + 1) * 64] = 1.0
    c["selg"] = selg
    c["tri64"] = (np.arange(64)[:, None] <= np.arange(64)[None, :]).astype(np.float32)
    return c


CONST_SHAPES = {"ident": [128, 128], "rbd": [128, 128], "r32": [128, 32], "invf": [128, 2], "tri": [4, 128, 512],
                "win": [8, 128, 512], "cmpm": [5, 128, 512], "ovl": [4, 128, 129], "eall": [128, N], "bv": [128, 254],
                "bf": [128, 254], "selg": [8, 384], "tri64": [64, 64]}

IN_SHAPES = {
    "pos": ([1, N], I32),
    "lru_x": ([2, 128, N], F32), "lru_w": ([2, 2, 64, 64], F32), "lru_v": ([128, 8], F32),
    "mla_cq": ([192, N], F32), "mla_ckv": ([128, N], F32), "mla_kr": ([32, N], F32),
    "mla_wuq": ([192, 192], F32), "mla_wk": ([128, 128], F32), "mla_wv": ([128, 128], F32), "mla_v": ([128, 3], F32),
    "nsa_q": ([2, 128, N], F32), "nsa_k": ([3, 64, N], F32), "nsa_vc": ([64, N], F32), "nsa_vs": ([N, 64], F32),
    "nsa_vw": ([N, 64], F32), "nsa_g": ([6, N], F32), "nsa_pos": ([128, 32], F32), "nsa_w1": ([2, 2048, 256], F32),
    "nsa_b1": ([128, 4], F32), "nsa_w2": ([2, 256, 64], F32),
    "gla_q": ([64, N], F32), "gla_k": ([64, N], F32), "gla_v": ([N, 128], F32), "gla_glr": ([16, N], F32),
    "gla_og": ([128, N], F32), "gla_wg2": ([16, 64], F32), "gla_v2": ([128, 2], F32),
}


class Ctx:
    pass


class YView:
    def __init__(self, ap):
        self.ap = ap

    def __getitem__(self, key):
        rs, cs = key
        half = cs.start // 4096
        assert (cs.stop - 1) // 4096 == half
        return self.ap[half * 512 + rs.start:half * 512 + rs.stop, cs.start - half * 4096:cs.stop - half * 4096]


SEL = [("a_x", 128, False, 128), ("a_gate", 128, False, 128), ("b_q", 128, False, 128), ("b_q", 128, True, 128),
       ("b_gate", 6, False, 6), ("c_q", 64, False, 64), ("c_k", 64, False, 64), ("c_og", 128, False, 128)]
SELROWS = sum(x[3] for x in SEL)


class Loader:
    def __init__(self, S, GP, GV, MYP, MYV):
        self.S = S
        self.GP, self.GV, self.MYP, self.MYV = GP, GV, MYP, MYV
        self.row0 = {}
        r = 0
        for nm, hpm, inv, n in SEL:
            self.row0[(nm, inv)] = r
            r += n

    @staticmethod
    def gprow(r, half):
        return (r // 128) * 256 + half * 128 + r % 128

    @staticmethod
    def gvrow(t):
        half, tl = t // 4096, t % 4096
        return (tl // 1024) * 2048 + half * 1024 + tl % 1024

    def select(self, names=None, with_v=True):
        S = self.S
        i = 0
        for nm, hpm, inv, n in SEL:
            if names is not None and nm not in names:
                i += 2
                continue
            r0 = self.row0[(nm, inv)]
            mult = 256 if hpm == 128 else hpm
            for hf in range(2):
                q = "sp" if i % 2 == 0 else "act"
                i += 1

                def emit(e, nm=nm, mult=mult, inv=inv, n=n, r0=r0, hf=hf, q=q):
                    hp = S.rt["hp_" + q]
                    start = ((1 - hp) if inv else hp) * mult + self.gprow(OFF[nm], hf)
                    return e.dma_start(out=self.MYP[r0:r0 + n, hf * 4096:(hf + 1) * 4096], in_=self.GP[bass.ds(start, n), :])
                S.dma(q, emit, writes=[f"MYP{i}"])
        for j in (range(4) if with_v else ()):
            S.dma("sp", lambda e, j=j: e.dma_start(out=self.MYV[j * 2048:(j + 1) * 2048, :],
                                                   in_=self.GV[:, bass.ds(S.rt["hp_sp"] * 128 + 128, 128)][j * 2048:(j + 1) * 2048, :]), writes=[f"MYV{j}"])
        S.barrier()

    def pt(self, off, hpm, n, c0, c1, inv=False):
        if hpm == 0:
            half = c0 // 4096
            assert off // 128 == (off + n - 1) // 128
            base = self.gprow(off, half)
            return self.GP[base:base + n, c0 - half * 4096:c1 - half * 4096]
        for nm, hm, iv, nn in SEL:
            if hm == hpm and iv == inv and OFF[nm] <= off and (off - OFF[nm]) + n <= nn:
                r0 = self.row0[(nm, inv)] + (off - OFF[nm])
                return self.MYP[r0:r0 + n, c0:c1]
        raise KeyError((off, hpm, n, inv))

    def pv(self, coff, hpm, n, t0, t1):
        assert t0 // 1024 == (t1 - 1) // 1024
        g0 = self.gvrow(t0)
        if hpm == 0:
            return self.GV[g0:g0 + (t1 - t0), coff:coff + n]
        assert coff == 128 and n == 128
        return self.MYV[g0:g0 + (t1 - t0), :]


def common_setup(kb, D):
    S = kb.S
    c = Ctx()
    c.ident = kb.alloc(128)
    c.ones_f = kb.alloc(128)
    c.ones_bf = kb.alloc(128, BF16)
    c.ident_bf = kb.alloc(128, BF16)
    S.dma("sp", lambda e: e.dma_start(out=c.ident, in_=D["ident"][:, :]), writes=["ident"])
    S.op("pool", lambda e: e.memset(c.ones_f, 1.0), writes=["ones_f"])
    S.op("pool", lambda e: e.memset(c.ones_bf, 1.0), writes=["ones_bf"])
    S.op("act", lambda e: e.copy(out=c.ident_bf, in_=c.ident), reads=["ident"], writes=["ident_bf"])
    c.invf = kb.alloc(2)
    S.dma("sp", lambda e: e.dma_start(out=c.invf, in_=D["invf"][:, :]), writes=["invf"])
    return c


def rope_tables(kb, c, posf, posk, r0, r1, col, T, bank, tag):
    S = kb.S
    P = kb.psb[bank]
    n = r1 - r0
    rs = slice(r0, r1)
    a, kf, ki, sn, cs = T["ang"], T["kf"], T["ki"], T["sin"], T["cos"]
    S.op("pe", lambda e: e.matmul(P[rs, :], lhsT=c.ones_f[0:1, 0:n], rhs=posf[0:1, :], start=True, stop=True),
         reads=["ones_f", posk], writes=[pk(bank)])
    S.op("dve", lambda e: e.tensor_scalar(out=a[rs, :], in0=P[rs, :], scalar1=c.invf[rs, col:col + 1], scalar2=None, op0=ALU.mult),
         reads=["invf"], writes=[pk(bank), tag + "ang"])
    S.op("dve", lambda e: e.tensor_scalar(out=ki[rs, :], in0=a[rs, :], scalar1=1.0 / TWO_PI, scalar2=None, op0=ALU.mult),
         reads=[tag + "ang"], writes=[tag + "ki"])
    S.op("dve", lambda e: e.tensor_copy(out=kf[rs, :], in_=ki[rs, :]), reads=[tag + "ki"], writes=[tag + "kf"])
    S.op("dve", lambda e: e.scalar_tensor_tensor(out=a[rs, :], in0=kf[rs, :], scalar=-TWO_PI, in1=a[rs, :], op0=ALU.mult, op1=ALU.add),
         reads=[tag + "kf"], writes=[tag + "ang"])
    S.op("dve", lambda e: e.tensor_scalar(out=kf[rs, :], in0=a[rs, :], scalar1=PI, scalar2=-TWO_PI, op0=ALU.is_gt, op1=ALU.mult),
         reads=[tag + "ang"], writes=[tag + "kf"])
    S.op("dve", lambda e: e.tensor_tensor(out=sn[rs, :], in0=a[rs, :], in1=kf[rs, :], op=ALU.add),
         reads=[tag + "ang", tag + "kf"], writes=[tag + "sin"])
    S.op("dve", lambda e: e.tensor_scalar(out=a[rs, :], in0=a[rs, :], scalar1=PI / 2, scalar2=None, op0=ALU.add),
         reads=[], writes=[tag + "ang"])
    S.op("dve", lambda e: e.tensor_scalar(out=kf[rs, :], in0=a[rs, :], scalar1=PI, scalar2=-TWO_PI, op0=ALU.is_gt, op1=ALU.mult),
         reads=[tag + "ang"], writes=[tag + "kf"])
    S.op("dve", lambda e: e.tensor_tensor(out=cs[rs, :], in0=a[rs, :], in1=kf[rs, :], op=ALU.add),
         reads=[tag + "ang", tag + "kf"], writes=[tag + "cos"])
    S.op("act", lambda e: e.activation(out=sn[rs, :], in_=sn[rs, :], func=AF.Sin), writes=[tag + "sin"])
    S.op("act", lambda e: e.activation(out=cs[rs, :], in_=cs[rs, :], func=AF.Sin), writes=[tag + "cos"])


def alloc_tables(kb):
    return {"ang": kb.alloc(512), "kf": kb.alloc(512), "ki": kb.alloc(512, I32), "sin": kb.alloc(512), "cos": kb.alloc(512)}


def load_pos(kb, D, posi, posf, t):
    S = kb.S
    S.dma("sp", lambda e: e.dma_start(out=posi[0:1, :], in_=D["pos"][0:1, t * TQ:(t + 1) * TQ]), writes=["posi"])
    S.op("dve", lambda e: e.tensor_copy(out=posf[0:1, :], in_=posi[0:1, :]), reads=["posi"], writes=["posf"])


class Attn:
    def __init__(self, kb, sbanks=(0, 1), npt=3):
        self.kb = kb
        self.sbanks = sbanks
        self.PT = [kb.alloc(512, BF16) for _ in range(npt)]
        self.pti = 0
        self.si = 0

    def run(self, blocks, obank, defer=None):
        kb = self.kb
        S = kb.S
        n = len(blocks)
        O = kb.psb[obank]
        banks = []

        def scores(i):
            bank = self.sbanks[self.si % len(self.sbanks)]
            self.si += 1
            banks.append(bank)
            mms = blocks[i][0]
            for j, (l, r, ks) in enumerate(mms):
                S.op("pe", lambda e, l=l, r=r, j=j, bank=bank, nm=len(mms): e.matmul(kb.psb[bank][:, :], lhsT=l, rhs=r, start=(j == 0), stop=(j == nm - 1)),
                     reads=ks, writes=[pk(bank)])

        scores(0)
        for i in range(n):
            if i + 1 < n:
                scores(i + 1)
            bank = banks[i]
            pi_ = self.pti % len(self.PT)
            self.pti += 1
            pt = self.PT[pi_]
            S.op("act", lambda e, pt=pt, bank=bank: e.activation(out=pt, in_=kb.psb[bank][:, :], func=AF.Exp), writes=[pk(bank), f"PT{pi_}"])
            v, vk = blocks[i][1], blocks[i][2]
            S.op("pe", lambda e, v=v, pt=pt, i=i: e.matmul(O[0:128, :], lhsT=v, rhs=pt, start=(i == 0), stop=(i == n - 1)),
                 reads=[f"PT{pi_}"] + vk, writes=[pk(obank)])
            if defer and i == min(2, n - 1):
                for f in defer:
                    f()
                del defer[:]


def norm_coef(kb, c, obank, rowbuf, bcbank, bcs):
    S = kb.S
    O = kb.psb[obank]
    B = kb.psb[bcbank]
    S.op("dve", lambda e: e.tensor_scalar_max(out=rowbuf[64:65, :], in0=O[64:65, :], scalar1=1e-30), writes=[pk(obank), "rowbuf"])
    S.op("dve", lambda e: e.reciprocal(out=rowbuf[64:65, :], in_=rowbuf[64:65, :]), writes=["rowbuf"])
    S.op("pe", lambda e: e.matmul(B[0:64, :], lhsT=c.ones_f[64:65, 0:64], rhs=rowbuf[64:65, :], start=True, stop=True),
         reads=["rowbuf", "ones_f"], writes=[pk(bcbank)])
    S.op("act", lambda e: e.copy(out=bcs[0:64, :], in_=B[0:64, :]), writes=[pk(bcbank), "bcs"])


def part_lru(kb, c, D, yT, L):
    S = kb.S
    m0 = kb.mark()
    xa = kb.alloc(N + 4)
    xc = kb.alloc(N)
    A = kb.alloc(N)
    U = kb.alloc(N)
    G = kb.alloc(N)
    xcb = kb.alloc(N, BF16)
    vec = kb.alloc(16)
    wtmp = kb.alloc(256)
    wbd = kb.alloc(256, BF16)
    T1 = xa[:, 0:N]
    P = kb.psb
    S.op("dve", lambda e: e.memset(xa[:, 0:3], 0.0), writes=["xa_pad"])
    for hf in range(2):
        S.dma("sp", lambda e, hf=hf: e.dma_start(out=xa[:, 3 + hf * 4096:3 + (hf + 1) * 4096], in_=L.pt(OFF["a_x"], 128, 128, hf * 4096, (hf + 1) * 4096)), writes=["xa"])
        S.dma("sp", lambda e, hf=hf: e.dma_start(out=G[:, hf * 4096:(hf + 1) * 4096], in_=L.pt(OFF["a_gate"], 128, 128, hf * 4096, (hf + 1) * 4096)), writes=["G"])
    S.dma("sp", lambda e: e.dma_start(out=vec[:, 0:8], in_=D["lru_v"][:, :]), writes=["vec"])
    S.op("dve", lambda e: e.memset(wtmp, 0.0), writes=["wtmp"])
    for a in range(2):
        for b in range(2):
            S.dma("sp", lambda e, a=a, b=b: e.dma_start(out=wtmp[b * 64:(b + 1) * 64, a * 128 + b * 64:a * 128 + (b + 1) * 64], in_=D["lru_w"][a, b]),
                  writes=["wtmp"])
    S.op("act", lambda e: e.copy(out=wbd, in_=wtmp), reads=["wtmp"], writes=["wbd"])
    S.op("act", lambda e: e.activation(out=vec[:, 8:9], in_=vec[:, 7:8], func=AF.Exp, scale=-1.0), reads=["vec"], writes=["vec8"])
    S.op("act", lambda e: e.activation(out=vec[:, 8:9], in_=vec[:, 8:9], func=AF.Ln, bias=1.0), writes=["vec8"])
    S.op("dve", lambda e: e.tensor_scalar(out=vec[:, 9:10], in0=vec[:, 8:9], scalar1=-8.0, scalar2=None, op0=ALU.mult), reads=["vec8"], writes=["vec9"])
    S.op("dve", lambda e: e.tensor_scalar(out=xc, in0=xa[:, 0:N], scalar1=vec[:, 0:1], scalar2=vec[:, 4:5], op0=ALU.mult, op1=ALU.add),
         reads=["xa", "xa_pad", "vec"], writes=["xc"])
    for j in range(1, 4):
        S.op("dve", lambda e, j=j: e.scalar_tensor_tensor(out=xc, in0=xa[:, j:j + N], scalar=vec[:, j:j + 1], in1=xc, op0=ALU.mult, op1=ALU.add),
             reads=["xa", "xa_pad", "vec"], writes=["xc"])
    S.op("act", lambda e: e.copy(out=xcb, in_=xc), reads=["xc"], writes=["xcb"])
    allA = [f"A{t}" for t in range(16)]
    allU = [f"U{t}" for t in range(16)]
    for t in range(16):
        ts = slice(t * 512, (t + 1) * 512)
        b0, b1 = 2 * (t % 2), 2 * (t % 2) + 1
        S.op("pe", lambda e, ts=ts, b0=b0: e.matmul(P[b0][:, :], lhsT=wbd[:, 0:128], rhs=xcb[:, ts], start=True, stop=True),
             reads=["xcb", "wbd"], writes=[pk(b0)])
        S.op("pe", lambda e, ts=ts, b1=b1: e.matmul(P[b1][:, :], lhsT=wbd[:, 128:256], rhs=xcb[:, ts], start=True, stop=True),
             reads=["xcb", "wbd"], writes=[pk(b1)])
        S.op("act", lambda e, ts=ts, b0=b0: e.activation(out=A[:, ts], in_=P[b0][:, :], func=AF.Sigmoid, bias=vec[:, 5:6]),
             reads=["vec"], writes=[pk(b0), f"A{t}"])
        S.op("act", lambda e, ts=ts, b1=b1: e.activation(out=U[:, ts], in_=P[b1][:, :], func=AF.Sigmoid, bias=vec[:, 6:7]),
             reads=["vec"], writes=[pk(b1), f"U{t}"])
    NP = 4
    W_ = N // NP
    for p_ in range(NP):
        cs = slice(p_ * W_, (p_ + 1) * W_)
        tA = [f"A{t}" for t in range(16) if p_ * W_ <= t * 512 < (p_ + 1) * W_]
        tU = [f"U{t}" for t in range(16) if p_ * W_ <= t * 512 < (p_ + 1) * W_]
        kA, kU, kT, kG, kH = f"Ap{p_}", f"Up{p_}", f"Tp{p_}", f"Gp{p_}", f"Hp{p_}"
        S.op("act", lambda e, cs=cs: e.activation(out=A[:, cs], in_=A[:, cs], func=AF.Exp, scale=vec[:, 9:10]), reads=["vec9"], writes=[kA] + tA)
        S.op("act", lambda e, cs=cs: e.activation(out=T1[:, cs], in_=A[:, cs], func=AF.Square), reads=[kA, "xc"], writes=[kT, "xa", "xa_pad"])
        S.op("act", lambda e, cs=cs: e.activation(out=T1[:, cs], in_=T1[:, cs], func=AF.Sqrt, bias=1.0, scale=-1.0), writes=[kT])
        S.op("dve", lambda e, cs=cs: e.tensor_tensor(out=U[:, cs], in0=U[:, cs], in1=xc[:, cs], op=ALU.mult), reads=["xc"], writes=[kU] + tU)
        S.op("dve", lambda e, cs=cs: e.tensor_tensor(out=U[:, cs], in0=U[:, cs], in1=T1[:, cs], op=ALU.mult), reads=[kT], writes=[kU])
    for p_ in range(NP):
        cs = slice(p_ * W_, (p_ + 1) * W_)
        init = 0.0 if p_ == 0 else xc[:, p_ * W_ - 1:p_ * W_]
        S.op("dve", lambda e, cs=cs, init=init: e.tensor_tensor_scan(out=xc[:, cs], data0=A[:, cs], data1=U[:, cs], initial=init, op0=ALU.mult, op1=ALU.add),
             reads=[f"Ap{p_}", f"Up{p_}"] + [f"Up{q_}" for q_ in range(NP)], writes=["xc", f"Hp{p_}"])
    for p_ in range(NP):
        cs = slice(p_ * W_, (p_ + 1) * W_)
        kT, kG = f"Tp{p_}", f"Gp{p_}"
        S.op("act", lambda e, cs=cs: e.activation(out=T1[:, cs], in_=G[:, cs], func=AF.Square), reads=["G", f"Up{p_}"], writes=[kT])
        S.op("dve", lambda e, cs=cs: e.tensor_scalar(out=T1[:, cs], in0=T1[:, cs], scalar1=0.044715, scalar2=1.0, op0=ALU.mult, op1=ALU.add), writes=[kT])
        S.op("dve", lambda e, cs=cs: e.tensor_tensor(out=T1[:, cs], in0=T1[:, cs], in1=G[:, cs], op=ALU.mult), reads=["G"], writes=[kT])
        S.op("act", lambda e, cs=cs: e.activation(out=T1[:, cs], in_=T1[:, cs], func=AF.Sigmoid, scale=1.5957691216057308), writes=[kT])
        S.op("dve", lambda e, cs=cs: e.tensor_tensor(out=G[:, cs], in0=G[:, cs], in1=T1[:, cs], op=ALU.mult), reads=[kT], writes=[kG])
        S.op("dve", lambda e, cs=cs: e.tensor_tensor(out=A[:, cs], in0=xc[:, cs], in1=G[:, cs], op=ALU.mult), reads=[f"Hp{p_}", kG], writes=[f"Ap{p_}"])
        for hf in range(2):
            if hf * 4096 >= p_ * W_ and (hf + 1) * 4096 <= (p_ + 1) * W_ or (p_ * W_ >= hf * 4096 and (p_ + 1) * W_ <= (hf + 1) * 4096):
                c0, c1 = max(hf * 4096, p_ * W_), min((hf + 1) * 4096, (p_ + 1) * W_)
                S.dma("sp", lambda e, c0=c0, c1=c1: e.dma_start(out=yT[0:128, c0:c1], in_=A[:, c0:c1]), reads=[f"Ap{p_}"])
    S.barrier()
    kb.release(m0)


def part_mla(kb, c, D, yT, L):
    S = kb.S
    m0 = kb.mark()
    P = kb.psb
    SC = float(96 ** -0.5)
    QD = [kb.alloc(N, BF16) for _ in range(2)]
    KD = [kb.alloc(N, BF16) for _ in range(2)]
    VD = kb.alloc(64 * 2 * 128, BF16).rearrange("p (b h d) -> p b h d", b=64, h=2)
    tri = kb.alloc(4 * 512, BF16).rearrange("p (i q) -> p i q", i=4)
    wuq = kb.alloc(2 * 192, BF16).rearrange("p (c n) -> p c n", c=2)
    wk = kb.alloc(128, BF16)
    wv = kb.alloc(128, BF16)
    vec = kb.alloc(4)
    r32 = kb.alloc(32)
    st = kb.alloc(512)
    S.op("pool", lambda e: e.memset(VD, 0.0), writes=["VD"])
    S.op("pool", lambda e: e.memset(VD[:, :, :, 64:65], 1.0), writes=["VD"])
    S.dma("sp", lambda e: e.dma_start(out=vec[:, 0:3], in_=D["mla_v"][:, :]), writes=["mvec"])
    S.dma("sp", lambda e: e.dma_start(out=r32, in_=D["r32"][:, :]), writes=["r32"])
    for i in range(4):
        S.dma("sp", lambda e, i=i: e.dma_start(out=st, in_=D["tri"][i]), writes=["st"])
        S.op("act", lambda e, i=i: e.copy(out=tri[:, i, :], in_=st), reads=["st"], writes=["tri"])
    S.dma("sp", lambda e: e.dma_start(out=st[:, 0:192], in_=D["mla_wuq"][0:128, :]), writes=["st"])
    S.op("act", lambda e: e.copy(out=wuq[:, 0, :], in_=st[:, 0:192]), reads=["st"], writes=["wuq"])
    S.dma("sp", lambda e: e.dma_start(out=st[0:64, 0:192], in_=D["mla_wuq"][128:192, :]), writes=["st"])
    S.op("act", lambda e: e.copy(out=wuq[0:64, 1, :], in_=st[0:64, 0:192]), reads=["st"], writes=["wuq"])
    S.dma("sp", lambda e: e.dma_start(out=st[:, 0:128], in_=D["mla_wk"][:, :]), writes=["st"])
    S.op("act", lambda e: e.copy(out=wk, in_=st[:, 0:128]), reads=["st"], writes=["wk"])
    S.dma("sp", lambda e: e.dma_start(out=st[:, 0:128], in_=D["mla_wv"][:, :]), writes=["st"])
    S.op("act", lambda e: e.copy(out=wv, in_=st[:, 0:128]), reads=["st"], writes=["wv"])

    m1 = kb.mark()
    cq0 = kb.alloc(512)
    cq1 = kb.alloc(512)
    ckv = kb.alloc(512)
    krt = kb.alloc(512)
    sq0 = kb.alloc(512, BF16)
    sq1 = kb.alloc(512, BF16)
    cn0 = kb.alloc(512, BF16)
    cn1 = kb.alloc(512, BF16)
    ckn = kb.alloc(512, BF16)
    rstd = kb.alloc(512)
    qr = kb.alloc(512)
    t1 = kb.alloc(512)
    t2 = kb.alloc(512)
    posi = kb.alloc(512, I32)
    posf = kb.alloc(512)
    T = alloc_tables(kb)
    R = slice(64, 96)
    for t in range(NQT):
        ts = slice(t * TQ, (t + 1) * TQ)
        load_pos(kb, D, posi, posf, t)
        S.dma("sp", lambda e, ts=ts: e.dma_start(out=cq0, in_=L.pt(OFF["d_cq"], 0, 128, ts.start, ts.stop)), writes=["cq0"])
        S.dma("sp", lambda e, ts=ts: e.dma_start(out=cq1[0:64, :], in_=L.pt(OFF["d_cq"] + 128, 0, 64, ts.start, ts.stop)), writes=["cq1"])
        S.dma("sp", lambda e, ts=ts: e.dma_start(out=ckv, in_=L.pt(OFF["d_ckv"], 0, 128, ts.start, ts.stop)), writes=["ckv"])
        S.dma("sp", lambda e, ts=ts: e.dma_start(out=krt[R, :], in_=L.pt(OFF["d_kr"], 0, 32, ts.start, ts.stop)), writes=["krt"])
        rope_tables(kb, c, posf, "posf", 64, 96, 1, T, 6, "m")
        S.op("act", lambda e: e.activation(out=sq0, in_=cq0, func=AF.Square), reads=["cq0"], writes=["sq0"])
        S.op("act", lambda e: e.activation(out=sq1[0:64, :], in_=cq1[0:64, :], func=AF.Square), reads=["cq1"], writes=["sq1"])
        S.op("pe", lambda e: e.matmul(P[0][:, :], lhsT=c.ones_bf, rhs=sq0, start=True, stop=False), reads=["sq0", "ones_bf"], writes=[pk(0)])
        S.op("pe", lambda e: e.matmul(P[0][:, :], lhsT=c.ones_bf[0:64, :], rhs=sq1[0:64, :], start=False, stop=True), reads=["sq1", "ones_bf"], writes=[pk(0)])
        S.op("act", lambda e: e.activation(out=rstd, in_=P[0][:, :], func=AF.Sqrt, bias=EPS, scale=1.0 / 192.0), writes=[pk(0), "rstd"])
        S.op("dve", lambda e: e.reciprocal(out=rstd, in_=rstd), writes=["rstd"])
        S.op("dve", lambda e: e.scalar_tensor_tensor(out=cn0, in0=cq0, scalar=vec[:, 0:1], in1=rstd, op0=ALU.mult, op1=ALU.mult),
             reads=["cq0", "rstd", "mvec"], writes=["cn0"])
        S.op("dve", lambda e: e.scalar_tensor_tensor(out=cn1[0:64, :], in0=cq1[0:64, :], scalar=vec[0:64, 1:2], in1=rstd[0:64, :], op0=ALU.mult, op1=ALU.mult),
             reads=["cq1", "rstd", "mvec"], writes=["cn1"])
        for h in range(2):
            hs = slice(h * 96, (h + 1) * 96)
            S.op("pe", lambda e, hs=hs: e.matmul(P[1][0:96, :], lhsT=wuq[:, 0, hs], rhs=cn0, start=True, stop=False), reads=["cn0", "wuq"], writes=[pk(1)])
            S.op("pe", lambda e, hs=hs: e.matmul(P[1][0:96, :], lhsT=wuq[0:64, 1, hs], rhs=cn1[0:64, :], start=False, stop=True), reads=["cn1", "wuq"], writes=[pk(1)])
            S.op("act", lambda e, h=h, ts=ts: e.mul(out=QD[h][0:64, ts], in_=P[1][0:64, :], mul=SC), writes=[pk(1), f"QD{h}"])
            S.op("dve", lambda e: e.tensor_copy(out=qr[R, :], in_=P[1][R, :]), writes=[pk(1), "qr"])
            S.op("pe", lambda e: e.matmul(P[2][R, :], lhsT=r32[R, 0:32], rhs=qr[R, :], start=True, stop=True), reads=["qr", "r32"], writes=[pk(2)])
            S.op("dve", lambda e: e.scalar_tensor_tensor(out=t1[R, :], in0=qr[R, :], scalar=SC, in1=T["cos"][R, :], op0=ALU.mult, op1=ALU.mult),
                 reads=["qr", "mcos"], writes=["t1"])
            S.op("dve", lambda e: e.scalar_tensor_tensor(out=t2[R, :], in0=P[2][R, :], scalar=SC, in1=T["sin"][R, :], op0=ALU.mult, op1=ALU.mult),
                 reads=["msin"], writes=[pk(2), "t2"])
            S.op("dve", lambda e, h=h, ts=ts: e.tensor_tensor(out=QD[h][R, ts], in0=t1[R, :], in1=t2[R, :], op=ALU.add), reads=["t1", "t2"], writes=[f"QD{h}"])
        S.op("act", lambda e: e.activation(out=sq0, in_=ckv, func=AF.Square), reads=["ckv"], writes=["sq0"])
        S.op("pe", lambda e: e.matmul(P[7][:, :], lhsT=c.ones_bf, rhs=sq0, start=True, stop=True), reads=["sq0", "ones_bf"], writes=[pk(7)])
        S.op("act", lambda e: e.activation(out=rstd, in_=P[7][:, :], func=AF.Sqrt, bias=EPS, scale=1.0 / 128.0), writes=[pk(7), "rstd"])
        S.op("dve", lambda e: e.reciprocal(out=rstd, in_=rstd), writes=["rstd"])
        S.op("dve", lambda e: e.scalar_tensor_tensor(out=ckn, in0=ckv, scalar=vec[:, 2:3], in1=rstd, op0=ALU.mult, op1=ALU.mult),
             reads=["ckv", "rstd", "mvec"], writes=["ckn"])
        for h in range(2):
            S.op("pe", lambda e, h=h: e.matmul(P[3][0:64, :], lhsT=wk[:, h * 64:(h + 1) * 64], rhs=ckn, start=True, stop=True), reads=["ckn", "wk"], writes=[pk(3)])
            S.op("act", lambda e, h=h, ts=ts: e.copy(out=KD[h][0:64, ts], in_=P[3][0:64, :]), writes=[pk(3), f"KD{h}"])
        for tb in range(4):
            S.op("pe", lambda e, tb=tb: e.matmul(P[4][:, 0:128], lhsT=ckn[:, tb * 128:(tb + 1) * 128], rhs=wv, start=True, stop=True), reads=["ckn", "wv"], writes=[pk(4)])
            S.op("act", lambda e, tb=tb, t=t: e.copy(out=VD[:, 4 * t + tb, :, 0:64], in_=P[4][:, 0:128].rearrange("p (h d) -> p h d", h=2)),
                 writes=[pk(4), "VD"])
        S.op("pe", lambda e: e.matmul(P[5][R, :], lhsT=r32[R, 0:32], rhs=krt[R, :], start=True, stop=True), reads=["krt", "r32"], writes=[pk(5)])
        S.op("dve", lambda e: e.tensor_tensor(out=t1[R, :], in0=krt[R, :], in1=T["cos"][R, :], op=ALU.mult), reads=["krt", "mcos"], writes=["t1"])
        S.op("dve", lambda e: e.tensor_tensor(out=t2[R, :], in0=P[5][R, :], in1=T["sin"][R, :], op=ALU.mult), reads=["msin"], writes=[pk(5), "t2"])
        for h in range(2):
            S.op("dve", lambda e, h=h, ts=ts: e.tensor_tensor(out=KD[h][R, ts], in0=t1[R, :], in1=t2[R, :], op=ALU.add), reads=["t1", "t2"], writes=[f"KD{h}"])
    S.barrier()
    kb.release(m1)
    att = Attn(kb, sbanks=(0, 1))
    rowbuf = kb.alloc(512)
    bcs = kb.alloc(512)
    yst = [kb.alloc(512), kb.alloc(512)]
    it = 0
    pending = []
    for qt in range(NQT):
        qs = slice(qt * TQ, (qt + 1) * TQ)
        for h in range(2):
            blocks = []
            for kbk in range(4 * qt + 4):
                mms = [(KD[h][0:96, kbk * 128:(kbk + 1) * 128], QD[h][0:96, qs], [f"KD{h}", f"QD{h}"])]
                if kbk >= 4 * qt:
                    mms.append((c.ident_bf, tri[:, kbk - 4 * qt, :], ["ident_bf", "tri"]))
                blocks.append((mms, VD[:, kbk, h, :], ["VD"]))
            ob = 2 + it % 2
            att.run(blocks, ob, defer=pending)

            def fin(ob=ob, ys=yst[it % 2], yk=f"yst{it % 2}", h=h, qs=qs):
                norm_coef(kb, c, ob, rowbuf, 4, bcs)
                S.op("dve", lambda e: e.tensor_tensor(out=ys[0:64, :], in0=P[ob][0:64, :], in1=bcs[0:64, :], op=ALU.mult),
                     reads=["bcs"], writes=[pk(ob), yk])
                S.dma("sp", lambda e: e.dma_start(out=yT[384 + h * 64:384 + (h + 1) * 64, qs], in_=ys[0:64, :]), reads=[yk])
            pending.append(fin)
            it += 1
    for f in pending:
        f()
    S.barrier()
    kb.release(m0)


def load_cast(kb, src_ap, dst_ap, stage, skey, dkey, eng="act", rows=slice(0, 128)):
    S = kb.S
    S.dma("sp", lambda e: e.dma_start(out=stage, in_=(src_ap() if callable(src_ap) else src_ap)), writes=[skey])
    if eng == "act":
        S.op("act", lambda e: e.copy(out=dst_ap, in_=stage), reads=[skey], writes=[dkey])
    else:
        S.op("pool", lambda e: e.tensor_copy(out=dst_ap, in_=stage), reads=[skey], writes=[dkey])


def gelu_tanh(kb, z, u, out, zk, uk, outk):
    S = kb.S
    S.op("pool", lambda e: e.tensor_tensor(out=u, in0=z, in1=z, op=ALU.mult), reads=[zk], writes=[uk])
    S.op("pool", lambda e: e.tensor_scalar(out=u, in0=u, scalar1=0.044715, scalar2=1.0, op0=ALU.mult, op1=ALU.add), writes=[uk])
    S.op("pool", lambda e: e.tensor_tensor(out=u, in0=u, in1=z, op=ALU.mult), reads=[zk], writes=[uk])
    S.op("act", lambda e: e.activation(out=u, in_=u, func=AF.Sigmoid, scale=1.5957691216057308), writes=[uk])
    S.op("dve", lambda e: e.tensor_tensor(out=out, in0=z, in1=u, op=ALU.mult), reads=[zk, uk], writes=[outk])


def part_nsa(kb, c, D, yT, L):
    S = kb.S
    P = kb.psb
    m0 = kb.mark()
    Qm = kb.alloc(N, BF16)
    Qo = kb.alloc(N, BF16)
    KSA = kb.alloc(N, BF16)
    KSB = kb.alloc(N, BF16)
    KW2 = kb.alloc(N, BF16)
    tri = kb.alloc(4 * 512, BF16).rearrange("p (i q) -> p i q", i=4)
    win = kb.alloc(8 * 512, BF16).rearrange("p (i q) -> p i q", i=8)
    cmpm = kb.alloc(5 * 512, BF16).rearrange("p (i q) -> p i q", i=5)
    rbd = kb.alloc(128)
    ovl = kb.alloc(4 * 130, BF16).rearrange("p (i q) -> p i q", i=4)
    bv = kb.alloc(254)
    bf = kb.alloc(254)
    selg = kb.alloc(384)
    KCA = kb.alloc(512, BF16)
    KCB = kb.alloc(512, BF16)
    VCMP = kb.alloc(4 * 128, BF16).rearrange("p (b d) -> p b d", b=4)
    st = kb.alloc(512)
    st3 = st.rearrange("p (b d) -> p b d", d=64)
    S.op("pool", lambda e: e.memset(KSA[64:128, :], 0.0), writes=["ksz"])
    S.op("pool", lambda e: e.memset(KSB[0:64, :], 0.0), writes=["ksz"])
    S.op("pool", lambda e: e.memset(KCA[64:128, :], 0.0), writes=["kcz"])
    S.op("pool", lambda e: e.memset(KCB[0:64, :], 0.0), writes=["kcz"])
    S.op("pool", lambda e: e.memset(VCMP, 0.0), writes=["VCMP"])
    S.op("pool", lambda e: e.memset(VCMP[:, :, 64:65], 1.0), writes=["VCMP"])
    S.dma("sp", lambda e: e.dma_start(out=rbd, in_=D["rbd"][:, :]), writes=["rbd"])
    S.dma("sp", lambda e: e.dma_start(out=bv, in_=D["bv"][:, :]), writes=["bv"])
    S.dma("sp", lambda e: e.dma_start(out=bf, in_=D["bf"][:, :]), writes=["bf"])
    S.dma("sp", lambda e: e.dma_start(out=selg[0:8, :], in_=D["selg"][:, :]), writes=["selg"])
    i = 0
    for nm, dst, cnt in (("tri", tri, 4), ("win", win, 8), ("cmpm", cmpm, 5)):
        for j in range(cnt):
            load_cast(kb, D[nm][j], dst[:, j, :], st[:, 0:512], "st", nm, "act" if i % 2 == 0 else "pool")
            i += 1
    for j in range(4):
        load_cast(kb, D["ovl"][j], ovl[:, j, 0:129], st[:, 0:129], "st", "ovl", "act")

    m1 = kb.mark()
    KCV = kb.alloc(N)
    xs = [kb.alloc(512), kb.alloc(512)]
    t1s = [kb.alloc(512), kb.alloc(512)]
    t2s = [kb.alloc(512), kb.alloc(512)]
    posi = kb.alloc(512, I32)
    posf = kb.alloc(512)
    T = alloc_tables(kb)
    for hf in range(2):
        S.dma("sp", lambda e, hf=hf: e.dma_start(out=KCV[64:128, hf * 4096:(hf + 1) * 4096], in_=L.pt(OFF["b_kv"] + 64, 0, 64, hf * 4096, (hf + 1) * 4096)), writes=["KCVv"])
    flat = []
    for t in range(NQT):
        ts = slice(t * TQ, (t + 1) * TQ)
        a0, a1 = ts.start, ts.stop
        flat += [(t, ts, "q0", [lambda a0=a0, a1=a1: L.pt(OFF["b_q"], 128, 128, a0, a1)], Qm, 0.125, 128),
                 (t, ts, "q1", [lambda a0=a0, a1=a1: L.pt(OFF["b_q"], 128, 128, a0, a1, inv=True)], Qo, 0.125, 128),
                 (t, ts, "ks", [lambda a0=a0, a1=a1: L.pt(OFF["b_kv"] + 128, 0, 64, a0, a1)] * 2, None, 1.0, 128),
                 (t, ts, "kw", [lambda a0=a0, a1=a1: L.pt(OFF["b_kv"] + 192, 0, 64, a0, a1)] * 2, KW2, 1.0, 128),
                 (t, ts, "kc", [lambda a0=a0, a1=a1: L.pt(OFF["b_kv"], 0, 64, a0, a1)], KCV, 1.0, 64)]

    def emit_load(i):
        t, ts, nm, srcs, dst, sc, rows = flat[i]
        x = xs[i % 2]
        xk = f"xs{i % 2}"
        if len(srcs) == 2:
            S.dma("sp", lambda e: e.dma_start(out=x[0:64, :], in_=srcs[0]()), writes=[xk])
            S.dma("sp", lambda e: e.dma_start(out=x[64:128, :], in_=srcs[1]()), writes=[xk])
        else:
            S.dma("sp", lambda e: e.dma_start(out=x[0:rows, :], in_=srcs[0]()), writes=[xk])

    def emit_compute(i):
        t, ts, nm, srcs, dst, sc, rows = flat[i]
        x = xs[i % 2]
        xk = f"xs{i % 2}"
        t1 = t1s[i % 2]
        t2 = t2s[i % 2]
        bank = 4 + i % 2
        R = slice(0, rows)
        S.op("pe", lambda e: e.matmul(P[bank][R, :], lhsT=rbd[R, 0:rows], rhs=x[R, :], start=True, stop=True),
             reads=[xk, "rbd"], writes=[pk(bank)])
        S.op("dve", lambda e: e.scalar_tensor_tensor(out=t1[R, :], in0=x[R, :], scalar=sc, in1=T["cos"][R, :], op0=ALU.mult, op1=ALU.mult),
             reads=[xk, "ncos"], writes=[f"t1{i % 2}"])
        S.op("dve", lambda e: e.scalar_tensor_tensor(out=t2[R, :], in0=P[bank][R, :], scalar=sc, in1=T["sin"][R, :], op0=ALU.mult, op1=ALU.mult),
             reads=["nsin"], writes=[pk(bank), f"t2{i % 2}"])
        dk = "KCVk" if nm == "kc" else nm
        if nm == "ks":
            for dst_, RR in ((KSA, slice(0, 64)), (KSB, slice(64, 128))):
                S.op("pool", lambda e, RR=RR, dst_=dst_: e.tensor_tensor(out=dst_[RR, ts], in0=t1[RR, :], in1=t2[RR, :], op=ALU.add),
                     reads=[f"t1{i % 2}", f"t2{i % 2}"], writes=[dk])
        else:
            S.op("pool", lambda e: e.tensor_tensor(out=dst[R, ts], in0=t1[R, :], in1=t2[R, :], op=ALU.add),
                 reads=[f"t1{i % 2}", f"t2{i % 2}"], writes=[dk])

    emit_load(0)
    for i in range(len(flat)):
        if i % 5 == 0:
            load_pos(kb, D, posi, posf, flat[i][0])
            rope_tables(kb, c, posf, "posf", 0, 128, 0, T, 6, "n")
        if i + 1 < len(flat):
            emit_load(i + 1)
        emit_compute(i)
    S.barrier()
    kb.release(m1)
    KCV = kb.alloc(N)
    BLK = kb.alloc(32 * 512, BF16).rearrange("p (l n) -> p l n", l=32)
    W1 = kb.alloc(32 * 256, BF16).rearrange("p (l h) -> p l h", l=32)
    pos2 = kb.alloc(32)
    b1 = kb.alloc(4)
    w2 = kb.alloc(2 * 2 * 64, BF16).rearrange("p (k m d) -> p k m d", k=2, m=2)
    HID = kb.alloc(2 * 2 * 512, BF16).rearrange("p (k m n) -> p k m n", k=2, m=2)
    zt = kb.alloc(512)
    ut = kb.alloc(512)
    stw = kb.alloc(1024).rearrange("p (l h) -> p l h", l=4)
    S.dma("sp", lambda e: e.dma_start(out=pos2, in_=D["nsa_pos"][:, :]), writes=["pos2"])
    S.dma("sp", lambda e: e.dma_start(out=b1, in_=D["nsa_b1"][:, :]), writes=["b1"])
    for kv in range(2):
        load_cast(kb, D["nsa_w2"][kv].rearrange("(m p) d -> p m d", p=128), w2[:, kv, :, :], st[:, 0:128].rearrange("p (m d) -> p m d", m=2), "st", "w2", "act")
    for l0 in range(0, 32, 4):
        for kv in range(2):
            src = D["nsa_w1"][kv].rearrange("(l d) h -> d l h", d=64)[:, l0:l0 + 4, :]
            S.dma("sp", lambda e, src=src, kv=kv: e.dma_start(out=stw[kv * 64:(kv + 1) * 64, :, :], in_=src), writes=["stw"])
        if (l0 // 4) % 2 == 0:
            S.op("act", lambda e, l0=l0: e.copy(out=W1[:, l0:l0 + 4, :], in_=stw), reads=["stw"], writes=["W1"])
        else:
            S.op("pool", lambda e, l0=l0: e.tensor_copy(out=W1[:, l0:l0 + 4, :], in_=stw), reads=["stw"], writes=["W1"])
    S.op("pool", lambda e: e.memset(BLK[:, :, 511:512], 0.0), writes=["BLKpad"])
    K3 = KCV.rearrange("p (g r) -> p g r", r=16)
    for l in range(32):
        src = K3[:, 0:511, l] if l < 16 else K3[:, 1:512, l - 16]
        eng = "dve" if l % 2 == 0 else "pool"
        S.op(eng, lambda e, l=l, src=src: e.tensor_scalar(out=BLK[:, l, 0:511], in0=src, scalar1=pos2[:, l:l + 1], scalar2=None, op0=ALU.add),
             reads=["pos2"], writes=[f"BLK{l}"])
    for kv in range(2):
        R = slice(kv * 64, kv * 64 + 64)
        for m in range(2):
            bank = 2 * kv + m
            for l in range(32):
                S.op("pe", lambda e, l=l, R=R, m=m, bank=bank: e.matmul(P[bank][:, :], lhsT=W1[R, l, m * 128:(m + 1) * 128], rhs=BLK[R, l, :], start=(l == 0), stop=(l == 31)),
                     reads=["W1", f"BLK{l}", "BLKpad"], writes=[pk(bank)])
            S.op("act", lambda e, kv=kv, m=m, bank=bank: e.activation(out=zt, in_=P[bank][:, :], func=AF.Identity, bias=b1[:, kv * 2 + m:kv * 2 + m + 1]),
                 reads=["b1"], writes=[pk(bank), "zt"])
            gelu_tanh(kb, zt, ut, HID[:, kv, m, :], "zt", "ut", f"HID{kv}")
    for m in range(2):
        S.op("pe", lambda e, m=m: e.matmul(P[4][0:64, :], lhsT=w2[:, 0, m, :], rhs=HID[:, 0, m, :], start=(m == 0), stop=(m == 1)), reads=["w2", "HID0"], writes=[pk(4)])
    S.op("act", lambda e: e.copy(out=KCA[0:64, :], in_=P[4][0:64, :]), writes=[pk(4), "KCMP2"])
    S.op("dve", lambda e: e.tensor_copy(out=KCB[64:128, :], in_=P[4][0:64, :]), writes=[pk(4), "KCMP2"])
    for nb in range(4):
        for m in range(2):
            S.op("pe", lambda e, m=m, nb=nb: e.matmul(P[5][:, 0:64], lhsT=HID[:, 1, m, nb * 128:(nb + 1) * 128], rhs=w2[:, 1, m, :], start=(m == 0), stop=(m == 1)),
                 reads=["w2", "HID1"], writes=[pk(5)])
        S.op("act", lambda e, nb=nb: e.copy(out=VCMP[:, nb, 0:64], in_=P[5][:, 0:64]), writes=[pk(5), "VCMP"])
    S.barrier()
    kb.release(m1)
    VS = kb.alloc(64 * 128, BF16).rearrange("p (b d) -> p b d", b=64)
    VW = kb.alloc(64 * 128, BF16).rearrange("p (b d) -> p b d", b=64)
    for vt_, vk_ in ((VS, "VS"), (VW, "VW")):
        S.op("pool", lambda e, vt_=vt_: e.memset(vt_, 0.0), writes=[vk_])
        S.op("pool", lambda e, vt_=vt_: e.memset(vt_[:, :, 64:65], 1.0), writes=[vk_])
    for nm, dst, coff in (("nsa_vs", VS, 0), ("nsa_vw", VW, 64)):
        for b0 in range(0, 64, 8):
            load_cast(kb, (lambda b0=b0, coff=coff: L.pv(coff, 0, 64, b0 * 128, (b0 + 8) * 128).rearrange("(b p) d -> p b d", p=128)),
                      dst[:, b0:b0 + 8, 0:64], st3[:, 0:8, :], "st", nm[-2:].upper(), "act" if (b0 // 8) % 2 == 0 else "pool")

    NST = kb.alloc(N, BF16)
    EALL = kb.alloc(N, BF16)
    for j in range(16):
        load_cast(kb, D["eall"][:, j * 512:(j + 1) * 512], EALL[:, j * 512:(j + 1) * 512], st[:, 0:512], "st", "EALL", "act" if j % 2 == 0 else "pool")
    ET = [kb.alloc(512, BF16) for _ in range(4)]
    IMP = kb.alloc(512).rearrange("p (a s) -> p a s", a=4)
    scr = kb.alloc(128)
    mr = kb.alloc(128)
    nsb = kb.alloc(128)
    mx = kb.alloc(16)
    thr = kb.alloc(2)
    rsum = kb.alloc(2)
    g6 = kb.alloc(512)
    gs = [[kb.alloc(512) for _ in range(3)] for _ in range(2)]
    acc = [kb.alloc(512), kb.alloc(512)]
    ctmp = kb.alloc(512)
    otmp = kb.alloc(512)
    rowbuf = ctmp
    bcs = kb.alloc(512)
    att = Attn(kb, sbanks=(0, 1))
    oi = 0

    def combine(ob, hA, br, first):
        norm_coef(kb, c, ob, rowbuf, 4, bcs)
        S.op("dve", lambda e: e.tensor_tensor(out=ctmp[0:64, :], in0=bcs[0:64, :], in1=gs[hA][br][0:64, :], op=ALU.mult),
             reads=["bcs", f"gs{hA}{br}"], writes=["ctmp"])
        if first:
            S.op("dve", lambda e: e.tensor_tensor(out=acc[hA][0:64, :], in0=P[ob][0:64, :], in1=ctmp[0:64, :], op=ALU.mult),
                 reads=["ctmp"], writes=[pk(ob), f"acc{hA}"])
        else:
            S.op("dve", lambda e: e.tensor_tensor(out=otmp[0:64, :], in0=P[ob][0:64, :], in1=ctmp[0:64, :], op=ALU.mult),
                 reads=["ctmp"], writes=[pk(ob), "otmp"])
            S.op("pool", lambda e: e.tensor_tensor(out=acc[hA][0:64, :], in0=acc[hA][0:64, :], in1=otmp[0:64, :], op=ALU.add),
                 reads=["otmp"], writes=[f"acc{hA}"])

    pending = []
    for qt in range(NQT):
        qs = slice(qt * TQ, (qt + 1) * TQ)
        for f in pending:
            f()
        del pending[:]
        S.dma("sp", lambda e, qs=qs: e.dma_start(out=g6[0:6, :], in_=L.pt(OFF["b_gate"], 6, 6, qs.start, qs.stop)), writes=["g6"])
        for hA in range(2):
            for br in range(3):
                r = hA * 3 + br
                S.op("pe", lambda e, r=r: e.matmul(P[5][0:64, :], lhsT=selg[0:6, r * 64:(r + 1) * 64], rhs=g6[0:6, :], start=True, stop=True),
                     reads=["g6", "selg"], writes=[pk(5)])
                S.op("act", lambda e, hA=hA, br=br: e.activation(out=gs[hA][br][0:64, :], in_=P[5][0:64, :], func=AF.Sigmoid), writes=[pk(5), f"gs{hA}{br}"])
        nbm = (512 * qt + 480) // 2048
        for hh in range(4):
            Qt = (Qm if hh < 2 else Qo)
            qk = "q0" if hh < 2 else "q1"
            R = slice(64 * (hh % 2), 64 * (hh % 2) + 64)
            ob = 2 + oi % 2
            for nb in range(nbm + 1):
                bank = nb % 2
                dl = 512 * qt - 2048 * nb
                mms = [((KCA if hh % 2 == 0 else KCB)[:, nb * 128:(nb + 1) * 128], Qt[:, qs], ["KCMP2", "kcz", qk])]
                if dl < 2560:
                    mms.append((c.ident_bf, cmpm[:, dl // 512, :], ["ident_bf", "cmpm"]))
                for j, (l_, r_, ks) in enumerate(mms):
                    S.op("pe", lambda e, l_=l_, r_=r_, j=j, bank=bank, nm=len(mms): e.matmul(P[bank][:, :], lhsT=l_, rhs=r_, start=(j == 0), stop=(j == nm - 1)),
                         reads=ks, writes=[pk(bank)])
                S.op("act", lambda e, nb=nb, bank=bank: e.activation(out=ET[nb], in_=P[bank][:, :], func=AF.Exp), writes=[pk(bank), f"ET{nb}"])
                if hh < 2:
                    S.op("pe", lambda e, nb=nb, ob=ob, nbm=nbm: e.matmul(P[ob][0:128, :], lhsT=VCMP[:, nb, :], rhs=ET[nb], start=(nb == 0), stop=(nb == nbm)),
                         reads=[f"ET{nb}", "VCMP"], writes=[pk(ob)])
            for s4 in range(4):
                for nb in range(nbm + 1):
                    S.op("pe", lambda e, nb=nb, s4=s4, nbm=nbm: e.matmul(P[6][:, 0:129], lhsT=ET[nb][:, s4 * 128:(s4 + 1) * 128], rhs=ovl[:, nb, 0:129], start=(nb == 0), stop=(nb == nbm)),
                         reads=[f"ET{nb}", "ovl"], writes=[pk(6)])
                S.op("dve", lambda e: e.tensor_scalar_max(out=rsum[:, 0:1], in0=P[6][:, 128:129], scalar1=1e-30), writes=[pk(6), "rsum"])
                S.op("dve", lambda e: e.reciprocal(out=rsum[:, 0:1], in_=rsum[:, 0:1]), writes=["rsum"])
                if hh == 0:
                    S.op("dve", lambda e, s4=s4: e.tensor_scalar(out=IMP[:, s4, :], in0=P[6][:, 0:128], scalar1=rsum[:, 0:1], scalar2=None, op0=ALU.mult),
                         reads=["rsum"], writes=[pk(6), f"IMP{s4}"])
                else:
                    S.op("dve", lambda e, s4=s4: e.scalar_tensor_tensor(out=IMP[:, s4, :], in0=P[6][:, 0:128], scalar=rsum[:, 0:1], in1=IMP[:, s4, :], op0=ALU.mult, op1=ALU.add),
                         reads=["rsum"], writes=[pk(6), f"IMP{s4}"])
            if hh < 2:
                combine(ob, hh, 0, True)
                oi += 1
        for s4 in range(4):
            qb = 4 * qt + s4
            o0 = 126 - 2 * qb
            S.op("dve", lambda e, s4=s4, o0=o0: e.tensor_tensor(out=scr, in0=IMP[:, s4, :], in1=bv[:, o0:o0 + 128], op=ALU.mult), reads=[f"IMP{s4}", "bv"], writes=["scr"])
            S.op("dve", lambda e, o0=o0: e.tensor_tensor(out=scr, in0=scr, in1=bf[:, o0:o0 + 128], op=ALU.add), reads=["bf"], writes=["scr"])
            S.op("dve", lambda e: e.memset(scr[:, 0:1], 1e9), writes=["scr"])
            S.op("dve", lambda e: e.max(out=mx[:, 0:8], in_=scr), reads=["scr"], writes=["mx"])
            S.op("dve", lambda e: e.match_replace(out=mr, in_to_replace=mx[:, 0:8], in_values=scr, imm_value=-2.0), reads=["scr", "mx"], writes=["mr"])
            S.op("dve", lambda e: e.max(out=mx[:, 8:16], in_=mr), reads=["mr"], writes=["mx"])
            S.op("dve", lambda e: e.tensor_reduce(out=thr[:, 0:1], in_=mx[:, 8:16], axis=AX.X, op=ALU.min), reads=["mx"], writes=["thr"])
            S.op("dve", lambda e: e.tensor_scalar(out=nsb, in0=scr, scalar1=thr[:, 0:1], scalar2=-NEG, op0=ALU.is_ge, op1=ALU.mult), reads=["scr", "thr"], writes=["nsb"])
            S.op("pool", lambda e: e.tensor_scalar(out=nsb, in0=nsb, scalar1=NEG, scalar2=None, op0=ALU.add), writes=["nsb"])
            S.op("pe", lambda e: e.transpose(P[7][:, 0:128], nsb, c.ident), reads=["nsb", "ident"], writes=[pk(7)])
            S.op("act", lambda e, qb=qb: e.copy(out=NST[:, qb * 128:(qb + 1) * 128], in_=P[7][:, 0:128]), writes=[pk(7), "NST"])
        for hA in range(2):
            R = slice(64 * hA, 64 * hA + 64)
            blocks = []
            for kbk in range(4 * qt + 4):
                mms = [((KSA if hA == 0 else KSB)[:, kbk * 128:(kbk + 1) * 128], Qm[:, qs], ["ks", "q0", "ksz"]),
                       (EALL[:, kbk * 128:(kbk + 1) * 128], NST[:, qs], ["EALL", "NST"])]
                if kbk >= 4 * qt:
                    mms.append((c.ident_bf, tri[:, kbk - 4 * qt, :], ["ident_bf", "tri"]))
                blocks.append((mms, VS[:, kbk, :], ["VS"]))
            ob = 2 + oi % 2
            oi += 1
            att.run(blocks, ob, defer=pending)
            pending.append(lambda ob=ob, hA=hA: combine(ob, hA, 1, False))
            blocks = []
            for kbk in range(max(0, 4 * qt - 4), 4 * qt + 4):
                mms = [(KW2[R, kbk * 128:(kbk + 1) * 128], Qm[R, qs], ["kw", "q0"]),
                       (c.ident_bf, win[:, kbk - 4 * qt + 4, :], ["ident_bf", "win"])]
                blocks.append((mms, VW[:, kbk, :], ["VW"]))
            ob = 2 + oi % 2
            oi += 1
            att.run(blocks, ob, defer=pending)

            def fin(ob=ob, hA=hA, qs=qs):
                combine(ob, hA, 2, False)
                S.dma("sp", lambda e: e.dma_start(out=yT[128 + hA * 64:128 + (hA + 1) * 64, qs], in_=acc[hA][0:64, :]), reads=[f"acc{hA}"])
            pending.append(fin)
    for f in pending:
        f()
    S.barrier()
    kb.release(m0)


def part_gla(kb, c, D, yT, L):
    STAGE = 9
    S = kb.S
    P = kb.psb
    m0 = kb.mark()
    QS = float(32 ** -0.5)
    A = kb.alloc(N)
    B = kb.alloc(N)
    C = kb.alloc(2048)
    QG = kb.alloc(N, BF16)
    KG = kb.alloc(N, BF16)
    KHT = kb.alloc(128 * 64, BF16).rearrange("p (b d) -> p b d", b=128)
    V = kb.alloc(128 * 128, BF16).rearrange("p (b d) -> p b d", b=128)
    SB = kb.alloc(128 * 64, BF16).rearrange("p (c v) -> p c v", c=128)
    DC = kb.alloc(128)
    wg2 = kb.alloc(64)
    glr = kb.alloc(512)
    v2 = kb.alloc(4)
    tri64 = kb.alloc(64)
    st = kb.alloc(1024)
    S.dma("sp", lambda e: e.dma_start(out=wg2[0:16, :], in_=D["gla_wg2"][:, :]), writes=["wg2"])
    S.dma("sp", lambda e: e.dma_start(out=v2[:, 0:2], in_=D["gla_v2"][:, :]), writes=["v2"])
    S.dma("sp", lambda e: e.dma_start(out=tri64[0:64, :], in_=D["tri64"][:, :]), writes=["tri64"])
    S.dma("sp", lambda e: e.dma_start(out=tri64[64:128, :], in_=D["tri64"][:, :]), writes=["tri64"])
    S.op("dve", lambda e: e.tensor_scalar(out=v2[:, 2:3], in0=v2[:, 0:1], scalar1=-1.0, scalar2=None, op0=ALU.mult), reads=["v2"], writes=["v2n"])
    R = slice(0, 64)
    st3 = st.rearrange("p (b d) -> p b d", d=128)
    for b0 in range(0, 128, 8):
        load_cast(kb, (lambda b0=b0: L.pv(128, 128, 128, b0 * 64, (b0 + 8) * 64).rearrange("(b p) d -> p b d", p=64)),
                  V[R, b0:b0 + 8, :], st3[R, :, :], "st", "V", "act" if (b0 // 8) % 2 == 0 else "pool")
    for t in range(NQT):
        ts = slice(t * TQ, (t + 1) * TQ)
        bank = t % 2
        S.dma("sp", lambda e, ts=ts: e.dma_start(out=glr[0:16, :], in_=L.pt(OFF["c_glr"], 0, 16, ts.start, ts.stop)), writes=["glr"])
        S.op("pe", lambda e, bank=bank: e.matmul(P[bank][R, :], lhsT=wg2[0:16, 0:64], rhs=glr[0:16, :], start=True, stop=True), reads=["glr", "wg2"], writes=[pk(bank)])
        S.op("act", lambda e, bank=bank, ts=ts: e.activation(out=A[R, ts], in_=P[bank][R, :], func=AF.Exp, bias=v2[R, 2:3], scale=-1.0),
             reads=["v2n"], writes=[pk(bank), "A"])
    S.op("act", lambda e: e.activation(out=A[R, :], in_=A[R, :], func=AF.Ln, bias=1.0), writes=["A"])
    for ch in range(128):
        cs = slice(ch * 64, (ch + 1) * 64)
        S.op("dve", lambda e, cs=cs: e.tensor_tensor_scan(out=B[R, cs], data0=c.ones_f[R, 0:64], data1=A[R, cs], initial=0.0, op0=ALU.mult, op1=ALU.add),
             reads=["A", "ones_f"], writes=["B"])
    if STAGE <= 1:
        S.barrier(); kb.release(m0); return
    B3 = B.rearrange("p (c j) -> p c j", j=64)
    A3 = A.rearrange("p (c j) -> p c j", j=64)
    S.op("act", lambda e: e.activation(out=DC[R, :], in_=B3[R, :, 63], func=AF.Exp, scale=-1.0 / 16.0), reads=["B"], writes=["DC"])
    S.op("act", lambda e: e.activation(out=A[R, :], in_=B[R, :], func=AF.Exp, scale=-1.0 / 16.0), reads=["B"], writes=["A"])
    for pc in range(4):
        ps_ = slice(pc * 2048, (pc + 1) * 2048)
        S.dma("sp", lambda e, ps_=ps_: e.dma_start(out=C[R, :], in_=L.pt(OFF["c_q"], 64, 64, ps_.start, ps_.stop)), writes=["C"])
        S.op("dve", lambda e, ps_=ps_: e.scalar_tensor_tensor(out=QG[R, ps_], in0=C[R, :], scalar=QS, in1=A[R, ps_], op0=ALU.mult, op1=ALU.mult),
             reads=["C", "A"], writes=["QG"])
    S.op("act", lambda e: e.activation(out=A[R, :], in_=B[R, :], func=AF.Exp, scale=1.0 / 16.0), reads=["B", "QG"], writes=["A"])
    for pc in range(4):
        ps_ = slice(pc * 2048, (pc + 1) * 2048)
        S.dma("sp", lambda e, ps_=ps_: e.dma_start(out=C[R, :], in_=L.pt(OFF["c_k"], 64, 64, ps_.start, ps_.stop)), writes=["C"])
        S.op("dve", lambda e, ps_=ps_: e.tensor_tensor(out=KG[R, ps_], in0=C[R, :], in1=A[R, ps_], op=ALU.mult), reads=["C", "A"], writes=["KG"])
    S.op("dve", lambda e: e.tensor_tensor(out=A3[R, :, :], in0=B3[R, :, :], in1=B3[R, :, 63:64].to_broadcast([64, 128, 64]), op=ALU.subtract),
         reads=["B", "KG"], writes=["A"])
    S.op("act", lambda e: e.activation(out=A[R, :], in_=A[R, :], func=AF.Exp, scale=1.0 / 16.0), writes=["A"])
    for pc in range(4):
        ps_ = slice(pc * 2048, (pc + 1) * 2048)
        S.dma("sp", lambda e, ps_=ps_: e.dma_start(out=C[R, :], in_=L.pt(OFF["c_k"], 64, 64, ps_.start, ps_.stop)), writes=["C"])
        S.op("dve", lambda e, ps_=ps_: e.tensor_tensor(out=A[R, ps_], in0=C[R, :], in1=A[R, ps_], op=ALU.mult), reads=["C"], writes=["A"])
    if STAGE <= 2:
        S.barrier(); kb.release(m0); return
    for blk in range(64):
        bank = blk % 2
        S.op("pe", lambda e, blk=blk, bank=bank: e.transpose(P[bank][:, 0:64], A[R, blk * 128:(blk + 1) * 128], c.ident[0:64, 0:64]),
             reads=["A", "ident"], writes=[pk(bank)])
        S.op("act", lambda e, blk=blk, bank=bank: e.copy(out=KHT[R, 2 * blk, :], in_=P[bank][0:64, 0:64]), writes=[pk(bank), "KHT"])
        S.op("dve", lambda e, blk=blk, bank=bank: e.tensor_copy(out=KHT[R, 2 * blk + 1, :], in_=P[bank][64:128, 0:64]), writes=[pk(bank), "KHT"])
    if STAGE <= 3:
        S.barrier(); kb.release(m0); return
    U3 = B.rearrange("p (v c) -> p v c", c=128)
    uev = [kb.alloc(512), kb.alloc(512)]
    S3 = A.rearrange("p (v c) -> p v c", c=128)
    for g in range(32):
        bank = 2 + g % 2
        for j in range(4):
            ch = 4 * g + j
            S.op("pe", lambda e, ch=ch, j=j, bank=bank: e.matmul(P[bank][R, j * 128:(j + 1) * 128], lhsT=KHT[R, ch, :], rhs=V[R, ch, :], start=True, stop=True),
                 reads=["KHT", "V"], writes=[pk(bank)])
        ue = uev[g % 2]
        S.op("act", lambda e, bank=bank, ue=ue: e.copy(out=ue[R, :], in_=P[bank][R, :]), writes=[pk(bank), f"uev{g % 2}"])
        for h in range(2):
            hr = slice(32 * h, 32 * h + 32)
            for j in range(4):
                eng = "dve" if j % 2 == 0 else "pool"
                S.op(eng, lambda e, hr=hr, ue=ue, g=g, j=j, h=h: e.tensor_copy(out=U3[hr, :, 4 * g + j], in_=ue[hr, j * 128 + h * 64:j * 128 + (h + 1) * 64]),
                     reads=[f"uev{g % 2}"], writes=["U3", "B"])
    if STAGE <= 4:
        S.barrier(); kb.release(m0); return
    for v in range(64):
        S.op("dve", lambda e, v=v: e.tensor_tensor_scan(out=S3[R, v, :], data0=DC[R, :], data1=U3[R, v, :], initial=0.0, op0=ALU.mult, op1=ALU.add),
             reads=["U3", "DC", "KHT"], writes=["S3", "A"])
    S.op("pool", lambda e: e.memset(SB[R, 0, :], 0.0), writes=["SB0"])
    S.op("act", lambda e: e.copy(out=SB[R, 1:128, :], in_=S3[R, :, 0:127].rearrange("p v c -> p c v")), reads=["S3"], writes=["SB"])
    if STAGE <= 5:
        S.barrier(); kb.release(m0); return
    osb = kb.alloc(512)
    sqb = kb.alloc(512, BF16)
    rstd = kb.alloc(512)
    og = kb.alloc(512)
    at = [kb.alloc(64, BF16), kb.alloc(64, BF16)]
    ai = 0
    for t in range(NQT):
        ts = slice(t * TQ, (t + 1) * TQ)
        for h in range(2):
            hr = slice(32 * h, 32 * h + 32)
            ob = 4 + (2 * t + h) % 2
            for j in range(8):
                ch = 8 * t + j
                cs = slice(ch * 64, (ch + 1) * 64)
                rr = R
                sbk = ai % 2
                a_t = at[ai % 2]
                ak = f"at{ai % 2}"
                ai += 1
                S.op("pe", lambda e, hr=hr, cs=cs, rr=rr, sbk=sbk: e.matmul(P[sbk][rr, 0:64], lhsT=KG[hr, cs], rhs=QG[hr, cs], start=True, stop=True),
                     reads=["KG", "QG"], writes=[pk(sbk)])
                S.op("dve", lambda e, rr=rr, sbk=sbk, a_t=a_t: e.tensor_tensor(out=a_t[rr, :], in0=P[sbk][rr, 0:64], in1=tri64[rr, :], op=ALU.mult),
                     reads=["tri64"], writes=[pk(sbk), ak])
                S.op("pe", lambda e, rr=rr, ch=ch, h=h, a_t=a_t, ob=ob, j=j: e.matmul(P[ob][R, j * 64:(j + 1) * 64], lhsT=V[rr, ch, h * 64:(h + 1) * 64], rhs=a_t[rr, :], start=True, stop=False),
                     reads=[ak, "V"], writes=[pk(ob)])
                S.op("pe", lambda e, hr=hr, ch=ch, cs=cs, ob=ob, j=j: e.matmul(P[ob][R, j * 64:(j + 1) * 64], lhsT=SB[hr, ch, :], rhs=QG[hr, cs], start=False, stop=True),
                     reads=["SB", "SB0", "QG"], writes=[pk(ob)])
            S.op("act", lambda e, ob=ob: e.copy(out=osb[R, :], in_=P[ob][R, :]), writes=[pk(ob), "osb"])
            S.op("act", lambda e: e.activation(out=sqb[R, :], in_=osb[R, :], func=AF.Square), reads=["osb"], writes=["sqb"])
            S.op("pe", lambda e: e.matmul(P[6][R, :], lhsT=c.ones_bf[R, 0:64], rhs=sqb[R, :], start=True, stop=True), reads=["sqb", "ones_bf"], writes=[pk(6)])
            S.op("act", lambda e: e.activation(out=rstd[R, :], in_=P[6][R, :], func=AF.Sqrt, bias=EPS, scale=1.0 / 64.0), writes=[pk(6), "rstd"])
            S.op("dve", lambda e: e.reciprocal(out=rstd[R, :], in_=rstd[R, :]), writes=["rstd"])
            S.op("dve", lambda e: e.scalar_tensor_tensor(out=osb[R, :], in0=osb[R, :], scalar=v2[R, 1:2], in1=rstd[R, :], op0=ALU.mult, op1=ALU.mult),
                 reads=["rstd", "v2"], writes=["osb"])
            S.dma("sp", lambda e, h=h, ts=ts: e.dma_start(out=og[R, :], in_=L.pt(OFF["c_og"] + h * 64, 128, 64, ts.start, ts.stop)), writes=["og"])
            S.op("act", lambda e: e.activation(out=og[R, :], in_=og[R, :], func=AF.Silu), writes=["og"])
            S.op("dve", lambda e: e.tensor_tensor(out=osb[R, :], in0=osb[R, :], in1=og[R, :], op=ALU.mult), reads=["og"], writes=["osb"])
            S.dma("sp", lambda e, h=h, ts=ts: e.dma_start(out=yT[256 + h * 64:256 + (h + 1) * 64, ts], in_=osb[R, :]), reads=["osb"])
    S.barrier()
    kb.release(m0)


WB = ["lru_w", "lru_v", "mla_wuq", "mla_wk", "mla_wv", "mla_v", "nsa_pos", "nsa_w1", "nsa_b1", "nsa_w2", "gla_wg2", "gla_v2"]
WAC = (("gain", [128, 8]), ("w_in", [1024, NCOLW]), ("gng", [128, 8]), ("nfg", [128, 8]), ("w_out", [1024, 1024]),
       ("w_gu", [1024, 2 * DFF]), ("w_dn", [DFF, 1024]))
RG = [[0, 1], [2, 3], [4, 5], [6, 7]]


def build_fused():
    kb = KB(arena_cols=53100)
    S = kb.S
    D = {k: kb.din(k, shp) for k, shp in CONST_SHAPES.items()}
    D["pos"] = kb.din("pos", [1, N], I32)
    xT = kb.din("xT", [1024, 4096])
    out = kb.dout("out", [1024, 4096])
    LW = []
    for l in range(2):
        d = {k: kb.din(f"{k}_{l}", IN_SHAPES[k][0]) for k in WB}
        for k, shp in WAC:
            d[k] = kb.din(f"{k}_{l}", shp)
        LW.append(d)
    fing = kb.din("fing", [128, 8])
    XA = kb.dint("XA", [NCOLP, 4096])
    XV = kb.dint("XV", [4096, NV])
    GP = kb.dint("GP", [2 * NCOLP, 4096])
    GV = kb.dint("GV", [N, NV])
    YB = kb.dint("YB", [1024, 4096])
    YV = YView(YB)
    GY = kb.dint("GY", [2048, 4096])
    XO = kb.dint("XO", [1024, 4096])
    MYP = kb.dint("MYP", [SELROWS, N])
    MYV = kb.dint("MYV", [N, 128])
    MYY = kb.dint("MYY", [1024, 4096])
    c = common_setup(kb, D)
    L = Loader(S, GP, GV, MYP, MYV)
    xs = xT
    for l in range(2):
        W = LW[l]
        ag = lambda i_, o_: (lambda e: e.collective_compute("AllGather", ALU.bypass, replica_groups=RG, ins=[i_], outs=[o_]))
        phase_A(kb, xs, W["gain"], W["w_in"], XA, XV, GP, GV, ag)
        ccA = S.ccn
        Dl = dict(D)
        Dl.update({k: W[k] for k in WB})
        S.cc_wait(kb.cc_lru)
        L.select(names=("a_x", "a_gate"), with_v=False)
        for part, g in ((part_lru, 0), (part_gla, 2), (part_mla, 3), (part_nsa, 1)):
            if g == 2:
                S.cc_wait(ccA)
                L.select(names=("b_q", "b_gate", "c_q", "c_k", "c_og"), with_v=True)
            part(kb, c, Dl, YV, L)
            for ch in range(2):
                S.collective_async(ag(YB[ch * 512 + g * 128:ch * 512 + (g + 1) * 128, :], GY[(g * 2 + ch) * 256:(g * 2 + ch + 1) * 256, :]))
        S.cc_wait_all()
        GY4 = GY.rearrange("(g ch q) t -> g ch q t", g=4, ch=2)
        S.dma("act", lambda e: e.dma_start(out=MYY.rearrange("(g q) t -> g q t", g=4), in_=GY4[:, bass.ds(S.rt["hp_act"], 1), :, :].rearrange("g o q t -> g (o q) t")),
              writes=["MYY"])
        S.barrier()
        phase_C(kb, l == 1, xs, MYY, W["gng"], W["nfg"], fing, W["w_out"], W["w_gu"], W["w_dn"], out if l == 1 else XO)
        xs = XO
    return kb.close()


def prep_W(W, l, hp):
    A = np.ascontiguousarray
    o = {}
    ch = slice(hp * 128, hp * 128 + 128)
    o["lru_w"] = A(np.stack([W["lru_wa"][l][2 * hp:2 * hp + 2], W["lru_wx"][l][2 * hp:2 * hp + 2]]))
    o["lru_v"] = A(np.stack([W["conv_w"][l][0, ch], W["conv_w"][l][1, ch], W["conv_w"][l][2, ch], W["conv_w"][l][3, ch],
                             W["conv_b"][l][ch], W["lru_ba"][l][ch], W["lru_bx"][l][ch], W["lru_lambda"][l][ch]], axis=1))
    o["mla_wuq"] = A(W["mla_w_uq"][l][:, hp * 192:(hp + 1) * 192])
    wkv = W["mla_w_ukv"][l].reshape(128, 4, 128)
    o["mla_wk"] = A(wkv[:, 2 * hp:2 * hp + 2, 0:64].reshape(128, 128))
    o["mla_wv"] = A(wkv[:, 2 * hp:2 * hp + 2, 64:128].reshape(128, 128))
    mv = np.zeros((128, 3), np.float32)
    mv[:, 0] = W["mla_q_norm"][l][0:128]
    mv[0:64, 1] = W["mla_q_norm"][l][128:192]
    mv[:, 2] = W["mla_kv_norm"][l]
    o["mla_v"] = mv
    o["nsa_pos"] = A(np.concatenate([W["cmp_pos"][l][0].T, W["cmp_pos"][l][1].T], axis=0))
    o["nsa_w1"] = A(W["cmp_w1"][l])
    o["nsa_b1"] = A(W["cmp_b1"][l].reshape(2, 2, 128).transpose(2, 0, 1).reshape(128, 4))
    o["nsa_w2"] = A(W["cmp_w2"][l])
    o["gla_wg2"] = A(W["gla_wg2"][l][:, hp * 64:(hp + 1) * 64])
    g2 = np.zeros((128, 2), np.float32)
    g2[0:64, 0] = W["gla_bg2"][l][hp * 64:(hp + 1) * 64]
    g2[:, 1] = np.tile(W["gla_norm"][l], 2)
    o["gla_v2"] = g2
    return o


_PROG = {}


def _arr8(g):
    return np.ascontiguousarray(np.asarray(g, np.float32).reshape(8, 128).T)


def kernel(**inp):
    W = {k: np.asarray(v) for k, v in inp.items()}
    x = W["x"]
    Bn, Sn, Dm = x.shape
    HT = Sn // 2
    cores = [(b, r) for b in range(Bn) for r in range(2)]
    if "F" not in _PROG:
        _PROG["F"] = build_fused()
    nc = _PROG["F"]
    consts = host_consts()
    pc = perm_cols()
    ins = []
    for (b, r) in cores:
        d = dict(consts)
        d["pos"] = np.ascontiguousarray(W["positions"][b][None, :].astype(np.int32))
        d["xT"] = np.ascontiguousarray(x[b, r * HT:(r + 1) * HT].T)
        d["fing"] = _arr8(W["final_norm"])
        for l in range(2):
            for k, v in prep_W(W, l, r).items():
                d[f"{k}_{l}"] = v
            d[f"gain_{l}"] = _arr8(W["norm_mix"][l])
            d[f"w_in_{l}"] = np.ascontiguousarray(W["w_in"][l][:, pc])
            d[f"gng_{l}"] = _arr8(W["group_norm"][l])
            d[f"nfg_{l}"] = _arr8(W["norm_ffn"][l])
            d[f"w_out_{l}"] = np.ascontiguousarray(W["w_out"][l])
            d[f"w_gu_{l}"] = np.ascontiguousarray(W["w_gate_up"][l])
            d[f"w_dn_{l}"] = np.ascontiguousarray(W["w_down"][l])
        ins.append(d)
    res = run_bass_kernel_spmd(nc, ins, core_ids=list(range(8))).results
    out = np.empty((Bn, Sn, Dm), np.float32)
    for ci, (b, r) in enumerate(cores):
        out[b, r * HT:(r + 1) * HT] = res[ci]["out"].T
    return out
```
